# Optimizing a Trainium2 kernel written in Bass

```python
import jax, jax.numpy as jnp
from jax import lax
import numpy as np

D_MODEL = 1024
BATCH = 32
SEQ = 2048
DEPTH = 1

GRID_W = 64
Q_BLOCK = 128
ROPE_THETA = 10000.0
MIX_WIDTH = D_MODEL
HEAD_DIM = 64
ATT_WIDTH = MIX_WIDTH // 2
ATT_HEADS = ATT_WIDTH // HEAD_DIM
ATT_KV_HEADS = 2
KV_WIDTH = ATT_KV_HEADS * HEAD_DIM
RWKV_WIDTH = MIX_WIDTH - ATT_WIDTH
RWKV_HEAD = 64
RWKV_HEADS = RWKV_WIDTH // RWKV_HEAD
DECAY_LORA = 64
ICLR_LORA = 64
GATE_LORA = 128
N_SHIFT = 3 * RWKV_WIDTH + DECAY_LORA + ICLR_LORA + GATE_LORA
N_IN = ATT_WIDTH + 2 * KV_WIDTH + N_SHIFT
DECAY_SCALE = 0.606531
GN_EPS = 64e-5
NORM_EPS = 1e-6
L2_EPS = 1e-12
N_EXPERTS = 16
EC_CAPACITY = 2
EXPERT_FF = 1024

kernel_name = "hybrid_attn_rwkv7_ec_moe_block"


def _rmsnorm(x, g):
    xf = x.astype(jnp.float32)
    y = xf * lax.rsqrt(jnp.mean(xf * xf, axis=-1, keepdims=True) + NORM_EPS)
    return y.astype(x.dtype) * g


def _axial_rope_tables(seq):
    rows = seq // GRID_W
    row = jnp.repeat(jnp.arange(rows, dtype=jnp.float32), GRID_W)
    col = jnp.tile(jnp.arange(GRID_W, dtype=jnp.float32), rows)
    n_pairs_axis = HEAD_DIM // 4
    freqs = ROPE_THETA ** (-jnp.arange(n_pairs_axis, dtype=jnp.float32) / n_pairs_axis)
    ang = jnp.concatenate([row[:, None] * freqs, col[:, None] * freqs], axis=-1)
    return jnp.cos(ang), jnp.sin(ang)


def _apply_rope(x, cos, sin):
    xp = x.reshape(*x.shape[:-1], HEAD_DIM // 2, 2)
    x0, x1 = xp[..., 0], xp[..., 1]
    c = cos[None, :, None, :].astype(x.dtype)
    s = sin[None, :, None, :].astype(x.dtype)
    return jnp.stack([x0 * c - x1 * s, x0 * s + x1 * c], axis=-1).reshape(x.shape)


def _block_attention(q, k, v):
    B, S = q.shape[:2]
    nb = S // Q_BLOCK
    grp = ATT_HEADS // ATT_KV_HEADS
    qb = q.reshape(B, nb, Q_BLOCK, ATT_KV_HEADS, grp, HEAD_DIM).transpose(1, 0, 2, 3, 4, 5)
    scale = HEAD_DIM ** -0.5

    def one_block(qi):
        s = jnp.einsum('bqhgd,bkhd->bhgqk', qi, k, preferred_element_type=jnp.float32) * scale
        p = jax.nn.softmax(s, axis=-1).astype(v.dtype)
        return jnp.einsum('bhgqk,bkhd->bqhgd', p, v)

    o = lax.map(one_block, qb)
    return o.transpose(1, 0, 2, 3, 4, 5).reshape(B, S, ATT_WIDTH)


def _centred_shift(p, mu):
    zero = jnp.zeros_like(p[:, :1])
    prev = jnp.concatenate([zero, p[:, :-1]], axis=1)
    nxt = jnp.concatenate([p[:, 1:], zero], axis=1)
    return p + mu * (0.5 * (prev + nxt) - p)


def _wkv7_scan(r, w, k, v, kk, a, reverse):
    B, T, H, N = r.shape

    def step(S, inp):
        r_t, w_t, k_t, v_t, kk_t, a_t = inp
        sa = jnp.einsum('bhvk,bhk->bhv', S, -kk_t)
        S = (S * w_t[:, :, None, :] + sa[..., None] * (kk_t * a_t)[:, :, None, :]
             + v_t[..., None] * k_t[:, :, None, :])
        return S, jnp.einsum('bhvk,bhk->bhv', S, r_t)

    xs = tuple(jnp.swapaxes(z, 0, 1) for z in (r, w, k, v, kk, a))
    S0 = jnp.zeros((B, H, N, N), jnp.float32)
    _, y = lax.scan(step, S0, xs, reverse=reverse)
    return jnp.swapaxes(y, 0, 1)


def _rwkv7_bidir(p, mu_shift, w0, w_up, a0, a_up, g_up, k_k, k_a, r_k, ln_w, ln_b):
    dt = p.dtype
    p = _centred_shift(p, mu_shift)
    R = RWKV_WIDTH
    r, k, v, dw, da, dg = jnp.split(
        p, [R, 2 * R, 3 * R, 3 * R + DECAY_LORA, 3 * R + DECAY_LORA + ICLR_LORA], axis=-1)
    B, T, _ = r.shape

    def heads(z):
        return z.reshape(B, T, RWKV_HEADS, RWKV_HEAD).astype(jnp.float32)

    kk = heads(k * k_k)
    kk = kk / jnp.maximum(jnp.sqrt(jnp.sum(kk * kk, axis=-1, keepdims=True)), L2_EPS)
    g = jax.nn.sigmoid(dg) @ g_up
    tw = jnp.tanh(dw)
    rh, vh = heads(r), heads(v)

    def direction(d, reverse):
        w = jnp.exp(-DECAY_SCALE * jax.nn.sigmoid(w0[d] + tw @ w_up[d]))
        a = jax.nn.sigmoid(a0[d] + da @ a_up[d])
        kd = heads(k * (1.0 + (a - 1.0) * k_a))
        ah = heads(a)
        y = _wkv7_scan(rh, heads(w), kd, vh, kk, ah, reverse)
        bonus = jnp.sum(rh * kd * r_k, axis=-1, keepdims=True) * vh
        return y, bonus

    y_f, bonus_f = direction(0, False)
    y_b, bonus_b = direction(1, True)
    y = y_f + y_b
    mean = jnp.mean(y, axis=-1, keepdims=True)
    var = jnp.mean(jnp.square(y - mean), axis=-1, keepdims=True)
    y = ((y - mean) * lax.rsqrt(var + GN_EPS)).reshape(B, T, R) * ln_w + ln_b
    y = y + (bonus_f + bonus_b).reshape(B, T, R)
    return (y * g).astype(dt)


def _expert_choice_ffn(h, w_router, w_gate, w_up, w_down):
    B, S, D = h.shape
    cap = EC_CAPACITY * S // N_EXPERTS
    aff = jax.nn.softmax((h @ w_router).astype(jnp.float32), axis=-1)
    vals, idx = lax.top_k(jnp.swapaxes(aff, 1, 2), cap)
    hg = jax.vmap(lambda hb, ib: hb[ib])(h, idx)
    hid = (jax.nn.silu(jnp.einsum('becd,edf->becf', hg, w_gate))
           * jnp.einsum('becd,edf->becf', hg, w_up))
    y = jnp.einsum('becf,efd->becd', hid, w_down) * vals[..., None].astype(h.dtype)
    return jax.vmap(
        lambda yb, ib: jnp.zeros((S, D), h.dtype).at[ib.reshape(-1)].add(yb.reshape(-1, D))
    )(y, idx)


def setup_inputs(seed: int = 0) -> dict:
    key = jax.random.key(seed)
    ks = jax.random.split(key, 28)
    L, D, E, F = DEPTH, D_MODEL, N_EXPERTS, EXPERT_FF

    def nrm(k, shape, s):
        return jax.random.normal(k, shape, jnp.float32) * s

    return {
        "x": nrm(ks[0], (BATCH, SEQ, D), 1.0),
        "c": nrm(ks[1], (BATCH, D), 1.0),
        "w_ada": nrm(ks[2], (L, D, 6 * D), 0.5 * D ** -0.5),
        "b_ada": nrm(ks[3], (L, 6 * D), 0.02),
        "g_mix": 1.0 + nrm(ks[4], (L, D), 0.02),
        "w_in": nrm(ks[5], (L, D, N_IN), D ** -0.5),
        "q_norm": 1.0 + nrm(ks[6], (L, HEAD_DIM), 0.02),
        "k_norm": 1.0 + nrm(ks[7], (L, HEAD_DIM), 0.02),
        "mu_shift": jax.random.uniform(ks[8], (L, N_SHIFT), jnp.float32, 0.0, 1.0),
        "w0": nrm(ks[9], (L, 2, RWKV_WIDTH), 1.0),
        "w_up": nrm(ks[10], (L, 2, DECAY_LORA, RWKV_WIDTH), 0.5 * DECAY_LORA ** -0.5),
        "a0": nrm(ks[11], (L, 2, RWKV_WIDTH), 0.5),
        "a_up": nrm(ks[12], (L, 2, ICLR_LORA, RWKV_WIDTH), 0.5 * ICLR_LORA ** -0.5),
        "g_up": nrm(ks[13], (L, GATE_LORA, RWKV_WIDTH), GATE_LORA ** -0.5),
        "k_k": 0.85 + nrm(ks[14], (L, RWKV_WIDTH), 0.05),
        "k_a": 1.0 + nrm(ks[15], (L, RWKV_WIDTH), 0.05),
        "r_k": nrm(ks[16], (L, RWKV_HEADS, RWKV_HEAD), 0.1),
        "ln_w": 1.0 + nrm(ks[17], (L, RWKV_WIDTH), 0.02),
        "ln_b": nrm(ks[18], (L, RWKV_WIDTH), 0.02),
        "w_out": nrm(ks[19], (L, MIX_WIDTH, D), MIX_WIDTH ** -0.5),
        "g_ffn": 1.0 + nrm(ks[20], (L, D), 0.02),
        "w_router": nrm(ks[21], (L, D, E), D ** -0.5),
        "w_gate": nrm(ks[22], (L, E, D, F), D ** -0.5),
        "w_up_e": nrm(ks[23], (L, E, D, F), D ** -0.5),
        "w_down": nrm(ks[24], (L, E, F, D), F ** -0.5),
    }


def reference(x, c, w_ada, b_ada, g_mix, w_in, q_norm, k_norm, mu_shift, w0, w_up, a0,
              a_up, g_up, k_k, k_a, r_k, ln_w, ln_b, w_out, g_ffn, w_router, w_gate,
              w_up_e, w_down):
    B, S, _ = x.shape
    cos, sin = _axial_rope_tables(S)
    cond = jax.nn.silu(c)
    for l in range(DEPTH):
        mod = (cond @ w_ada[l] + b_ada[l])[:, None, :]
        sh1, sc1, gt1, sh2, sc2, gt2 = jnp.split(mod, 6, axis=-1)

        h = _rmsnorm(x, g_mix[l]) * (1.0 + sc1) + sh1
        p = h @ w_in[l]
        pq, pk, pv, pr = jnp.split(
            p, [ATT_WIDTH, ATT_WIDTH + KV_WIDTH, ATT_WIDTH + 2 * KV_WIDTH], axis=-1)
        q = _apply_rope(_rmsnorm(pq.reshape(B, S, ATT_HEADS, HEAD_DIM), q_norm[l]), cos, sin)
        k = _apply_rope(_rmsnorm(pk.reshape(B, S, ATT_KV_HEADS, HEAD_DIM), k_norm[l]), cos, sin)
        v = pv.reshape(B, S, ATT_KV_HEADS, HEAD_DIM)
        o_att = _block_attention(q, k, v)
        o_rwkv = _rwkv7_bidir(pr, mu_shift[l], w0[l], w_up[l], a0[l], a_up[l], g_up[l],
                              k_k[l], k_a[l], r_k[l], ln_w[l], ln_b[l])
        x = x + gt1 * (jnp.concatenate([o_att, o_rwkv], axis=-1) @ w_out[l])

        h2 = _rmsnorm(x, g_ffn[l]) * (1.0 + sc2) + sh2
        x = x + gt2 * _expert_choice_ffn(h2, w_router[l], w_gate[l], w_up_e[l], w_down[l])
    return x
```

```python
import numpy as np
from contextlib import ExitStack, contextmanager
import concourse.bass as bass
import concourse.mybir as mybir
from concourse.bass_utils import run_bass_kernel_spmd

F32 = mybir.dt.float32
BF16 = mybir.dt.bfloat16
AF = mybir.ActivationFunctionType
ALU = mybir.AluOpType
AX = mybir.AxisListType

D = 1024
KD = 8
HD = 64
NE = 16
DECAY = 0.606531
GN_EPS = 64e-5
NORM_EPS = 1e-6
N_IN = 2560
REBASE_T = 3000
BAR_EVERY = 4


class Tok:
    __slots__ = ("w", "r")

    def __init__(self):
        self.w = None
        self.r = {}


class Cnt:
    def __init__(self, sem, incv, eng=None, name=""):
        self.sem = sem
        self.incv = incv
        self.cnt = 0
        self.eng = eng
        self.seen = {}
        self.name = name
        self.gen = 0


class Ctx:
    def __init__(self, nc):
        self.nc = nc
        self.stacks = [ExitStack()]
        self.uid = 0
        self.allc = []
        mk = self._mkc
        self.PE = mk(nc.tensor, 1, "pe")
        self.ACT = mk(nc.scalar, 1, "act")
        self.DVE = mk(nc.vector, 1, "dve")
        self.POOL = mk(nc.gpsimd, 1, "pool")
        self.SP = Cnt(None, 0, nc.sync, "sp")
        self.dma_free = []
        self.outc = []

    def _mkc(self, eng, incv, name):
        sem = self.stacks[0].enter_context(self.nc.semaphore(f"s_{name}_{self.uid}"))
        self.uid += 1
        c = Cnt(sem, incv, eng, name)
        self.allc.append(c)
        return c

    def dmac(self, name="d"):
        return self._mkc(None, 16, name)

    def nm(self, s):
        self.uid += 1
        return f"{s}_{self.uid}"

    def sb(self, shape, dt, name="t"):
        return self.stacks[-1].enter_context(self.nc.sbuf_tensor(self.nm(name), list(shape), dt))

    def ps(self, shape, dt=F32, name="p"):
        return self.stacks[-1].enter_context(self.nc.psum_tensor(self.nm(name), list(shape), dt))

    @contextmanager
    def scope(self):
        self.stacks.append(ExitStack())
        try:
            yield
        finally:
            self.barrier()
            self.stacks.pop().close()

    def barrier(self):
        for e in (self.PE, self.ACT, self.DVE, self.POOL, self.SP):
            for f in self.allc:
                if f.cnt > 0 and e.seen.get(f, 0) < f.cnt:
                    e.eng.wait_ge(f.sem, f.cnt * f.incv)
                    e.seen[f] = f.cnt
        for f in (self.PE, self.ACT, self.DVE, self.POOL):
            if f.cnt > REBASE_T:
                f.sem = self.stacks[0].enter_context(self.nc.semaphore(f"s_{f.name}_rb{self.uid}"))
                self.uid += 1
                f.cnt = 0
                f.gen += 1
                for e in (self.PE, self.ACT, self.DVE, self.POOL, self.SP):
                    e.seen.pop(f, None)

    def op(self, e, fn, R=(), W=(), comp=None, inc=True):
        comp = comp or e
        need = {}
        for t in R:
            if t.w is not None:
                f, c, g = t.w
                if g == f.gen and c > need.get(f, 0):
                    need[f] = c
        for t in W:
            if t.w is not None:
                f, c, g = t.w
                if g == f.gen and c > need.get(f, 0):
                    need[f] = c
            for f, (c, g) in t.r.items():
                if g == f.gen and c > need.get(f, 0):
                    need[f] = c
        for f, c in need.items():
            if f is e and e is self.PE:
                continue
            if e.seen.get(f, 0) < c:
                e.eng.wait_ge(f.sem, c * f.incv)
                e.seen[f] = c
        ins = fn(e.eng)
        if inc:
            comp.cnt += 1
            ins.then_inc(comp.sem, comp.incv)
            cc = comp.cnt
        else:
            cc = comp.cnt + 1
        for t in R:
            pr = t.r.get(comp)
            if pr is None or pr[1] != comp.gen or pr[0] < cc:
                t.r[comp] = (cc, comp.gen)
        for t in W:
            t.w = (comp, cc, comp.gen)
            t.r = {}
        return ins

    def mm(self, out, pairs, R=(), W=(), start=True, stop=True, inc=True):
        n = len(pairs)
        for i, (l, r) in enumerate(pairs):
            last = i == n - 1
            self.op(self.PE,
                    lambda e, l=l, r=r, i=i, last=last: e.matmul(out, lhsT=l, rhs=r, start=(start and i == 0), stop=(stop and last)),
                    R=R if i == 0 else (), W=W, inc=(inc and last))

    def tr(self, out, in_, ident, R=(), W=(), inc=True):
        self.op(self.PE, lambda e: e.transpose(out, in_, ident), R=R, W=W, inc=inc)

    def dma(self, issuer, comp, out, in_, R=(), W=()):
        self.op(issuer, lambda e: e.dma_start(out=out, in_=in_), R=R, W=W, comp=comp)


def bc(ap, shape):
    return ap.to_broadcast(list(shape))


class _Stop(Exception):
    pass


def build(S=2048, NSEQ=4, dumps=(), stop=None):
    try:
        return _build(S, NSEQ, dumps, stop)
    except _Stop as ex:
        ex.args[1].barrier()
        ex.args[1].stacks[0].close()
        return ex.args[0]


def _build(S, NSEQ, dumps, stop):
    NT = S // 128
    QB = min(512, S)
    NQB = S // QB
    QT = QB // 128
    CAP = 2 * S // NE
    NCT = max(1, CAP // 128)
    CP = min(CAP, 128)
    TB = min(512, S)
    NTB = S // TB
    TOKS = NSEQ * S
    nc = bass.Bass("TRN2", target_bir_lowering=False)
    K = Ctx(nc)

    def din(name, shape):
        return nc.dram_tensor(name, list(shape), F32, kind="ExternalInput").ap()

    x_d = din("x", [TOKS, D])
    cT_d = din("cT", [128, KD, NSEQ])
    wada_d = din("w_ada", [D, 6 * D])
    bada_d = din("b_ada", [128, 48])
    gmix_d = din("g_mix", [128, KD])
    gffn_d = din("g_ffn", [128, KD])
    win_d = din("w_in", [D, N_IN])
    qn_d = din("q_norm", [1, HD])
    kn_d = din("k_norm", [1, HD])
    mu_d = din("mu", [128, 14])
    w0_d = din("w0", [128, 2, 4])
    a0_d = din("a0", [128, 2, 4])
    wup_d = din("w_up", [2, 64, 512])
    aup_d = din("a_up", [2, 64, 512])
    gup_d = din("g_up", [128, 512])
    kk_d = din("k_k", [128, 4])
    ka_d = din("k_a", [128, 4])
    rk_d = din("r_k", [128, 4])
    lnw_d = din("ln_w", [128, 4])
    lnb_d = din("ln_b", [128, 4])
    wout_d = din("w_out", [D, D])
    wr_d = din("w_router", [D, NE])
    wg_d = din("w_gate", [NE, D, D])
    wu_d = din("w_up_e", [NE, D, D])
    wd_d = din("w_down", [NE, D, D])
    cos_d = din("cos", [128, NT, 32])
    sin_d = din("sin", [128, NT, 32])
    out_d = nc.dram_tensor("out", [TOKS, D], F32, kind="ExternalOutput").ap()
    dump_d = {}

    PE, ACT, DVE, POOL, SP = K.PE, K.ACT, K.DVE, K.POOL, K.SP
    import os as _os0
    PL = POOL if _os0.environ.get('POOLC', '0') == '1' else DVE
    outc = K.dmac("outc")

    def ckpt(name):
        if stop == name:
            raise _Stop((nc, dump_d), K)

    def dump(name, src_ap, shape, R=()):
        if name not in dumps:
            return
        dd = nc.dram_tensor("dump_" + name, list(shape), F32, kind="ExternalOutput").ap()
        dump_d[name] = dd
        tmp = K.sb(shape, F32, "dmp")
        tk = Tok()
        K.op(DVE, lambda e: e.tensor_copy(out=tmp[:], in_=src_ap), R=R, W=[tk])
        K.dma(SP, outc, dd, tmp[:], R=[tk])

    cst = Tok()
    identf = K.sb([128, 128], F32, "identf")
    identb = K.sb([128, 128], BF16, "identb")
    onesf = K.sb([128, 128], F32, "onesf")
    bdones = K.sb([128, 128], BF16, "bdones")
    MP = [K.sb([128, 256], BF16, "MP0"), K.sb([128, 256], BF16, "MP1")]
    ML = [K.sb([128, 128], BF16, "ML0"), K.sb([128, 128], BF16, "ML1")]
    iotac = K.sb([128, 256], F32, "iotac")
    iotap = K.sb([128, 2], F32, "iotap")
    hmask = K.sb([128, 4], F32, "hmask")

    K.op(POOL, lambda e: e.memset(onesf[:], 1.0), W=[cst])
    K.stacks.append(ExitStack())
    mUPs = K.sb([128, 128], F32, "mUPs")
    mUPi = K.sb([128, 128], F32, "mUPi")
    mLOs = K.sb([128, 128], F32, "mLOs")
    mLOi = K.sb([128, 128], F32, "mLOi")
    def aff(dst, base, cm, step, cmp):
        K.op(POOL, lambda e: e.affine_select(out=dst[:], in_=onesf[:], pattern=[[step, 128]], compare_op=cmp,
                                             fill=0.0, base=base, channel_multiplier=cm), R=[cst], W=[cst])
    aff(identf, 0, 1, -1, ALU.is_equal)
    aff(mUPs, 0, -1, 1, ALU.is_gt)
    aff(mUPi, 0, -1, 1, ALU.is_ge)
    aff(mLOs, 0, 1, -1, ALU.is_gt)
    aff(mLOi, 0, 1, -1, ALU.is_ge)
    K.op(DVE, lambda e: e.tensor_copy(out=identb[:], in_=identf[:]), R=[cst], W=[cst])
    K.op(DVE, lambda e: e.memset(bdones[:], 0.0), W=[cst])
    K.op(DVE, lambda e: e.memset(bdones[0:64, 0:64], 1.0), W=[cst])
    K.op(DVE, lambda e: e.memset(bdones[64:128, 64:128], 1.0), W=[cst])
    K.op(DVE, lambda e: e.tensor_copy(out=MP[0][:, 0:128], in_=mUPs[:]), R=[cst], W=[cst])
    K.op(DVE, lambda e: e.tensor_copy(out=MP[0][:, 128:256], in_=mUPi[:]), R=[cst], W=[cst])
    K.op(DVE, lambda e: e.tensor_copy(out=MP[1][:, 0:128], in_=mLOs[:]), R=[cst], W=[cst])
    K.op(DVE, lambda e: e.tensor_copy(out=MP[1][:, 128:256], in_=mLOi[:]), R=[cst], W=[cst])
    K.op(DVE, lambda e: e.tensor_copy(out=ML[0][:], in_=mLOs[:]), R=[cst], W=[cst])
    K.op(DVE, lambda e: e.tensor_copy(out=ML[1][:], in_=mUPs[:]), R=[cst], W=[cst])
    K.barrier()
    K.stacks.pop().close()
    K.op(POOL, lambda e: e.iota(iotac[:], pattern=[[1, 256]], base=0, channel_multiplier=0,
                                allow_small_or_imprecise_dtypes=True), W=[cst])
    K.op(POOL, lambda e: e.iota(iotap[:], pattern=[[128, 2]], base=0, channel_multiplier=1,
                                allow_small_or_imprecise_dtypes=True), W=[cst])
    K.op(DVE, lambda e: e.memset(hmask[:], 0.0), W=[cst])
    K.op(DVE, lambda e: e.memset(hmask[0:64, 0:1], 1.0), W=[cst])
    K.op(DVE, lambda e: e.memset(hmask[64:128, 1:2], 1.0), W=[cst])
    K.op(DVE, lambda e: e.memset(hmask[0:64, 2:3], -1.0), W=[cst])
    K.op(DVE, lambda e: e.memset(hmask[64:128, 3:4], -1.0), W=[cst])

    ckpt('consts')
    def ldsmall(dram, shape, name, dt=F32, eng=None):
        t = K.sb(shape, dt, name)
        c = K.dmac(name)
        tk = Tok()
        K.dma(SP if dt == F32 else POOL, c, t[:], dram, W=[tk])
        return t, tk

    cT, cT_k = ldsmall(cT_d, [128, KD, NSEQ], "cT")
    bada, bada_k = ldsmall(bada_d, [128, 48], "bada")
    gmix, gmix_k = ldsmall(gmix_d, [128, KD], "gmix")
    gffn, gffn_k = ldsmall(gffn_d, [128, KD], "gffn")
    mu, mu_k = ldsmall(mu_d, [128, 14], "mu")
    w0, w0_k = ldsmall(w0_d, [128, 2, 4], "w0")
    a0, a0_k = ldsmall(a0_d, [128, 2, 4], "a0")
    kkp, kkp_k = ldsmall(kk_d, [128, 4], "kkp")
    kap, kap_k = ldsmall(ka_d, [128, 4], "kap")
    rkp, rkp_k = ldsmall(rk_d, [128, 4], "rkp")
    lnw, lnw_k = ldsmall(lnw_d, [128, 4], "lnw")
    lnb, lnb_k = ldsmall(lnb_d, [128, 4], "lnb")
    cosT, cos_k = ldsmall(cos_d, [128, NT, 32], "cos")
    sinT, sin_k = ldsmall(sin_d, [128, NT, 32], "sin")
    gain = K.sb([128, 10, HD], F32, "gain")
    gain_k = Tok()
    gc_ = K.dmac("gain")
    K.dma(SP, gc_, gain[:, 0, :], qn_d.partition_broadcast(128), W=[gain_k])
    K.dma(SP, gc_, gain[:, 8, :], kn_d.partition_broadcast(128), W=[gain_k])
    for h in range(1, 8):
        K.op(DVE, lambda e, h=h: e.tensor_copy(out=gain[:, h, :], in_=gain[:, 0, :]), R=[gain_k], W=[gain_k])
    K.op(DVE, lambda e: e.tensor_copy(out=gain[:, 9, :], in_=gain[:, 8, :]), R=[gain_k], W=[gain_k])
    K.op(DVE, lambda e: e.tensor_scalar(out=gain[:, 0:8, :], in0=gain[:, 0:8, :], scalar1=HD ** -0.5, scalar2=None, op0=ALU.mult),
         R=[gain_k], W=[gain_k])
    hmu = K.sb([128, 14], F32, "hmu")
    omm = K.sb([128, 14], F32, "omm")
    omka = K.sb([128, 4], F32, "omka")
    K.op(DVE, lambda e: e.tensor_scalar(out=hmu[:], in0=mu[:], scalar1=0.5, scalar2=None, op0=ALU.mult), R=[mu_k], W=[mu_k])
    K.op(DVE, lambda e: e.tensor_scalar(out=omm[:], in0=mu[:], scalar1=-1.0, scalar2=1.0, op0=ALU.mult, op1=ALU.add), R=[mu_k], W=[mu_k])
    K.op(DVE, lambda e: e.tensor_scalar(out=omka[:], in0=kap[:], scalar1=-1.0, scalar2=1.0, op0=ALU.mult, op1=ALU.add), R=[kap_k], W=[kap_k])
    wup = K.sb([128, 2, 512], BF16, "wup")
    aup = K.sb([128, 2, 512], BF16, "aup")
    gup = K.sb([128, 512], BF16, "gup")
    wrt = K.sb([128, KD, NE], BF16, "wrt")
    wsm_k = Tok()
    wsc = K.dmac("wsm")
    K.dma(POOL, wsc, wup[0:64, :, :], wup_d.rearrange("d k n -> k d n"), W=[wsm_k])
    K.dma(POOL, wsc, aup[64:128, :, :], aup_d.rearrange("d k n -> k d n"), W=[wsm_k])
    K.dma(POOL, wsc, gup[:], gup_d, W=[wsm_k])
    K.dma(POOL, wsc, wrt[:], wr_d.rearrange("(j p) n -> p j n", p=128), W=[wsm_k])

    ckpt('small')
    modT = K.sb([128, 48, NSEQ], F32, "modT")
    mod_k = Tok()
    S1 = K.sb([128, KD, NSEQ], F32, "S1")
    S2 = K.sb([128, KD, NSEQ], F32, "S2")
    with K.scope():
        cond = K.sb([128, KD, NSEQ], F32, "cond")
        cond_k = Tok()
        K.op(ACT, lambda e: e.activation(out=cond[:], in_=cT[:], func=AF.Silu), R=[cT_k], W=[cond_k])
        wa = [K.sb([128, KD, 512], F32, "wa") for _ in range(2)]
        wa_k = [Tok(), Tok()]
        wac = [K.dmac("wa0"), K.dmac("wa1")]
        pm = [K.ps([128, 4, NSEQ], F32, "pm") for _ in range(2)]
        pm_k = [Tok(), Tok()]
        for g in range(12):
            b = g % 2
            K.dma(SP, wac[b], wa[b][:], wada_d[:, g * 512:(g + 1) * 512].rearrange("(j p) n -> p j n", p=128), W=[wa_k[b]])
            for mm_ in range(4):
                K.mm(pm[b][:, mm_, :], [(wa[b][:, j, mm_ * 128:(mm_ + 1) * 128], cond[:, j, :]) for j in range(KD)],
                     R=[wa_k[b], cond_k], W=[pm_k[b]])
            K.op(DVE, lambda e, b=b, g=g: e.tensor_tensor(out=modT[:, g * 4:(g + 1) * 4, :], in0=pm[b][:],
                                                          in1=bc(bada[:, g * 4:(g + 1) * 4].unsqueeze(2), [128, 4, NSEQ]), op=ALU.add),
                 R=[pm_k[b], bada_k], W=[mod_k])
        for (Sx, gv, gk, off) in ((S1, gmix, gmix_k, 8), (S2, gffn, gffn_k, 32)):
            K.op(DVE, lambda e, Sx=Sx, off=off: e.tensor_scalar(out=Sx[:], in0=modT[:, off:off + 8, :], scalar1=1.0, scalar2=None, op0=ALU.add),
                 R=[mod_k], W=[mod_k])
            K.op(DVE, lambda e, Sx=Sx, gv=gv: e.tensor_tensor(out=Sx[:], in0=Sx[:], in1=bc(gv[:].unsqueeze(2), [128, KD, NSEQ]), op=ALU.mult),
                 R=[mod_k, gk], W=[mod_k])
    dump("modT", modT[:].rearrange("p m b -> p (m b)"), [128, 48 * NSEQ], R=[mod_k])

    ckpt('phaseA')
    catT = K.sb([128, KD, S], BF16, "catT")
    cat_k = [Tok() for _ in range(KD)]
    def bcast_rows(b, bct, bct_k, srcs):
        with K.scope():
            dg = [K.sb([128, 128], F32, "dg") for _ in range(2)]
            dg_k = [Tok(), Tok()]
            pb_ = [K.ps([128, 512], F32, "pb") for _ in range(2)]
            pb_k = [Tok(), Tok()]
            i = 0
            for r, (src, off) in enumerate(srcs):
                for half in range(2):
                    pi = (r * 2 + half) % 2
                    for jj in range(4):
                        j = half * 4 + jj
                        di = i % 2
                        i += 1
                        K.op(DVE, lambda e, di=di, src=src, off=off, j=j: e.tensor_scalar(
                            out=dg[di][:], in0=identf[:], scalar1=src[:, off + j, b:b + 1], scalar2=None, op0=ALU.mult),
                            R=[cst, mod_k], W=[dg_k[di]])
                        K.mm(pb_[pi][:, jj * 128:(jj + 1) * 128], [(onesf[:], dg[di][:])], R=[dg_k[di], cst], W=[pb_k[pi]])
                    K.op(ACT, lambda e, pi=pi, r=r, half=half: e.activation(out=bct[:, r, half * 512:(half + 1) * 512], in_=pb_[pi][:], func=AF.Copy),
                         R=[pb_k[pi]], W=[bct_k])

    for b in range(NSEQ):
        tok0 = b * S
        with K.scope():
            hT = K.sb([128, KD, S], BF16, "hT")
            hT_k = [Tok() for _ in range(NT)]
            with K.scope():
                xt = [K.sb([128, D], F32, "xt") for _ in range(2)]
                xt_k = [Tok(), Tok()]
                xc = [K.dmac("x0"), K.dmac("x1")]
                xn = [K.sb([128, D], BF16, "xn") for _ in range(2)]
                xn_k = [Tok(), Tok()]
                junk = K.sb([128, D], BF16, "junk")
                junk_k = Tok()
                st_ = [K.sb([128, 2], F32, "st") for _ in range(2)]
                st_k = [Tok(), Tok()]
                pt = [K.ps([128, KD, 128], BF16, "pt") for _ in range(2)]
                pt_k = [Tok(), Tok()]
                for i in range(NT):
                    if i % 4 == 0 and i > 0:
                        K.barrier()
                    u = i % 2
                    K.dma(SP, xc[u], xt[u][:], x_d[tok0 + i * 128: tok0 + (i + 1) * 128, :], W=[xt_k[u]])
                    K.op(ACT, lambda e, u=u: e.activation(out=junk[:], in_=xt[u][:], func=AF.Square, accum_out=st_[u][:, 0:1]),
                         R=[xt_k[u]], W=[junk_k, st_k[u]])
                    K.op(ACT, lambda e, u=u: e.activation(out=st_[u][:, 1:2], in_=st_[u][:, 0:1], func=AF.Sqrt, scale=1.0 / D, bias=NORM_EPS),
                         R=[st_k[u]], W=[st_k[u]])
                    K.op(DVE, lambda e, u=u: e.reciprocal(out=st_[u][:, 1:2], in_=st_[u][:, 1:2]), R=[st_k[u]], W=[st_k[u]])
                    K.op(DVE, lambda e, u=u: e.tensor_scalar(out=xn[u][:], in0=xt[u][:], scalar1=st_[u][:, 1:2], scalar2=None, op0=ALU.mult),
                         R=[xt_k[u], st_k[u]], W=[xn_k[u]])
                    for j in range(KD):
                        K.tr(pt[u][:, j, :], xn[u][:, j * 128:(j + 1) * 128], identb[:], R=[xn_k[u], cst], W=[pt_k[u]], inc=(j == KD - 1))
                    for j in range(KD):
                        eng = ACT if j % 2 == 0 else DVE
                        if eng is ACT:
                            K.op(ACT, lambda e, j=j, u=u, i=i: e.activation(out=hT[:, j, i * 128:(i + 1) * 128], in_=pt[u][:, j, :], func=AF.Identity,
                                                                             scale=S1[:, j, b:b + 1], bias=modT[:, j, b:b + 1]),
                                 R=[pt_k[u], mod_k], W=[hT_k[i]])
                        else:
                            K.op(DVE, lambda e, j=j, u=u, i=i: e.tensor_scalar(out=hT[:, j, i * 128:(i + 1) * 128], in0=pt[u][:, j, :],
                                                                                scalar1=S1[:, j, b:b + 1], scalar2=modT[:, j, b:b + 1], op0=ALU.mult, op1=ALU.add),
                                 R=[pt_k[u], mod_k], W=[hT_k[i]])
            if b == 0:
                dump("hT", hT[:, 0, :], [128, S], R=hT_k)

            ckpt('B1')
            with K.scope():
                watt = K.sb([128, KD, 768], BF16, "watt")
                watt_k = Tok()
                K.dma(POOL, K.dmac("watt"), watt[:], win_d[:, 0:768].rearrange("(j p) n -> p j n", p=128), W=[watt_k])
                qT = K.sb([128, 4, S], BF16, "qT")
                qT_k = [Tok() for _ in range(NT)]
                kT2 = K.sb([128, 2, S], BF16, "kT2")
                kT_k = [Tok() for _ in range(NT)]
                vaug = K.sb([128, NT, 2, HD + 1], BF16, "vaug")
                v_k = [Tok() for _ in range(NT)]
                K.op(DVE, lambda e: e.memset(vaug[:], 1.0), W=v_k)
                with K.scope():
                    pq = K.ps([128, 512], F32, "pq")
                    pq_k = Tok()
                    pkv = K.ps([128, 256], F32, "pkv")
                    pkv_k = Tok()
                    ptq = K.ps([128, 4, 128], BF16, "ptq")
                    ptq_k = Tok()
                    ptk = K.ps([128, 2, 128], BF16, "ptk")
                    ptk_k = Tok()
                    qk = K.sb([128, 10, HD], F32, "qk")
                    qk_k = Tok()
                    sq = K.sb([128, 10, HD], F32, "sq")
                    sq_k = Tok()
                    ss = K.sb([128, 10], F32, "ss")
                    ss_k = Tok()
                    t1 = K.sb([128, 10, 32], F32, "t1")
                    t2 = K.sb([128, 10, 32], F32, "t2")
                    t3 = K.sb([128, 10, 32], F32, "t3")
                    t4 = K.sb([128, 10, 32], F32, "t4")
                    t_k = [Tok() for _ in range(4)]
                    qr = K.sb([128, 8, HD], BF16, "qr")
                    qr_k = Tok()
                    kr = K.sb([128, 2, 2, HD], BF16, "kr")
                    kr_k = Tok()
                    for i in range(NT):
                        if i % 4 == 0 and i > 0:
                            K.barrier()
                        sl = slice(i * 128, (i + 1) * 128)
                        K.mm(pq[:], [(hT[:, j, sl], watt[:, j, 0:512]) for j in range(KD)], R=[hT_k[i], watt_k], W=[pq_k])
                        K.mm(pkv[:], [(hT[:, j, sl], watt[:, j, 512:768]) for j in range(KD)], R=[hT_k[i], watt_k], W=[pkv_k])
                        K.op(ACT, lambda e: e.activation(out=qk[:, 0:8, :].rearrange("p h d -> p (h d)"), in_=pq[:], func=AF.Copy), R=[pq_k], W=[qk_k])
                        K.op(ACT, lambda e: e.activation(out=qk[:, 8:10, :].rearrange("p h d -> p (h d)"), in_=pkv[:, 0:128], func=AF.Copy), R=[pkv_k], W=[qk_k])
                        K.op(ACT, lambda e, i=i: e.activation(out=vaug[:, i, :, 0:HD], in_=pkv[:, 128:256].rearrange("p (g d) -> p g d", g=2), func=AF.Copy),
                             R=[pkv_k], W=[v_k[i]])
                        K.op(DVE, lambda e: e.tensor_tensor(out=sq[:], in0=qk[:], in1=qk[:], op=ALU.mult), R=[qk_k], W=[sq_k])
                        K.op(DVE, lambda e: e.tensor_reduce(out=ss[:], in_=sq[:], axis=AX.X, op=ALU.add), R=[sq_k], W=[ss_k])
                        K.op(ACT, lambda e: e.activation(out=ss[:], in_=ss[:], func=AF.Sqrt, scale=1.0 / HD, bias=NORM_EPS), R=[ss_k], W=[ss_k])
                        K.op(DVE, lambda e: e.reciprocal(out=ss[:], in_=ss[:]), R=[ss_k], W=[ss_k])
                        K.op(DVE, lambda e: e.tensor_tensor(out=qk[:], in0=qk[:], in1=bc(ss[:].unsqueeze(2), [128, 10, HD]), op=ALU.mult),
                             R=[qk_k, ss_k], W=[qk_k])
                        K.op(DVE, lambda e: e.tensor_tensor(out=qk[:], in0=qk[:], in1=gain[:], op=ALU.mult), R=[qk_k, gain_k], W=[qk_k])
                        qv = qk[:].rearrange("p h (k two) -> p h k two", two=2)
                        x0, x1 = qv[:, :, :, 0], qv[:, :, :, 1]
                        cb = bc(cosT[:, i, :].unsqueeze(1), [128, 10, 32])
                        sb_ = bc(sinT[:, i, :].unsqueeze(1), [128, 10, 32])
                        K.op(DVE, lambda e: e.tensor_tensor(out=t1[:], in0=x0, in1=cb, op=ALU.mult), R=[qk_k, cos_k], W=[t_k[0]])
                        K.op(PL, lambda e: e.tensor_tensor(out=t2[:], in0=x1, in1=sb_, op=ALU.mult), R=[qk_k, sin_k], W=[t_k[1]])
                        K.op(DVE, lambda e: e.tensor_tensor(out=t3[:], in0=x0, in1=sb_, op=ALU.mult), R=[qk_k, sin_k], W=[t_k[2]])
                        K.op(PL, lambda e: e.tensor_tensor(out=t4[:], in0=x1, in1=cb, op=ALU.mult), R=[qk_k, cos_k], W=[t_k[3]])
                        qrv = qr[:].rearrange("p h (k two) -> p h k two", two=2)
                        krv = kr[:].rearrange("p g u (k two) -> p g u k two", two=2)
                        K.op(DVE, lambda e: e.tensor_tensor(out=qrv[:, :, :, 0], in0=t1[:, 0:8, :], in1=t2[:, 0:8, :], op=ALU.subtract),
                             R=[t_k[0], t_k[1]], W=[qr_k])
                        K.op(DVE, lambda e: e.tensor_tensor(out=qrv[:, :, :, 1], in0=t3[:, 0:8, :], in1=t4[:, 0:8, :], op=ALU.add),
                             R=[t_k[2], t_k[3]], W=[qr_k])
                        for u_ in range(2):
                            K.op(PL, lambda e, u_=u_: e.tensor_tensor(out=krv[:, :, u_, :, 0], in0=t1[:, 8:10, :], in1=t2[:, 8:10, :], op=ALU.subtract),
                                 R=[t_k[0], t_k[1]], W=[kr_k])
                            K.op(PL, lambda e, u_=u_: e.tensor_tensor(out=krv[:, :, u_, :, 1], in0=t3[:, 8:10, :], in1=t4[:, 8:10, :], op=ALU.add),
                                 R=[t_k[2], t_k[3]], W=[kr_k])
                        for pr in range(4):
                            K.tr(ptq[:, pr, :], qr[:, 2 * pr:2 * pr + 2, :].rearrange("p h d -> p (h d)"), identb[:], R=[qr_k, cst], W=[ptq_k], inc=(pr == 3))
                        K.op(ACT, lambda e, sl=sl: e.activation(out=qT[:, :, sl], in_=ptq[:], func=AF.Copy), R=[ptq_k], W=[qT_k[i]])
                        for g in range(2):
                            K.tr(ptk[:, g, :], kr[:, g, :, :].rearrange("p u d -> p (u d)"), identb[:], R=[kr_k, cst], W=[ptk_k], inc=(g == 1))
                        K.op(DVE, lambda e, sl=sl: e.tensor_copy(out=kT2[:, :, sl], in_=ptk[:]), R=[ptk_k], W=[kT_k[i]])
                if b == 0:
                    dump("qT", qT[:, 0, :], [128, S], R=qT_k)
                    dump("kT", kT2[:, 0, :], [128, S], R=kT_k)
                ckpt('attproj')
                with K.scope():
                    NPS = 2
                    psc = [K.ps([128, QB], F32, "psc") for _ in range(NPS)]
                    psc_k = [Tok() for _ in range(NPS)]
                    pTa = [K.sb([128, NT, QB], BF16, "pTa") for _ in range(2)]
                    pTa_k = [Tok(), Tok()]
                    oacc = [K.ps([128, QT, 128], F32, "oacc") for _ in range(2)]
                    oacc_k = [Tok(), Tok()]
                    rs = K.sb([128, QT], F32, "rs")
                    rs_k = Tok()
                    otm = K.sb([128, QT, 512], BF16, "otm")
                    otm_k = Tok()
                    pto = K.ps([128, 4, 128], BF16, "pto")
                    pto_k = Tok()
                    it = 0
                    for qb in range(NQB):
                        qsl = slice(qb * QB, (qb + 1) * QB)
                        qtoks = qT_k[qb * QT:(qb + 1) * QT]
                        for hq in range(8):
                            g = hq // 4
                            pb0 = 64 * (hq % 2)
                            pr = hq // 2
                            oa = oacc[hq % 2]
                            oa_k = oacc_k[hq % 2]
                            pa = pTa[hq % 2]
                            pa_k = pTa_k[hq % 2]
                            for kt in range(NT):
                                u = it % NPS
                                it += 1
                                K.mm(psc[u][:], [(kT2[pb0:pb0 + 64, g, kt * 128:(kt + 1) * 128], qT[pb0:pb0 + 64, pr, qsl])],
                                     R=[kT_k[kt]] + qtoks, W=[psc_k[u]])
                                K.op(ACT, lambda e, u=u, pa=pa, kt=kt: e.activation(out=pa[:, kt, :], in_=psc[u][:], func=AF.Exp), R=[psc_k[u]], W=[pa_k])
                            for qt in range(QT):
                                K.mm(oa[:, qt, 0:HD + 1], [(pa[:, kt, qt * 128:(qt + 1) * 128], vaug[:, kt, g, :]) for kt in range(NT)],
                                     R=[pa_k] + v_k, W=[oa_k], inc=(qt == QT - 1))
                            K.op(DVE, lambda e, oa=oa: e.reciprocal(out=rs[:], in_=oa[:, :, HD]), R=[oa_k], W=[rs_k])
                            K.op(DVE, lambda e, oa=oa, hq=hq: e.tensor_tensor(out=otm[:, :, hq * HD:(hq + 1) * HD], in0=oa[:, :, 0:HD],
                                                                               in1=bc(rs[:].unsqueeze(2), [128, QT, HD]), op=ALU.mult),
                                 R=[oa_k, rs_k], W=[otm_k])
                        for qt in range(QT):
                            ti = qb * QT + qt
                            for c in range(4):
                                K.tr(pto[:, c, :], otm[:, qt, c * 128:(c + 1) * 128], identb[:], R=[otm_k, cst], W=[pto_k], inc=(c == 3))
                            K.op(ACT, lambda e, ti=ti: e.activation(out=catT[:, 0:4, ti * 128:(ti + 1) * 128], in_=pto[:], func=AF.Copy),
                                 R=[pto_k], W=cat_k[0:4])
            if b == 0:
                dump("oatt", catT[:, 0, :], [128, S], R=cat_k[0:4])

            ckpt('attcore')
            with K.scope():
                NC = NT
                c_ = DECAY
                wrw = [K.sb([128, KD, 128], BF16, "wrw") for _ in range(1)]
                wrw_k = [Tok() for _ in range(1)]
                wrc = [K.dmac("wrw") for _ in range(1)]
                wri = [0]
                T1 = K.sb([128, S + 2], F32, "T1")
                T2 = K.sb([128, S], F32, "T2")
                T3 = K.sb([128, S], F32, "T3")
                T4 = K.sb([128, S], F32, "T4")
                T_k = [Tok() for _ in range(4)]
                r32 = K.sb([128, S], BF16, "r32")
                k32 = K.sb([128, S], F32, "k32")
                kk32 = K.sb([128, S], F32, "kk32")
                yacc = K.sb([128, S], F32, "yacc")
                bacc = K.sb([128, S], BF16, "bacc")
                r_k_, k_k_, kk_k_, ya_k, ba_k = Tok(), Tok(), Tok(), Tok(), Tok()
                twda = K.sb([128, S], BF16, "twda")
                sg = K.sb([128, S], BF16, "sg")
                vb = K.sb([128, S], BF16, "vb")
                gTb = K.sb([128, S], BF16, "gTb")
                sqb = K.sb([128, S], BF16, "sqb")
                twda_k, sg_k, vb_k, gT_k, sqb_k = Tok(), Tok(), Tok(), Tok(), Tok()
                ART = K.sb([128, 2, NC, 2, 128], BF16, "ART")
                BT = K.sb([128, S], BF16, "BT")
                KT = K.sb([128, S], BF16, "KT")
                ART_k, BT_k, KT_k = Tok(), Tok(), Tok()
                gC = K.sb([128, NC], F32, "gC")
                gC_k = Tok()
                ppj = [K.ps([128, TB], F32, "ppj") for _ in range(2)]
                ppj_k = [Tok(), Tok()]
                pji = [0]
                K.op(DVE, lambda e: e.memset(T1[:, 0:1], 0.0), W=[T_k[0]])
                K.op(DVE, lambda e: e.memset(T1[:, S + 1:S + 2], 0.0), W=[T_k[0]])

                def project_shift(m, dst_fn):
                    wi = 0
                    wri[0] += 1
                    K.dma(POOL, wrc[wi], wrw[wi][:], win_d[:, 768 + m * 128: 768 + (m + 1) * 128].rearrange("(j p) n -> p j n", p=128), W=[wrw_k[wi]])
                    for tb in range(NTB):
                        u = pji[0] % 2
                        pji[0] += 1
                        K.mm(ppj[u][:], [(wrw[wi][:, j, :], hT[:, j, tb * TB:(tb + 1) * TB]) for j in range(KD)],
                             R=[wrw_k[wi]] + hT_k[tb * (TB // 128):(tb + 1) * (TB // 128)], W=[ppj_k[u]])
                        K.op(ACT, lambda e, u=u, tb=tb: e.activation(out=T1[:, 1 + tb * TB:1 + (tb + 1) * TB], in_=ppj[u][:], func=AF.Copy),
                             R=[ppj_k[u]], W=[T_k[0]])
                    K.op(PL, lambda e: e.tensor_tensor(out=T2[:], in0=T1[:, 0:S], in1=T1[:, 2:S + 2], op=ALU.add), R=[T_k[0]], W=[T_k[1]])
                    K.op(DVE, lambda e: e.tensor_scalar(out=T2[:], in0=T2[:], scalar1=hmu[:, m:m + 1], scalar2=None, op0=ALU.mult), R=[T_k[1], mu_k], W=[T_k[1]])
                    K.op(DVE, lambda e: e.scalar_tensor_tensor(out=T3[:], in0=T1[:, 1:S + 1], scalar=omm[:, m:m + 1], in1=T2[:], op0=ALU.mult, op1=ALU.add),
                         R=[T_k[0], T_k[1], mu_k], W=[T_k[2]])
                    if b == 0 and m == 4:
                        dump("T1k", T1[:, 0:S], [128, S], R=[T_k[0]])
                        dump("T2k", T2[:], [128, S], R=[T_k[1]])
                        dump("T3k", T3[:], [128, S], R=[T_k[2]])
                        dump("hmu", hmu[:], [128, 14], R=[mu_k])
                        dump("omm", omm[:], [128, 14], R=[mu_k])
                    dst_fn()

                def d12():
                    K.op(ACT, lambda e: e.activation(out=twda[0:64, :], in_=T3[0:64, :], func=AF.Tanh), R=[T_k[2]], W=[twda_k])
                    K.op(DVE, lambda e: e.tensor_copy(out=twda[64:128, :], in_=T3[64:128, :]), R=[T_k[2]], W=[twda_k])
                project_shift(12, d12)

                def d13():
                    K.op(ACT, lambda e: e.activation(out=sg[:], in_=T3[:], func=AF.Sigmoid), R=[T_k[2]], W=[sg_k])
                project_shift(13, d13)

                ckpt('lora')
                pbd = [K.ps([128, TB], F32, "pbd") for _ in range(2)]
                pbd_k = [Tok(), Tok()]
                bdi = [0]
                nck = [0]
                ptk3 = K.ps([128, 3, 128], BF16, "ptk3")
                ptk3_k = Tok()
                pP = K.ps([128, 2, 256], F32, "pP")
                pP_k = Tok()
                pQ = K.ps([128, 2, 128], F32, "pQ")
                pQ_k = Tok()
                pZY = K.ps([128, 512], F32, "pZY")
                pZ = pZY[:, 0:256].rearrange("p (a v) -> p a v", v=64)
                pZY_k = Tok()
                pZ_k = [pZY_k, pZY_k]
                pY = pZY[:, 256:448]
                pY_k = [pZY_k, pZY_k]
                tok3s = [K.sb([128, 3, 2, 128], BF16, "tok3") for _ in range(2)]
                tok3s_k = [Tok(), Tok()]
                for q_ in range(2):
                    K.op(DVE, lambda e, q_=q_: e.memset(tok3s[q_][:], 0.0), W=[tok3s_k[q_]])
                Ub2 = K.sb([128, 2, 128], BF16, "Ub2")
                Ub2_k = Tok()
                K.op(DVE, lambda e: e.memset(Ub2[:], 0.0), W=[Ub2_k])
                Hbd = K.sb([128, 128], BF16, "Hbd")
                Hbd_k = Tok()
                M1s = [K.sb([128, 2, 256], BF16, "M1") for _ in range(2)]
                M2s = [K.sb([128, 2, 256], BF16, "M2") for _ in range(2)]
                M1s_k, M2s_k = [Tok(), Tok()], [Tok(), Tok()]
                XAs = [[K.sb([128, 2, 2, 128], BF16, "XA") for _ in range(2)] for _ in range(2)]
                XAs_k = [[Tok(), Tok()], [Tok(), Tok()]]
                PAs = [[K.sb([128, 2, 128], BF16, "PA") for _ in range(2)] for _ in range(2)]
                PAs_k = [[Tok(), Tok()], [Tok(), Tok()]]
                Zb = K.sb([128, 2, 64], BF16, "Zb")
                Ub = K.sb([128, 2, 64], BF16, "Ub")
                Zb_k, Ub_k = Tok(), Tok()
                H32 = K.sb([128, 64], F32, "H32")
                Hb = K.sb([128, 64], BF16, "Hb")
                Htmp = K.sb([128, 64], F32, "Htmp")
                H_k, Hb_k, Ht_k = Tok(), Tok(), Tok()
                identb2 = bc(identb[:].unsqueeze(1), [128, 2, 128])

                def bdsum(src_bf, src_k, consume):
                    for tb in range(NTB):
                        u = bdi[0] % 2
                        bdi[0] += 1
                        K.mm(pbd[u][:], [(bdones[:], src_bf[:, tb * TB:(tb + 1) * TB])], R=[src_k, cst], W=[pbd_k[u]])
                        consume(pbd[u], pbd_k[u], tb)

                import os as _os
                for c4 in [int(q) for q in _os.environ.get('C4LIST', '0,1,2,3').split(',')]:
                    K.barrier()
                    csl = slice(c4 * 128, (c4 + 1) * 128)
                    project_shift(c4, lambda: K.op(PL, lambda e: e.tensor_copy(out=r32[:], in_=T3[:]), R=[T_k[2]], W=[r_k_]))
                    project_shift(4 + c4, lambda: K.op(PL, lambda e: e.tensor_copy(out=k32[:], in_=T3[:]), R=[T_k[2]], W=[k_k_]))
                    project_shift(8 + c4, lambda: K.op(ACT, lambda e: e.activation(out=vb[:], in_=T3[:], func=AF.Copy), R=[T_k[2]], W=[vb_k]))
                    ckpt('rw_proj%d' % c4)
                    for tb in range(NTB):
                        u = pji[0] % 2
                        pji[0] += 1
                        K.mm(ppj[u][:], [(gup[:, csl], sg[:, tb * TB:(tb + 1) * TB])], R=[wsm_k, sg_k], W=[ppj_k[u]])
                        K.op(ACT, lambda e, u=u, tb=tb: e.activation(out=gTb[:, tb * TB:(tb + 1) * TB], in_=ppj[u][:], func=AF.Copy), R=[ppj_k[u]], W=[gT_k])
                    K.op(DVE, lambda e: e.tensor_scalar(out=kk32[:], in0=k32[:], scalar1=kkp[:, c4:c4 + 1], scalar2=None, op0=ALU.mult), R=[k_k_, kkp_k], W=[kk_k_])
                    K.op(PL, lambda e: e.tensor_tensor(out=sqb[:], in0=kk32[:], in1=kk32[:], op=ALU.mult), R=[kk_k_], W=[sqb_k])

                    def cons_kk(ps_, pk, tb):
                        tsl = slice(tb * TB, (tb + 1) * TB)
                        K.op(ACT, lambda e: e.activation(out=T4[:, tsl], in_=ps_[:], func=AF.Sqrt, bias=1e-24), R=[pk], W=[T_k[3]])
                        K.op(DVE, lambda e: e.reciprocal(out=T4[:, tsl], in_=T4[:, tsl]), R=[T_k[3]], W=[T_k[3]])
                    bdsum(sqb, sqb_k, cons_kk)
                    K.op(DVE, lambda e: e.tensor_tensor(out=kk32[:], in0=kk32[:], in1=T4[:], op=ALU.mult), R=[kk_k_, T_k[3]], W=[kk_k_])
                    if b == 0 and c4 == 0:
                        dump("kk", kk32[:], [128, S], R=[kk_k_])
                        pass

                    ckpt('rw_kk%d' % c4)
                    for d in range(2):
                        lw = T1[:, 1:S + 1]
                        for tb in range(NTB):
                            tsl = slice(tb * TB, (tb + 1) * TB)
                            u = pji[0] % 2
                            pji[0] += 1
                            K.mm(ppj[u][:], [(wup[0:64, d, csl], twda[0:64, tsl])], R=[wsm_k, twda_k], W=[ppj_k[u]])
                            K.op(ACT, lambda e, u=u, tsl=tsl: e.activation(out=lw[:, tsl], in_=ppj[u][:], func=AF.Sigmoid, bias=w0[:, d, c4:c4 + 1]),
                                 R=[ppj_k[u], w0_k], W=[T_k[0]])
                        for n_ in range(NC):
                            K.op(DVE, lambda e, n_=n_: e.tensor_tensor_scan(out=T2[:, n_ * 128:(n_ + 1) * 128], data0=onesf[:], data1=lw[:, n_ * 128:(n_ + 1) * 128],
                                                                        initial=0.0, op0=ALU.mult, op1=ALU.add),
                                 R=[T_k[0], cst], W=[T_k[1]])
                        cs3 = T2[:].rearrange("p (c t) -> p c t", t=128)
                        K.op(ACT, lambda e: e.activation(out=gC[:], in_=cs3[:, :, 127], func=AF.Exp, scale=-c_), R=[T_k[1]], W=[gC_k])
                        if d == 0:
                            K.op(DVE, lambda e: e.tensor_tensor(out=lw, in0=T2[:], in1=lw, op=ALU.subtract), R=[T_k[0], T_k[1]], W=[T_k[0]])
                            gexc, gexc_k, ginc, ginc_k = lw, T_k[0], T2[:], T_k[1]
                        else:
                            K.op(PL, lambda e: e.tensor_copy(out=T4[:].rearrange("p (c t) -> p c t", t=128), in_=bc(cs3[:, :, 127:128], [128, NC, 128])),
                                 R=[T_k[1]], W=[T_k[3]])
                            K.op(DVE, lambda e: e.tensor_tensor(out=T2[:], in0=T4[:], in1=T2[:], op=ALU.subtract), R=[T_k[1], T_k[3]], W=[T_k[1]])
                            K.op(DVE, lambda e: e.tensor_tensor(out=lw, in0=lw, in1=T2[:], op=ALU.add), R=[T_k[0], T_k[1]], W=[T_k[0]])
                            gexc, gexc_k, ginc, ginc_k = T2[:], T_k[1], lw, T_k[0]
                        A3 = [ART[:, 0, :, 0, :], ART[:, 1, :, 0, :]]
                        R3 = [ART[:, 0, :, 1, :], ART[:, 1, :, 1, :]]
                        v3 = lambda ap: ap.rearrange("p (c t) -> p c t", t=128)
                        K.op(ACT, lambda e: e.activation(out=gexc, in_=gexc, func=AF.Exp, scale=-c_), R=[gexc_k], W=[gexc_k])
                        for hh in range(2):
                            K.op(DVE, lambda e, hh=hh: e.scalar_tensor_tensor(out=A3[hh], in0=v3(kk32[:]), scalar=hmask[:, 2 + hh:3 + hh], in1=v3(gexc), op0=ALU.mult, op1=ALU.mult),
                                 R=[kk_k_, gexc_k, cst], W=[ART_k])
                        K.op(ACT, lambda e: e.activation(out=T3[:], in_=ginc, func=AF.Exp, scale=-c_), R=[ginc_k], W=[T_k[2]])
                        for hh in range(2):
                            K.op(DVE, lambda e, hh=hh: e.scalar_tensor_tensor(out=R3[hh], in0=v3(r32[:]), scalar=hmask[:, hh:hh + 1], in1=v3(T3[:]), op0=ALU.mult, op1=ALU.mult),
                                 R=[r_k_, T_k[2], cst], W=[ART_k])
                        K.op(ACT, lambda e: e.activation(out=ginc, in_=ginc, func=AF.Exp, scale=c_), R=[ginc_k], W=[ginc_k])
                        for tb in range(NTB):
                            tsl = slice(tb * TB, (tb + 1) * TB)
                            u = pji[0] % 2
                            pji[0] += 1
                            K.mm(ppj[u][:], [(aup[64:128, d, csl], twda[64:128, tsl])], R=[wsm_k, twda_k], W=[ppj_k[u]])
                            K.op(ACT, lambda e, u=u, tsl=tsl: e.activation(out=T3[:, tsl], in_=ppj[u][:], func=AF.Sigmoid, bias=a0[:, d, c4:c4 + 1]),
                                 R=[ppj_k[u], a0_k], W=[T_k[2]])
                        Tg = gexc
                        K.op(DVE, lambda e: e.tensor_tensor(out=Tg, in0=kk32[:], in1=T3[:], op=ALU.mult), R=[kk_k_, T_k[2], ART_k], W=[gexc_k])
                        K.op(DVE, lambda e: e.tensor_tensor(out=BT[:], in0=Tg, in1=ginc, op=ALU.mult), R=[gexc_k, ginc_k], W=[BT_k])
                        K.op(DVE, lambda e: e.tensor_scalar(out=Tg, in0=T3[:], scalar1=kap[:, c4:c4 + 1], scalar2=omka[:, c4:c4 + 1], op0=ALU.mult, op1=ALU.add),
                             R=[T_k[2], kap_k, BT_k], W=[gexc_k])
                        K.op(DVE, lambda e: e.tensor_tensor(out=Tg, in0=Tg, in1=k32[:], op=ALU.mult), R=[gexc_k, k_k_], W=[gexc_k])
                        K.op(PL, lambda e: e.tensor_tensor(out=KT[:], in0=Tg, in1=ginc, op=ALU.mult), R=[gexc_k, ginc_k], W=[KT_k])
                        K.op(DVE, lambda e: e.scalar_tensor_tensor(out=sqb[:], in0=r32[:], scalar=rkp[:, c4:c4 + 1], in1=Tg, op0=ALU.mult, op1=ALU.mult),
                             R=[r_k_, gexc_k, rkp_k], W=[sqb_k])

                        def cons_b(ps_, pk, tb, d=d):
                            tsl = slice(tb * TB, (tb + 1) * TB)
                            if d == 0:
                                K.op(DVE, lambda e: e.tensor_tensor(out=bacc[:, tsl], in0=ps_[:], in1=vb[:, tsl], op=ALU.mult), R=[pk, vb_k], W=[ba_k])
                            else:
                                K.op(DVE, lambda e: e.tensor_tensor(out=T3[:, tsl], in0=ps_[:], in1=vb[:, tsl], op=ALU.mult), R=[pk, vb_k], W=[T_k[2]])
                                K.op(PL, lambda e: e.tensor_tensor(out=bacc[:, tsl], in0=bacc[:, tsl], in1=T3[:, tsl], op=ALU.add), R=[T_k[2]], W=[ba_k])
                        bdsum(sqb, sqb_k, cons_b)
                        if b == 0 and c4 == 0:
                            dump(f"AT{d}", ART[:, 0, :, 0, :], [128, NC, 128], R=[ART_k])
                            dump(f"BT{d}", BT[:], [128, S], R=[BT_k])

                        ckpt('rw_prep%d_%d' % (c4, d))
                        K.op(DVE, lambda e: e.memset(H32[:], 0.0), W=[H_k])
                        K.op(DVE, lambda e: e.memset(Hb[:], 0.0), W=[Hb_k])
                        K.op(DVE, lambda e: e.memset(Hbd[:], 0.0), W=[Hbd_k])
                        order = range(NC) if d == 0 else range(NC - 1, -1, -1)
                        hs = [slice(0, 64), slice(64, 128)]

                        def prep(n, q, d=d):
                            nsl = slice(n * 128, (n + 1) * 128)
                            tk, tk_k = tok3s[q], tok3s_k[q]
                            m1, m1_k, m2, m2_k = M1s[q], M1s_k[q], M2s[q], M2s_k[q]
                            xa, xa_k, pa, pa_k = XAs[q], XAs_k[q], PAs[q], PAs_k[q]
                            K.tr(ptk3[:, 0, :], BT[:, nsl], identb[:], R=[BT_k, cst], W=[ptk3_k], inc=False)
                            K.tr(ptk3[:, 1, :], KT[:, nsl], identb[:], R=[KT_k], W=[ptk3_k], inc=False)
                            K.tr(ptk3[:, 2, :], vb[:, nsl], identb[:], R=[vb_k], W=[ptk3_k])
                            for hh in range(2):
                                K.op(ACT if hh == 0 else DVE, (lambda e, hh=hh: e.activation(out=tk[:, :, hh, hh * 64:(hh + 1) * 64], in_=ptk3[:, :, hh * 64:(hh + 1) * 64], func=AF.Copy)) if hh == 0 else
                                     (lambda e, hh=hh: e.tensor_copy(out=tk[:, :, hh, hh * 64:(hh + 1) * 64], in_=ptk3[:, :, hh * 64:(hh + 1) * 64])), R=[ptk3_k], W=[tk_k])
                            yield
                            for hh in range(2):
                                K.mm(pP[:, hh, :], [(BT[:, nsl], ART[:, hh, n, :, :].rearrange("p a t -> p (a t)"))], R=[BT_k, ART_k], W=[pP_k], inc=(hh == 1))
                            for hh in range(2):
                                K.op(DVE, lambda e, hh=hh: e.tensor_tensor(out=m1[:, hh, :], in0=pP[:, hh, :], in1=MP[d][:], op=ALU.mult), R=[pP_k, cst], W=[m1_k])
                            yield
                            for hh in range(2):
                                K.mm(pP[:, hh, :], [(KT[:, nsl], ART[:, hh, n, :, :].rearrange("p a t -> p (a t)"))], R=[KT_k, ART_k], W=[pP_k], inc=(hh == 1))
                            for hh in range(2):
                                K.op(DVE, lambda e, hh=hh: e.tensor_tensor(out=m2[:, hh, :], in0=pP[:, hh, :], in1=MP[d][:], op=ALU.mult), R=[pP_k, cst], W=[m2_k])
                            yield
                            for hh in range(2):
                                K.mm(pQ[:, hh, :], [(ART[:, hh, n, 0, :], BT[:, nsl])], R=[BT_k, ART_k], W=[pQ_k], inc=(hh == 1))
                            for hh in range(2):
                                K.op(DVE, lambda e, hh=hh: e.tensor_tensor(out=pa[0][:, hh, :], in0=pQ[:, hh, :], in1=ML[d][:], op=ALU.mult), R=[pQ_k, cst], W=[pa_k[0]])
                            yield
                            K.op(PL, lambda e: e.tensor_tensor(out=xa[1][:, :, 1, :], in0=m1[:, :, 0:128], in1=identb2, op=ALU.add), R=[m1_k, cst], W=[xa_k[1]])
                            for hh in range(2):
                                K.mm(pP[:, hh, 0:128], [(pa[0][:, hh, :], m1[:, hh, 0:128])], R=[pa_k[0], m1_k], W=[pP_k], inc=(hh == 1))
                            K.op(ACT, lambda e: e.activation(out=xa[1][:, :, 0, :], in_=pP[:, :, 0:128], func=AF.Copy), R=[pP_k], W=[xa_k[1]])
                            for hh in range(2):
                                K.mm(pQ[:, hh, :], [(m1[:, hh, 0:128], pa[0][:, hh, :])], R=[pa_k[0], m1_k], W=[pQ_k], inc=(hh == 1))
                            K.op(DVE, lambda e: e.tensor_copy(out=pa[1][:], in_=pQ[:]), R=[pQ_k], W=[pa_k[1]])
                            yield
                            cur = 1
                            for lev in range(1, 7):
                                nx = 1 - cur
                                if lev < 6:
                                    for hh in range(2):
                                        K.mm(pP[:, hh, :], [(pa[cur][:, hh, :], xa[cur][:, hh, :, :].rearrange("p a t -> p (a t)"))],
                                             R=[pa_k[cur], xa_k[cur]], W=[pP_k], inc=(hh == 1))
                                    K.op(ACT, lambda e, nx=nx: e.activation(out=xa[nx][:, :, 0, :], in_=pP[:, :, 0:128], func=AF.Copy), R=[pP_k], W=[xa_k[nx]])
                                    K.op(DVE, lambda e, nx=nx, cur=cur: e.tensor_tensor(out=xa[nx][:, :, 1, :], in0=pP[:, :, 128:256], in1=xa[cur][:, :, 1, :], op=ALU.add),
                                         R=[pP_k, xa_k[cur]], W=[xa_k[nx]])
                                    for hh in range(2):
                                        K.mm(pQ[:, hh, :], [(xa[cur][:, hh, 0, :], pa[cur][:, hh, :])], R=[pa_k[cur], xa_k[cur]], W=[pQ_k], inc=(hh == 1))
                                    K.op(DVE, lambda e, nx=nx: e.tensor_copy(out=pa[nx][:], in_=pQ[:]), R=[pQ_k], W=[pa_k[nx]])
                                else:
                                    for hh in range(2):
                                        K.mm(pP[:, hh, 128:256], [(pa[cur][:, hh, :], xa[cur][:, hh, 1, :])], R=[pa_k[cur], xa_k[cur]], W=[pP_k], inc=(hh == 1))
                                    K.op(DVE, lambda e, nx=nx, cur=cur: e.tensor_tensor(out=xa[nx][:, :, 1, :], in0=pP[:, :, 128:256], in1=xa[cur][:, :, 1, :], op=ALU.add),
                                         R=[pP_k, xa_k[cur]], W=[xa_k[nx]])
                                cur = nx
                                yield
                            assert cur == 1

                        def chain(n, q, d=d):
                            nsl = slice(n * 128, (n + 1) * 128)
                            tk, tk_k = tok3s[q], tok3s_k[q]
                            m1, m1_k, m2, m2_k = M1s[q], M1s_k[q], M2s[q], M2s_k[q]
                            Wf, Wf_k = XAs[q][1], XAs_k[q][1]
                            for hh in range(2):
                                K.mm(pZ[:, hh, :], [(ART[:, hh, n, 0, :], Hb[:, :]), (m2[:, hh, 0:128], tk[:, 2, hh, hs[hh]])],
                                     R=[ART_k, Hb_k, m2_k, tk_k], W=[pZ_k[0]])
                            K.op(ACT, lambda e: e.activation(out=Zb[:], in_=pZ[:, 0:2, :], func=AF.Copy), R=[pZ_k[0]], W=[Zb_k])
                            yield
                            for hh in range(2):
                                K.mm(pZ[:, 2 + hh, :], [(Wf[:, hh, 1, :], Zb[:, hh, :])], R=[Wf_k, Zb_k], W=[pZ_k[1]])
                            K.op(ACT, lambda e: e.activation(out=Ub[:], in_=pZ[:, 2:4, :], func=AF.Copy), R=[pZ_k[1]], W=[Ub_k])
                            for hh in range(2):
                                K.op(DVE, lambda e, hh=hh: e.tensor_copy(out=Ub2[:, hh, hh * 64:(hh + 1) * 64], in_=Ub[:, hh, :]), R=[Ub_k], W=[Ub2_k])
                            yield
                            K.mm(pY[:, 128:192], [(tk[:, 0, 0, :], Ub[:, 0, :]), (tk[:, 0, 1, :], Ub[:, 1, :]),
                                                  (tk[:, 1, 0, :], tk[:, 2, 0, 0:64]), (tk[:, 1, 1, :], tk[:, 2, 1, 64:128])],
                                 R=[tk_k, Ub_k], W=[pY_k[1]])
                            K.mm(pY[:, 0:128], [(Hbd[:], ART[:, 0, n, 1, :]), (Hbd[:], ART[:, 1, n, 1, :]),
                                                (Ub2[:, 0, :], m1[:, 0, 128:256]), (Ub2[:, 1, :], m1[:, 1, 128:256]),
                                                (tk[:, 2, 0, :], m2[:, 0, 128:256]), (tk[:, 2, 1, :], m2[:, 1, 128:256])],
                                 R=[Hbd_k, ART_k, Ub2_k, m1_k, m2_k, tk_k], W=[pY_k[0]])
                            K.op(DVE, lambda e: e.tensor_tensor(out=Htmp[:], in0=pY[:, 128:192], in1=H32[:], op=ALU.add), R=[pY_k[1], H_k], W=[Ht_k])
                            if d == 0:
                                K.op(DVE, lambda e, nsl=nsl: e.tensor_copy(out=yacc[:, nsl], in_=pY[:, 0:128]), R=[pY_k[0]], W=[ya_k])
                            else:
                                K.op(DVE, lambda e, nsl=nsl: e.tensor_tensor(out=yacc[:, nsl], in0=pY[:, 0:128], in1=yacc[:, nsl], op=ALU.add), R=[pY_k[0], ya_k], W=[ya_k])
                            yield
                            K.op(DVE, lambda e, n=n: e.tensor_scalar(out=H32[:], in0=Htmp[:], scalar1=gC[:, n:n + 1], scalar2=None, op0=ALU.mult), R=[Ht_k, gC_k], W=[H_k])
                            K.op(ACT, lambda e, n=n: e.activation(out=Hb[:], in_=Htmp[:], func=AF.Copy, scale=gC[:, n:n + 1]), R=[Ht_k, gC_k], W=[Hb_k])
                            for hh in range(2):
                                K.op(PL, lambda e, hh=hh: e.tensor_copy(out=Hbd[hs[hh], hh * 64:(hh + 1) * 64], in_=H32[hs[hh], :]), R=[H_k], W=[Hbd_k])
                            yield

                        order = list(order)
                        for _ in prep(order[0], 0):
                            pass
                        for kk_ in range(len(order)):
                            gens = [chain(order[kk_], kk_ % 2)]
                            if kk_ + 1 < len(order):
                                gens.append(prep(order[kk_ + 1], (kk_ + 1) % 2))
                            while gens:
                                for g_ in list(gens):
                                    try:
                                        next(g_)
                                    except StopIteration:
                                        gens.remove(g_)
                            nck[0] += 1
                    if b == 0 and c4 == 0:
                        dump("yacc", yacc[:], [128, S], R=[ya_k])
                        dump("bacc", bacc[:], [128, S], R=[ba_k])
                    ckpt('rw_loops%d' % c4)
                    K.op(ACT, lambda e: e.activation(out=sqb[:], in_=yacc[:], func=AF.Copy), R=[ya_k], W=[sqb_k])

                    def cons_m(ps_, pk, tb):
                        tsl = slice(tb * TB, (tb + 1) * TB)
                        K.op(DVE, lambda e: e.scalar_tensor_tensor(out=yacc[:, tsl], in0=ps_[:], scalar=-1.0 / 64, in1=yacc[:, tsl], op0=ALU.mult, op1=ALU.add),
                             R=[pk, ya_k], W=[ya_k])
                    bdsum(sqb, sqb_k, cons_m)
                    K.op(PL, lambda e: e.tensor_tensor(out=sqb[:], in0=yacc[:], in1=yacc[:], op=ALU.mult), R=[ya_k], W=[sqb_k])

                    def cons_v(ps_, pk, tb):
                        tsl = slice(tb * TB, (tb + 1) * TB)
                        K.op(ACT, lambda e: e.activation(out=T4[:, tsl], in_=ps_[:], func=AF.Sqrt, scale=1.0 / 64, bias=GN_EPS), R=[pk], W=[T_k[3]])
                        K.op(DVE, lambda e: e.reciprocal(out=T4[:, tsl], in_=T4[:, tsl]), R=[T_k[3]], W=[T_k[3]])
                    bdsum(sqb, sqb_k, cons_v)
                    K.op(DVE, lambda e: e.tensor_tensor(out=yacc[:], in0=yacc[:], in1=T4[:], op=ALU.mult), R=[ya_k, T_k[3]], W=[ya_k])
                    K.op(DVE, lambda e: e.tensor_scalar(out=yacc[:], in0=yacc[:], scalar1=lnw[:, c4:c4 + 1], scalar2=lnb[:, c4:c4 + 1], op0=ALU.mult, op1=ALU.add),
                         R=[ya_k, lnw_k, lnb_k], W=[ya_k])
                    K.op(PL, lambda e: e.tensor_tensor(out=yacc[:], in0=yacc[:], in1=bacc[:], op=ALU.add), R=[ya_k, ba_k], W=[ya_k])
                    K.op(DVE, lambda e: e.tensor_tensor(out=catT[:, 4 + c4, :], in0=yacc[:], in1=gTb[:], op=ALU.mult), R=[ya_k, gT_k], W=[cat_k[4 + c4]])
                    ckpt('rw_gn%d' % c4)
            if b == 0:
                dump("orw", catT[:, 4, :], [128, S], R=cat_k)

        ckpt('rwkv')
        with K.scope():
            x1 = K.sb([128, NT, D], F32, "x1")
            x1_k = [Tok() for _ in range(NT)]
            h2t = K.sb([128, NT, D], BF16, "h2t")
            h2_k = [Tok() for _ in range(NT)]
            afft = K.sb([128, NT, NE], F32, "afft")
            aff_k = [Tok() for _ in range(NT)]
            posm = K.sb([16, S], F32, "posm")
            posm_k = Tok()
            post = K.sb([128, NT, NE], F32, "post")
            post_k = Tok()
            gt2b = K.sb([128, 1, D], F32, "gt2b")
            gt2b_k = Tok()
            bcast_rows(b, gt2b, gt2b_k, [(modT, 40)])
            K.stacks.append(ExitStack())
            affT = K.sb([16, S], F32, "affT")
            affT_k = Tok()
            with K.scope():
                bct = K.sb([128, 3, D], F32, "bct")
                bct_k = Tok()
                bcast_rows(b, bct, bct_k, [(modT, 16), (S2, 0), (modT, 24)])
                wo = K.sb([128, KD, D], BF16, "wo")
                wo_k = Tok()
                K.dma(POOL, K.dmac("wo"), wo[:], wout_d.rearrange("(j p) n -> p j n", p=128), W=[wo_k])
                xc = [K.dmac("x0")]
                xt = [K.sb([128, D], F32, "xt")]
                xt_k = [Tok()]
                po = [K.ps([128, 512], F32, "po") for _ in range(2)]
                po_k = [Tok(), Tok()]
                st_ = [K.sb([128, 4], F32, "st") for _ in range(2)]
                st_k = [Tok(), Tok()]
                pt = [K.ps([128, KD, 128], BF16, "pt") for _ in range(2)]
                pt_k = [Tok(), Tok()]
                h2T = [K.sb([128, KD, 128], BF16, "h2T") for _ in range(2)]
                h2T_k = [Tok(), Tok()]
                plg = K.ps([128, NE], F32, "plg")
                plg_k = Tok()
                lg = K.sb([128, NE], F32, "lg")
                lg_k = Tok()
                paT = K.ps([16, 128], F32, "paT")
                paT_k = Tok()
                for i in range(NT):
                    if i % 4 == 0 and i > 0:
                        K.barrier()
                    u = i % 2
                    sl = slice(i * 128, (i + 1) * 128)
                    K.dma(SP, xc[0], xt[0][:], x_d[tok0 + i * 128: tok0 + (i + 1) * 128, :], W=[xt_k[0]])
                    for half in range(2):
                        hsl = slice(half * 512, (half + 1) * 512)
                        K.mm(po[half][:], [(catT[:, j, sl], wo[:, j, hsl]) for j in range(KD)], R=cat_k + [wo_k], W=[po_k[half]])
                        K.op(DVE, lambda e, half=half, hsl=hsl, i=i: e.tensor_tensor(out=x1[:, i, hsl], in0=po[half][:], in1=bct[:, 0, hsl], op=ALU.mult),
                             R=[po_k[half], bct_k], W=[x1_k[i]])
                    K.op(PL, lambda e, i=i: e.tensor_tensor(out=x1[:, i, :], in0=x1[:, i, :], in1=xt[0][:], op=ALU.add), R=[x1_k[i], xt_k[0]], W=[x1_k[i]])
                    K.op(ACT, lambda e, u=u, i=i: e.activation(out=h2T[u][:].rearrange("p j t -> p (j t)"), in_=x1[:, i, :], func=AF.Square, accum_out=st_[u][:, 0:1]),
                         R=[x1_k[i]], W=[h2T_k[u], st_k[u]])
                    K.op(ACT, lambda e, u=u: e.activation(out=st_[u][:, 1:2], in_=st_[u][:, 0:1], func=AF.Sqrt, scale=1.0 / D, bias=NORM_EPS), R=[st_k[u]], W=[st_k[u]])
                    K.op(DVE, lambda e, u=u: e.reciprocal(out=st_[u][:, 1:2], in_=st_[u][:, 1:2]), R=[st_k[u]], W=[st_k[u]])
                    K.op(DVE, lambda e, u=u, i=i: e.scalar_tensor_tensor(out=h2t[:, i, :], in0=x1[:, i, :], scalar=st_[u][:, 1:2], in1=bct[:, 1, :], op0=ALU.mult, op1=ALU.mult),
                         R=[x1_k[i], st_k[u], bct_k], W=[h2_k[i]])
                    K.op(PL, lambda e, i=i: e.tensor_tensor(out=h2t[:, i, :], in0=h2t[:, i, :], in1=bct[:, 2, :], op=ALU.add), R=[h2_k[i], bct_k], W=[h2_k[i]])
                    for j in range(KD):
                        K.tr(pt[u][:, j, :], h2t[:, i, j * 128:(j + 1) * 128], identb[:], R=[h2_k[i], cst], W=[pt_k[u]], inc=(j == KD - 1))
                    K.op(ACT, lambda e, u=u: e.activation(out=h2T[u][:], in_=pt[u][:], func=AF.Copy), R=[pt_k[u]], W=[h2T_k[u]])
                    K.mm(plg[:], [(h2T[u][:, j, :], wrt[:, j, :]) for j in range(KD)], R=[h2T_k[u], wsm_k], W=[plg_k])
                    K.op(DVE, lambda e, u=u: e.tensor_reduce(out=st_[u][:, 2:3], in_=plg[:], axis=AX.X, op=ALU.max), R=[plg_k], W=[st_k[u]])
                    K.op(DVE, lambda e, u=u: e.tensor_scalar(out=st_[u][:, 2:3], in0=st_[u][:, 2:3], scalar1=-1.0, scalar2=None, op0=ALU.mult), R=[st_k[u]], W=[st_k[u]])
                    K.op(ACT, lambda e, u=u: e.activation(out=lg[:], in_=plg[:], func=AF.Exp, bias=st_[u][:, 2:3], accum_out=st_[u][:, 3:4]),
                         R=[plg_k, st_k[u]], W=[lg_k, st_k[u]])
                    K.op(DVE, lambda e, u=u: e.reciprocal(out=st_[u][:, 3:4], in_=st_[u][:, 3:4]), R=[st_k[u]], W=[st_k[u]])
                    K.op(DVE, lambda e, u=u, i=i: e.tensor_scalar(out=afft[:, i, :], in0=lg[:], scalar1=st_[u][:, 3:4], scalar2=None, op0=ALU.mult),
                         R=[lg_k, st_k[u]], W=[aff_k[i]])
                    K.tr(paT[:], afft[:, i, :], identf[:], R=[aff_k[i], cst], W=[paT_k])
                    K.op(DVE, lambda e, sl=sl: e.tensor_copy(out=affT[:, sl], in_=paT[:]), R=[paT_k], W=[affT_k])
            if b == 0:
                dump("x1", x1[:, 0, :], [128, D], R=x1_k)
                dump("affT", affT[:], [16, S], R=[affT_k])

            ckpt('outproj')
            with K.scope():
                wk = [K.sb([16, S], F32, "wk") for _ in range(2)]
                wk_k = [Tok(), Tok()]
                m8 = K.sb([16, 8], F32, "m8")
                m8_k = Tok()
                mk_ = K.sb([16, S], F32, "mk")
                mk_k = Tok()
                ppo = K.ps([128, NE], F32, "ppo")
                ppo_k = Tok()
                K.op(DVE, lambda e: e.tensor_copy(out=wk[0][:], in_=affT[:]), R=[affT_k], W=[wk_k[0]])
                nit = CAP // 8
                cur = 0
                for it in range(nit):
                    K.op(DVE, lambda e, cur=cur: e.max(out=m8[:], in_=wk[cur][:]), R=[wk_k[cur]], W=[m8_k])
                    if it < nit - 1:
                        K.op(DVE, lambda e, cur=cur: e.match_replace(out=wk[1 - cur][:], in_to_replace=m8[:], in_values=wk[cur][:], imm_value=-1.0),
                             R=[wk_k[cur], m8_k], W=[wk_k[1 - cur]])
                        cur = 1 - cur
                K.op(DVE, lambda e: e.tensor_scalar(out=mk_[:], in0=affT[:], scalar1=m8[:, 7:8], scalar2=None, op0=ALU.is_ge), R=[affT_k, m8_k], W=[mk_k])
                K.op(DVE, lambda e: e.tensor_tensor_scan(out=posm[:], data0=onesf[0:16, 0:1].to_broadcast([16, S]), data1=mk_[:], initial=0.0, op0=ALU.mult, op1=ALU.add),
                     R=[mk_k, cst], W=[posm_k])
                K.op(DVE, lambda e: e.tensor_tensor(out=posm[:], in0=posm[:], in1=mk_[:], op=ALU.mult), R=[posm_k, mk_k], W=[posm_k])
                K.op(DVE, lambda e: e.tensor_scalar(out=posm[:], in0=posm[:], scalar1=-1.0, scalar2=None, op0=ALU.add), R=[posm_k], W=[posm_k])
                for i in range(NT):
                    K.tr(ppo[:], posm[:, i * 128:(i + 1) * 128], identf[0:16, 0:16], R=[posm_k, cst], W=[ppo_k])
                    K.op(DVE, lambda e, i=i: e.tensor_copy(out=post[:, i, :], in_=ppo[:]), R=[ppo_k], W=[post_k])
            if b == 0:
                dump("posm", posm[:], [16, S], R=[posm_k])

            K.barrier()
            K.stacks.pop().close()
            ckpt('topk')
            with K.scope():
                NWS = 6
                if S >= 2048:
                    wsl = [catT[:, :, q * 512:(q + 1) * 512] for q in range(4)]
                    wsl += [K.sb([128, KD, 512], BF16, "wsl") for _ in range(NWS - 4)]
                else:
                    wsl = [K.sb([128, KD, 512], BF16, "wsl") for _ in range(NWS)]
                wsl_k = [Tok() for _ in range(NWS)]
                wsc_ = [K.dmac("wsl") for _ in range(NWS)]
                Sel = K.sb([128, NT, CAP], BF16, "Sel")
                Sel_k = Tok()
                SelT = K.sb([128, NCT, S], BF16, "SelT")
                SelT_k = Tok()
                hgT = K.sb([128, KD, CAP], BF16, "hgT")
                hgT_k = Tok()
                hidT = K.sb([128, KD, CAP], BF16, "hidT")
                hid_k = Tok()
                sgt = K.sb([128, CAP], F32, "sgt")
                sgt_k = Tok()
                ysb = K.sb([128, NCT, D], BF16, "ysb")
                ysb_k = Tok()
                ppb = K.ps([128, TB], F32, "ppb")
                ppb_k = Tok()
                pg = K.ps([128, CAP], F32, "pg")
                pg_k = Tok()
                pu = K.ps([128, CAP], F32, "pu")
                pu_k = Tok()
                ph = [K.ps([128, CAP], F32, "ph") for _ in range(2)]
                ph_k = [Tok(), Tok()]
                py = [K.ps([128, 512], F32, "py") for _ in range(2)]
                py_k = [Tok(), Tok()]
                wcount = [0]
                ohe = K.sb([16, 128], F32, "ohe")
                ohe_k = Tok()

                def wload(src_d, e_, half):
                    s = wcount[0] % NWS
                    wcount[0] += 1
                    K.dma(POOL, wsc_[s], wsl[s][:], src_d[e_, :, half * 512:(half + 1) * 512].rearrange("(j p) n -> p j n", p=128), W=[wsl_k[s]])
                    return s

                for e_ in range(NE):
                    sg0 = wload(wg_d, e_, 0)
                    sg1 = wload(wg_d, e_, 1)
                    su0 = wload(wu_d, e_, 0)
                    su1 = wload(wu_d, e_, 1)
                    for i in range(NT):
                        K.op(DVE, lambda e, i=i, e_=e_: e.tensor_scalar(out=Sel[:, i, :], in0=iotac[:, 0:CAP], scalar1=post[:, i, e_:e_ + 1], scalar2=None, op0=ALU.is_equal),
                             R=[post_k, cst], W=[Sel_k])
                    K.op(DVE, lambda e, e_=e_: e.tensor_copy(out=ohe[:], in_=bc(identf[0:16, e_:e_ + 1], [16, 128])), R=[cst], W=[ohe_k])
                    for tb in range(NTB):
                        tsl = slice(tb * TB, (tb + 1) * TB)
                        K.mm(ppb[:], [(ohe[:], posm[:, tsl])], R=[posm_k, ohe_k], W=[ppb_k])
                        for ct in range(NCT):
                            K.op(DVE, lambda e, ct=ct, tsl=tsl: e.tensor_scalar(out=SelT[:, ct, tsl], in0=ppb[:], scalar1=iotap[:, ct:ct + 1], scalar2=None, op0=ALU.is_equal),
                                 R=[ppb_k, cst], W=[SelT_k])
                    for fc in range(KD):
                        u = fc % 2
                        K.mm(ph[u][:], [(h2t[:, i, fc * 128:(fc + 1) * 128], Sel[:, i, :]) for i in range(NT)], R=h2_k + [Sel_k], W=[ph_k[u]])
                        K.op(ACT if u == 0 else DVE, (lambda e, u=u, fc=fc: e.activation(out=hgT[:, fc, :], in_=ph[u][:], func=AF.Copy)) if u == 0 else
                             (lambda e, u=u, fc=fc: e.tensor_copy(out=hgT[:, fc, :], in_=ph[u][:])), R=[ph_k[u]], W=[hgT_k])
                    for fc in range(KD):
                        gs = sg0 if fc < 4 else sg1
                        us = su0 if fc < 4 else su1
                        fo = (fc % 4) * 128
                        K.mm(pg[:], [(wsl[gs][:, j, fo:fo + 128], hgT[:, j, :]) for j in range(KD)], R=[wsl_k[gs], hgT_k], W=[pg_k])
                        K.mm(pu[:], [(wsl[us][:, j, fo:fo + 128], hgT[:, j, :]) for j in range(KD)], R=[wsl_k[us], hgT_k], W=[pu_k])
                        K.op(ACT, lambda e: e.activation(out=sgt[:], in_=pg[:], func=AF.Silu), R=[pg_k], W=[sgt_k])
                        K.op(DVE, lambda e, fc=fc: e.tensor_tensor(out=hidT[:, fc, :], in0=pu[:], in1=sgt[:], op=ALU.mult), R=[pu_k, sgt_k], W=[hid_k])
                    sd0 = wload(wd_d, e_, 0)
                    sd1 = wload(wd_d, e_, 1)
                    for ct in range(NCT):
                        for half in range(2):
                            ds_ = sd0 if half == 0 else sd1
                            hsl = slice(half * 512, (half + 1) * 512)
                            K.mm(py[half][0:CP, :], [(hidT[:, fc, ct * 128:ct * 128 + CP], wsl[ds_][:, fc, :]) for fc in range(KD)], R=[hid_k, wsl_k[ds_]], W=[py_k[half]])
                            K.op(DVE, lambda e, ct=ct, half=half, hsl=hsl: e.tensor_tensor(out=ysb[0:CP, ct, hsl], in0=py[half][0:CP, :], in1=gt2b[0:CP, 0, hsl], op=ALU.mult),
                                 R=[py_k[half], gt2b_k], W=[ysb_k])
                    for i in range(NT):
                        sl = slice(i * 128, (i + 1) * 128)
                        for half in range(2):
                            hsl = slice(half * 512, (half + 1) * 512)
                            K.mm(py[half][:], [(SelT[0:CP, ct, sl], ysb[0:CP, ct, hsl]) for ct in range(NCT)], R=[SelT_k, ysb_k], W=[py_k[half]])
                            K.op(DVE, lambda e, i=i, half=half, hsl=hsl, e_=e_: e.scalar_tensor_tensor(out=x1[:, i, hsl], in0=py[half][:], scalar=afft[:, i, e_:e_ + 1],
                                                                                                    in1=x1[:, i, hsl], op0=ALU.mult, op1=ALU.add),
                                 R=[py_k[half], aff_k[i], x1_k[i]], W=[x1_k[i]])
                for i in range(NT):
                    K.dma(SP, outc, out_d[tok0 + i * 128: tok0 + (i + 1) * 128, :], x1[:, i, :], R=[x1_k[i]])
    K.barrier()
    K.stacks[0].close()
    return nc, dump_d


def rope_tables(S):
    rows = S // 64
    row = np.repeat(np.arange(rows, dtype=np.float32), 64)
    col = np.tile(np.arange(64, dtype=np.float32), rows)
    freqs = (np.float32(10000.0) ** (-np.arange(16, dtype=np.float32) / np.float32(16))).astype(np.float32)
    ang = np.concatenate([row[:, None] * freqs, col[:, None] * freqs], axis=-1).astype(np.float32)
    return np.cos(ang).astype(np.float32), np.sin(ang).astype(np.float32)


def fm(v, n):
    return np.ascontiguousarray(np.asarray(v, np.float32).reshape(n, 128).T)


def make_in_maps(inputs, S, NSEQ, ncores):
    f = lambda a: np.ascontiguousarray(np.asarray(a, np.float32))
    NT = S // 128
    cos, sin = rope_tables(S)
    cosl = np.ascontiguousarray(cos.reshape(NT, 128, 32).transpose(1, 0, 2))
    sinl = np.ascontiguousarray(sin.reshape(NT, 128, 32).transpose(1, 0, 2))
    x = f(inputs["x"])
    c = f(inputs["c"])
    shared = {
        "w_ada": f(inputs["w_ada"][0]),
        "b_ada": fm(inputs["b_ada"][0], 48),
        "g_mix": fm(inputs["g_mix"][0], 8),
        "g_ffn": fm(inputs["g_ffn"][0], 8),
        "w_in": f(inputs["w_in"][0]),
        "q_norm": f(inputs["q_norm"][0]).reshape(1, 64),
        "k_norm": f(inputs["k_norm"][0]).reshape(1, 64),
        "mu": fm(inputs["mu_shift"][0], 14),
        "w0": np.ascontiguousarray(f(inputs["w0"][0]).reshape(2, 4, 128).transpose(2, 0, 1)),
        "a0": np.ascontiguousarray(f(inputs["a0"][0]).reshape(2, 4, 128).transpose(2, 0, 1)),
        "w_up": f(inputs["w_up"][0]),
        "a_up": f(inputs["a_up"][0]),
        "g_up": f(inputs["g_up"][0]),
        "k_k": fm(inputs["k_k"][0], 4),
        "k_a": fm(inputs["k_a"][0], 4),
        "r_k": fm(f(inputs["r_k"][0]).reshape(-1), 4),
        "ln_w": fm(inputs["ln_w"][0], 4),
        "ln_b": fm(inputs["ln_b"][0], 4),
        "w_out": f(inputs["w_out"][0]),
        "w_router": f(inputs["w_router"][0]),
        "w_gate": f(inputs["w_gate"][0]),
        "w_up_e": f(inputs["w_up_e"][0]),
        "w_down": f(inputs["w_down"][0]),
        "cos": cosl,
        "sin": sinl,
    }
    maps = []
    for i in range(ncores):
        m = dict(shared)
        m["x"] = np.ascontiguousarray(x[i * NSEQ:(i + 1) * NSEQ].reshape(NSEQ * S, D))
        cc = c[i * NSEQ:(i + 1) * NSEQ]
        m["cT"] = np.ascontiguousarray(cc.reshape(NSEQ, KD, 128).transpose(2, 1, 0))
        maps.append(m)
    return maps


def kernel(**inputs):
    x = np.asarray(inputs["x"])
    B, S, _ = x.shape
    ncores = 8
    NSEQ = B // ncores
    nc, _ = build(S=S, NSEQ=NSEQ)
    maps = make_in_maps(inputs, S, NSEQ, ncores)
    res = run_bass_kernel_spmd(nc, maps, core_ids=list(range(ncores)))
    outs = [np.asarray(r["out"]).reshape(NSEQ, S, D) for r in res.results]
    return np.concatenate(outs, axis=0).astype(np.float32)
```

```python
import numpy as np
from contextlib import ExitStack, contextmanager
import concourse.bass as bass
import concourse.mybir as mybir
from concourse.bass_utils import run_bass_kernel_spmd

F32 = mybir.dt.float32
BF16 = mybir.dt.bfloat16
AF = mybir.ActivationFunctionType
ALU = mybir.AluOpType
AX = mybir.AxisListType

D = 1024
KD = 8
HD = 64
NE = 16
DECAY = 0.606531
GN_EPS = 64e-5
NORM_EPS = 1e-6
N_IN = 2560
REBASE_T = 3000
BAR_EVERY = 4


class Tok:
    __slots__ = ("w", "r")

    def __init__(self):
        self.w = None
        self.r = {}


class Cnt:
    def __init__(self, sem, incv, eng=None, name=""):
        self.sem = sem
        self.incv = incv
        self.cnt = 0
        self.eng = eng
        self.seen = {}
        self.name = name
        self.gen = 0


class Ctx:
    def __init__(self, nc):
        self.nc = nc
        self.stacks = [ExitStack()]
        self.uid = 0
        self.allc = []
        mk = self._mkc
        self.PE = mk(nc.tensor, 1, "pe")
        self.ACT = mk(nc.scalar, 1, "act")
        self.DVE = mk(nc.vector, 1, "dve")
        self.POOL = mk(nc.gpsimd, 1, "pool")
        self.SP = Cnt(None, 0, nc.sync, "sp")
        self.dma_free = []
        self.outc = []

    def _mkc(self, eng, incv, name):
        sem = self.stacks[0].enter_context(self.nc.semaphore(f"s_{name}_{self.uid}"))
        self.uid += 1
        c = Cnt(sem, incv, eng, name)
        self.allc.append(c)
        return c

    def dmac(self, name="d"):
        return self._mkc(None, 16, name)

    def nm(self, s):
        self.uid += 1
        return f"{s}_{self.uid}"

    def sb(self, shape, dt, name="t"):
        return self.stacks[-1].enter_context(self.nc.sbuf_tensor(self.nm(name), list(shape), dt))

    def ps(self, shape, dt=F32, name="p"):
        return self.stacks[-1].enter_context(self.nc.psum_tensor(self.nm(name), list(shape), dt))

    @contextmanager
    def scope(self):
        self.stacks.append(ExitStack())
        try:
            yield
        finally:
            self.barrier()
            self.stacks.pop().close()

    def barrier(self):
        for e in (self.PE, self.ACT, self.DVE, self.POOL, self.SP):
            for f in self.allc:
                if f.cnt > 0 and e.seen.get(f, 0) < f.cnt:
                    e.eng.wait_ge(f.sem, f.cnt * f.incv)
                    e.seen[f] = f.cnt
        for f in (self.PE, self.ACT, self.DVE, self.POOL):
            if f.cnt > REBASE_T:
                f.sem = self.stacks[0].enter_context(self.nc.semaphore(f"s_{f.name}_rb{self.uid}"))
                self.uid += 1
                f.cnt = 0
                f.gen += 1
                for e in (self.PE, self.ACT, self.DVE, self.POOL, self.SP):
                    e.seen.pop(f, None)

    def op(self, e, fn, R=(), W=(), comp=None, inc=True):
        comp = comp or e
        need = {}
        for t in R:
            if t.w is not None:
                f, c, g = t.w
                if g == f.gen and c > need.get(f, 0):
                    need[f] = c
        for t in W:
            if t.w is not None:
                f, c, g = t.w
                if g == f.gen and c > need.get(f, 0):
                    need[f] = c
            for f, (c, g) in t.r.items():
                if g == f.gen and c > need.get(f, 0):
                    need[f] = c
        for f, c in need.items():
            if f is e and e is self.PE:
                continue
            if e.seen.get(f, 0) < c:
                e.eng.wait_ge(f.sem, c * f.incv)
                e.seen[f] = c
        ins = fn(e.eng)
        if inc:
            comp.cnt += 1
            ins.then_inc(comp.sem, comp.incv)
            cc = comp.cnt
        else:
            cc = comp.cnt + 1
        for t in R:
            pr = t.r.get(comp)
            if pr is None or pr[1] != comp.gen or pr[0] < cc:
                t.r[comp] = (cc, comp.gen)
        for t in W:
            t.w = (comp, cc, comp.gen)
            t.r = {}
        return ins

    def mm(self, out, pairs, R=(), W=(), start=True, stop=True, inc=True):
        n = len(pairs)
        for i, (l, r) in enumerate(pairs):
            last = i == n - 1
            self.op(self.PE,
                    lambda e, l=l, r=r, i=i, last=last: e.matmul(out, lhsT=l, rhs=r, start=(start and i == 0), stop=(stop and last)),
                    R=R if i == 0 else (), W=W, inc=(inc and last))

    def tr(self, out, in_, ident, R=(), W=(), inc=True):
        self.op(self.PE, lambda e: e.transpose(out, in_, ident), R=R, W=W, inc=inc)

    def dma(self, issuer, comp, out, in_, R=(), W=()):
        self.op(issuer, lambda e: e.dma_start(out=out, in_=in_), R=R, W=W, comp=comp)


def bc(ap, shape):
    return ap.to_broadcast(list(shape))


class _Stop(Exception):
    pass


def build(S=2048, NSEQ=4, dumps=(), stop=None):
    try:
        return _build(S, NSEQ, dumps, stop)
    except _Stop as ex:
        ex.args[1].barrier()
        ex.args[1].stacks[0].close()
        return ex.args[0]


def _build(S, NSEQ, dumps, stop):
    NT = S // 128
    QB = min(512, S)
    NQB = S // QB
    QT = QB // 128
    CAP = 2 * S // NE
    NCT = max(1, CAP // 128)
    CP = min(CAP, 128)
    TB = min(512, S)
    NTB = S // TB
    TOKS = NSEQ * S
    nc = bass.Bass("TRN2", target_bir_lowering=False)
    K = Ctx(nc)

    def din(name, shape):
        return nc.dram_tensor(name, list(shape), F32, kind="ExternalInput").ap()

    x_d = din("x", [TOKS, D])
    cT_d = din("cT", [128, KD, NSEQ])
    wada_d = din("w_ada", [D, 6 * D])
    bada_d = din("b_ada", [128, 48])
    gmix_d = din("g_mix", [128, KD])
    gffn_d = din("g_ffn", [128, KD])
    win_d = din("w_in", [D, N_IN])
    qn_d = din("q_norm", [1, HD])
    kn_d = din("k_norm", [1, HD])
    mu_d = din("mu", [128, 14])
    w0_d = din("w0", [128, 2, 4])
    a0_d = din("a0", [128, 2, 4])
    wup_d = din("w_up", [2, 64, 512])
    aup_d = din("a_up", [2, 64, 512])
    gup_d = din("g_up", [128, 512])
    kk_d = din("k_k", [128, 4])
    ka_d = din("k_a", [128, 4])
    rk_d = din("r_k", [128, 4])
    lnw_d = din("ln_w", [128, 4])
    lnb_d = din("ln_b", [128, 4])
    wout_d = din("w_out", [D, D])
    wr_d = din("w_router", [D, NE])
    wg_d = din("w_gate", [NE, D, D])
    wu_d = din("w_up_e", [NE, D, D])
    wd_d = din("w_down", [NE, D, D])
    cos_d = din("cos", [128, NT, 32])
    sin_d = din("sin", [128, NT, 32])
    out_d = nc.dram_tensor("out", [TOKS, D], F32, kind="ExternalOutput").ap()
    dump_d = {}

    PE, ACT, DVE, POOL, SP = K.PE, K.ACT, K.DVE, K.POOL, K.SP
    import os as _os0
    PL = POOL if _os0.environ.get('POOLC', '0') == '1' else DVE
    outc = K.dmac("outc")

    def ckpt(name):
        if stop == name:
            raise _Stop((nc, dump_d), K)

    def dump(name, src_ap, shape, R=()):
        if name not in dumps:
            return
        dd = nc.dram_tensor("dump_" + name, list(shape), F32, kind="ExternalOutput").ap()
        dump_d[name] = dd
        tmp = K.sb(shape, F32, "dmp")
        tk = Tok()
        K.op(DVE, lambda e: e.tensor_copy(out=tmp[:], in_=src_ap), R=R, W=[tk])
        K.dma(SP, outc, dd, tmp[:], R=[tk])

    cst = Tok()
    identf = K.sb([128, 128], F32, "identf")
    identb = K.sb([128, 128], BF16, "identb")
    onesf = K.sb([128, 128], F32, "onesf")
    bdones = K.sb([128, 128], BF16, "bdones")
    MP = [K.sb([128, 256], BF16, "MP0"), K.sb([128, 256], BF16, "MP1")]
    ML = [K.sb([128, 128], BF16, "ML0"), K.sb([128, 128], BF16, "ML1")]
    iotac = K.sb([128, 256], F32, "iotac")
    iotap = K.sb([128, 2], F32, "iotap")
    hmask = K.sb([128, 4], F32, "hmask")

    K.op(POOL, lambda e: e.memset(onesf[:], 1.0), W=[cst])
    K.stacks.append(ExitStack())
    mUPs = K.sb([128, 128], F32, "mUPs")
    mUPi = K.sb([128, 128], F32, "mUPi")
    mLOs = K.sb([128, 128], F32, "mLOs")
    mLOi = K.sb([128, 128], F32, "mLOi")
    def aff(dst, base, cm, step, cmp):
        K.op(POOL, lambda e: e.affine_select(out=dst[:], in_=onesf[:], pattern=[[step, 128]], compare_op=cmp,
                                             fill=0.0, base=base, channel_multiplier=cm), R=[cst], W=[cst])
    aff(identf, 0, 1, -1, ALU.is_equal)
    aff(mUPs, 0, -1, 1, ALU.is_gt)
    aff(mUPi, 0, -1, 1, ALU.is_ge)
    aff(mLOs, 0, 1, -1, ALU.is_gt)
    aff(mLOi, 0, 1, -1, ALU.is_ge)
    K.op(DVE, lambda e: e.tensor_copy(out=identb[:], in_=identf[:]), R=[cst], W=[cst])
    K.op(DVE, lambda e: e.memset(bdones[:], 0.0), W=[cst])
    K.op(DVE, lambda e: e.memset(bdones[0:64, 0:64], 1.0), W=[cst])
    K.op(DVE, lambda e: e.memset(bdones[64:128, 64:128], 1.0), W=[cst])
    K.op(DVE, lambda e: e.tensor_copy(out=MP[0][:, 0:128], in_=mUPs[:]), R=[cst], W=[cst])
    K.op(DVE, lambda e: e.tensor_copy(out=MP[0][:, 128:256], in_=mUPi[:]), R=[cst], W=[cst])
    K.op(DVE, lambda e: e.tensor_copy(out=MP[1][:, 0:128], in_=mLOs[:]), R=[cst], W=[cst])
    K.op(DVE, lambda e: e.tensor_copy(out=MP[1][:, 128:256], in_=mLOi[:]), R=[cst], W=[cst])
    K.op(DVE, lambda e: e.tensor_copy(out=ML[0][:], in_=mLOs[:]), R=[cst], W=[cst])
    K.op(DVE, lambda e: e.tensor_copy(out=ML[1][:], in_=mUPs[:]), R=[cst], W=[cst])
    K.barrier()
    K.stacks.pop().close()
    K.op(POOL, lambda e: e.iota(iotac[:], pattern=[[1, 256]], base=0, channel_multiplier=0,
                                allow_small_or_imprecise_dtypes=True), W=[cst])
    K.op(POOL, lambda e: e.iota(iotap[:], pattern=[[128, 2]], base=0, channel_multiplier=1,
                                allow_small_or_imprecise_dtypes=True), W=[cst])
    K.op(DVE, lambda e: e.memset(hmask[:], 0.0), W=[cst])
    K.op(DVE, lambda e: e.memset(hmask[0:64, 0:1], 1.0), W=[cst])
    K.op(DVE, lambda e: e.memset(hmask[64:128, 1:2], 1.0), W=[cst])
    K.op(DVE, lambda e: e.memset(hmask[0:64, 2:3], -1.0), W=[cst])
    K.op(DVE, lambda e: e.memset(hmask[64:128, 3:4], -1.0), W=[cst])

    ckpt('consts')
    def ldsmall(dram, shape, name, dt=F32, eng=None):
        t = K.sb(shape, dt, name)
        c = K.dmac(name)
        tk = Tok()
        K.dma(SP if dt == F32 else POOL, c, t[:], dram, W=[tk])
        return t, tk

    cT, cT_k = ldsmall(cT_d, [128, KD, NSEQ], "cT")
    bada, bada_k = ldsmall(bada_d, [128, 48], "bada")
    gmix, gmix_k = ldsmall(gmix_d, [128, KD], "gmix")
    gffn, gffn_k = ldsmall(gffn_d, [128, KD], "gffn")
    mu, mu_k = ldsmall(mu_d, [128, 14], "mu")
    w0, w0_k = ldsmall(w0_d, [128, 2, 4], "w0")
    a0, a0_k = ldsmall(a0_d, [128, 2, 4], "a0")
    kkp, kkp_k = ldsmall(kk_d, [128, 4], "kkp")
    kap, kap_k = ldsmall(ka_d, [128, 4], "kap")
    rkp, rkp_k = ldsmall(rk_d, [128, 4], "rkp")
    lnw, lnw_k = ldsmall(lnw_d, [128, 4], "lnw")
    lnb, lnb_k = ldsmall(lnb_d, [128, 4], "lnb")
    cosT, cos_k = ldsmall(cos_d, [128, NT, 32], "cos")
    sinT, sin_k = ldsmall(sin_d, [128, NT, 32], "sin")
    gain = K.sb([128, 10, HD], F32, "gain")
    gain_k = Tok()
    gc_ = K.dmac("gain")
    K.dma(SP, gc_, gain[:, 0, :], qn_d.partition_broadcast(128), W=[gain_k])
    K.dma(SP, gc_, gain[:, 8, :], kn_d.partition_broadcast(128), W=[gain_k])
    for h in range(1, 8):
        K.op(DVE, lambda e, h=h: e.tensor_copy(out=gain[:, h, :], in_=gain[:, 0, :]), R=[gain_k], W=[gain_k])
    K.op(DVE, lambda e: e.tensor_copy(out=gain[:, 9, :], in_=gain[:, 8, :]), R=[gain_k], W=[gain_k])
    K.op(DVE, lambda e: e.tensor_scalar(out=gain[:, 0:8, :], in0=gain[:, 0:8, :], scalar1=HD ** -0.5, scalar2=None, op0=ALU.mult),
         R=[gain_k], W=[gain_k])
    hmu = K.sb([128, 14], F32, "hmu")
    omm = K.sb([128, 14], F32, "omm")
    omka = K.sb([128, 4], F32, "omka")
    K.op(DVE, lambda e: e.tensor_scalar(out=hmu[:], in0=mu[:], scalar1=0.5, scalar2=None, op0=ALU.mult), R=[mu_k], W=[mu_k])
    K.op(DVE, lambda e: e.tensor_scalar(out=omm[:], in0=mu[:], scalar1=-1.0, scalar2=1.0, op0=ALU.mult, op1=ALU.add), R=[mu_k], W=[mu_k])
    K.op(DVE, lambda e: e.tensor_scalar(out=omka[:], in0=kap[:], scalar1=-1.0, scalar2=1.0, op0=ALU.mult, op1=ALU.add), R=[kap_k], W=[kap_k])
    wup = K.sb([128, 2, 512], BF16, "wup")
    aup = K.sb([128, 2, 512], BF16, "aup")
    gup = K.sb([128, 512], BF16, "gup")
    wrt = K.sb([128, KD, NE], BF16, "wrt")
    wsm_k = Tok()
    wsc = K.dmac("wsm")
    K.dma(POOL, wsc, wup[0:64, :, :], wup_d.rearrange("d k n -> k d n"), W=[wsm_k])
    K.dma(POOL, wsc, aup[64:128, :, :], aup_d.rearrange("d k n -> k d n"), W=[wsm_k])
    K.dma(POOL, wsc, gup[:], gup_d, W=[wsm_k])
    K.dma(POOL, wsc, wrt[:], wr_d.rearrange("(j p) n -> p j n", p=128), W=[wsm_k])

    ckpt('small')
    modT = K.sb([128, 48, NSEQ], F32, "modT")
    mod_k = Tok()
    S1 = K.sb([128, KD, NSEQ], F32, "S1")
    S2 = K.sb([128, KD, NSEQ], F32, "S2")
    with K.scope():
        cond = K.sb([128, KD, NSEQ], F32, "cond")
        cond_k = Tok()
        K.op(ACT, lambda e: e.activation(out=cond[:], in_=cT[:], func=AF.Silu), R=[cT_k], W=[cond_k])
        wa = [K.sb([128, KD, 512], F32, "wa") for _ in range(2)]
        wa_k = [Tok(), Tok()]
        wac = [K.dmac("wa0"), K.dmac("wa1")]
        pm = [K.ps([128, 4, NSEQ], F32, "pm") for _ in range(2)]
        pm_k = [Tok(), Tok()]
        for g in range(12):
            b = g % 2
            K.dma(SP, wac[b], wa[b][:], wada_d[:, g * 512:(g + 1) * 512].rearrange("(j p) n -> p j n", p=128), W=[wa_k[b]])
            for mm_ in range(4):
                K.mm(pm[b][:, mm_, :], [(wa[b][:, j, mm_ * 128:(mm_ + 1) * 128], cond[:, j, :]) for j in range(KD)],
                     R=[wa_k[b], cond_k], W=[pm_k[b]])
            K.op(DVE, lambda e, b=b, g=g: e.tensor_tensor(out=modT[:, g * 4:(g + 1) * 4, :], in0=pm[b][:],
                                                          in1=bc(bada[:, g * 4:(g + 1) * 4].unsqueeze(2), [128, 4, NSEQ]), op=ALU.add),
                 R=[pm_k[b], bada_k], W=[mod_k])
        for (Sx, gv, gk, off) in ((S1, gmix, gmix_k, 8), (S2, gffn, gffn_k, 32)):
            K.op(DVE, lambda e, Sx=Sx, off=off: e.tensor_scalar(out=Sx[:], in0=modT[:, off:off + 8, :], scalar1=1.0, scalar2=None, op0=ALU.add),
                 R=[mod_k], W=[mod_k])
            K.op(DVE, lambda e, Sx=Sx, gv=gv: e.tensor_tensor(out=Sx[:], in0=Sx[:], in1=bc(gv[:].unsqueeze(2), [128, KD, NSEQ]), op=ALU.mult),
                 R=[mod_k, gk], W=[mod_k])
    dump("modT", modT[:].rearrange("p m b -> p (m b)"), [128, 48 * NSEQ], R=[mod_k])

    ckpt('phaseA')
    catT = K.sb([128, KD, S], BF16, "catT")
    cat_k = [Tok() for _ in range(KD)]
    def bcast_rows(b, bct, bct_k, srcs):
        with K.scope():
            dg = [K.sb([128, 128], F32, "dg") for _ in range(2)]
            dg_k = [Tok(), Tok()]
            pb_ = [K.ps([128, 512], F32, "pb") for _ in range(2)]
            pb_k = [Tok(), Tok()]
            i = 0
            for r, (src, off) in enumerate(srcs):
                for half in range(2):
                    pi = (r * 2 + half) % 2
                    for jj in range(4):
                        j = half * 4 + jj
                        di = i % 2
                        i += 1
                        K.op(DVE, lambda e, di=di, src=src, off=off, j=j: e.tensor_scalar(
                            out=dg[di][:], in0=identf[:], scalar1=src[:, off + j, b:b + 1], scalar2=None, op0=ALU.mult),
                            R=[cst, mod_k], W=[dg_k[di]])
                        K.mm(pb_[pi][:, jj * 128:(jj + 1) * 128], [(onesf[:], dg[di][:])], R=[dg_k[di], cst], W=[pb_k[pi]])
                    K.op(ACT, lambda e, pi=pi, r=r, half=half: e.activation(out=bct[:, r, half * 512:(half + 1) * 512], in_=pb_[pi][:], func=AF.Copy),
                         R=[pb_k[pi]], W=[bct_k])

    for b in range(NSEQ):
        tok0 = b * S
        with K.scope():
            hT = K.sb([128, KD, S], BF16, "hT")
            hT_k = [Tok() for _ in range(NT)]
            with K.scope():
                xt = [K.sb([128, D], F32, "xt") for _ in range(2)]
                xt_k = [Tok(), Tok()]
                xc = [K.dmac("x0"), K.dmac("x1")]
                xn = [K.sb([128, D], BF16, "xn") for _ in range(2)]
                xn_k = [Tok(), Tok()]
                junk = K.sb([128, D], BF16, "junk")
                junk_k = Tok()
                st_ = [K.sb([128, 2], F32, "st") for _ in range(2)]
                st_k = [Tok(), Tok()]
                pt = [K.ps([128, KD, 128], BF16, "pt") for _ in range(2)]
                pt_k = [Tok(), Tok()]
                for i in range(NT):
                    if i % 4 == 0 and i > 0:
                        K.barrier()
                    u = i % 2
                    K.dma(SP, xc[u], xt[u][:], x_d[tok0 + i * 128: tok0 + (i + 1) * 128, :], W=[xt_k[u]])
                    K.op(ACT, lambda e, u=u: e.activation(out=junk[:], in_=xt[u][:], func=AF.Square, accum_out=st_[u][:, 0:1]),
                         R=[xt_k[u]], W=[junk_k, st_k[u]])
                    K.op(ACT, lambda e, u=u: e.activation(out=st_[u][:, 1:2], in_=st_[u][:, 0:1], func=AF.Sqrt, scale=1.0 / D, bias=NORM_EPS),
                         R=[st_k[u]], W=[st_k[u]])
                    K.op(DVE, lambda e, u=u: e.reciprocal(out=st_[u][:, 1:2], in_=st_[u][:, 1:2]), R=[st_k[u]], W=[st_k[u]])
                    K.op(DVE, lambda e, u=u: e.tensor_scalar(out=xn[u][:], in0=xt[u][:], scalar1=st_[u][:, 1:2], scalar2=None, op0=ALU.mult),
                         R=[xt_k[u], st_k[u]], W=[xn_k[u]])
                    for j in range(KD):
                        K.tr(pt[u][:, j, :], xn[u][:, j * 128:(j + 1) * 128], identb[:], R=[xn_k[u], cst], W=[pt_k[u]], inc=(j == KD - 1))
                    for j in range(KD):
                        eng = ACT if j % 2 == 0 else DVE
                        if eng is ACT:
                            K.op(ACT, lambda e, j=j, u=u, i=i: e.activation(out=hT[:, j, i * 128:(i + 1) * 128], in_=pt[u][:, j, :], func=AF.Identity,
                                                                             scale=S1[:, j, b:b + 1], bias=modT[:, j, b:b + 1]),
                                 R=[pt_k[u], mod_k], W=[hT_k[i]])
                        else:
                            K.op(DVE, lambda e, j=j, u=u, i=i: e.tensor_scalar(out=hT[:, j, i * 128:(i + 1) * 128], in0=pt[u][:, j, :],
                                                                                scalar1=S1[:, j, b:b + 1], scalar2=modT[:, j, b:b + 1], op0=ALU.mult, op1=ALU.add),
                                 R=[pt_k[u], mod_k], W=[hT_k[i]])
            if b == 0:
                dump("hT", hT[:, 0, :], [128, S], R=hT_k)

            ckpt('B1')
            with K.scope():
                watt = K.sb([128, KD, 768], BF16, "watt")
                watt_k = Tok()
                K.dma(POOL, K.dmac("watt"), watt[:], win_d[:, 0:768].rearrange("(j p) n -> p j n", p=128), W=[watt_k])
                qT = K.sb([128, 4, S], BF16, "qT")
                qT_k = [Tok() for _ in range(NT)]
                kT2 = K.sb([128, 2, S], BF16, "kT2")
                kT_k = [Tok() for _ in range(NT)]
                vaug = K.sb([128, NT, 2, HD + 1], BF16, "vaug")
                v_k = [Tok() for _ in range(NT)]
                K.op(DVE, lambda e: e.memset(vaug[:], 1.0), W=v_k)
                with K.scope():
                    pq = K.ps([128, 512], F32, "pq")
                    pq_k = Tok()
                    pkv = K.ps([128, 256], F32, "pkv")
                    pkv_k = Tok()
                    ptq = K.ps([128, 4, 128], BF16, "ptq")
                    ptq_k = Tok()
                    ptk = K.ps([128, 2, 128], BF16, "ptk")
                    ptk_k = Tok()
                    qk = K.sb([128, 10, HD], F32, "qk")
                    qk_k = Tok()
                    sq = K.sb([128, 10, HD], F32, "sq")
                    sq_k = Tok()
                    ss = K.sb([128, 10], F32, "ss")
                    ss_k = Tok()
                    t1 = K.sb([128, 10, 32], F32, "t1")
                    t2 = K.sb([128, 10, 32], F32, "t2")
                    t3 = K.sb([128, 10, 32], F32, "t3")
                    t4 = K.sb([128, 10, 32], F32, "t4")
                    t_k = [Tok() for _ in range(4)]
                    qr = K.sb([128, 8, HD], BF16, "qr")
                    qr_k = Tok()
                    kr = K.sb([128, 2, 2, HD], BF16, "kr")
                    kr_k = Tok()
                    for i in range(NT):
                        if i % 4 == 0 and i > 0:
                            K.barrier()
                        sl = slice(i * 128, (i + 1) * 128)
                        K.mm(pq[:], [(hT[:, j, sl], watt[:, j, 0:512]) for j in range(KD)], R=[hT_k[i], watt_k], W=[pq_k])
                        K.mm(pkv[:], [(hT[:, j, sl], watt[:, j, 512:768]) for j in range(KD)], R=[hT_k[i], watt_k], W=[pkv_k])
                        K.op(ACT, lambda e: e.activation(out=qk[:, 0:8, :].rearrange("p h d -> p (h d)"), in_=pq[:], func=AF.Copy), R=[pq_k], W=[qk_k])
                        K.op(ACT, lambda e: e.activation(out=qk[:, 8:10, :].rearrange("p h d -> p (h d)"), in_=pkv[:, 0:128], func=AF.Copy), R=[pkv_k], W=[qk_k])
                        K.op(ACT, lambda e, i=i: e.activation(out=vaug[:, i, :, 0:HD], in_=pkv[:, 128:256].rearrange("p (g d) -> p g d", g=2), func=AF.Copy),
                             R=[pkv_k], W=[v_k[i]])
                        K.op(DVE, lambda e: e.tensor_tensor(out=sq[:], in0=qk[:], in1=qk[:], op=ALU.mult), R=[qk_k], W=[sq_k])
                        K.op(DVE, lambda e: e.tensor_reduce(out=ss[:], in_=sq[:], axis=AX.X, op=ALU.add), R=[sq_k], W=[ss_k])
                        K.op(ACT, lambda e: e.activation(out=ss[:], in_=ss[:], func=AF.Sqrt, scale=1.0 / HD, bias=NORM_EPS), R=[ss_k], W=[ss_k])
                        K.op(DVE, lambda e: e.reciprocal(out=ss[:], in_=ss[:]), R=[ss_k], W=[ss_k])
                        K.op(DVE, lambda e: e.tensor_tensor(out=qk[:], in0=qk[:], in1=bc(ss[:].unsqueeze(2), [128, 10, HD]), op=ALU.mult),
                             R=[qk_k, ss_k], W=[qk_k])
                        K.op(DVE, lambda e: e.tensor_tensor(out=qk[:], in0=qk[:], in1=gain[:], op=ALU.mult), R=[qk_k, gain_k], W=[qk_k])
                        qv = qk[:].rearrange("p h (k two) -> p h k two", two=2)
                        x0, x1 = qv[:, :, :, 0], qv[:, :, :, 1]
                        cb = bc(cosT[:, i, :].unsqueeze(1), [128, 10, 32])
                        sb_ = bc(sinT[:, i, :].unsqueeze(1), [128, 10, 32])
                        K.op(DVE, lambda e: e.tensor_tensor(out=t1[:], in0=x0, in1=cb, op=ALU.mult), R=[qk_k, cos_k], W=[t_k[0]])
                        K.op(PL, lambda e: e.tensor_tensor(out=t2[:], in0=x1, in1=sb_, op=ALU.mult), R=[qk_k, sin_k], W=[t_k[1]])
                        K.op(DVE, lambda e: e.tensor_tensor(out=t3[:], in0=x0, in1=sb_, op=ALU.mult), R=[qk_k, sin_k], W=[t_k[2]])
                        K.op(PL, lambda e: e.tensor_tensor(out=t4[:], in0=x1, in1=cb, op=ALU.mult), R=[qk_k, cos_k], W=[t_k[3]])
                        qrv = qr[:].rearrange("p h (k two) -> p h k two", two=2)
                        krv = kr[:].rearrange("p g u (k two) -> p g u k two", two=2)
                        K.op(DVE, lambda e: e.tensor_tensor(out=qrv[:, :, :, 0], in0=t1[:, 0:8, :], in1=t2[:, 0:8, :], op=ALU.subtract),
                             R=[t_k[0], t_k[1]], W=[qr_k])
                        K.op(DVE, lambda e: e.tensor_tensor(out=qrv[:, :, :, 1], in0=t3[:, 0:8, :], in1=t4[:, 0:8, :], op=ALU.add),
                             R=[t_k[2], t_k[3]], W=[qr_k])
                        for u_ in range(2):
                            K.op(PL, lambda e, u_=u_: e.tensor_tensor(out=krv[:, :, u_, :, 0], in0=t1[:, 8:10, :], in1=t2[:, 8:10, :], op=ALU.subtract),
                                 R=[t_k[0], t_k[1]], W=[kr_k])
                            K.op(PL, lambda e, u_=u_: e.tensor_tensor(out=krv[:, :, u_, :, 1], in0=t3[:, 8:10, :], in1=t4[:, 8:10, :], op=ALU.add),
                                 R=[t_k[2], t_k[3]], W=[kr_k])
                        for pr in range(4):
                            K.tr(ptq[:, pr, :], qr[:, 2 * pr:2 * pr + 2, :].rearrange("p h d -> p (h d)"), identb[:], R=[qr_k, cst], W=[ptq_k], inc=(pr == 3))
                        K.op(ACT, lambda e, sl=sl: e.activation(out=qT[:, :, sl], in_=ptq[:], func=AF.Copy), R=[ptq_k], W=[qT_k[i]])
                        for g in range(2):
                            K.tr(ptk[:, g, :], kr[:, g, :, :].rearrange("p u d -> p (u d)"), identb[:], R=[kr_k, cst], W=[ptk_k], inc=(g == 1))
                        K.op(DVE, lambda e, sl=sl: e.tensor_copy(out=kT2[:, :, sl], in_=ptk[:]), R=[ptk_k], W=[kT_k[i]])
                if b == 0:
                    dump("qT", qT[:, 0, :], [128, S], R=qT_k)
                    dump("kT", kT2[:, 0, :], [128, S], R=kT_k)
                ckpt('attproj')
                with K.scope():
                    NPS = 2
                    psc = [K.ps([128, QB], F32, "psc") for _ in range(NPS)]
                    psc_k = [Tok() for _ in range(NPS)]
                    pTa = [K.sb([128, NT, QB], BF16, "pTa") for _ in range(2)]
                    pTa_k = [Tok(), Tok()]
                    oacc = [K.ps([128, QT, 128], F32, "oacc") for _ in range(2)]
                    oacc_k = [Tok(), Tok()]
                    rs = K.sb([128, QT], F32, "rs")
                    rs_k = Tok()
                    otm = K.sb([128, QT, 512], BF16, "otm")
                    otm_k = Tok()
                    pto = K.ps([128, 4, 128], BF16, "pto")
                    pto_k = Tok()
                    it = 0
                    for qb in range(NQB):
                        qsl = slice(qb * QB, (qb + 1) * QB)
                        qtoks = qT_k[qb * QT:(qb + 1) * QT]
                        for hq in range(8):
                            g = hq // 4
                            pb0 = 64 * (hq % 2)
                            pr = hq // 2
                            oa = oacc[hq % 2]
                            oa_k = oacc_k[hq % 2]
                            pa = pTa[hq % 2]
                            pa_k = pTa_k[hq % 2]
                            for kt in range(NT):
                                u = it % NPS
                                it += 1
                                K.mm(psc[u][:], [(kT2[pb0:pb0 + 64, g, kt * 128:(kt + 1) * 128], qT[pb0:pb0 + 64, pr, qsl])],
                                     R=[kT_k[kt]] + qtoks, W=[psc_k[u]])
                                K.op(ACT, lambda e, u=u, pa=pa, kt=kt: e.activation(out=pa[:, kt, :], in_=psc[u][:], func=AF.Exp), R=[psc_k[u]], W=[pa_k])
                            for qt in range(QT):
                                K.mm(oa[:, qt, 0:HD + 1], [(pa[:, kt, qt * 128:(qt + 1) * 128], vaug[:, kt, g, :]) for kt in range(NT)],
                                     R=[pa_k] + v_k, W=[oa_k], inc=(qt == QT - 1))
                            K.op(DVE, lambda e, oa=oa: e.reciprocal(out=rs[:], in_=oa[:, :, HD]), R=[oa_k], W=[rs_k])
                            K.op(DVE, lambda e, oa=oa, hq=hq: e.tensor_tensor(out=otm[:, :, hq * HD:(hq + 1) * HD], in0=oa[:, :, 0:HD],
                                                                               in1=bc(rs[:].unsqueeze(2), [128, QT, HD]), op=ALU.mult),
                                 R=[oa_k, rs_k], W=[otm_k])
                        for qt in range(QT):
                            ti = qb * QT + qt
                            for c in range(4):
                                K.tr(pto[:, c, :], otm[:, qt, c * 128:(c + 1) * 128], identb[:], R=[otm_k, cst], W=[pto_k], inc=(c == 3))
                            K.op(ACT, lambda e, ti=ti: e.activation(out=catT[:, 0:4, ti * 128:(ti + 1) * 128], in_=pto[:], func=AF.Copy),
                                 R=[pto_k], W=cat_k[0:4])
            if b == 0:
                dump("oatt", catT[:, 0, :], [128, S], R=cat_k[0:4])

            ckpt('attcore')
            with K.scope():
                NC = NT
                c_ = DECAY
                wrw = [K.sb([128, KD, 128], BF16, "wrw") for _ in range(1)]
                wrw_k = [Tok() for _ in range(1)]
                wrc = [K.dmac("wrw") for _ in range(1)]
                wri = [0]
                T1 = K.sb([128, S + 2], F32, "T1")
                T2 = K.sb([128, S], F32, "T2")
                T3 = K.sb([128, S], F32, "T3")
                T4 = K.sb([128, S], F32, "T4")
                T_k = [Tok() for _ in range(4)]
                r32 = K.sb([128, S], BF16, "r32")
                k32 = K.sb([128, S], F32, "k32")
                kk32 = K.sb([128, S], F32, "kk32")
                yacc = K.sb([128, S], F32, "yacc")
                bacc = K.sb([128, S], BF16, "bacc")
                r_k_, k_k_, kk_k_, ya_k, ba_k = Tok(), Tok(), Tok(), Tok(), Tok()
                twda = K.sb([128, S], BF16, "twda")
                sg = K.sb([128, S], BF16, "sg")
                vb = K.sb([128, S], BF16, "vb")
                gTb = K.sb([128, S], BF16, "gTb")
                sqb = K.sb([128, S], BF16, "sqb")
                twda_k, sg_k, vb_k, gT_k, sqb_k = Tok(), Tok(), Tok(), Tok(), Tok()
                ART = K.sb([128, 2, NC, 2, 128], BF16, "ART")
                BT = K.sb([128, S], BF16, "BT")
                KT = K.sb([128, S], BF16, "KT")
                ART_k, BT_k, KT_k = Tok(), Tok(), Tok()
                gC = K.sb([128, NC], F32, "gC")
                gC_k = Tok()
                ppj = [K.ps([128, 512], F32, "ppj") for _ in range(2)]
                ppj_k = [Tok(), Tok()]
                pji = [0]
                K.op(DVE, lambda e: e.memset(T1[:, 0:1], 0.0), W=[T_k[0]])
                K.op(DVE, lambda e: e.memset(T1[:, S + 1:S + 2], 0.0), W=[T_k[0]])

                def project_shift(m, dst_fn):
                    wi = 0
                    wri[0] += 1
                    K.dma(POOL, wrc[wi], wrw[wi][:], win_d[:, 768 + m * 128: 768 + (m + 1) * 128].rearrange("(j p) n -> p j n", p=128), W=[wrw_k[wi]])
                    for tb in range(NTB):
                        u = pji[0] % 2
                        pji[0] += 1
                        K.mm(ppj[u][:, 0:TB], [(wrw[wi][:, j, :], hT[:, j, tb * TB:(tb + 1) * TB]) for j in range(KD)],
                             R=[wrw_k[wi]] + hT_k[tb * (TB // 128):(tb + 1) * (TB // 128)], W=[ppj_k[u]])
                        K.op(ACT, lambda e, u=u, tb=tb: e.activation(out=T1[:, 1 + tb * TB:1 + (tb + 1) * TB], in_=ppj[u][:, 0:TB], func=AF.Copy),
                             R=[ppj_k[u]], W=[T_k[0]])
                    K.op(PL, lambda e: e.tensor_tensor(out=T2[:], in0=T1[:, 0:S], in1=T1[:, 2:S + 2], op=ALU.add), R=[T_k[0]], W=[T_k[1]])
                    K.op(DVE, lambda e: e.tensor_scalar(out=T2[:], in0=T2[:], scalar1=hmu[:, m:m + 1], scalar2=None, op0=ALU.mult), R=[T_k[1], mu_k], W=[T_k[1]])
                    K.op(DVE, lambda e: e.scalar_tensor_tensor(out=T3[:], in0=T1[:, 1:S + 1], scalar=omm[:, m:m + 1], in1=T2[:], op0=ALU.mult, op1=ALU.add),
                         R=[T_k[0], T_k[1], mu_k], W=[T_k[2]])
                    if b == 0 and m == 4:
                        dump("T1k", T1[:, 0:S], [128, S], R=[T_k[0]])
                        dump("T2k", T2[:], [128, S], R=[T_k[1]])
                        dump("T3k", T3[:], [128, S], R=[T_k[2]])
                        dump("hmu", hmu[:], [128, 14], R=[mu_k])
                        dump("omm", omm[:], [128, 14], R=[mu_k])
                    dst_fn()

                def d12():
                    K.op(ACT, lambda e: e.activation(out=twda[0:64, :], in_=T3[0:64, :], func=AF.Tanh), R=[T_k[2]], W=[twda_k])
                    K.op(DVE, lambda e: e.tensor_copy(out=twda[64:128, :], in_=T3[64:128, :]), R=[T_k[2]], W=[twda_k])
                project_shift(12, d12)

                def d13():
                    K.op(ACT, lambda e: e.activation(out=sg[:], in_=T3[:], func=AF.Sigmoid), R=[T_k[2]], W=[sg_k])
                project_shift(13, d13)

                ckpt('lora')
                pbd = [K.ps([128, 512], F32, "pbd") for _ in range(2)]
                pbd_k = [Tok(), Tok()]
                bdi = [0]
                nck = [0]
                ptk3 = K.ps([128, 3, 128], BF16, "ptk3")
                ptk3_k = Tok()
                pP = K.ps([128, 2, 256], F32, "pP")
                pP_k = Tok()
                pQ = K.ps([128, 2, 128], F32, "pQ")
                pQ_k = Tok()
                pZY = K.ps([128, 512], F32, "pZY")
                pZ = pZY[:, 0:256].rearrange("p (a v) -> p a v", v=64)
                pZY_k = Tok()
                pZ_k = [pZY_k, pZY_k]
                pY = pZY[:, 256:448]
                pY_k = [pZY_k, pZY_k]
                tok3s = [K.sb([128, 3, 2, 128], BF16, "tok3") for _ in range(2)]
                tok3s_k = [Tok(), Tok()]
                for q_ in range(2):
                    K.op(DVE, lambda e, q_=q_: e.memset(tok3s[q_][:], 0.0), W=[tok3s_k[q_]])
                Ub2 = K.sb([128, 2, 128], BF16, "Ub2")
                Ub2_k = Tok()
                K.op(DVE, lambda e: e.memset(Ub2[:], 0.0), W=[Ub2_k])
                Hbd = K.sb([128, 128], BF16, "Hbd")
                Hbd_k = Tok()
                M1s = [K.sb([128, 2, 256], BF16, "M1") for _ in range(2)]
                M2s = [K.sb([128, 2, 256], BF16, "M2") for _ in range(2)]
                M1s_k, M2s_k = [Tok(), Tok()], [Tok(), Tok()]
                XAs = [[K.sb([128, 2, 2, 128], BF16, "XA") for _ in range(2)] for _ in range(2)]
                XAs_k = [[Tok(), Tok()], [Tok(), Tok()]]
                PAs = [[K.sb([128, 2, 128], BF16, "PA") for _ in range(2)] for _ in range(2)]
                PAs_k = [[Tok(), Tok()], [Tok(), Tok()]]
                Zb = K.sb([128, 2, 64], BF16, "Zb")
                Ub = K.sb([128, 2, 64], BF16, "Ub")
                Zb_k, Ub_k = Tok(), Tok()
                H32 = K.sb([128, 64], F32, "H32")
                Hb = K.sb([128, 64], BF16, "Hb")
                Htmp = K.sb([128, 64], F32, "Htmp")
                H_k, Hb_k, Ht_k = Tok(), Tok(), Tok()
                identb2 = bc(identb[:].unsqueeze(1), [128, 2, 128])
                T4b = T4[:].bitcast(BF16)
                if 2 * S >= 3328:
                    tok3s.append(T4b[:, 0:768].rearrange("p (x h c) -> p x h c", x=3, h=2))
                    M1s.append(T4b[:, 768:1280].rearrange("p (h t) -> p h t", h=2))
                    M2s.append(T4b[:, 1280:1792].rearrange("p (h t) -> p h t", h=2))
                    XAs.append([T4b[:, 1792 + i_ * 512:1792 + (i_ + 1) * 512].rearrange("p (h a t) -> p h a t", h=2, a=2) for i_ in range(2)])
                    PAs.append([T4b[:, 2816 + i_ * 256:2816 + (i_ + 1) * 256].rearrange("p (h t) -> p h t", h=2) for i_ in range(2)])
                    tok3s_k.append(Tok()); M1s_k.append(Tok()); M2s_k.append(Tok())
                    XAs_k.append([Tok(), Tok()]); PAs_k.append([Tok(), Tok()])
                NSETS = len(tok3s)
                pPs = [pP, ppj[1][:].rearrange("p (a t) -> p a t", a=2)]
                pP_ks = [pP_k, ppj_k[1]]
                pQs = [pQ, pbd[1][:, 0:256].rearrange("p (a t) -> p a t", a=2)]
                pQ_ks = [pQ_k, pbd_k[1]]

                def bdsum(src_bf, src_k, consume):
                    for tb in range(NTB):
                        u = bdi[0] % 2
                        bdi[0] += 1
                        K.mm(pbd[u][:, 0:TB], [(bdones[:], src_bf[:, tb * TB:(tb + 1) * TB])], R=[src_k, cst], W=[pbd_k[u]])
                        consume(pbd[u][:, 0:TB], pbd_k[u], tb)

                import os as _os
                for c4 in [int(q) for q in _os.environ.get('C4LIST', '0,1,2,3').split(',')]:
                    K.barrier()
                    csl = slice(c4 * 128, (c4 + 1) * 128)
                    project_shift(c4, lambda: K.op(PL, lambda e: e.tensor_copy(out=r32[:], in_=T3[:]), R=[T_k[2]], W=[r_k_]))
                    project_shift(4 + c4, lambda: K.op(PL, lambda e: e.tensor_copy(out=k32[:], in_=T3[:]), R=[T_k[2]], W=[k_k_]))
                    project_shift(8 + c4, lambda: K.op(ACT, lambda e: e.activation(out=vb[:], in_=T3[:], func=AF.Copy), R=[T_k[2]], W=[vb_k]))
                    ckpt('rw_proj%d' % c4)
                    for tb in range(NTB):
                        u = pji[0] % 2
                        pji[0] += 1
                        K.mm(ppj[u][:, 0:TB], [(gup[:, csl], sg[:, tb * TB:(tb + 1) * TB])], R=[wsm_k, sg_k], W=[ppj_k[u]])
                        K.op(ACT, lambda e, u=u, tb=tb: e.activation(out=gTb[:, tb * TB:(tb + 1) * TB], in_=ppj[u][:, 0:TB], func=AF.Copy), R=[ppj_k[u]], W=[gT_k])
                    K.op(DVE, lambda e: e.tensor_scalar(out=kk32[:], in0=k32[:], scalar1=kkp[:, c4:c4 + 1], scalar2=None, op0=ALU.mult), R=[k_k_, kkp_k], W=[kk_k_])
                    K.op(PL, lambda e: e.tensor_tensor(out=sqb[:], in0=kk32[:], in1=kk32[:], op=ALU.mult), R=[kk_k_], W=[sqb_k])

                    def cons_kk(ps_, pk, tb):
                        tsl = slice(tb * TB, (tb + 1) * TB)
                        K.op(ACT, lambda e: e.activation(out=T4[:, tsl], in_=ps_[:], func=AF.Sqrt, bias=1e-24), R=[pk], W=[T_k[3]])
                        K.op(DVE, lambda e: e.reciprocal(out=T4[:, tsl], in_=T4[:, tsl]), R=[T_k[3]], W=[T_k[3]])
                    bdsum(sqb, sqb_k, cons_kk)
                    K.op(DVE, lambda e: e.tensor_tensor(out=kk32[:], in0=kk32[:], in1=T4[:], op=ALU.mult), R=[kk_k_, T_k[3]], W=[kk_k_])
                    if b == 0 and c4 == 0:
                        dump("kk", kk32[:], [128, S], R=[kk_k_])
                        pass

                    ckpt('rw_kk%d' % c4)
                    for d in range(2):
                        lw = T1[:, 1:S + 1]
                        for tb in range(NTB):
                            tsl = slice(tb * TB, (tb + 1) * TB)
                            u = pji[0] % 2
                            pji[0] += 1
                            K.mm(ppj[u][:, 0:TB], [(wup[0:64, d, csl], twda[0:64, tsl])], R=[wsm_k, twda_k], W=[ppj_k[u]])
                            K.op(ACT, lambda e, u=u, tsl=tsl: e.activation(out=lw[:, tsl], in_=ppj[u][:, 0:TB], func=AF.Sigmoid, bias=w0[:, d, c4:c4 + 1]),
                                 R=[ppj_k[u], w0_k], W=[T_k[0]])
                        for n_ in range(NC):
                            K.op(DVE, lambda e, n_=n_: e.tensor_tensor_scan(out=T2[:, n_ * 128:(n_ + 1) * 128], data0=onesf[:], data1=lw[:, n_ * 128:(n_ + 1) * 128],
                                                                        initial=0.0, op0=ALU.mult, op1=ALU.add),
                                 R=[T_k[0], cst], W=[T_k[1]])
                        cs3 = T2[:].rearrange("p (c t) -> p c t", t=128)
                        K.op(ACT, lambda e: e.activation(out=gC[:], in_=cs3[:, :, 127], func=AF.Exp, scale=-c_), R=[T_k[1]], W=[gC_k])
                        if d == 0:
                            K.op(DVE, lambda e: e.tensor_tensor(out=lw, in0=T2[:], in1=lw, op=ALU.subtract), R=[T_k[0], T_k[1]], W=[T_k[0]])
                            gexc, gexc_k, ginc, ginc_k = lw, T_k[0], T2[:], T_k[1]
                        else:
                            K.op(PL, lambda e: e.tensor_copy(out=T4[:].rearrange("p (c t) -> p c t", t=128), in_=bc(cs3[:, :, 127:128], [128, NC, 128])),
                                 R=[T_k[1]], W=[T_k[3]])
                            K.op(DVE, lambda e: e.tensor_tensor(out=T2[:], in0=T4[:], in1=T2[:], op=ALU.subtract), R=[T_k[1], T_k[3]], W=[T_k[1]])
                            K.op(DVE, lambda e: e.tensor_tensor(out=lw, in0=lw, in1=T2[:], op=ALU.add), R=[T_k[0], T_k[1]], W=[T_k[0]])
                            gexc, gexc_k, ginc, ginc_k = T2[:], T_k[1], lw, T_k[0]
                        A3 = [ART[:, 0, :, 0, :], ART[:, 1, :, 0, :]]
                        R3 = [ART[:, 0, :, 1, :], ART[:, 1, :, 1, :]]
                        v3 = lambda ap: ap.rearrange("p (c t) -> p c t", t=128)
                        K.op(ACT, lambda e: e.activation(out=gexc, in_=gexc, func=AF.Exp, scale=-c_), R=[gexc_k], W=[gexc_k])
                        for hh in range(2):
                            K.op(DVE, lambda e, hh=hh: e.scalar_tensor_tensor(out=A3[hh], in0=v3(kk32[:]), scalar=hmask[:, 2 + hh:3 + hh], in1=v3(gexc), op0=ALU.mult, op1=ALU.mult),
                                 R=[kk_k_, gexc_k, cst], W=[ART_k])
                        K.op(ACT, lambda e: e.activation(out=T3[:], in_=ginc, func=AF.Exp, scale=-c_), R=[ginc_k], W=[T_k[2]])
                        for hh in range(2):
                            K.op(DVE, lambda e, hh=hh: e.scalar_tensor_tensor(out=R3[hh], in0=v3(r32[:]), scalar=hmask[:, hh:hh + 1], in1=v3(T3[:]), op0=ALU.mult, op1=ALU.mult),
                                 R=[r_k_, T_k[2], cst], W=[ART_k])
                        K.op(ACT, lambda e: e.activation(out=ginc, in_=ginc, func=AF.Exp, scale=c_), R=[ginc_k], W=[ginc_k])
                        for tb in range(NTB):
                            tsl = slice(tb * TB, (tb + 1) * TB)
                            u = pji[0] % 2
                            pji[0] += 1
                            K.mm(ppj[u][:, 0:TB], [(aup[64:128, d, csl], twda[64:128, tsl])], R=[wsm_k, twda_k], W=[ppj_k[u]])
                            K.op(ACT, lambda e, u=u, tsl=tsl: e.activation(out=T3[:, tsl], in_=ppj[u][:, 0:TB], func=AF.Sigmoid, bias=a0[:, d, c4:c4 + 1]),
                                 R=[ppj_k[u], a0_k], W=[T_k[2]])
                        Tg = gexc
                        K.op(DVE, lambda e: e.tensor_tensor(out=Tg, in0=kk32[:], in1=T3[:], op=ALU.mult), R=[kk_k_, T_k[2], ART_k], W=[gexc_k])
                        K.op(DVE, lambda e: e.tensor_tensor(out=BT[:], in0=Tg, in1=ginc, op=ALU.mult), R=[gexc_k, ginc_k], W=[BT_k])
                        K.op(DVE, lambda e: e.tensor_scalar(out=Tg, in0=T3[:], scalar1=kap[:, c4:c4 + 1], scalar2=omka[:, c4:c4 + 1], op0=ALU.mult, op1=ALU.add),
                             R=[T_k[2], kap_k, BT_k], W=[gexc_k])
                        K.op(DVE, lambda e: e.tensor_tensor(out=Tg, in0=Tg, in1=k32[:], op=ALU.mult), R=[gexc_k, k_k_], W=[gexc_k])
                        K.op(PL, lambda e: e.tensor_tensor(out=KT[:], in0=Tg, in1=ginc, op=ALU.mult), R=[gexc_k, ginc_k], W=[KT_k])
                        K.op(DVE, lambda e: e.scalar_tensor_tensor(out=sqb[:], in0=r32[:], scalar=rkp[:, c4:c4 + 1], in1=Tg, op0=ALU.mult, op1=ALU.mult),
                             R=[r_k_, gexc_k, rkp_k], W=[sqb_k])

                        def cons_b(ps_, pk, tb, d=d):
                            tsl = slice(tb * TB, (tb + 1) * TB)
                            if d == 0:
                                K.op(DVE, lambda e: e.tensor_tensor(out=bacc[:, tsl], in0=ps_[:], in1=vb[:, tsl], op=ALU.mult), R=[pk, vb_k], W=[ba_k])
                            else:
                                K.op(DVE, lambda e: e.tensor_tensor(out=T3[:, tsl], in0=ps_[:], in1=vb[:, tsl], op=ALU.mult), R=[pk, vb_k], W=[T_k[2]])
                                K.op(PL, lambda e: e.tensor_tensor(out=bacc[:, tsl], in0=bacc[:, tsl], in1=T3[:, tsl], op=ALU.add), R=[T_k[2]], W=[ba_k])
                        bdsum(sqb, sqb_k, cons_b)
                        if b == 0 and c4 == 0:
                            dump(f"AT{d}", ART[:, 0, :, 0, :], [128, NC, 128], R=[ART_k])
                            dump(f"BT{d}", BT[:], [128, S], R=[BT_k])

                        ckpt('rw_prep%d_%d' % (c4, d))
                        K.op(DVE, lambda e: e.memset(H32[:], 0.0), W=[H_k])
                        K.op(DVE, lambda e: e.memset(Hb[:], 0.0), W=[Hb_k])
                        K.op(DVE, lambda e: e.memset(Hbd[:], 0.0), W=[Hbd_k])
                        order = range(NC) if d == 0 else range(NC - 1, -1, -1)
                        hs = [slice(0, 64), slice(64, 128)]

                        def prep(n, q, r, d=d):
                            nsl = slice(n * 128, (n + 1) * 128)
                            tk, tk_k = tok3s[q], tok3s_k[q]
                            m1, m1_k, m2, m2_k = M1s[q], M1s_k[q], M2s[q], M2s_k[q]
                            xa, xa_k, pa, pa_k = XAs[q], XAs_k[q], PAs[q], PAs_k[q]
                            pP, pP_k, pQ, pQ_k = pPs[r], pP_ks[r], pQs[r], pQ_ks[r]
                            K.tr(ptk3[:, 0, :], BT[:, nsl], identb[:], R=[BT_k, cst], W=[ptk3_k], inc=False)
                            K.tr(ptk3[:, 1, :], KT[:, nsl], identb[:], R=[KT_k], W=[ptk3_k], inc=False)
                            K.tr(ptk3[:, 2, :], vb[:, nsl], identb[:], R=[vb_k], W=[ptk3_k])
                            for hh in range(2):
                                K.op(ACT if hh == 0 else DVE, (lambda e, hh=hh: e.activation(out=tk[:, :, hh, hh * 64:(hh + 1) * 64], in_=ptk3[:, :, hh * 64:(hh + 1) * 64], func=AF.Copy)) if hh == 0 else
                                     (lambda e, hh=hh: e.tensor_copy(out=tk[:, :, hh, hh * 64:(hh + 1) * 64], in_=ptk3[:, :, hh * 64:(hh + 1) * 64])), R=[ptk3_k], W=[tk_k])
                            yield
                            for hh in range(2):
                                K.mm(pP[:, hh, :], [(BT[:, nsl], ART[:, hh, n, :, :].rearrange("p a t -> p (a t)"))], R=[BT_k, ART_k], W=[pP_k], inc=(hh == 1))
                            for hh in range(2):
                                K.op(DVE, lambda e, hh=hh: e.tensor_tensor(out=m1[:, hh, :], in0=pP[:, hh, :], in1=MP[d][:], op=ALU.mult), R=[pP_k, cst], W=[m1_k])
                            yield
                            for hh in range(2):
                                K.mm(pP[:, hh, :], [(KT[:, nsl], ART[:, hh, n, :, :].rearrange("p a t -> p (a t)"))], R=[KT_k, ART_k], W=[pP_k], inc=(hh == 1))
                            for hh in range(2):
                                K.op(DVE, lambda e, hh=hh: e.tensor_tensor(out=m2[:, hh, :], in0=pP[:, hh, :], in1=MP[d][:], op=ALU.mult), R=[pP_k, cst], W=[m2_k])
                            yield
                            for hh in range(2):
                                K.mm(pQ[:, hh, :], [(ART[:, hh, n, 0, :], BT[:, nsl])], R=[BT_k, ART_k], W=[pQ_k], inc=(hh == 1))
                            for hh in range(2):
                                K.op(DVE, lambda e, hh=hh: e.tensor_tensor(out=pa[0][:, hh, :], in0=pQ[:, hh, :], in1=ML[d][:], op=ALU.mult), R=[pQ_k, cst], W=[pa_k[0]])
                            yield
                            K.op(PL, lambda e: e.tensor_tensor(out=xa[1][:, :, 1, :], in0=m1[:, :, 0:128], in1=identb2, op=ALU.add), R=[m1_k, cst], W=[xa_k[1]])
                            for hh in range(2):
                                K.mm(pP[:, hh, 0:128], [(pa[0][:, hh, :], m1[:, hh, 0:128])], R=[pa_k[0], m1_k], W=[pP_k], inc=(hh == 1))
                            K.op(ACT, lambda e: e.activation(out=xa[1][:, :, 0, :], in_=pP[:, :, 0:128], func=AF.Copy), R=[pP_k], W=[xa_k[1]])
                            for hh in range(2):
                                K.mm(pQ[:, hh, :], [(m1[:, hh, 0:128], pa[0][:, hh, :])], R=[pa_k[0], m1_k], W=[pQ_k], inc=(hh == 1))
                            K.op(DVE, lambda e: e.tensor_copy(out=pa[1][:], in_=pQ[:]), R=[pQ_k], W=[pa_k[1]])
                            yield
                            cur = 1
                            for lev in range(1, 7):
                                nx = 1 - cur
                                if lev < 6:
                                    for hh in range(2):
                                        K.mm(pP[:, hh, :], [(pa[cur][:, hh, :], xa[cur][:, hh, :, :].rearrange("p a t -> p (a t)"))],
                                             R=[pa_k[cur], xa_k[cur]], W=[pP_k], inc=(hh == 1))
                                    K.op(ACT, lambda e, nx=nx: e.activation(out=xa[nx][:, :, 0, :], in_=pP[:, :, 0:128], func=AF.Copy), R=[pP_k], W=[xa_k[nx]])
                                    K.op(DVE, lambda e, nx=nx, cur=cur: e.tensor_tensor(out=xa[nx][:, :, 1, :], in0=pP[:, :, 128:256], in1=xa[cur][:, :, 1, :], op=ALU.add),
                                         R=[pP_k, xa_k[cur]], W=[xa_k[nx]])
                                    for hh in range(2):
                                        K.mm(pQ[:, hh, :], [(xa[cur][:, hh, 0, :], pa[cur][:, hh, :])], R=[pa_k[cur], xa_k[cur]], W=[pQ_k], inc=(hh == 1))
                                    K.op(DVE, lambda e, nx=nx: e.tensor_copy(out=pa[nx][:], in_=pQ[:]), R=[pQ_k], W=[pa_k[nx]])
                                else:
                                    for hh in range(2):
                                        K.mm(pP[:, hh, 128:256], [(pa[cur][:, hh, :], xa[cur][:, hh, 1, :])], R=[pa_k[cur], xa_k[cur]], W=[pP_k], inc=(hh == 1))
                                    K.op(DVE, lambda e, nx=nx, cur=cur: e.tensor_tensor(out=xa[nx][:, :, 1, :], in0=pP[:, :, 128:256], in1=xa[cur][:, :, 1, :], op=ALU.add),
                                         R=[pP_k, xa_k[cur]], W=[xa_k[nx]])
                                cur = nx
                                yield
                            assert cur == 1

                        def chain(n, q, d=d):
                            nsl = slice(n * 128, (n + 1) * 128)
                            tk, tk_k = tok3s[q], tok3s_k[q]
                            m1, m1_k, m2, m2_k = M1s[q], M1s_k[q], M2s[q], M2s_k[q]
                            Wf, Wf_k = XAs[q][1], XAs_k[q][1]
                            for hh in range(2):
                                K.mm(pZ[:, hh, :], [(ART[:, hh, n, 0, :], Hb[:, :]), (m2[:, hh, 0:128], tk[:, 2, hh, hs[hh]])],
                                     R=[ART_k, Hb_k, m2_k, tk_k], W=[pZ_k[0]])
                            K.op(ACT, lambda e: e.activation(out=Zb[:], in_=pZ[:, 0:2, :], func=AF.Copy), R=[pZ_k[0]], W=[Zb_k])
                            yield
                            for hh in range(2):
                                K.mm(pZ[:, 2 + hh, :], [(Wf[:, hh, 1, :], Zb[:, hh, :])], R=[Wf_k, Zb_k], W=[pZ_k[1]])
                            K.op(ACT, lambda e: e.activation(out=Ub[:], in_=pZ[:, 2:4, :], func=AF.Copy), R=[pZ_k[1]], W=[Ub_k])
                            for hh in range(2):
                                K.op(DVE, lambda e, hh=hh: e.tensor_copy(out=Ub2[:, hh, hh * 64:(hh + 1) * 64], in_=Ub[:, hh, :]), R=[Ub_k], W=[Ub2_k])
                            yield
                            K.mm(pY[:, 128:192], [(tk[:, 0, 0, :], Ub[:, 0, :]), (tk[:, 0, 1, :], Ub[:, 1, :]),
                                                  (tk[:, 1, 0, :], tk[:, 2, 0, 0:64]), (tk[:, 1, 1, :], tk[:, 2, 1, 64:128])],
                                 R=[tk_k, Ub_k], W=[pY_k[1]])
                            K.mm(pY[:, 0:128], [(Hbd[:], ART[:, 0, n, 1, :]), (Hbd[:], ART[:, 1, n, 1, :]),
                                                (Ub2[:, 0, :], m1[:, 0, 128:256]), (Ub2[:, 1, :], m1[:, 1, 128:256]),
                                                (tk[:, 2, 0, :], m2[:, 0, 128:256]), (tk[:, 2, 1, :], m2[:, 1, 128:256])],
                                 R=[Hbd_k, ART_k, Ub2_k, m1_k, m2_k, tk_k], W=[pY_k[0]])
                            K.op(DVE, lambda e: e.tensor_tensor(out=Htmp[:], in0=pY[:, 128:192], in1=H32[:], op=ALU.add), R=[pY_k[1], H_k], W=[Ht_k])
                            if d == 0:
                                K.op(DVE, lambda e, nsl=nsl: e.tensor_copy(out=yacc[:, nsl], in_=pY[:, 0:128]), R=[pY_k[0]], W=[ya_k])
                            else:
                                K.op(DVE, lambda e, nsl=nsl: e.tensor_tensor(out=yacc[:, nsl], in0=pY[:, 0:128], in1=yacc[:, nsl], op=ALU.add), R=[pY_k[0], ya_k], W=[ya_k])
                            yield
                            K.op(DVE, lambda e, n=n: e.tensor_scalar(out=H32[:], in0=Htmp[:], scalar1=gC[:, n:n + 1], scalar2=None, op0=ALU.mult), R=[Ht_k, gC_k], W=[H_k])
                            K.op(ACT, lambda e, n=n: e.activation(out=Hb[:], in_=Htmp[:], func=AF.Copy, scale=gC[:, n:n + 1]), R=[Ht_k, gC_k], W=[Hb_k])
                            for hh in range(2):
                                K.op(PL, lambda e, hh=hh: e.tensor_copy(out=Hbd[hs[hh], hh * 64:(hh + 1) * 64], in_=H32[hs[hh], :]), R=[H_k], W=[Hbd_k])
                            yield

                        order = list(order)
                        K.barrier()
                        if NSETS > 2:
                            K.op(DVE, lambda e: e.memset(tok3s[2], 0.0), W=[tok3s_k[2]])
                        nch = len(order)
                        act_preps, done_prep = [], set()
                        next_prep, chain_k, completed, chain_gen = 0, 0, 0, None
                        while chain_k < nch:
                            while len(act_preps) < 2 and next_prep < nch and next_prep <= completed + NSETS - 1:
                                act_preps.append((next_prep, prep(order[next_prep], next_prep % NSETS, next_prep % 2)))
                                next_prep += 1
                            if chain_gen is None and chain_k in done_prep:
                                chain_gen = chain(order[chain_k], chain_k % NSETS)
                            if chain_gen is not None:
                                try:
                                    next(chain_gen)
                                except StopIteration:
                                    chain_gen = None
                                    completed += 1
                                    chain_k += 1
                                    nck[0] += 1
                            for it_ in list(act_preps):
                                try:
                                    next(it_[1])
                                except StopIteration:
                                    act_preps.remove(it_)
                                    done_prep.add(it_[0])
                        K.barrier()
                    if b == 0 and c4 == 0:
                        dump("yacc", yacc[:], [128, S], R=[ya_k])
                        dump("bacc", bacc[:], [128, S], R=[ba_k])
                    ckpt('rw_loops%d' % c4)
                    K.op(ACT, lambda e: e.activation(out=sqb[:], in_=yacc[:], func=AF.Copy), R=[ya_k], W=[sqb_k])

                    def cons_m(ps_, pk, tb):
                        tsl = slice(tb * TB, (tb + 1) * TB)
                        K.op(DVE, lambda e: e.scalar_tensor_tensor(out=yacc[:, tsl], in0=ps_[:], scalar=-1.0 / 64, in1=yacc[:, tsl], op0=ALU.mult, op1=ALU.add),
                             R=[pk, ya_k], W=[ya_k])
                    bdsum(sqb, sqb_k, cons_m)
                    K.op(PL, lambda e: e.tensor_tensor(out=sqb[:], in0=yacc[:], in1=yacc[:], op=ALU.mult), R=[ya_k], W=[sqb_k])

                    def cons_v(ps_, pk, tb):
                        tsl = slice(tb * TB, (tb + 1) * TB)
                        K.op(ACT, lambda e: e.activation(out=T4[:, tsl], in_=ps_[:], func=AF.Sqrt, scale=1.0 / 64, bias=GN_EPS), R=[pk], W=[T_k[3]])
                        K.op(DVE, lambda e: e.reciprocal(out=T4[:, tsl], in_=T4[:, tsl]), R=[T_k[3]], W=[T_k[3]])
                    bdsum(sqb, sqb_k, cons_v)
                    K.op(DVE, lambda e: e.tensor_tensor(out=yacc[:], in0=yacc[:], in1=T4[:], op=ALU.mult), R=[ya_k, T_k[3]], W=[ya_k])
                    K.op(DVE, lambda e: e.tensor_scalar(out=yacc[:], in0=yacc[:], scalar1=lnw[:, c4:c4 + 1], scalar2=lnb[:, c4:c4 + 1], op0=ALU.mult, op1=ALU.add),
                         R=[ya_k, lnw_k, lnb_k], W=[ya_k])
                    K.op(PL, lambda e: e.tensor_tensor(out=yacc[:], in0=yacc[:], in1=bacc[:], op=ALU.add), R=[ya_k, ba_k], W=[ya_k])
                    K.op(DVE, lambda e: e.tensor_tensor(out=catT[:, 4 + c4, :], in0=yacc[:], in1=gTb[:], op=ALU.mult), R=[ya_k, gT_k], W=[cat_k[4 + c4]])
                    ckpt('rw_gn%d' % c4)
            if b == 0:
                dump("orw", catT[:, 4, :], [128, S], R=cat_k)

        ckpt('rwkv')
        with K.scope():
            x1 = K.sb([128, NT, D], F32, "x1")
            x1_k = [Tok() for _ in range(NT)]
            h2t = K.sb([128, NT, D], BF16, "h2t")
            h2_k = [Tok() for _ in range(NT)]
            afft = K.sb([128, NT, NE], F32, "afft")
            aff_k = [Tok() for _ in range(NT)]
            posm = K.sb([16, S], F32, "posm")
            posm_k = Tok()
            post = K.sb([128, NT, NE], F32, "post")
            post_k = Tok()
            gt2b = K.sb([128, 1, D], F32, "gt2b")
            gt2b_k = Tok()
            bcast_rows(b, gt2b, gt2b_k, [(modT, 40)])
            K.stacks.append(ExitStack())
            affT = K.sb([16, S], F32, "affT")
            affT_k = Tok()
            with K.scope():
                bct = K.sb([128, 3, D], F32, "bct")
                bct_k = Tok()
                bcast_rows(b, bct, bct_k, [(modT, 16), (S2, 0), (modT, 24)])
                wo = K.sb([128, KD, D], BF16, "wo")
                wo_k = Tok()
                K.dma(POOL, K.dmac("wo"), wo[:], wout_d.rearrange("(j p) n -> p j n", p=128), W=[wo_k])
                xc = [K.dmac("x0")]
                xt = [K.sb([128, D], F32, "xt")]
                xt_k = [Tok()]
                po = [K.ps([128, 512], F32, "po") for _ in range(2)]
                po_k = [Tok(), Tok()]
                st_ = [K.sb([128, 4], F32, "st") for _ in range(2)]
                st_k = [Tok(), Tok()]
                pt = [K.ps([128, KD, 128], BF16, "pt") for _ in range(2)]
                pt_k = [Tok(), Tok()]
                h2T = [K.sb([128, KD, 128], BF16, "h2T") for _ in range(2)]
                h2T_k = [Tok(), Tok()]
                plg = K.ps([128, NE], F32, "plg")
                plg_k = Tok()
                lg = K.sb([128, NE], F32, "lg")
                lg_k = Tok()
                paT = K.ps([16, 128], F32, "paT")
                paT_k = Tok()
                for i in range(NT):
                    if i % 4 == 0 and i > 0:
                        K.barrier()
                    u = i % 2
                    sl = slice(i * 128, (i + 1) * 128)
                    K.dma(SP, xc[0], xt[0][:], x_d[tok0 + i * 128: tok0 + (i + 1) * 128, :], W=[xt_k[0]])
                    for half in range(2):
                        hsl = slice(half * 512, (half + 1) * 512)
                        K.mm(po[half][:], [(catT[:, j, sl], wo[:, j, hsl]) for j in range(KD)], R=cat_k + [wo_k], W=[po_k[half]])
                        K.op(DVE, lambda e, half=half, hsl=hsl, i=i: e.tensor_tensor(out=x1[:, i, hsl], in0=po[half][:], in1=bct[:, 0, hsl], op=ALU.mult),
                             R=[po_k[half], bct_k], W=[x1_k[i]])
                    K.op(PL, lambda e, i=i: e.tensor_tensor(out=x1[:, i, :], in0=x1[:, i, :], in1=xt[0][:], op=ALU.add), R=[x1_k[i], xt_k[0]], W=[x1_k[i]])
                    K.op(ACT, lambda e, u=u, i=i: e.activation(out=h2T[u][:].rearrange("p j t -> p (j t)"), in_=x1[:, i, :], func=AF.Square, accum_out=st_[u][:, 0:1]),
                         R=[x1_k[i]], W=[h2T_k[u], st_k[u]])
                    K.op(ACT, lambda e, u=u: e.activation(out=st_[u][:, 1:2], in_=st_[u][:, 0:1], func=AF.Sqrt, scale=1.0 / D, bias=NORM_EPS), R=[st_k[u]], W=[st_k[u]])
                    K.op(DVE, lambda e, u=u: e.reciprocal(out=st_[u][:, 1:2], in_=st_[u][:, 1:2]), R=[st_k[u]], W=[st_k[u]])
                    K.op(DVE, lambda e, u=u, i=i: e.scalar_tensor_tensor(out=h2t[:, i, :], in0=x1[:, i, :], scalar=st_[u][:, 1:2], in1=bct[:, 1, :], op0=ALU.mult, op1=ALU.mult),
                         R=[x1_k[i], st_k[u], bct_k], W=[h2_k[i]])
                    K.op(PL, lambda e, i=i: e.tensor_tensor(out=h2t[:, i, :], in0=h2t[:, i, :], in1=bct[:, 2, :], op=ALU.add), R=[h2_k[i], bct_k], W=[h2_k[i]])
                    for j in range(KD):
                        K.tr(pt[u][:, j, :], h2t[:, i, j * 128:(j + 1) * 128], identb[:], R=[h2_k[i], cst], W=[pt_k[u]], inc=(j == KD - 1))
                    K.op(ACT, lambda e, u=u: e.activation(out=h2T[u][:], in_=pt[u][:], func=AF.Copy), R=[pt_k[u]], W=[h2T_k[u]])
                    K.mm(plg[:], [(h2T[u][:, j, :], wrt[:, j, :]) for j in range(KD)], R=[h2T_k[u], wsm_k], W=[plg_k])
                    K.op(DVE, lambda e, u=u: e.tensor_reduce(out=st_[u][:, 2:3], in_=plg[:], axis=AX.X, op=ALU.max), R=[plg_k], W=[st_k[u]])
                    K.op(DVE, lambda e, u=u: e.tensor_scalar(out=st_[u][:, 2:3], in0=st_[u][:, 2:3], scalar1=-1.0, scalar2=None, op0=ALU.mult), R=[st_k[u]], W=[st_k[u]])
                    K.op(ACT, lambda e, u=u: e.activation(out=lg[:], in_=plg[:], func=AF.Exp, bias=st_[u][:, 2:3], accum_out=st_[u][:, 3:4]),
                         R=[plg_k, st_k[u]], W=[lg_k, st_k[u]])
                    K.op(DVE, lambda e, u=u: e.reciprocal(out=st_[u][:, 3:4], in_=st_[u][:, 3:4]), R=[st_k[u]], W=[st_k[u]])
                    K.op(DVE, lambda e, u=u, i=i: e.tensor_scalar(out=afft[:, i, :], in0=lg[:], scalar1=st_[u][:, 3:4], scalar2=None, op0=ALU.mult),
                         R=[lg_k, st_k[u]], W=[aff_k[i]])
                    K.tr(paT[:], afft[:, i, :], identf[:], R=[aff_k[i], cst], W=[paT_k])
                    K.op(DVE, lambda e, sl=sl: e.tensor_copy(out=affT[:, sl], in_=paT[:]), R=[paT_k], W=[affT_k])
            if b == 0:
                dump("x1", x1[:, 0, :], [128, D], R=x1_k)
                dump("affT", affT[:], [16, S], R=[affT_k])

            ckpt('outproj')
            with K.scope():
                wk = [K.sb([16, S], F32, "wk") for _ in range(2)]
                wk_k = [Tok(), Tok()]
                m8 = K.sb([16, 8], F32, "m8")
                m8_k = Tok()
                mk_ = K.sb([16, S], F32, "mk")
                mk_k = Tok()
                ppo = K.ps([128, NE], F32, "ppo")
                ppo_k = Tok()
                K.op(DVE, lambda e: e.tensor_copy(out=wk[0][:], in_=affT[:]), R=[affT_k], W=[wk_k[0]])
                nit = CAP // 8
                cur = 0
                for it in range(nit):
                    K.op(DVE, lambda e, cur=cur: e.max(out=m8[:], in_=wk[cur][:]), R=[wk_k[cur]], W=[m8_k])
                    if it < nit - 1:
                        K.op(DVE, lambda e, cur=cur: e.match_replace(out=wk[1 - cur][:], in_to_replace=m8[:], in_values=wk[cur][:], imm_value=-1.0),
                             R=[wk_k[cur], m8_k], W=[wk_k[1 - cur]])
                        cur = 1 - cur
                K.op(DVE, lambda e: e.tensor_scalar(out=mk_[:], in0=affT[:], scalar1=m8[:, 7:8], scalar2=None, op0=ALU.is_ge), R=[affT_k, m8_k], W=[mk_k])
                K.op(DVE, lambda e: e.tensor_tensor_scan(out=posm[:], data0=onesf[0:16, 0:1].to_broadcast([16, S]), data1=mk_[:], initial=0.0, op0=ALU.mult, op1=ALU.add),
                     R=[mk_k, cst], W=[posm_k])
                K.op(DVE, lambda e: e.tensor_tensor(out=posm[:], in0=posm[:], in1=mk_[:], op=ALU.mult), R=[posm_k, mk_k], W=[posm_k])
                K.op(DVE, lambda e: e.tensor_scalar(out=posm[:], in0=posm[:], scalar1=-1.0, scalar2=None, op0=ALU.add), R=[posm_k], W=[posm_k])
                for i in range(NT):
                    K.tr(ppo[:], posm[:, i * 128:(i + 1) * 128], identf[0:16, 0:16], R=[posm_k, cst], W=[ppo_k])
                    K.op(DVE, lambda e, i=i: e.tensor_copy(out=post[:, i, :], in_=ppo[:]), R=[ppo_k], W=[post_k])
            if b == 0:
                dump("posm", posm[:], [16, S], R=[posm_k])

            K.barrier()
            K.stacks.pop().close()
            ckpt('topk')
            with K.scope():
                NWS = 6
                if S >= 2048:
                    wsl = [catT[:, :, q * 512:(q + 1) * 512] for q in range(4)]
                    wsl += [K.sb([128, KD, 512], BF16, "wsl") for _ in range(NWS - 4)]
                else:
                    wsl = [K.sb([128, KD, 512], BF16, "wsl") for _ in range(NWS)]
                wsl_k = [Tok() for _ in range(NWS)]
                wsc_ = [K.dmac("wsl") for _ in range(NWS)]
                Sel = K.sb([128, NT, CAP], BF16, "Sel")
                Sel_k = Tok()
                SelT = K.sb([128, NCT, S], BF16, "SelT")
                SelT_k = Tok()
                hgT = K.sb([128, KD, CAP], BF16, "hgT")
                hgT_k = Tok()
                hidT = K.sb([128, KD, CAP], BF16, "hidT")
                hid_k = Tok()
                sgt = K.sb([128, CAP], F32, "sgt")
                sgt_k = Tok()
                ysb = K.sb([128, NCT, D], BF16, "ysb")
                ysb_k = Tok()
                ppb = K.ps([128, TB], F32, "ppb")
                ppb_k = Tok()
                pg = K.ps([128, CAP], F32, "pg")
                pg_k = Tok()
                pu = K.ps([128, CAP], F32, "pu")
                pu_k = Tok()
                ph = [K.ps([128, CAP], F32, "ph") for _ in range(2)]
                ph_k = [Tok(), Tok()]
                py = [K.ps([128, 512], F32, "py") for _ in range(2)]
                py_k = [Tok(), Tok()]
                wcount = [0]
                ohe = K.sb([16, 128], F32, "ohe")
                ohe_k = Tok()

                def wload(src_d, e_, half):
                    s = wcount[0] % NWS
                    wcount[0] += 1
                    K.dma(POOL, wsc_[s], wsl[s][:], src_d[e_, :, half * 512:(half + 1) * 512].rearrange("(j p) n -> p j n", p=128), W=[wsl_k[s]])
                    return s

                for e_ in range(NE):
                    sg0 = wload(wg_d, e_, 0)
                    sg1 = wload(wg_d, e_, 1)
                    su0 = wload(wu_d, e_, 0)
                    su1 = wload(wu_d, e_, 1)
                    for i in range(NT):
                        K.op(DVE, lambda e, i=i, e_=e_: e.tensor_scalar(out=Sel[:, i, :], in0=iotac[:, 0:CAP], scalar1=post[:, i, e_:e_ + 1], scalar2=None, op0=ALU.is_equal),
                             R=[post_k, cst], W=[Sel_k])
                    K.op(DVE, lambda e, e_=e_: e.tensor_copy(out=ohe[:], in_=bc(identf[0:16, e_:e_ + 1], [16, 128])), R=[cst], W=[ohe_k])
                    for tb in range(NTB):
                        tsl = slice(tb * TB, (tb + 1) * TB)
                        K.mm(ppb[:], [(ohe[:], posm[:, tsl])], R=[posm_k, ohe_k], W=[ppb_k])
                        for ct in range(NCT):
                            K.op(DVE, lambda e, ct=ct, tsl=tsl: e.tensor_scalar(out=SelT[:, ct, tsl], in0=ppb[:], scalar1=iotap[:, ct:ct + 1], scalar2=None, op0=ALU.is_equal),
                                 R=[ppb_k, cst], W=[SelT_k])
                    for fc in range(KD):
                        u = fc % 2
                        K.mm(ph[u][:], [(h2t[:, i, fc * 128:(fc + 1) * 128], Sel[:, i, :]) for i in range(NT)], R=h2_k + [Sel_k], W=[ph_k[u]])
                        K.op(ACT if u == 0 else DVE, (lambda e, u=u, fc=fc: e.activation(out=hgT[:, fc, :], in_=ph[u][:], func=AF.Copy)) if u == 0 else
                             (lambda e, u=u, fc=fc: e.tensor_copy(out=hgT[:, fc, :], in_=ph[u][:])), R=[ph_k[u]], W=[hgT_k])
                    for fc in range(KD):
                        gs = sg0 if fc < 4 else sg1
                        us = su0 if fc < 4 else su1
                        fo = (fc % 4) * 128
                        K.mm(pg[:], [(wsl[gs][:, j, fo:fo + 128], hgT[:, j, :]) for j in range(KD)], R=[wsl_k[gs], hgT_k], W=[pg_k])
                        K.mm(pu[:], [(wsl[us][:, j, fo:fo + 128], hgT[:, j, :]) for j in range(KD)], R=[wsl_k[us], hgT_k], W=[pu_k])
                        K.op(ACT, lambda e: e.activation(out=sgt[:], in_=pg[:], func=AF.Silu), R=[pg_k], W=[sgt_k])
                        K.op(DVE, lambda e, fc=fc: e.tensor_tensor(out=hidT[:, fc, :], in0=pu[:], in1=sgt[:], op=ALU.mult), R=[pu_k, sgt_k], W=[hid_k])
                    sd0 = wload(wd_d, e_, 0)
                    sd1 = wload(wd_d, e_, 1)
                    for ct in range(NCT):
                        for half in range(2):
                            ds_ = sd0 if half == 0 else sd1
                            hsl = slice(half * 512, (half + 1) * 512)
                            K.mm(py[half][0:CP, :], [(hidT[:, fc, ct * 128:ct * 128 + CP], wsl[ds_][:, fc, :]) for fc in range(KD)], R=[hid_k, wsl_k[ds_]], W=[py_k[half]])
                            K.op(DVE, lambda e, ct=ct, half=half, hsl=hsl: e.tensor_tensor(out=ysb[0:CP, ct, hsl], in0=py[half][0:CP, :], in1=gt2b[0:CP, 0, hsl], op=ALU.mult),
                                 R=[py_k[half], gt2b_k], W=[ysb_k])
                    for i in range(NT):
                        sl = slice(i * 128, (i + 1) * 128)
                        for half in range(2):
                            hsl = slice(half * 512, (half + 1) * 512)
                            K.mm(py[half][:], [(SelT[0:CP, ct, sl], ysb[0:CP, ct, hsl]) for ct in range(NCT)], R=[SelT_k, ysb_k], W=[py_k[half]])
                            K.op(DVE, lambda e, i=i, half=half, hsl=hsl, e_=e_: e.scalar_tensor_tensor(out=x1[:, i, hsl], in0=py[half][:], scalar=afft[:, i, e_:e_ + 1],
                                                                                                    in1=x1[:, i, hsl], op0=ALU.mult, op1=ALU.add),
                                 R=[py_k[half], aff_k[i], x1_k[i]], W=[x1_k[i]])
                for i in range(NT):
                    K.dma(SP, outc, out_d[tok0 + i * 128: tok0 + (i + 1) * 128, :], x1[:, i, :], R=[x1_k[i]])
    K.barrier()
    K.stacks[0].close()
    return nc, dump_d


def rope_tables(S):
    rows = S // 64
    row = np.repeat(np.arange(rows, dtype=np.float32), 64)
    col = np.tile(np.arange(64, dtype=np.float32), rows)
    freqs = (np.float32(10000.0) ** (-np.arange(16, dtype=np.float32) / np.float32(16))).astype(np.float32)
    ang = np.concatenate([row[:, None] * freqs, col[:, None] * freqs], axis=-1).astype(np.float32)
    return np.cos(ang).astype(np.float32), np.sin(ang).astype(np.float32)


def fm(v, n):
    return np.ascontiguousarray(np.asarray(v, np.float32).reshape(n, 128).T)


def make_in_maps(inputs, S, NSEQ, ncores):
    f = lambda a: np.ascontiguousarray(np.asarray(a, np.float32))
    NT = S // 128
    cos, sin = rope_tables(S)
    cosl = np.ascontiguousarray(cos.reshape(NT, 128, 32).transpose(1, 0, 2))
    sinl = np.ascontiguousarray(sin.reshape(NT, 128, 32).transpose(1, 0, 2))
    x = f(inputs["x"])
    c = f(inputs["c"])
    shared = {
        "w_ada": f(inputs["w_ada"][0]),
        "b_ada": fm(inputs["b_ada"][0], 48),
        "g_mix": fm(inputs["g_mix"][0], 8),
        "g_ffn": fm(inputs["g_ffn"][0], 8),
        "w_in": f(inputs["w_in"][0]),
        "q_norm": f(inputs["q_norm"][0]).reshape(1, 64),
        "k_norm": f(inputs["k_norm"][0]).reshape(1, 64),
        "mu": fm(inputs["mu_shift"][0], 14),
        "w0": np.ascontiguousarray(f(inputs["w0"][0]).reshape(2, 4, 128).transpose(2, 0, 1)),
        "a0": np.ascontiguousarray(f(inputs["a0"][0]).reshape(2, 4, 128).transpose(2, 0, 1)),
        "w_up": f(inputs["w_up"][0]),
        "a_up": f(inputs["a_up"][0]),
        "g_up": f(inputs["g_up"][0]),
        "k_k": fm(inputs["k_k"][0], 4),
        "k_a": fm(inputs["k_a"][0], 4),
        "r_k": fm(f(inputs["r_k"][0]).reshape(-1), 4),
        "ln_w": fm(inputs["ln_w"][0], 4),
        "ln_b": fm(inputs["ln_b"][0], 4),
        "w_out": f(inputs["w_out"][0]),
        "w_router": f(inputs["w_router"][0]),
        "w_gate": f(inputs["w_gate"][0]),
        "w_up_e": f(inputs["w_up_e"][0]),
        "w_down": f(inputs["w_down"][0]),
        "cos": cosl,
        "sin": sinl,
    }
    maps = []
    for i in range(ncores):
        m = dict(shared)
        m["x"] = np.ascontiguousarray(x[i * NSEQ:(i + 1) * NSEQ].reshape(NSEQ * S, D))
        cc = c[i * NSEQ:(i + 1) * NSEQ]
        m["cT"] = np.ascontiguousarray(cc.reshape(NSEQ, KD, 128).transpose(2, 1, 0))
        maps.append(m)
    return maps


def kernel(**inputs):
    x = np.asarray(inputs["x"])
    B, S, _ = x.shape
    ncores = 8
    NSEQ = B // ncores
    nc, _ = build(S=S, NSEQ=NSEQ)
    maps = make_in_maps(inputs, S, NSEQ, ncores)
    res = run_bass_kernel_spmd(nc, maps, core_ids=list(range(ncores)))
    outs = [np.asarray(r["out"]).reshape(NSEQ, S, D) for r in res.results]
    return np.concatenate(outs, axis=0).astype(np.float32)
```

```python
import numpy as np
from contextlib import ExitStack, contextmanager
import concourse.bass as bass
import concourse.mybir as mybir
from concourse.bass_utils import run_bass_kernel_spmd

F32 = mybir.dt.float32
BF16 = mybir.dt.bfloat16
AF = mybir.ActivationFunctionType
ALU = mybir.AluOpType
AX = mybir.AxisListType

D = 1024
KD = 8
HD = 64
NE = 16
DECAY = 0.606531
GN_EPS = 64e-5
NORM_EPS = 1e-6
N_IN = 2560
REBASE_T = 3000
BAR_EVERY = 4


class Tok:
    __slots__ = ("w", "r")

    def __init__(self):
        self.w = None
        self.r = {}


class Cnt:
    def __init__(self, sem, incv, eng=None, name=""):
        self.sem = sem
        self.incv = incv
        self.cnt = 0
        self.eng = eng
        self.seen = {}
        self.name = name
        self.gen = 0


class Ctx:
    def __init__(self, nc):
        self.nc = nc
        self.stacks = [ExitStack()]
        self.uid = 0
        self.allc = []
        mk = self._mkc
        self.PE = mk(nc.tensor, 1, "pe")
        self.ACT = mk(nc.scalar, 1, "act")
        self.DVE = mk(nc.vector, 1, "dve")
        self.POOL = mk(nc.gpsimd, 1, "pool")
        self.SP = Cnt(None, 0, nc.sync, "sp")
        self.dma_free = []
        self.outc = []

    def _mkc(self, eng, incv, name):
        sem = self.stacks[0].enter_context(self.nc.semaphore(f"s_{name}_{self.uid}"))
        self.uid += 1
        c = Cnt(sem, incv, eng, name)
        self.allc.append(c)
        return c

    def dmac(self, name="d"):
        return self._mkc(None, 16, name)

    def nm(self, s):
        self.uid += 1
        return f"{s}_{self.uid}"

    def sb(self, shape, dt, name="t"):
        return self.stacks[-1].enter_context(self.nc.sbuf_tensor(self.nm(name), list(shape), dt))

    def ps(self, shape, dt=F32, name="p"):
        return self.stacks[-1].enter_context(self.nc.psum_tensor(self.nm(name), list(shape), dt))

    @contextmanager
    def scope(self):
        self.stacks.append(ExitStack())
        try:
            yield
        finally:
            self.barrier()
            self.stacks.pop().close()

    def barrier(self):
        for e in (self.PE, self.ACT, self.DVE, self.POOL, self.SP):
            for f in self.allc:
                if f.cnt > 0 and e.seen.get(f, 0) < f.cnt:
                    e.eng.wait_ge(f.sem, f.cnt * f.incv)
                    e.seen[f] = f.cnt
        for f in (self.PE, self.ACT, self.DVE, self.POOL):
            if f.cnt > REBASE_T:
                f.sem = self.stacks[0].enter_context(self.nc.semaphore(f"s_{f.name}_rb{self.uid}"))
                self.uid += 1
                f.cnt = 0
                f.gen += 1
                for e in (self.PE, self.ACT, self.DVE, self.POOL, self.SP):
                    e.seen.pop(f, None)

    def op(self, e, fn, R=(), W=(), comp=None, inc=True):
        comp = comp or e
        need = {}
        for t in R:
            if t.w is not None:
                f, c, g = t.w
                if g == f.gen and c > need.get(f, 0):
                    need[f] = c
        for t in W:
            if t.w is not None:
                f, c, g = t.w
                if g == f.gen and c > need.get(f, 0):
                    need[f] = c
            for f, (c, g) in t.r.items():
                if g == f.gen and c > need.get(f, 0):
                    need[f] = c
        for f, c in need.items():
            if f is e and e is self.PE:
                continue
            if e.seen.get(f, 0) < c:
                e.eng.wait_ge(f.sem, c * f.incv)
                e.seen[f] = c
        ins = fn(e.eng)
        if inc:
            comp.cnt += 1
            ins.then_inc(comp.sem, comp.incv)
            cc = comp.cnt
        else:
            cc = comp.cnt + 1
        for t in R:
            pr = t.r.get(comp)
            if pr is None or pr[1] != comp.gen or pr[0] < cc:
                t.r[comp] = (cc, comp.gen)
        for t in W:
            t.w = (comp, cc, comp.gen)
            t.r = {}
        return ins

    def mm(self, out, pairs, R=(), W=(), start=True, stop=True, inc=True):
        n = len(pairs)
        for i, (l, r) in enumerate(pairs):
            last = i == n - 1
            self.op(self.PE,
                    lambda e, l=l, r=r, i=i, last=last: e.matmul(out, lhsT=l, rhs=r, start=(start and i == 0), stop=(stop and last)),
                    R=R if i == 0 else (), W=W, inc=(inc and last))

    def tr(self, out, in_, ident, R=(), W=(), inc=True):
        self.op(self.PE, lambda e: e.transpose(out, in_, ident), R=R, W=W, inc=inc)

    def dma(self, issuer, comp, out, in_, R=(), W=()):
        self.op(issuer, lambda e: e.dma_start(out=out, in_=in_), R=R, W=W, comp=comp)


def bc(ap, shape):
    return ap.to_broadcast(list(shape))


class _Stop(Exception):
    pass


def build(S=2048, NSEQ=4, dumps=(), stop=None):
    try:
        return _build(S, NSEQ, dumps, stop)
    except _Stop as ex:
        ex.args[1].barrier()
        ex.args[1].stacks[0].close()
        return ex.args[0]


def _build(S, NSEQ, dumps, stop):
    NT = S // 128
    QB = min(512, S)
    NQB = S // QB
    QT = QB // 128
    CAP = 2 * S // NE
    NCT = max(1, CAP // 128)
    CP = min(CAP, 128)
    TB = min(512, S)
    NTB = S // TB
    TOKS = NSEQ * S
    nc = bass.Bass("TRN2", target_bir_lowering=False)
    K = Ctx(nc)

    def din(name, shape):
        return nc.dram_tensor(name, list(shape), F32, kind="ExternalInput").ap()

    x_d = din("x", [TOKS, D])
    cT_d = din("cT", [128, KD, NSEQ])
    wada_d = din("w_ada", [D, 6 * D])
    bada_d = din("b_ada", [128, 48])
    gmix_d = din("g_mix", [128, KD])
    gffn_d = din("g_ffn", [128, KD])
    win_d = din("w_in", [D, N_IN])
    qn_d = din("q_norm", [1, HD])
    kn_d = din("k_norm", [1, HD])
    mu_d = din("mu", [128, 14])
    w0_d = din("w0", [128, 2, 4])
    a0_d = din("a0", [128, 2, 4])
    wup_d = din("w_up", [2, 64, 512])
    aup_d = din("a_up", [2, 64, 512])
    gup_d = din("g_up", [128, 512])
    kk_d = din("k_k", [128, 4])
    ka_d = din("k_a", [128, 4])
    rk_d = din("r_k", [128, 4])
    lnw_d = din("ln_w", [128, 4])
    lnb_d = din("ln_b", [128, 4])
    wout_d = din("w_out", [D, D])
    wr_d = din("w_router", [D, NE])
    wg_d = din("w_gate", [NE, D, D])
    wu_d = din("w_up_e", [NE, D, D])
    wd_d = din("w_down", [NE, D, D])
    cos_d = din("cos", [128, NT, 32])
    sin_d = din("sin", [128, NT, 32])
    out_d = nc.dram_tensor("out", [TOKS, D], F32, kind="ExternalOutput").ap()
    dump_d = {}

    PE, ACT, DVE, POOL, SP = K.PE, K.ACT, K.DVE, K.POOL, K.SP
    import os as _os0
    PL = POOL if _os0.environ.get('POOLC', '0') == '1' else DVE
    outc = K.dmac("outc")

    def ckpt(name):
        if stop == name:
            raise _Stop((nc, dump_d), K)

    def dump(name, src_ap, shape, R=()):
        if name not in dumps:
            return
        dd = nc.dram_tensor("dump_" + name, list(shape), F32, kind="ExternalOutput").ap()
        dump_d[name] = dd
        tmp = K.sb(shape, F32, "dmp")
        tk = Tok()
        K.op(DVE, lambda e: e.tensor_copy(out=tmp[:], in_=src_ap), R=R, W=[tk])
        K.dma(SP, outc, dd, tmp[:], R=[tk])

    cst = Tok()
    identf = K.sb([128, 128], F32, "identf")
    identb = K.sb([128, 128], BF16, "identb")
    onesf = K.sb([128, 128], F32, "onesf")
    bdones = K.sb([128, 128], BF16, "bdones")
    MP = [K.sb([128, 256], BF16, "MP0"), K.sb([128, 256], BF16, "MP1")]
    ML = [K.sb([128, 128], BF16, "ML0"), K.sb([128, 128], BF16, "ML1")]
    iotac = K.sb([128, 256], F32, "iotac")
    iotap = K.sb([128, 2], F32, "iotap")
    hmask = K.sb([128, 4], F32, "hmask")

    K.op(POOL, lambda e: e.memset(onesf[:], 1.0), W=[cst])
    K.stacks.append(ExitStack())
    mUPs = K.sb([128, 128], F32, "mUPs")
    mUPi = K.sb([128, 128], F32, "mUPi")
    mLOs = K.sb([128, 128], F32, "mLOs")
    mLOi = K.sb([128, 128], F32, "mLOi")
    def aff(dst, base, cm, step, cmp):
        K.op(POOL, lambda e: e.affine_select(out=dst[:], in_=onesf[:], pattern=[[step, 128]], compare_op=cmp,
                                             fill=0.0, base=base, channel_multiplier=cm), R=[cst], W=[cst])
    aff(identf, 0, 1, -1, ALU.is_equal)
    aff(mUPs, 0, -1, 1, ALU.is_gt)
    aff(mUPi, 0, -1, 1, ALU.is_ge)
    aff(mLOs, 0, 1, -1, ALU.is_gt)
    aff(mLOi, 0, 1, -1, ALU.is_ge)
    K.op(DVE, lambda e: e.tensor_copy(out=identb[:], in_=identf[:]), R=[cst], W=[cst])
    K.op(DVE, lambda e: e.memset(bdones[:], 0.0), W=[cst])
    K.op(DVE, lambda e: e.memset(bdones[0:64, 0:64], 1.0), W=[cst])
    K.op(DVE, lambda e: e.memset(bdones[64:128, 64:128], 1.0), W=[cst])
    K.op(DVE, lambda e: e.tensor_copy(out=MP[0][:, 0:128], in_=mUPs[:]), R=[cst], W=[cst])
    K.op(DVE, lambda e: e.tensor_copy(out=MP[0][:, 128:256], in_=mUPi[:]), R=[cst], W=[cst])
    K.op(DVE, lambda e: e.tensor_copy(out=MP[1][:, 0:128], in_=mLOs[:]), R=[cst], W=[cst])
    K.op(DVE, lambda e: e.tensor_copy(out=MP[1][:, 128:256], in_=mLOi[:]), R=[cst], W=[cst])
    K.op(DVE, lambda e: e.tensor_copy(out=ML[0][:], in_=mLOs[:]), R=[cst], W=[cst])
    K.op(DVE, lambda e: e.tensor_copy(out=ML[1][:], in_=mUPs[:]), R=[cst], W=[cst])
    K.barrier()
    K.stacks.pop().close()
    K.op(POOL, lambda e: e.iota(iotac[:], pattern=[[1, 256]], base=0, channel_multiplier=0,
                                allow_small_or_imprecise_dtypes=True), W=[cst])
    K.op(POOL, lambda e: e.iota(iotap[:], pattern=[[128, 2]], base=0, channel_multiplier=1,
                                allow_small_or_imprecise_dtypes=True), W=[cst])
    K.op(DVE, lambda e: e.memset(hmask[:], 0.0), W=[cst])
    K.op(DVE, lambda e: e.memset(hmask[0:64, 0:1], 1.0), W=[cst])
    K.op(DVE, lambda e: e.memset(hmask[64:128, 1:2], 1.0), W=[cst])
    K.op(DVE, lambda e: e.memset(hmask[0:64, 2:3], -1.0), W=[cst])
    K.op(DVE, lambda e: e.memset(hmask[64:128, 3:4], -1.0), W=[cst])

    ckpt('consts')
    def ldsmall(dram, shape, name, dt=F32, eng=None):
        t = K.sb(shape, dt, name)
        c = K.dmac(name)
        tk = Tok()
        K.dma(SP if dt == F32 else POOL, c, t[:], dram, W=[tk])
        return t, tk

    cT, cT_k = ldsmall(cT_d, [128, KD, NSEQ], "cT")
    bada, bada_k = ldsmall(bada_d, [128, 48], "bada")
    gmix, gmix_k = ldsmall(gmix_d, [128, KD], "gmix")
    gffn, gffn_k = ldsmall(gffn_d, [128, KD], "gffn")
    mu, mu_k = ldsmall(mu_d, [128, 14], "mu")
    w0, w0_k = ldsmall(w0_d, [128, 2, 4], "w0")
    a0, a0_k = ldsmall(a0_d, [128, 2, 4], "a0")
    kkp, kkp_k = ldsmall(kk_d, [128, 4], "kkp")
    kap, kap_k = ldsmall(ka_d, [128, 4], "kap")
    rkp, rkp_k = ldsmall(rk_d, [128, 4], "rkp")
    lnw, lnw_k = ldsmall(lnw_d, [128, 4], "lnw")
    lnb, lnb_k = ldsmall(lnb_d, [128, 4], "lnb")
    cosT, cos_k = ldsmall(cos_d, [128, NT, 32], "cos")
    sinT, sin_k = ldsmall(sin_d, [128, NT, 32], "sin")
    gain = K.sb([128, 10, HD], F32, "gain")
    gain_k = Tok()
    gc_ = K.dmac("gain")
    K.dma(SP, gc_, gain[:, 0, :], qn_d.partition_broadcast(128), W=[gain_k])
    K.dma(SP, gc_, gain[:, 8, :], kn_d.partition_broadcast(128), W=[gain_k])
    for h in range(1, 8):
        K.op(DVE, lambda e, h=h: e.tensor_copy(out=gain[:, h, :], in_=gain[:, 0, :]), R=[gain_k], W=[gain_k])
    K.op(DVE, lambda e: e.tensor_copy(out=gain[:, 9, :], in_=gain[:, 8, :]), R=[gain_k], W=[gain_k])
    K.op(DVE, lambda e: e.tensor_scalar(out=gain[:, 0:8, :], in0=gain[:, 0:8, :], scalar1=HD ** -0.5, scalar2=None, op0=ALU.mult),
         R=[gain_k], W=[gain_k])
    hmu = K.sb([128, 14], F32, "hmu")
    omm = K.sb([128, 14], F32, "omm")
    omka = K.sb([128, 4], F32, "omka")
    K.op(DVE, lambda e: e.tensor_scalar(out=hmu[:], in0=mu[:], scalar1=0.5, scalar2=None, op0=ALU.mult), R=[mu_k], W=[mu_k])
    K.op(DVE, lambda e: e.tensor_scalar(out=omm[:], in0=mu[:], scalar1=-1.0, scalar2=1.0, op0=ALU.mult, op1=ALU.add), R=[mu_k], W=[mu_k])
    K.op(DVE, lambda e: e.tensor_scalar(out=omka[:], in0=kap[:], scalar1=-1.0, scalar2=1.0, op0=ALU.mult, op1=ALU.add), R=[kap_k], W=[kap_k])
    wup = K.sb([128, 2, 512], BF16, "wup")
    aup = K.sb([128, 2, 512], BF16, "aup")
    gup = K.sb([128, 512], BF16, "gup")
    wrt = K.sb([128, KD, NE], BF16, "wrt")
    wsm_k = Tok()
    wsc = K.dmac("wsm")
    K.dma(POOL, wsc, wup[0:64, :, :], wup_d.rearrange("d k n -> k d n"), W=[wsm_k])
    K.dma(POOL, wsc, aup[64:128, :, :], aup_d.rearrange("d k n -> k d n"), W=[wsm_k])
    K.dma(POOL, wsc, gup[:], gup_d, W=[wsm_k])
    K.dma(POOL, wsc, wrt[:], wr_d.rearrange("(j p) n -> p j n", p=128), W=[wsm_k])

    ckpt('small')
    modT = K.sb([128, 48, NSEQ], F32, "modT")
    mod_k = Tok()
    S1 = K.sb([128, KD, NSEQ], F32, "S1")
    S2 = K.sb([128, KD, NSEQ], F32, "S2")
    with K.scope():
        cond = K.sb([128, KD, NSEQ], F32, "cond")
        cond_k = Tok()
        K.op(ACT, lambda e: e.activation(out=cond[:], in_=cT[:], func=AF.Silu), R=[cT_k], W=[cond_k])
        wa = [K.sb([128, KD, 512], F32, "wa") for _ in range(2)]
        wa_k = [Tok(), Tok()]
        wac = [K.dmac("wa0"), K.dmac("wa1")]
        pm = [K.ps([128, 4, NSEQ], F32, "pm") for _ in range(2)]
        pm_k = [Tok(), Tok()]
        for g in range(12):
            b = g % 2
            K.dma(SP, wac[b], wa[b][:], wada_d[:, g * 512:(g + 1) * 512].rearrange("(j p) n -> p j n", p=128), W=[wa_k[b]])
            for mm_ in range(4):
                K.mm(pm[b][:, mm_, :], [(wa[b][:, j, mm_ * 128:(mm_ + 1) * 128], cond[:, j, :]) for j in range(KD)],
                     R=[wa_k[b], cond_k], W=[pm_k[b]])
            K.op(DVE, lambda e, b=b, g=g: e.tensor_tensor(out=modT[:, g * 4:(g + 1) * 4, :], in0=pm[b][:],
                                                          in1=bc(bada[:, g * 4:(g + 1) * 4].unsqueeze(2), [128, 4, NSEQ]), op=ALU.add),
                 R=[pm_k[b], bada_k], W=[mod_k])
        for (Sx, gv, gk, off) in ((S1, gmix, gmix_k, 8), (S2, gffn, gffn_k, 32)):
            K.op(DVE, lambda e, Sx=Sx, off=off: e.tensor_scalar(out=Sx[:], in0=modT[:, off:off + 8, :], scalar1=1.0, scalar2=None, op0=ALU.add),
                 R=[mod_k], W=[mod_k])
            K.op(DVE, lambda e, Sx=Sx, gv=gv: e.tensor_tensor(out=Sx[:], in0=Sx[:], in1=bc(gv[:].unsqueeze(2), [128, KD, NSEQ]), op=ALU.mult),
                 R=[mod_k, gk], W=[mod_k])
    dump("modT", modT[:].rearrange("p m b -> p (m b)"), [128, 48 * NSEQ], R=[mod_k])

    ckpt('phaseA')
    catT = K.sb([128, KD, S], BF16, "catT")
    cat_k = [Tok() for _ in range(KD)]
    def bcast_rows(b, bct, bct_k, srcs):
        with K.scope():
            dg = [K.sb([128, 128], F32, "dg") for _ in range(2)]
            dg_k = [Tok(), Tok()]
            pb_ = [K.ps([128, 512], F32, "pb") for _ in range(2)]
            pb_k = [Tok(), Tok()]
            i = 0
            for r, (src, off) in enumerate(srcs):
                for half in range(2):
                    pi = (r * 2 + half) % 2
                    for jj in range(4):
                        j = half * 4 + jj
                        di = i % 2
                        i += 1
                        K.op(DVE, lambda e, di=di, src=src, off=off, j=j: e.tensor_scalar(
                            out=dg[di][:], in0=identf[:], scalar1=src[:, off + j, b:b + 1], scalar2=None, op0=ALU.mult),
                            R=[cst, mod_k], W=[dg_k[di]])
                        K.mm(pb_[pi][:, jj * 128:(jj + 1) * 128], [(onesf[:], dg[di][:])], R=[dg_k[di], cst], W=[pb_k[pi]])
                    K.op(ACT, lambda e, pi=pi, r=r, half=half: e.activation(out=bct[:, r, half * 512:(half + 1) * 512], in_=pb_[pi][:], func=AF.Copy),
                         R=[pb_k[pi]], W=[bct_k])

    for b in range(NSEQ):
        tok0 = b * S
        with K.scope():
            hT = K.sb([128, KD, S], BF16, "hT")
            hT_k = [Tok() for _ in range(NT)]
            with K.scope():
                xt = [K.sb([128, D], F32, "xt") for _ in range(2)]
                xt_k = [Tok(), Tok()]
                xc = [K.dmac("x0"), K.dmac("x1")]
                xn = [K.sb([128, D], BF16, "xn") for _ in range(2)]
                xn_k = [Tok(), Tok()]
                junk = K.sb([128, D], BF16, "junk")
                junk_k = Tok()
                st_ = [K.sb([128, 2], F32, "st") for _ in range(2)]
                st_k = [Tok(), Tok()]
                pt = [K.ps([128, KD, 128], BF16, "pt") for _ in range(2)]
                pt_k = [Tok(), Tok()]
                for i in range(NT):
                    if i % 4 == 0 and i > 0:
                        K.barrier()
                    u = i % 2
                    K.dma(SP, xc[u], xt[u][:], x_d[tok0 + i * 128: tok0 + (i + 1) * 128, :], W=[xt_k[u]])
                    K.op(ACT, lambda e, u=u: e.activation(out=junk[:], in_=xt[u][:], func=AF.Square, accum_out=st_[u][:, 0:1]),
                         R=[xt_k[u]], W=[junk_k, st_k[u]])
                    K.op(ACT, lambda e, u=u: e.activation(out=st_[u][:, 1:2], in_=st_[u][:, 0:1], func=AF.Sqrt, scale=1.0 / D, bias=NORM_EPS),
                         R=[st_k[u]], W=[st_k[u]])
                    K.op(DVE, lambda e, u=u: e.reciprocal(out=st_[u][:, 1:2], in_=st_[u][:, 1:2]), R=[st_k[u]], W=[st_k[u]])
                    K.op(DVE, lambda e, u=u: e.tensor_scalar(out=xn[u][:], in0=xt[u][:], scalar1=st_[u][:, 1:2], scalar2=None, op0=ALU.mult),
                         R=[xt_k[u], st_k[u]], W=[xn_k[u]])
                    for j in range(KD):
                        K.tr(pt[u][:, j, :], xn[u][:, j * 128:(j + 1) * 128], identb[:], R=[xn_k[u], cst], W=[pt_k[u]], inc=(j == KD - 1))
                    for j in range(KD):
                        eng = ACT if j % 2 == 0 else DVE
                        if eng is ACT:
                            K.op(ACT, lambda e, j=j, u=u, i=i: e.activation(out=hT[:, j, i * 128:(i + 1) * 128], in_=pt[u][:, j, :], func=AF.Identity,
                                                                             scale=S1[:, j, b:b + 1], bias=modT[:, j, b:b + 1]),
                                 R=[pt_k[u], mod_k], W=[hT_k[i]])
                        else:
                            K.op(DVE, lambda e, j=j, u=u, i=i: e.tensor_scalar(out=hT[:, j, i * 128:(i + 1) * 128], in0=pt[u][:, j, :],
                                                                                scalar1=S1[:, j, b:b + 1], scalar2=modT[:, j, b:b + 1], op0=ALU.mult, op1=ALU.add),
                                 R=[pt_k[u], mod_k], W=[hT_k[i]])
            if b == 0:
                dump("hT", hT[:, 0, :], [128, S], R=hT_k)

            ckpt('B1')
            with K.scope():
                watt = K.sb([128, KD, 768], BF16, "watt")
                watt_k = Tok()
                K.dma(POOL, K.dmac("watt"), watt[:], win_d[:, 0:768].rearrange("(j p) n -> p j n", p=128), W=[watt_k])
                qT = K.sb([128, 4, S], BF16, "qT")
                qT_k = [Tok() for _ in range(NT)]
                kT2 = K.sb([128, 2, S], BF16, "kT2")
                kT_k = [Tok() for _ in range(NT)]
                vaug = K.sb([128, NT, 2, HD + 1], BF16, "vaug")
                v_k = [Tok() for _ in range(NT)]
                K.op(DVE, lambda e: e.memset(vaug[:], 1.0), W=v_k)
                with K.scope():
                    pq = K.ps([128, 512], F32, "pq")
                    pq_k = Tok()
                    pkv = K.ps([128, 256], F32, "pkv")
                    pkv_k = Tok()
                    ptq = K.ps([128, 4, 128], BF16, "ptq")
                    ptq_k = Tok()
                    ptk = K.ps([128, 2, 128], BF16, "ptk")
                    ptk_k = Tok()
                    qk = K.sb([128, 10, HD], F32, "qk")
                    qk_k = Tok()
                    sq = K.sb([128, 10, HD], F32, "sq")
                    sq_k = Tok()
                    ss = K.sb([128, 10], F32, "ss")
                    ss_k = Tok()
                    t1 = K.sb([128, 10, 32], F32, "t1")
                    t2 = K.sb([128, 10, 32], F32, "t2")
                    t3 = K.sb([128, 10, 32], F32, "t3")
                    t4 = K.sb([128, 10, 32], F32, "t4")
                    t_k = [Tok() for _ in range(4)]
                    qr = K.sb([128, 8, HD], BF16, "qr")
                    qr_k = Tok()
                    kr = K.sb([128, 2, 2, HD], BF16, "kr")
                    kr_k = Tok()
                    for i in range(NT):
                        if i % 4 == 0 and i > 0:
                            K.barrier()
                        sl = slice(i * 128, (i + 1) * 128)
                        K.mm(pq[:], [(hT[:, j, sl], watt[:, j, 0:512]) for j in range(KD)], R=[hT_k[i], watt_k], W=[pq_k])
                        K.mm(pkv[:], [(hT[:, j, sl], watt[:, j, 512:768]) for j in range(KD)], R=[hT_k[i], watt_k], W=[pkv_k])
                        K.op(ACT, lambda e: e.activation(out=qk[:, 0:8, :].rearrange("p h d -> p (h d)"), in_=pq[:], func=AF.Copy), R=[pq_k], W=[qk_k])
                        K.op(ACT, lambda e: e.activation(out=qk[:, 8:10, :].rearrange("p h d -> p (h d)"), in_=pkv[:, 0:128], func=AF.Copy), R=[pkv_k], W=[qk_k])
                        K.op(ACT, lambda e, i=i: e.activation(out=vaug[:, i, :, 0:HD], in_=pkv[:, 128:256].rearrange("p (g d) -> p g d", g=2), func=AF.Copy),
                             R=[pkv_k], W=[v_k[i]])
                        K.op(DVE, lambda e: e.tensor_tensor(out=sq[:], in0=qk[:], in1=qk[:], op=ALU.mult), R=[qk_k], W=[sq_k])
                        K.op(DVE, lambda e: e.tensor_reduce(out=ss[:], in_=sq[:], axis=AX.X, op=ALU.add), R=[sq_k], W=[ss_k])
                        K.op(ACT, lambda e: e.activation(out=ss[:], in_=ss[:], func=AF.Sqrt, scale=1.0 / HD, bias=NORM_EPS), R=[ss_k], W=[ss_k])
                        K.op(DVE, lambda e: e.reciprocal(out=ss[:], in_=ss[:]), R=[ss_k], W=[ss_k])
                        K.op(DVE, lambda e: e.tensor_tensor(out=qk[:], in0=qk[:], in1=bc(ss[:].unsqueeze(2), [128, 10, HD]), op=ALU.mult),
                             R=[qk_k, ss_k], W=[qk_k])
                        K.op(DVE, lambda e: e.tensor_tensor(out=qk[:], in0=qk[:], in1=gain[:], op=ALU.mult), R=[qk_k, gain_k], W=[qk_k])
                        qv = qk[:].rearrange("p h (k two) -> p h k two", two=2)
                        x0, x1 = qv[:, :, :, 0], qv[:, :, :, 1]
                        cb = bc(cosT[:, i, :].unsqueeze(1), [128, 10, 32])
                        sb_ = bc(sinT[:, i, :].unsqueeze(1), [128, 10, 32])
                        K.op(DVE, lambda e: e.tensor_tensor(out=t1[:], in0=x0, in1=cb, op=ALU.mult), R=[qk_k, cos_k], W=[t_k[0]])
                        K.op(PL, lambda e: e.tensor_tensor(out=t2[:], in0=x1, in1=sb_, op=ALU.mult), R=[qk_k, sin_k], W=[t_k[1]])
                        K.op(DVE, lambda e: e.tensor_tensor(out=t3[:], in0=x0, in1=sb_, op=ALU.mult), R=[qk_k, sin_k], W=[t_k[2]])
                        K.op(PL, lambda e: e.tensor_tensor(out=t4[:], in0=x1, in1=cb, op=ALU.mult), R=[qk_k, cos_k], W=[t_k[3]])
                        qrv = qr[:].rearrange("p h (k two) -> p h k two", two=2)
                        krv = kr[:].rearrange("p g u (k two) -> p g u k two", two=2)
                        K.op(DVE, lambda e: e.tensor_tensor(out=qrv[:, :, :, 0], in0=t1[:, 0:8, :], in1=t2[:, 0:8, :], op=ALU.subtract),
                             R=[t_k[0], t_k[1]], W=[qr_k])
                        K.op(DVE, lambda e: e.tensor_tensor(out=qrv[:, :, :, 1], in0=t3[:, 0:8, :], in1=t4[:, 0:8, :], op=ALU.add),
                             R=[t_k[2], t_k[3]], W=[qr_k])
                        for u_ in range(2):
                            K.op(PL, lambda e, u_=u_: e.tensor_tensor(out=krv[:, :, u_, :, 0], in0=t1[:, 8:10, :], in1=t2[:, 8:10, :], op=ALU.subtract),
                                 R=[t_k[0], t_k[1]], W=[kr_k])
                            K.op(PL, lambda e, u_=u_: e.tensor_tensor(out=krv[:, :, u_, :, 1], in0=t3[:, 8:10, :], in1=t4[:, 8:10, :], op=ALU.add),
                                 R=[t_k[2], t_k[3]], W=[kr_k])
                        for pr in range(4):
                            K.tr(ptq[:, pr, :], qr[:, 2 * pr:2 * pr + 2, :].rearrange("p h d -> p (h d)"), identb[:], R=[qr_k, cst], W=[ptq_k], inc=(pr == 3))
                        K.op(ACT, lambda e, sl=sl: e.activation(out=qT[:, :, sl], in_=ptq[:], func=AF.Copy), R=[ptq_k], W=[qT_k[i]])
                        for g in range(2):
                            K.tr(ptk[:, g, :], kr[:, g, :, :].rearrange("p u d -> p (u d)"), identb[:], R=[kr_k, cst], W=[ptk_k], inc=(g == 1))
                        K.op(DVE, lambda e, sl=sl: e.tensor_copy(out=kT2[:, :, sl], in_=ptk[:]), R=[ptk_k], W=[kT_k[i]])
                if b == 0:
                    dump("qT", qT[:, 0, :], [128, S], R=qT_k)
                    dump("kT", kT2[:, 0, :], [128, S], R=kT_k)
                ckpt('attproj')
                with K.scope():
                    NPS = 2
                    psc = [K.ps([128, QB], F32, "psc") for _ in range(NPS)]
                    psc_k = [Tok() for _ in range(NPS)]
                    pTa = [K.sb([128, NT, QB], BF16, "pTa") for _ in range(2)]
                    pTa_k = [Tok(), Tok()]
                    oacc = [K.ps([128, QT, 128], F32, "oacc") for _ in range(2)]
                    oacc_k = [Tok(), Tok()]
                    rs = K.sb([128, QT], F32, "rs")
                    rs_k = Tok()
                    otm = K.sb([128, QT, 512], BF16, "otm")
                    otm_k = Tok()
                    pto = K.ps([128, 4, 128], BF16, "pto")
                    pto_k = Tok()
                    it = 0
                    for qb in range(NQB):
                        qsl = slice(qb * QB, (qb + 1) * QB)
                        qtoks = qT_k[qb * QT:(qb + 1) * QT]
                        for hq in range(8):
                            g = hq // 4
                            pb0 = 64 * (hq % 2)
                            pr = hq // 2
                            oa = oacc[hq % 2]
                            oa_k = oacc_k[hq % 2]
                            pa = pTa[hq % 2]
                            pa_k = pTa_k[hq % 2]
                            for kt in range(NT):
                                u = it % NPS
                                it += 1
                                K.mm(psc[u][:], [(kT2[pb0:pb0 + 64, g, kt * 128:(kt + 1) * 128], qT[pb0:pb0 + 64, pr, qsl])],
                                     R=[kT_k[kt]] + qtoks, W=[psc_k[u]])
                                K.op(ACT, lambda e, u=u, pa=pa, kt=kt: e.activation(out=pa[:, kt, :], in_=psc[u][:], func=AF.Exp), R=[psc_k[u]], W=[pa_k])
                            for qt in range(QT):
                                K.mm(oa[:, qt, 0:HD + 1], [(pa[:, kt, qt * 128:(qt + 1) * 128], vaug[:, kt, g, :]) for kt in range(NT)],
                                     R=[pa_k] + v_k, W=[oa_k], inc=(qt == QT - 1))
                            K.op(DVE, lambda e, oa=oa: e.reciprocal(out=rs[:], in_=oa[:, :, HD]), R=[oa_k], W=[rs_k])
                            K.op(DVE, lambda e, oa=oa, hq=hq: e.tensor_tensor(out=otm[:, :, hq * HD:(hq + 1) * HD], in0=oa[:, :, 0:HD],
                                                                               in1=bc(rs[:].unsqueeze(2), [128, QT, HD]), op=ALU.mult),
                                 R=[oa_k, rs_k], W=[otm_k])
                        for qt in range(QT):
                            ti = qb * QT + qt
                            for c in range(4):
                                K.tr(pto[:, c, :], otm[:, qt, c * 128:(c + 1) * 128], identb[:], R=[otm_k, cst], W=[pto_k], inc=(c == 3))
                            K.op(ACT, lambda e, ti=ti: e.activation(out=catT[:, 0:4, ti * 128:(ti + 1) * 128], in_=pto[:], func=AF.Copy),
                                 R=[pto_k], W=cat_k[0:4])
            if b == 0:
                dump("oatt", catT[:, 0, :], [128, S], R=cat_k[0:4])

            ckpt('attcore')
            with K.scope():
                NC = NT
                c_ = DECAY
                wrw = [K.sb([128, KD, 128], BF16, "wrw") for _ in range(1)]
                wrw_k = [Tok() for _ in range(1)]
                wrc = [K.dmac("wrw") for _ in range(1)]
                wri = [0]
                T1 = K.sb([128, S + 2], F32, "T1")
                T2 = K.sb([128, S], F32, "T2")
                T3 = K.sb([128, S], F32, "T3")
                T4 = K.sb([128, S], F32, "T4")
                T_k = [Tok() for _ in range(4)]
                r32 = K.sb([128, S], BF16, "r32")
                k32 = K.sb([128, S], F32, "k32")
                kk32 = K.sb([128, S], F32, "kk32")
                yacc = K.sb([128, S], F32, "yacc")
                bacc = K.sb([128, S], BF16, "bacc")
                r_k_, k_k_, kk_k_, ya_k, ba_k = Tok(), Tok(), Tok(), Tok(), Tok()
                twda = K.sb([128, S], BF16, "twda")
                sg = K.sb([128, S], BF16, "sg")
                vb = K.sb([128, S], BF16, "vb")
                gTb = K.sb([128, S], BF16, "gTb")
                sqb = K.sb([128, S], BF16, "sqb")
                twda_k, sg_k, vb_k, gT_k, sqb_k = Tok(), Tok(), Tok(), Tok(), Tok()
                ART = K.sb([128, 2, NC, 2, 128], BF16, "ART")
                BT = K.sb([128, S], BF16, "BT")
                KT = K.sb([128, S], BF16, "KT")
                ART_k, BT_k, KT_k = Tok(), Tok(), Tok()
                gC = K.sb([128, NC], F32, "gC")
                gC_k = Tok()
                ppj = [K.ps([128, 512], F32, "ppj") for _ in range(2)]
                ppj_k = [Tok(), Tok()]
                pji = [0]
                K.op(DVE, lambda e: e.memset(T1[:, 0:1], 0.0), W=[T_k[0]])
                K.op(DVE, lambda e: e.memset(T1[:, S + 1:S + 2], 0.0), W=[T_k[0]])

                def project_shift(m, dst_fn):
                    wi = 0
                    wri[0] += 1
                    K.dma(POOL, wrc[wi], wrw[wi][:], win_d[:, 768 + m * 128: 768 + (m + 1) * 128].rearrange("(j p) n -> p j n", p=128), W=[wrw_k[wi]])
                    for tb in range(NTB):
                        u = pji[0] % 2
                        pji[0] += 1
                        K.mm(ppj[u][:, 0:TB], [(wrw[wi][:, j, :], hT[:, j, tb * TB:(tb + 1) * TB]) for j in range(KD)],
                             R=[wrw_k[wi]] + hT_k[tb * (TB // 128):(tb + 1) * (TB // 128)], W=[ppj_k[u]])
                        K.op(ACT, lambda e, u=u, tb=tb: e.activation(out=T1[:, 1 + tb * TB:1 + (tb + 1) * TB], in_=ppj[u][:, 0:TB], func=AF.Copy),
                             R=[ppj_k[u]], W=[T_k[0]])
                    K.op(PL, lambda e: e.tensor_tensor(out=T2[:], in0=T1[:, 0:S], in1=T1[:, 2:S + 2], op=ALU.add), R=[T_k[0]], W=[T_k[1]])
                    K.op(DVE, lambda e: e.tensor_scalar(out=T2[:], in0=T2[:], scalar1=hmu[:, m:m + 1], scalar2=None, op0=ALU.mult), R=[T_k[1], mu_k], W=[T_k[1]])
                    K.op(DVE, lambda e: e.scalar_tensor_tensor(out=T3[:], in0=T1[:, 1:S + 1], scalar=omm[:, m:m + 1], in1=T2[:], op0=ALU.mult, op1=ALU.add),
                         R=[T_k[0], T_k[1], mu_k], W=[T_k[2]])
                    if b == 0 and m == 4:
                        dump("T1k", T1[:, 0:S], [128, S], R=[T_k[0]])
                        dump("T2k", T2[:], [128, S], R=[T_k[1]])
                        dump("T3k", T3[:], [128, S], R=[T_k[2]])
                        dump("hmu", hmu[:], [128, 14], R=[mu_k])
                        dump("omm", omm[:], [128, 14], R=[mu_k])
                    dst_fn()

                def d12():
                    K.op(ACT, lambda e: e.activation(out=twda[0:64, :], in_=T3[0:64, :], func=AF.Tanh), R=[T_k[2]], W=[twda_k])
                    K.op(DVE, lambda e: e.tensor_copy(out=twda[64:128, :], in_=T3[64:128, :]), R=[T_k[2]], W=[twda_k])
                project_shift(12, d12)

                def d13():
                    K.op(ACT, lambda e: e.activation(out=sg[:], in_=T3[:], func=AF.Sigmoid), R=[T_k[2]], W=[sg_k])
                project_shift(13, d13)

                ckpt('lora')
                pbd = [K.ps([128, 512], F32, "pbd") for _ in range(2)]
                pbd_k = [Tok(), Tok()]
                bdi = [0]
                nck = [0]
                ptk3 = K.ps([128, 3, 128], BF16, "ptk3")
                ptk3_k = Tok()
                pP = K.ps([128, 2, 256], F32, "pP")
                pP_k = Tok()
                pQ = K.ps([128, 2, 128], F32, "pQ")
                pQ_k = Tok()
                pZY = K.ps([128, 512], F32, "pZY")
                pZ = pZY[:, 0:256].rearrange("p (a v) -> p a v", v=64)
                pZY_k = Tok()
                pZ_k = [pZY_k, pZY_k]
                pY = pZY[:, 256:448]
                pY_k = [pZY_k, pZY_k]
                tok3s = [K.sb([128, 3, 2, 128], BF16, "tok3") for _ in range(2)]
                tok3s_k = [Tok(), Tok()]
                for q_ in range(2):
                    K.op(DVE, lambda e, q_=q_: e.memset(tok3s[q_][:], 0.0), W=[tok3s_k[q_]])
                Ub2 = K.sb([128, 2, 128], BF16, "Ub2")
                Ub2_k = Tok()
                K.op(DVE, lambda e: e.memset(Ub2[:], 0.0), W=[Ub2_k])
                Hbd = K.sb([128, 128], BF16, "Hbd")
                Hbd_k = Tok()
                M1s = [K.sb([128, 2, 256], BF16, "M1") for _ in range(2)]
                M2s = [K.sb([128, 2, 256], BF16, "M2") for _ in range(2)]
                M1s_k, M2s_k = [Tok(), Tok()], [Tok(), Tok()]
                XAs = [[K.sb([128, 2, 2, 128], BF16, "XA") for _ in range(2)] for _ in range(2)]
                XAs_k = [[Tok(), Tok()], [Tok(), Tok()]]
                PAs = [[K.sb([128, 2, 128], BF16, "PA") for _ in range(2)] for _ in range(2)]
                PAs_k = [[Tok(), Tok()], [Tok(), Tok()]]
                Zb = K.sb([128, 2, 64], BF16, "Zb")
                Ub = K.sb([128, 2, 64], BF16, "Ub")
                Zb_k, Ub_k = Tok(), Tok()
                H32 = K.sb([128, 64], F32, "H32")
                Hb = K.sb([128, 64], BF16, "Hb")
                Htmp = K.sb([128, 64], F32, "Htmp")
                H_k, Hb_k, Ht_k = Tok(), Tok(), Tok()
                identb2 = bc(identb[:].unsqueeze(1), [128, 2, 128])
                T4b = T4[:].bitcast(BF16)
                if 2 * S >= 3328:
                    tok3s.append(T4b[:, 0:768].rearrange("p (x h c) -> p x h c", x=3, h=2))
                    M1s.append(T4b[:, 768:1280].rearrange("p (h t) -> p h t", h=2))
                    M2s.append(T4b[:, 1280:1792].rearrange("p (h t) -> p h t", h=2))
                    XAs.append([T4b[:, 1792 + i_ * 512:1792 + (i_ + 1) * 512].rearrange("p (h a t) -> p h a t", h=2, a=2) for i_ in range(2)])
                    PAs.append([T4b[:, 2816 + i_ * 256:2816 + (i_ + 1) * 256].rearrange("p (h t) -> p h t", h=2) for i_ in range(2)])
                    tok3s_k.append(Tok()); M1s_k.append(Tok()); M2s_k.append(Tok())
                    XAs_k.append([Tok(), Tok()]); PAs_k.append([Tok(), Tok()])
                    T3b = T3[:].bitcast(BF16)
                    tok3s.append(T3b[:, 0:768].rearrange("p (x h c) -> p x h c", x=3, h=2))
                    M1s.append(T3b[:, 768:1280].rearrange("p (h t) -> p h t", h=2))
                    M2s.append(T3b[:, 1280:1792].rearrange("p (h t) -> p h t", h=2))
                    XAs.append([T3b[:, 1792 + i_ * 512:1792 + (i_ + 1) * 512].rearrange("p (h a t) -> p h a t", h=2, a=2) for i_ in range(2)])
                    PAs.append([T3b[:, 2816 + i_ * 256:2816 + (i_ + 1) * 256].rearrange("p (h t) -> p h t", h=2) for i_ in range(2)])
                    tok3s_k.append(Tok()); M1s_k.append(Tok()); M2s_k.append(Tok())
                    XAs_k.append([Tok(), Tok()]); PAs_k.append([Tok(), Tok()])
                NSETS = len(tok3s)
                pPs = [pP, ppj[1][:].rearrange("p (a t) -> p a t", a=2), ppj[0][:].rearrange("p (a t) -> p a t", a=2)]
                pP_ks = [pP_k, ppj_k[1], ppj_k[0]]
                pQs = [pQ, pbd[1][:, 0:256].rearrange("p (a t) -> p a t", a=2), pbd[0][:, 0:256].rearrange("p (a t) -> p a t", a=2)]
                pQ_ks = [pQ_k, pbd_k[1], pbd_k[0]]
                NPIPE = 3 if NSETS >= 4 else 2

                def bdsum(src_bf, src_k, consume):
                    for tb in range(NTB):
                        u = bdi[0] % 2
                        bdi[0] += 1
                        K.mm(pbd[u][:, 0:TB], [(bdones[:], src_bf[:, tb * TB:(tb + 1) * TB])], R=[src_k, cst], W=[pbd_k[u]])
                        consume(pbd[u][:, 0:TB], pbd_k[u], tb)

                import os as _os
                for c4 in [int(q) for q in _os.environ.get('C4LIST', '0,1,2,3').split(',')]:
                    K.barrier()
                    csl = slice(c4 * 128, (c4 + 1) * 128)
                    project_shift(c4, lambda: K.op(PL, lambda e: e.tensor_copy(out=r32[:], in_=T3[:]), R=[T_k[2]], W=[r_k_]))
                    project_shift(4 + c4, lambda: K.op(PL, lambda e: e.tensor_copy(out=k32[:], in_=T3[:]), R=[T_k[2]], W=[k_k_]))
                    project_shift(8 + c4, lambda: K.op(ACT, lambda e: e.activation(out=vb[:], in_=T3[:], func=AF.Copy), R=[T_k[2]], W=[vb_k]))
                    ckpt('rw_proj%d' % c4)
                    for tb in range(NTB):
                        u = pji[0] % 2
                        pji[0] += 1
                        K.mm(ppj[u][:, 0:TB], [(gup[:, csl], sg[:, tb * TB:(tb + 1) * TB])], R=[wsm_k, sg_k], W=[ppj_k[u]])
                        K.op(ACT, lambda e, u=u, tb=tb: e.activation(out=gTb[:, tb * TB:(tb + 1) * TB], in_=ppj[u][:, 0:TB], func=AF.Copy), R=[ppj_k[u]], W=[gT_k])
                    K.op(DVE, lambda e: e.tensor_scalar(out=kk32[:], in0=k32[:], scalar1=kkp[:, c4:c4 + 1], scalar2=None, op0=ALU.mult), R=[k_k_, kkp_k], W=[kk_k_])
                    K.op(PL, lambda e: e.tensor_tensor(out=sqb[:], in0=kk32[:], in1=kk32[:], op=ALU.mult), R=[kk_k_], W=[sqb_k])

                    def cons_kk(ps_, pk, tb):
                        tsl = slice(tb * TB, (tb + 1) * TB)
                        K.op(ACT, lambda e: e.activation(out=T4[:, tsl], in_=ps_[:], func=AF.Sqrt, bias=1e-24), R=[pk], W=[T_k[3]])
                        K.op(DVE, lambda e: e.reciprocal(out=T4[:, tsl], in_=T4[:, tsl]), R=[T_k[3]], W=[T_k[3]])
                    bdsum(sqb, sqb_k, cons_kk)
                    K.op(DVE, lambda e: e.tensor_tensor(out=kk32[:], in0=kk32[:], in1=T4[:], op=ALU.mult), R=[kk_k_, T_k[3]], W=[kk_k_])
                    if b == 0 and c4 == 0:
                        dump("kk", kk32[:], [128, S], R=[kk_k_])
                        pass

                    ckpt('rw_kk%d' % c4)
                    for d in range(2):
                        lw = T1[:, 1:S + 1]
                        for tb in range(NTB):
                            tsl = slice(tb * TB, (tb + 1) * TB)
                            u = pji[0] % 2
                            pji[0] += 1
                            K.mm(ppj[u][:, 0:TB], [(wup[0:64, d, csl], twda[0:64, tsl])], R=[wsm_k, twda_k], W=[ppj_k[u]])
                            K.op(ACT, lambda e, u=u, tsl=tsl: e.activation(out=lw[:, tsl], in_=ppj[u][:, 0:TB], func=AF.Sigmoid, bias=w0[:, d, c4:c4 + 1]),
                                 R=[ppj_k[u], w0_k], W=[T_k[0]])
                        for n_ in range(NC):
                            K.op(DVE, lambda e, n_=n_: e.tensor_tensor_scan(out=T2[:, n_ * 128:(n_ + 1) * 128], data0=onesf[:], data1=lw[:, n_ * 128:(n_ + 1) * 128],
                                                                        initial=0.0, op0=ALU.mult, op1=ALU.add),
                                 R=[T_k[0], cst], W=[T_k[1]])
                        cs3 = T2[:].rearrange("p (c t) -> p c t", t=128)
                        K.op(ACT, lambda e: e.activation(out=gC[:], in_=cs3[:, :, 127], func=AF.Exp, scale=-c_), R=[T_k[1]], W=[gC_k])
                        if d == 0:
                            K.op(DVE, lambda e: e.tensor_tensor(out=lw, in0=T2[:], in1=lw, op=ALU.subtract), R=[T_k[0], T_k[1]], W=[T_k[0]])
                            gexc, gexc_k, ginc, ginc_k = lw, T_k[0], T2[:], T_k[1]
                        else:
                            K.op(PL, lambda e: e.tensor_copy(out=T4[:].rearrange("p (c t) -> p c t", t=128), in_=bc(cs3[:, :, 127:128], [128, NC, 128])),
                                 R=[T_k[1]], W=[T_k[3]])
                            K.op(DVE, lambda e: e.tensor_tensor(out=T2[:], in0=T4[:], in1=T2[:], op=ALU.subtract), R=[T_k[1], T_k[3]], W=[T_k[1]])
                            K.op(DVE, lambda e: e.tensor_tensor(out=lw, in0=lw, in1=T2[:], op=ALU.add), R=[T_k[0], T_k[1]], W=[T_k[0]])
                            gexc, gexc_k, ginc, ginc_k = T2[:], T_k[1], lw, T_k[0]
                        A3 = [ART[:, 0, :, 0, :], ART[:, 1, :, 0, :]]
                        R3 = [ART[:, 0, :, 1, :], ART[:, 1, :, 1, :]]
                        v3 = lambda ap: ap.rearrange("p (c t) -> p c t", t=128)
                        K.op(ACT, lambda e: e.activation(out=gexc, in_=gexc, func=AF.Exp, scale=-c_), R=[gexc_k], W=[gexc_k])
                        for hh in range(2):
                            K.op(DVE, lambda e, hh=hh: e.scalar_tensor_tensor(out=A3[hh], in0=v3(kk32[:]), scalar=hmask[:, 2 + hh:3 + hh], in1=v3(gexc), op0=ALU.mult, op1=ALU.mult),
                                 R=[kk_k_, gexc_k, cst], W=[ART_k])
                        K.op(ACT, lambda e: e.activation(out=T3[:], in_=ginc, func=AF.Exp, scale=-c_), R=[ginc_k], W=[T_k[2]])
                        for hh in range(2):
                            K.op(DVE, lambda e, hh=hh: e.scalar_tensor_tensor(out=R3[hh], in0=v3(r32[:]), scalar=hmask[:, hh:hh + 1], in1=v3(T3[:]), op0=ALU.mult, op1=ALU.mult),
                                 R=[r_k_, T_k[2], cst], W=[ART_k])
                        K.op(ACT, lambda e: e.activation(out=ginc, in_=ginc, func=AF.Exp, scale=c_), R=[ginc_k], W=[ginc_k])
                        for tb in range(NTB):
                            tsl = slice(tb * TB, (tb + 1) * TB)
                            u = pji[0] % 2
                            pji[0] += 1
                            K.mm(ppj[u][:, 0:TB], [(aup[64:128, d, csl], twda[64:128, tsl])], R=[wsm_k, twda_k], W=[ppj_k[u]])
                            K.op(ACT, lambda e, u=u, tsl=tsl: e.activation(out=T3[:, tsl], in_=ppj[u][:, 0:TB], func=AF.Sigmoid, bias=a0[:, d, c4:c4 + 1]),
                                 R=[ppj_k[u], a0_k], W=[T_k[2]])
                        Tg = gexc
                        K.op(DVE, lambda e: e.tensor_tensor(out=Tg, in0=kk32[:], in1=T3[:], op=ALU.mult), R=[kk_k_, T_k[2], ART_k], W=[gexc_k])
                        K.op(DVE, lambda e: e.tensor_tensor(out=BT[:], in0=Tg, in1=ginc, op=ALU.mult), R=[gexc_k, ginc_k], W=[BT_k])
                        K.op(DVE, lambda e: e.tensor_scalar(out=Tg, in0=T3[:], scalar1=kap[:, c4:c4 + 1], scalar2=omka[:, c4:c4 + 1], op0=ALU.mult, op1=ALU.add),
                             R=[T_k[2], kap_k, BT_k], W=[gexc_k])
                        K.op(DVE, lambda e: e.tensor_tensor(out=Tg, in0=Tg, in1=k32[:], op=ALU.mult), R=[gexc_k, k_k_], W=[gexc_k])
                        K.op(PL, lambda e: e.tensor_tensor(out=KT[:], in0=Tg, in1=ginc, op=ALU.mult), R=[gexc_k, ginc_k], W=[KT_k])
                        K.op(DVE, lambda e: e.scalar_tensor_tensor(out=sqb[:], in0=r32[:], scalar=rkp[:, c4:c4 + 1], in1=Tg, op0=ALU.mult, op1=ALU.mult),
                             R=[r_k_, gexc_k, rkp_k], W=[sqb_k])

                        def cons_b(ps_, pk, tb, d=d):
                            tsl = slice(tb * TB, (tb + 1) * TB)
                            if d == 0:
                                K.op(DVE, lambda e: e.tensor_tensor(out=bacc[:, tsl], in0=ps_[:], in1=vb[:, tsl], op=ALU.mult), R=[pk, vb_k], W=[ba_k])
                            else:
                                K.op(DVE, lambda e: e.tensor_tensor(out=T3[:, tsl], in0=ps_[:], in1=vb[:, tsl], op=ALU.mult), R=[pk, vb_k], W=[T_k[2]])
                                K.op(PL, lambda e: e.tensor_tensor(out=bacc[:, tsl], in0=bacc[:, tsl], in1=T3[:, tsl], op=ALU.add), R=[T_k[2]], W=[ba_k])
                        bdsum(sqb, sqb_k, cons_b)
                        if b == 0 and c4 == 0:
                            dump(f"AT{d}", ART[:, 0, :, 0, :], [128, NC, 128], R=[ART_k])
                            dump(f"BT{d}", BT[:], [128, S], R=[BT_k])

                        ckpt('rw_prep%d_%d' % (c4, d))
                        K.op(DVE, lambda e: e.memset(H32[:], 0.0), W=[H_k])
                        K.op(DVE, lambda e: e.memset(Hb[:], 0.0), W=[Hb_k])
                        K.op(DVE, lambda e: e.memset(Hbd[:], 0.0), W=[Hbd_k])
                        order = range(NC) if d == 0 else range(NC - 1, -1, -1)
                        hs = [slice(0, 64), slice(64, 128)]

                        def prep(n, q, r, d=d):
                            nsl = slice(n * 128, (n + 1) * 128)
                            tk, tk_k = tok3s[q], tok3s_k[q]
                            m1, m1_k, m2, m2_k = M1s[q], M1s_k[q], M2s[q], M2s_k[q]
                            xa, xa_k, pa, pa_k = XAs[q], XAs_k[q], PAs[q], PAs_k[q]
                            pP, pP_k, pQ, pQ_k = pPs[r], pP_ks[r], pQs[r], pQ_ks[r]
                            K.tr(ptk3[:, 0, :], BT[:, nsl], identb[:], R=[BT_k, cst], W=[ptk3_k], inc=False)
                            K.tr(ptk3[:, 1, :], KT[:, nsl], identb[:], R=[KT_k], W=[ptk3_k], inc=False)
                            K.tr(ptk3[:, 2, :], vb[:, nsl], identb[:], R=[vb_k], W=[ptk3_k])
                            for hh in range(2):
                                K.op(ACT if hh == 0 else DVE, (lambda e, hh=hh: e.activation(out=tk[:, :, hh, hh * 64:(hh + 1) * 64], in_=ptk3[:, :, hh * 64:(hh + 1) * 64], func=AF.Copy)) if hh == 0 else
                                     (lambda e, hh=hh: e.tensor_copy(out=tk[:, :, hh, hh * 64:(hh + 1) * 64], in_=ptk3[:, :, hh * 64:(hh + 1) * 64])), R=[ptk3_k], W=[tk_k])
                            yield
                            for hh in range(2):
                                K.mm(pP[:, hh, :], [(BT[:, nsl], ART[:, hh, n, :, :].rearrange("p a t -> p (a t)"))], R=[BT_k, ART_k], W=[pP_k], inc=(hh == 1))
                            for hh in range(2):
                                K.op(DVE, lambda e, hh=hh: e.tensor_tensor(out=m1[:, hh, :], in0=pP[:, hh, :], in1=MP[d][:], op=ALU.mult), R=[pP_k, cst], W=[m1_k])
                            yield
                            for hh in range(2):
                                K.mm(pP[:, hh, :], [(KT[:, nsl], ART[:, hh, n, :, :].rearrange("p a t -> p (a t)"))], R=[KT_k, ART_k], W=[pP_k], inc=(hh == 1))
                            for hh in range(2):
                                K.op(DVE, lambda e, hh=hh: e.tensor_tensor(out=m2[:, hh, :], in0=pP[:, hh, :], in1=MP[d][:], op=ALU.mult), R=[pP_k, cst], W=[m2_k])
                            yield
                            for hh in range(2):
                                K.mm(pQ[:, hh, :], [(ART[:, hh, n, 0, :], BT[:, nsl])], R=[BT_k, ART_k], W=[pQ_k], inc=(hh == 1))
                            for hh in range(2):
                                K.op(DVE, lambda e, hh=hh: e.tensor_tensor(out=pa[0][:, hh, :], in0=pQ[:, hh, :], in1=ML[d][:], op=ALU.mult), R=[pQ_k, cst], W=[pa_k[0]])
                            yield
                            K.op(PL, lambda e: e.tensor_tensor(out=xa[1][:, :, 1, :], in0=m1[:, :, 0:128], in1=identb2, op=ALU.add), R=[m1_k, cst], W=[xa_k[1]])
                            for hh in range(2):
                                K.mm(pP[:, hh, 0:128], [(pa[0][:, hh, :], m1[:, hh, 0:128])], R=[pa_k[0], m1_k], W=[pP_k], inc=(hh == 1))
                            K.op(ACT, lambda e: e.activation(out=xa[1][:, :, 0, :], in_=pP[:, :, 0:128], func=AF.Copy), R=[pP_k], W=[xa_k[1]])
                            for hh in range(2):
                                K.mm(pQ[:, hh, :], [(m1[:, hh, 0:128], pa[0][:, hh, :])], R=[pa_k[0], m1_k], W=[pQ_k], inc=(hh == 1))
                            K.op(DVE, lambda e: e.tensor_copy(out=pa[1][:], in_=pQ[:]), R=[pQ_k], W=[pa_k[1]])
                            yield
                            cur = 1
                            for lev in range(1, 7):
                                nx = 1 - cur
                                if lev < 6:
                                    for hh in range(2):
                                        K.mm(pP[:, hh, :], [(pa[cur][:, hh, :], xa[cur][:, hh, :, :].rearrange("p a t -> p (a t)"))],
                                             R=[pa_k[cur], xa_k[cur]], W=[pP_k], inc=(hh == 1))
                                    K.op(ACT, lambda e, nx=nx: e.activation(out=xa[nx][:, :, 0, :], in_=pP[:, :, 0:128], func=AF.Copy), R=[pP_k], W=[xa_k[nx]])
                                    K.op(DVE, lambda e, nx=nx, cur=cur: e.tensor_tensor(out=xa[nx][:, :, 1, :], in0=pP[:, :, 128:256], in1=xa[cur][:, :, 1, :], op=ALU.add),
                                         R=[pP_k, xa_k[cur]], W=[xa_k[nx]])
                                    for hh in range(2):
                                        K.mm(pQ[:, hh, :], [(xa[cur][:, hh, 0, :], pa[cur][:, hh, :])], R=[pa_k[cur], xa_k[cur]], W=[pQ_k], inc=(hh == 1))
                                    K.op(DVE, lambda e, nx=nx: e.tensor_copy(out=pa[nx][:], in_=pQ[:]), R=[pQ_k], W=[pa_k[nx]])
                                else:
                                    for hh in range(2):
                                        K.mm(pP[:, hh, 128:256], [(pa[cur][:, hh, :], xa[cur][:, hh, 1, :])], R=[pa_k[cur], xa_k[cur]], W=[pP_k], inc=(hh == 1))
                                    K.op(DVE, lambda e, nx=nx, cur=cur: e.tensor_tensor(out=xa[nx][:, :, 1, :], in0=pP[:, :, 128:256], in1=xa[cur][:, :, 1, :], op=ALU.add),
                                         R=[pP_k, xa_k[cur]], W=[xa_k[nx]])
                                cur = nx
                                yield
                            assert cur == 1

                        def chain(n, q, d=d):
                            nsl = slice(n * 128, (n + 1) * 128)
                            tk, tk_k = tok3s[q], tok3s_k[q]
                            m1, m1_k, m2, m2_k = M1s[q], M1s_k[q], M2s[q], M2s_k[q]
                            Wf, Wf_k = XAs[q][1], XAs_k[q][1]
                            for hh in range(2):
                                K.mm(pZ[:, hh, :], [(ART[:, hh, n, 0, :], Hb[:, :]), (m2[:, hh, 0:128], tk[:, 2, hh, hs[hh]])],
                                     R=[ART_k, Hb_k, m2_k, tk_k], W=[pZ_k[0]])
                            K.op(ACT, lambda e: e.activation(out=Zb[:], in_=pZ[:, 0:2, :], func=AF.Copy), R=[pZ_k[0]], W=[Zb_k])
                            yield
                            for hh in range(2):
                                K.mm(pZ[:, 2 + hh, :], [(Wf[:, hh, 1, :], Zb[:, hh, :])], R=[Wf_k, Zb_k], W=[pZ_k[1]])
                            K.op(ACT, lambda e: e.activation(out=Ub[:], in_=pZ[:, 2:4, :], func=AF.Copy), R=[pZ_k[1]], W=[Ub_k])
                            for hh in range(2):
                                K.op(DVE, lambda e, hh=hh: e.tensor_copy(out=Ub2[:, hh, hh * 64:(hh + 1) * 64], in_=Ub[:, hh, :]), R=[Ub_k], W=[Ub2_k])
                            yield
                            K.mm(pY[:, 128:192], [(tk[:, 0, 0, :], Ub[:, 0, :]), (tk[:, 0, 1, :], Ub[:, 1, :]),
                                                  (tk[:, 1, 0, :], tk[:, 2, 0, 0:64]), (tk[:, 1, 1, :], tk[:, 2, 1, 64:128])],
                                 R=[tk_k, Ub_k], W=[pY_k[1]])
                            K.mm(pY[:, 0:128], [(Hbd[:], ART[:, 0, n, 1, :]), (Hbd[:], ART[:, 1, n, 1, :]),
                                                (Ub2[:, 0, :], m1[:, 0, 128:256]), (Ub2[:, 1, :], m1[:, 1, 128:256]),
                                                (tk[:, 2, 0, :], m2[:, 0, 128:256]), (tk[:, 2, 1, :], m2[:, 1, 128:256])],
                                 R=[Hbd_k, ART_k, Ub2_k, m1_k, m2_k, tk_k], W=[pY_k[0]])
                            K.op(DVE, lambda e: e.tensor_tensor(out=Htmp[:], in0=pY[:, 128:192], in1=H32[:], op=ALU.add), R=[pY_k[1], H_k], W=[Ht_k])
                            if d == 0:
                                K.op(DVE, lambda e, nsl=nsl: e.tensor_copy(out=yacc[:, nsl], in_=pY[:, 0:128]), R=[pY_k[0]], W=[ya_k])
                            else:
                                K.op(DVE, lambda e, nsl=nsl: e.tensor_tensor(out=yacc[:, nsl], in0=pY[:, 0:128], in1=yacc[:, nsl], op=ALU.add), R=[pY_k[0], ya_k], W=[ya_k])
                            yield
                            K.op(DVE, lambda e, n=n: e.tensor_scalar(out=H32[:], in0=Htmp[:], scalar1=gC[:, n:n + 1], scalar2=None, op0=ALU.mult), R=[Ht_k, gC_k], W=[H_k])
                            K.op(ACT, lambda e, n=n: e.activation(out=Hb[:], in_=Htmp[:], func=AF.Copy, scale=gC[:, n:n + 1]), R=[Ht_k, gC_k], W=[Hb_k])
                            for hh in range(2):
                                K.op(PL, lambda e, hh=hh: e.tensor_copy(out=Hbd[hs[hh], hh * 64:(hh + 1) * 64], in_=H32[hs[hh], :]), R=[H_k], W=[Hbd_k])
                            yield

                        order = list(order)
                        K.barrier()
                        for q_ in range(2, NSETS):
                            K.op(DVE, lambda e, q_=q_: e.memset(tok3s[q_], 0.0), W=[tok3s_k[q_]])
                        nch = len(order)
                        act_preps, done_prep = [], set()
                        next_prep, chain_k, completed, chain_gen = 0, 0, 0, None
                        while chain_k < nch:
                            while len(act_preps) < NPIPE and next_prep < nch and next_prep <= completed + NSETS - 1:
                                act_preps.append((next_prep, prep(order[next_prep], next_prep % NSETS, next_prep % NPIPE)))
                                next_prep += 1
                            if chain_gen is None and chain_k in done_prep:
                                chain_gen = chain(order[chain_k], chain_k % NSETS)
                            if chain_gen is not None:
                                try:
                                    next(chain_gen)
                                except StopIteration:
                                    chain_gen = None
                                    completed += 1
                                    chain_k += 1
                                    nck[0] += 1
                            for it_ in list(act_preps):
                                try:
                                    next(it_[1])
                                except StopIteration:
                                    act_preps.remove(it_)
                                    done_prep.add(it_[0])
                        K.barrier()
                    if b == 0 and c4 == 0:
                        dump("yacc", yacc[:], [128, S], R=[ya_k])
                        dump("bacc", bacc[:], [128, S], R=[ba_k])
                    ckpt('rw_loops%d' % c4)
                    K.op(ACT, lambda e: e.activation(out=sqb[:], in_=yacc[:], func=AF.Copy), R=[ya_k], W=[sqb_k])

                    def cons_m(ps_, pk, tb):
                        tsl = slice(tb * TB, (tb + 1) * TB)
                        K.op(DVE, lambda e: e.scalar_tensor_tensor(out=yacc[:, tsl], in0=ps_[:], scalar=-1.0 / 64, in1=yacc[:, tsl], op0=ALU.mult, op1=ALU.add),
                             R=[pk, ya_k], W=[ya_k])
                    bdsum(sqb, sqb_k, cons_m)
                    K.op(PL, lambda e: e.tensor_tensor(out=sqb[:], in0=yacc[:], in1=yacc[:], op=ALU.mult), R=[ya_k], W=[sqb_k])

                    def cons_v(ps_, pk, tb):
                        tsl = slice(tb * TB, (tb + 1) * TB)
                        K.op(ACT, lambda e: e.activation(out=T4[:, tsl], in_=ps_[:], func=AF.Sqrt, scale=1.0 / 64, bias=GN_EPS), R=[pk], W=[T_k[3]])
                        K.op(DVE, lambda e: e.reciprocal(out=T4[:, tsl], in_=T4[:, tsl]), R=[T_k[3]], W=[T_k[3]])
                    bdsum(sqb, sqb_k, cons_v)
                    K.op(DVE, lambda e: e.tensor_tensor(out=yacc[:], in0=yacc[:], in1=T4[:], op=ALU.mult), R=[ya_k, T_k[3]], W=[ya_k])
                    K.op(DVE, lambda e: e.tensor_scalar(out=yacc[:], in0=yacc[:], scalar1=lnw[:, c4:c4 + 1], scalar2=lnb[:, c4:c4 + 1], op0=ALU.mult, op1=ALU.add),
                         R=[ya_k, lnw_k, lnb_k], W=[ya_k])
                    K.op(PL, lambda e: e.tensor_tensor(out=yacc[:], in0=yacc[:], in1=bacc[:], op=ALU.add), R=[ya_k, ba_k], W=[ya_k])
                    K.op(DVE, lambda e: e.tensor_tensor(out=catT[:, 4 + c4, :], in0=yacc[:], in1=gTb[:], op=ALU.mult), R=[ya_k, gT_k], W=[cat_k[4 + c4]])
                    ckpt('rw_gn%d' % c4)
            if b == 0:
                dump("orw", catT[:, 4, :], [128, S], R=cat_k)

        ckpt('rwkv')
        with K.scope():
            x1 = K.sb([128, NT, D], F32, "x1")
            x1_k = [Tok() for _ in range(NT)]
            h2t = K.sb([128, NT, D], BF16, "h2t")
            h2_k = [Tok() for _ in range(NT)]
            afft = K.sb([128, NT, NE], F32, "afft")
            aff_k = [Tok() for _ in range(NT)]
            posm = K.sb([16, S], F32, "posm")
            posm_k = Tok()
            post = K.sb([128, NT, NE], F32, "post")
            post_k = Tok()
            gt2b = K.sb([128, 1, D], F32, "gt2b")
            gt2b_k = Tok()
            bcast_rows(b, gt2b, gt2b_k, [(modT, 40)])
            K.stacks.append(ExitStack())
            affT = K.sb([16, S], F32, "affT")
            affT_k = Tok()
            with K.scope():
                bct = K.sb([128, 3, D], F32, "bct")
                bct_k = Tok()
                bcast_rows(b, bct, bct_k, [(modT, 16), (S2, 0), (modT, 24)])
                wo = K.sb([128, KD, D], BF16, "wo")
                wo_k = Tok()
                K.dma(POOL, K.dmac("wo"), wo[:], wout_d.rearrange("(j p) n -> p j n", p=128), W=[wo_k])
                xc = [K.dmac("x0")]
                xt = [K.sb([128, D], F32, "xt")]
                xt_k = [Tok()]
                po = [K.ps([128, 512], F32, "po") for _ in range(2)]
                po_k = [Tok(), Tok()]
                st_ = [K.sb([128, 4], F32, "st") for _ in range(2)]
                st_k = [Tok(), Tok()]
                pt = [K.ps([128, KD, 128], BF16, "pt") for _ in range(2)]
                pt_k = [Tok(), Tok()]
                h2T = [K.sb([128, KD, 128], BF16, "h2T") for _ in range(2)]
                h2T_k = [Tok(), Tok()]
                plg = K.ps([128, NE], F32, "plg")
                plg_k = Tok()
                lg = K.sb([128, NE], F32, "lg")
                lg_k = Tok()
                paT = K.ps([16, 128], F32, "paT")
                paT_k = Tok()
                for i in range(NT):
                    if i % 4 == 0 and i > 0:
                        K.barrier()
                    u = i % 2
                    sl = slice(i * 128, (i + 1) * 128)
                    K.dma(SP, xc[0], xt[0][:], x_d[tok0 + i * 128: tok0 + (i + 1) * 128, :], W=[xt_k[0]])
                    for half in range(2):
                        hsl = slice(half * 512, (half + 1) * 512)
                        K.mm(po[half][:], [(catT[:, j, sl], wo[:, j, hsl]) for j in range(KD)], R=cat_k + [wo_k], W=[po_k[half]])
                        K.op(DVE, lambda e, half=half, hsl=hsl, i=i: e.tensor_tensor(out=x1[:, i, hsl], in0=po[half][:], in1=bct[:, 0, hsl], op=ALU.mult),
                             R=[po_k[half], bct_k], W=[x1_k[i]])
                    K.op(PL, lambda e, i=i: e.tensor_tensor(out=x1[:, i, :], in0=x1[:, i, :], in1=xt[0][:], op=ALU.add), R=[x1_k[i], xt_k[0]], W=[x1_k[i]])
                    K.op(ACT, lambda e, u=u, i=i: e.activation(out=h2T[u][:].rearrange("p j t -> p (j t)"), in_=x1[:, i, :], func=AF.Square, accum_out=st_[u][:, 0:1]),
                         R=[x1_k[i]], W=[h2T_k[u], st_k[u]])
                    K.op(ACT, lambda e, u=u: e.activation(out=st_[u][:, 1:2], in_=st_[u][:, 0:1], func=AF.Sqrt, scale=1.0 / D, bias=NORM_EPS), R=[st_k[u]], W=[st_k[u]])
                    K.op(DVE, lambda e, u=u: e.reciprocal(out=st_[u][:, 1:2], in_=st_[u][:, 1:2]), R=[st_k[u]], W=[st_k[u]])
                    K.op(DVE, lambda e, u=u, i=i: e.scalar_tensor_tensor(out=h2t[:, i, :], in0=x1[:, i, :], scalar=st_[u][:, 1:2], in1=bct[:, 1, :], op0=ALU.mult, op1=ALU.mult),
                         R=[x1_k[i], st_k[u], bct_k], W=[h2_k[i]])
                    K.op(PL, lambda e, i=i: e.tensor_tensor(out=h2t[:, i, :], in0=h2t[:, i, :], in1=bct[:, 2, :], op=ALU.add), R=[h2_k[i], bct_k], W=[h2_k[i]])
                    for j in range(KD):
                        K.tr(pt[u][:, j, :], h2t[:, i, j * 128:(j + 1) * 128], identb[:], R=[h2_k[i], cst], W=[pt_k[u]], inc=(j == KD - 1))
                    K.op(ACT, lambda e, u=u: e.activation(out=h2T[u][:], in_=pt[u][:], func=AF.Copy), R=[pt_k[u]], W=[h2T_k[u]])
                    K.mm(plg[:], [(h2T[u][:, j, :], wrt[:, j, :]) for j in range(KD)], R=[h2T_k[u], wsm_k], W=[plg_k])
                    K.op(DVE, lambda e, u=u: e.tensor_reduce(out=st_[u][:, 2:3], in_=plg[:], axis=AX.X, op=ALU.max), R=[plg_k], W=[st_k[u]])
                    K.op(DVE, lambda e, u=u: e.tensor_scalar(out=st_[u][:, 2:3], in0=st_[u][:, 2:3], scalar1=-1.0, scalar2=None, op0=ALU.mult), R=[st_k[u]], W=[st_k[u]])
                    K.op(ACT, lambda e, u=u: e.activation(out=lg[:], in_=plg[:], func=AF.Exp, bias=st_[u][:, 2:3], accum_out=st_[u][:, 3:4]),
                         R=[plg_k, st_k[u]], W=[lg_k, st_k[u]])
                    K.op(DVE, lambda e, u=u: e.reciprocal(out=st_[u][:, 3:4], in_=st_[u][:, 3:4]), R=[st_k[u]], W=[st_k[u]])
                    K.op(DVE, lambda e, u=u, i=i: e.tensor_scalar(out=afft[:, i, :], in0=lg[:], scalar1=st_[u][:, 3:4], scalar2=None, op0=ALU.mult),
                         R=[lg_k, st_k[u]], W=[aff_k[i]])
                    K.tr(paT[:], afft[:, i, :], identf[:], R=[aff_k[i], cst], W=[paT_k])
                    K.op(DVE, lambda e, sl=sl: e.tensor_copy(out=affT[:, sl], in_=paT[:]), R=[paT_k], W=[affT_k])
            if b == 0:
                dump("x1", x1[:, 0, :], [128, D], R=x1_k)
                dump("affT", affT[:], [16, S], R=[affT_k])

            ckpt('outproj')
            with K.scope():
                wk = [K.sb([16, S], F32, "wk") for _ in range(2)]
                wk_k = [Tok(), Tok()]
                m8 = K.sb([16, 8], F32, "m8")
                m8_k = Tok()
                mk_ = K.sb([16, S], F32, "mk")
                mk_k = Tok()
                ppo = K.ps([128, NE], F32, "ppo")
                ppo_k = Tok()
                K.op(DVE, lambda e: e.tensor_copy(out=wk[0][:], in_=affT[:]), R=[affT_k], W=[wk_k[0]])
                nit = CAP // 8
                cur = 0
                for it in range(nit):
                    K.op(DVE, lambda e, cur=cur: e.max(out=m8[:], in_=wk[cur][:]), R=[wk_k[cur]], W=[m8_k])
                    if it < nit - 1:
                        K.op(DVE, lambda e, cur=cur: e.match_replace(out=wk[1 - cur][:], in_to_replace=m8[:], in_values=wk[cur][:], imm_value=-1.0),
                             R=[wk_k[cur], m8_k], W=[wk_k[1 - cur]])
                        cur = 1 - cur
                K.op(DVE, lambda e: e.tensor_scalar(out=mk_[:], in0=affT[:], scalar1=m8[:, 7:8], scalar2=None, op0=ALU.is_ge), R=[affT_k, m8_k], W=[mk_k])
                K.op(DVE, lambda e: e.tensor_tensor_scan(out=posm[:], data0=onesf[0:16, 0:1].to_broadcast([16, S]), data1=mk_[:], initial=0.0, op0=ALU.mult, op1=ALU.add),
                     R=[mk_k, cst], W=[posm_k])
                K.op(DVE, lambda e: e.tensor_tensor(out=posm[:], in0=posm[:], in1=mk_[:], op=ALU.mult), R=[posm_k, mk_k], W=[posm_k])
                K.op(DVE, lambda e: e.tensor_scalar(out=posm[:], in0=posm[:], scalar1=-1.0, scalar2=None, op0=ALU.add), R=[posm_k], W=[posm_k])
                for i in range(NT):
                    K.tr(ppo[:], posm[:, i * 128:(i + 1) * 128], identf[0:16, 0:16], R=[posm_k, cst], W=[ppo_k])
                    K.op(DVE, lambda e, i=i: e.tensor_copy(out=post[:, i, :], in_=ppo[:]), R=[ppo_k], W=[post_k])
            if b == 0:
                dump("posm", posm[:], [16, S], R=[posm_k])

            K.barrier()
            K.stacks.pop().close()
            ckpt('topk')
            with K.scope():
                NWS = 6
                if S >= 2048:
                    wsl = [catT[:, :, q * 512:(q + 1) * 512] for q in range(4)]
                    wsl += [K.sb([128, KD, 512], BF16, "wsl") for _ in range(NWS - 4)]
                else:
                    wsl = [K.sb([128, KD, 512], BF16, "wsl") for _ in range(NWS)]
                wsl_k = [Tok() for _ in range(NWS)]
                wsc_ = [K.dmac("wsl") for _ in range(NWS)]
                Sel = K.sb([128, NT, CAP], BF16, "Sel")
                Sel_k = Tok()
                SelT = K.sb([128, NCT, S], BF16, "SelT")
                SelT_k = Tok()
                hgT = K.sb([128, KD, CAP], BF16, "hgT")
                hgT_k = Tok()
                hidT = K.sb([128, KD, CAP], BF16, "hidT")
                hid_k = Tok()
                sgt = K.sb([128, CAP], F32, "sgt")
                sgt_k = Tok()
                ysb = K.sb([128, NCT, D], BF16, "ysb")
                ysb_k = Tok()
                ppb = K.ps([128, TB], F32, "ppb")
                ppb_k = Tok()
                pg = K.ps([128, CAP], F32, "pg")
                pg_k = Tok()
                pu = K.ps([128, CAP], F32, "pu")
                pu_k = Tok()
                ph = [K.ps([128, CAP], F32, "ph") for _ in range(2)]
                ph_k = [Tok(), Tok()]
                py = [K.ps([128, 512], F32, "py") for _ in range(2)]
                py_k = [Tok(), Tok()]
                wcount = [0]
                ohe = K.sb([16, 128], F32, "ohe")
                ohe_k = Tok()

                def wload(src_d, e_, half):
                    s = wcount[0] % NWS
                    wcount[0] += 1
                    K.dma(POOL, wsc_[s], wsl[s][:], src_d[e_, :, half * 512:(half + 1) * 512].rearrange("(j p) n -> p j n", p=128), W=[wsl_k[s]])
                    return s

                for e_ in range(NE):
                    sg0 = wload(wg_d, e_, 0)
                    sg1 = wload(wg_d, e_, 1)
                    su0 = wload(wu_d, e_, 0)
                    su1 = wload(wu_d, e_, 1)
                    for i in range(NT):
                        K.op(DVE, lambda e, i=i, e_=e_: e.tensor_scalar(out=Sel[:, i, :], in0=iotac[:, 0:CAP], scalar1=post[:, i, e_:e_ + 1], scalar2=None, op0=ALU.is_equal),
                             R=[post_k, cst], W=[Sel_k])
                    K.op(DVE, lambda e, e_=e_: e.tensor_copy(out=ohe[:], in_=bc(identf[0:16, e_:e_ + 1], [16, 128])), R=[cst], W=[ohe_k])
                    for tb in range(NTB):
                        tsl = slice(tb * TB, (tb + 1) * TB)
                        K.mm(ppb[:], [(ohe[:], posm[:, tsl])], R=[posm_k, ohe_k], W=[ppb_k])
                        for ct in range(NCT):
                            K.op(DVE, lambda e, ct=ct, tsl=tsl: e.tensor_scalar(out=SelT[:, ct, tsl], in0=ppb[:], scalar1=iotap[:, ct:ct + 1], scalar2=None, op0=ALU.is_equal),
                                 R=[ppb_k, cst], W=[SelT_k])
                    for fc in range(KD):
                        u = fc % 2
                        K.mm(ph[u][:], [(h2t[:, i, fc * 128:(fc + 1) * 128], Sel[:, i, :]) for i in range(NT)], R=h2_k + [Sel_k], W=[ph_k[u]])
                        K.op(ACT if u == 0 else DVE, (lambda e, u=u, fc=fc: e.activation(out=hgT[:, fc, :], in_=ph[u][:], func=AF.Copy)) if u == 0 else
                             (lambda e, u=u, fc=fc: e.tensor_copy(out=hgT[:, fc, :], in_=ph[u][:])), R=[ph_k[u]], W=[hgT_k])
                    for fc in range(KD):
                        gs = sg0 if fc < 4 else sg1
                        us = su0 if fc < 4 else su1
                        fo = (fc % 4) * 128
                        K.mm(pg[:], [(wsl[gs][:, j, fo:fo + 128], hgT[:, j, :]) for j in range(KD)], R=[wsl_k[gs], hgT_k], W=[pg_k])
                        K.mm(pu[:], [(wsl[us][:, j, fo:fo + 128], hgT[:, j, :]) for j in range(KD)], R=[wsl_k[us], hgT_k], W=[pu_k])
                        K.op(ACT, lambda e: e.activation(out=sgt[:], in_=pg[:], func=AF.Silu), R=[pg_k], W=[sgt_k])
                        K.op(DVE, lambda e, fc=fc: e.tensor_tensor(out=hidT[:, fc, :], in0=pu[:], in1=sgt[:], op=ALU.mult), R=[pu_k, sgt_k], W=[hid_k])
                    sd0 = wload(wd_d, e_, 0)
                    sd1 = wload(wd_d, e_, 1)
                    for ct in range(NCT):
                        for half in range(2):
                            ds_ = sd0 if half == 0 else sd1
                            hsl = slice(half * 512, (half + 1) * 512)
                            K.mm(py[half][0:CP, :], [(hidT[:, fc, ct * 128:ct * 128 + CP], wsl[ds_][:, fc, :]) for fc in range(KD)], R=[hid_k, wsl_k[ds_]], W=[py_k[half]])
                            K.op(DVE, lambda e, ct=ct, half=half, hsl=hsl: e.tensor_tensor(out=ysb[0:CP, ct, hsl], in0=py[half][0:CP, :], in1=gt2b[0:CP, 0, hsl], op=ALU.mult),
                                 R=[py_k[half], gt2b_k], W=[ysb_k])
                    for i in range(NT):
                        sl = slice(i * 128, (i + 1) * 128)
                        for half in range(2):
                            hsl = slice(half * 512, (half + 1) * 512)
                            K.mm(py[half][:], [(SelT[0:CP, ct, sl], ysb[0:CP, ct, hsl]) for ct in range(NCT)], R=[SelT_k, ysb_k], W=[py_k[half]])
                            K.op(DVE, lambda e, i=i, half=half, hsl=hsl, e_=e_: e.scalar_tensor_tensor(out=x1[:, i, hsl], in0=py[half][:], scalar=afft[:, i, e_:e_ + 1],
                                                                                                    in1=x1[:, i, hsl], op0=ALU.mult, op1=ALU.add),
                                 R=[py_k[half], aff_k[i], x1_k[i]], W=[x1_k[i]])
                for i in range(NT):
                    K.dma(SP, outc, out_d[tok0 + i * 128: tok0 + (i + 1) * 128, :], x1[:, i, :], R=[x1_k[i]])
    K.barrier()
    K.stacks[0].close()
    return nc, dump_d


def rope_tables(S):
    rows = S // 64
    row = np.repeat(np.arange(rows, dtype=np.float32), 64)
    col = np.tile(np.arange(64, dtype=np.float32), rows)
    freqs = (np.float32(10000.0) ** (-np.arange(16, dtype=np.float32) / np.float32(16))).astype(np.float32)
    ang = np.concatenate([row[:, None] * freqs, col[:, None] * freqs], axis=-1).astype(np.float32)
    return np.cos(ang).astype(np.float32), np.sin(ang).astype(np.float32)


def fm(v, n):
    return np.ascontiguousarray(np.asarray(v, np.float32).reshape(n, 128).T)


def make_in_maps(inputs, S, NSEQ, ncores):
    f = lambda a: np.ascontiguousarray(np.asarray(a, np.float32))
    NT = S // 128
    cos, sin = rope_tables(S)
    cosl = np.ascontiguousarray(cos.reshape(NT, 128, 32).transpose(1, 0, 2))
    sinl = np.ascontiguousarray(sin.reshape(NT, 128, 32).transpose(1, 0, 2))
    x = f(inputs["x"])
    c = f(inputs["c"])
    shared = {
        "w_ada": f(inputs["w_ada"][0]),
        "b_ada": fm(inputs["b_ada"][0], 48),
        "g_mix": fm(inputs["g_mix"][0], 8),
        "g_ffn": fm(inputs["g_ffn"][0], 8),
        "w_in": f(inputs["w_in"][0]),
        "q_norm": f(inputs["q_norm"][0]).reshape(1, 64),
        "k_norm": f(inputs["k_norm"][0]).reshape(1, 64),
        "mu": fm(inputs["mu_shift"][0], 14),
        "w0": np.ascontiguousarray(f(inputs["w0"][0]).reshape(2, 4, 128).transpose(2, 0, 1)),
        "a0": np.ascontiguousarray(f(inputs["a0"][0]).reshape(2, 4, 128).transpose(2, 0, 1)),
        "w_up": f(inputs["w_up"][0]),
        "a_up": f(inputs["a_up"][0]),
        "g_up": f(inputs["g_up"][0]),
        "k_k": fm(inputs["k_k"][0], 4),
        "k_a": fm(inputs["k_a"][0], 4),
        "r_k": fm(f(inputs["r_k"][0]).reshape(-1), 4),
        "ln_w": fm(inputs["ln_w"][0], 4),
        "ln_b": fm(inputs["ln_b"][0], 4),
        "w_out": f(inputs["w_out"][0]),
        "w_router": f(inputs["w_router"][0]),
        "w_gate": f(inputs["w_gate"][0]),
        "w_up_e": f(inputs["w_up_e"][0]),
        "w_down": f(inputs["w_down"][0]),
        "cos": cosl,
        "sin": sinl,
    }
    maps = []
    for i in range(ncores):
        m = dict(shared)
        m["x"] = np.ascontiguousarray(x[i * NSEQ:(i + 1) * NSEQ].reshape(NSEQ * S, D))
        cc = c[i * NSEQ:(i + 1) * NSEQ]
        m["cT"] = np.ascontiguousarray(cc.reshape(NSEQ, KD, 128).transpose(2, 1, 0))
        maps.append(m)
    return maps


def kernel(**inputs):
    x = np.asarray(inputs["x"])
    B, S, _ = x.shape
    ncores = 8
    NSEQ = B // ncores
    nc, _ = build(S=S, NSEQ=NSEQ)
    maps = make_in_maps(inputs, S, NSEQ, ncores)
    res = run_bass_kernel_spmd(nc, maps, core_ids=list(range(ncores)))
    outs = [np.asarray(r["out"]).reshape(NSEQ, S, D) for r in res.results]
    return np.concatenate(outs, axis=0).astype(np.float32)
```

```python
import numpy as np
from contextlib import ExitStack, contextmanager
import concourse.bass as bass
import concourse.mybir as mybir
from concourse.bass_utils import run_bass_kernel_spmd

F32 = mybir.dt.float32
BF16 = mybir.dt.bfloat16
AF = mybir.ActivationFunctionType
ALU = mybir.AluOpType
AX = mybir.AxisListType

D = 1024
KD = 8
HD = 64
NE = 16
DECAY = 0.606531
GN_EPS = 64e-5
NORM_EPS = 1e-6
N_IN = 2560
REBASE_T = 3000
BAR_EVERY = 4


class Tok:
    __slots__ = ("w", "r")

    def __init__(self):
        self.w = None
        self.r = {}


class Cnt:
    def __init__(self, sem, incv, eng=None, name=""):
        self.sem = sem
        self.incv = incv
        self.cnt = 0
        self.eng = eng
        self.seen = {}
        self.name = name
        self.gen = 0


class Ctx:
    def __init__(self, nc):
        self.nc = nc
        self.stacks = [ExitStack()]
        self.uid = 0
        self.allc = []
        mk = self._mkc
        self.PE = mk(nc.tensor, 1, "pe")
        self.ACT = mk(nc.scalar, 1, "act")
        self.DVE = mk(nc.vector, 1, "dve")
        self.POOL = mk(nc.gpsimd, 1, "pool")
        self.SP = Cnt(None, 0, nc.sync, "sp")
        self.dma_free = []
        self.outc = []

    def _mkc(self, eng, incv, name):
        sem = self.stacks[0].enter_context(self.nc.semaphore(f"s_{name}_{self.uid}"))
        self.uid += 1
        c = Cnt(sem, incv, eng, name)
        self.allc.append(c)
        return c

    def dmac(self, name="d"):
        return self._mkc(None, 16, name)

    def nm(self, s):
        self.uid += 1
        return f"{s}_{self.uid}"

    def sb(self, shape, dt, name="t"):
        return self.stacks[-1].enter_context(self.nc.sbuf_tensor(self.nm(name), list(shape), dt))

    def ps(self, shape, dt=F32, name="p"):
        return self.stacks[-1].enter_context(self.nc.psum_tensor(self.nm(name), list(shape), dt))

    @contextmanager
    def scope(self):
        self.stacks.append(ExitStack())
        try:
            yield
        finally:
            self.barrier()
            self.stacks.pop().close()

    def barrier(self):
        for e in (self.PE, self.ACT, self.DVE, self.POOL, self.SP):
            for f in self.allc:
                if f.cnt > 0 and e.seen.get(f, 0) < f.cnt:
                    e.eng.wait_ge(f.sem, f.cnt * f.incv)
                    e.seen[f] = f.cnt
        for f in (self.PE, self.ACT, self.DVE, self.POOL):
            if f.cnt > REBASE_T:
                f.sem = self.stacks[0].enter_context(self.nc.semaphore(f"s_{f.name}_rb{self.uid}"))
                self.uid += 1
                f.cnt = 0
                f.gen += 1
                for e in (self.PE, self.ACT, self.DVE, self.POOL, self.SP):
                    e.seen.pop(f, None)

    def op(self, e, fn, R=(), W=(), comp=None, inc=True):
        comp = comp or e
        need = {}
        for t in R:
            if t.w is not None:
                f, c, g = t.w
                if g == f.gen and c > need.get(f, 0):
                    need[f] = c
        for t in W:
            if t.w is not None:
                f, c, g = t.w
                if g == f.gen and c > need.get(f, 0):
                    need[f] = c
            for f, (c, g) in t.r.items():
                if g == f.gen and c > need.get(f, 0):
                    need[f] = c
        for f, c in need.items():
            if f is e and e is self.PE:
                continue
            if e.seen.get(f, 0) < c:
                e.eng.wait_ge(f.sem, c * f.incv)
                e.seen[f] = c
        ins = fn(e.eng)
        if inc:
            comp.cnt += 1
            ins.then_inc(comp.sem, comp.incv)
            cc = comp.cnt
        else:
            cc = comp.cnt + 1
        for t in R:
            pr = t.r.get(comp)
            if pr is None or pr[1] != comp.gen or pr[0] < cc:
                t.r[comp] = (cc, comp.gen)
        for t in W:
            t.w = (comp, cc, comp.gen)
            t.r = {}
        return ins

    def mm(self, out, pairs, R=(), W=(), start=True, stop=True, inc=True):
        n = len(pairs)
        for i, (l, r) in enumerate(pairs):
            last = i == n - 1
            self.op(self.PE,
                    lambda e, l=l, r=r, i=i, last=last: e.matmul(out, lhsT=l, rhs=r, start=(start and i == 0), stop=(stop and last)),
                    R=R if i == 0 else (), W=W, inc=(inc and last))

    def tr(self, out, in_, ident, R=(), W=(), inc=True):
        self.op(self.PE, lambda e: e.transpose(out, in_, ident), R=R, W=W, inc=inc)

    def dma(self, issuer, comp, out, in_, R=(), W=()):
        self.op(issuer, lambda e: e.dma_start(out=out, in_=in_), R=R, W=W, comp=comp)


def bc(ap, shape):
    return ap.to_broadcast(list(shape))


class _Stop(Exception):
    pass


def build(S=2048, NSEQ=4, dumps=(), stop=None):
    try:
        return _build(S, NSEQ, dumps, stop)
    except _Stop as ex:
        ex.args[1].barrier()
        ex.args[1].stacks[0].close()
        return ex.args[0]


def _build(S, NSEQ, dumps, stop):
    NT = S // 128
    QB = min(512, S)
    NQB = S // QB
    QT = QB // 128
    CAP = 2 * S // NE
    NCT = max(1, CAP // 128)
    CP = min(CAP, 128)
    TB = min(512, S)
    NTB = S // TB
    TOKS = NSEQ * S
    nc = bass.Bass("TRN2", target_bir_lowering=False)
    K = Ctx(nc)

    def din(name, shape):
        return nc.dram_tensor(name, list(shape), F32, kind="ExternalInput").ap()

    x_d = din("x", [TOKS, D])
    cT_d = din("cT", [128, KD, NSEQ])
    wada_d = din("w_ada", [D, 6 * D])
    bada_d = din("b_ada", [128, 48])
    gmix_d = din("g_mix", [128, KD])
    gffn_d = din("g_ffn", [128, KD])
    win_d = din("w_in", [D, N_IN])
    qn_d = din("q_norm", [1, HD])
    kn_d = din("k_norm", [1, HD])
    mu_d = din("mu", [128, 14])
    w0_d = din("w0", [128, 2, 4])
    a0_d = din("a0", [128, 2, 4])
    wup_d = din("w_up", [2, 64, 512])
    aup_d = din("a_up", [2, 64, 512])
    gup_d = din("g_up", [128, 512])
    kk_d = din("k_k", [128, 4])
    ka_d = din("k_a", [128, 4])
    rk_d = din("r_k", [128, 4])
    lnw_d = din("ln_w", [128, 4])
    lnb_d = din("ln_b", [128, 4])
    wout_d = din("w_out", [D, D])
    wr_d = din("w_router", [D, NE])
    wg_d = din("w_gate", [NE, D, D])
    wu_d = din("w_up_e", [NE, D, D])
    wd_d = din("w_down", [NE, D, D])
    cos_d = din("cos", [128, NT, 32])
    sin_d = din("sin", [128, NT, 32])
    out_d = nc.dram_tensor("out", [TOKS, D], F32, kind="ExternalOutput").ap()
    dump_d = {}

    PE, ACT, DVE, POOL, SP = K.PE, K.ACT, K.DVE, K.POOL, K.SP
    import os as _os0
    PL = POOL if _os0.environ.get('POOLC', '1') == '1' else DVE
    outc = K.dmac("outc")

    def ckpt(name):
        if stop == name:
            raise _Stop((nc, dump_d), K)

    def dump(name, src_ap, shape, R=()):
        if name not in dumps:
            return
        dd = nc.dram_tensor("dump_" + name, list(shape), F32, kind="ExternalOutput").ap()
        dump_d[name] = dd
        tmp = K.sb(shape, F32, "dmp")
        tk = Tok()
        K.op(DVE, lambda e: e.tensor_copy(out=tmp[:], in_=src_ap), R=R, W=[tk])
        K.dma(SP, outc, dd, tmp[:], R=[tk])

    cst = Tok()
    identf = K.sb([128, 128], F32, "identf")
    identb = K.sb([128, 128], BF16, "identb")
    onesf = K.sb([128, 128], F32, "onesf")
    bdones = K.sb([128, 128], BF16, "bdones")
    MP = [K.sb([128, 256], BF16, "MP0"), K.sb([128, 256], BF16, "MP1")]
    ML = [K.sb([128, 128], BF16, "ML0"), K.sb([128, 128], BF16, "ML1")]
    iotac = K.sb([128, 256], F32, "iotac")
    iotap = K.sb([128, 2], F32, "iotap")
    hmask = K.sb([128, 4], F32, "hmask")

    K.op(POOL, lambda e: e.memset(onesf[:], 1.0), W=[cst])
    K.stacks.append(ExitStack())
    mUPs = K.sb([128, 128], F32, "mUPs")
    mUPi = K.sb([128, 128], F32, "mUPi")
    mLOs = K.sb([128, 128], F32, "mLOs")
    mLOi = K.sb([128, 128], F32, "mLOi")
    def aff(dst, base, cm, step, cmp):
        K.op(POOL, lambda e: e.affine_select(out=dst[:], in_=onesf[:], pattern=[[step, 128]], compare_op=cmp,
                                             fill=0.0, base=base, channel_multiplier=cm), R=[cst], W=[cst])
    aff(identf, 0, 1, -1, ALU.is_equal)
    aff(mUPs, 0, -1, 1, ALU.is_gt)
    aff(mUPi, 0, -1, 1, ALU.is_ge)
    aff(mLOs, 0, 1, -1, ALU.is_gt)
    aff(mLOi, 0, 1, -1, ALU.is_ge)
    K.op(DVE, lambda e: e.tensor_copy(out=identb[:], in_=identf[:]), R=[cst], W=[cst])
    K.op(DVE, lambda e: e.memset(bdones[:], 0.0), W=[cst])
    K.op(DVE, lambda e: e.memset(bdones[0:64, 0:64], 1.0), W=[cst])
    K.op(DVE, lambda e: e.memset(bdones[64:128, 64:128], 1.0), W=[cst])
    K.op(DVE, lambda e: e.tensor_copy(out=MP[0][:, 0:128], in_=mUPs[:]), R=[cst], W=[cst])
    K.op(DVE, lambda e: e.tensor_copy(out=MP[0][:, 128:256], in_=mUPi[:]), R=[cst], W=[cst])
    K.op(DVE, lambda e: e.tensor_copy(out=MP[1][:, 0:128], in_=mLOs[:]), R=[cst], W=[cst])
    K.op(DVE, lambda e: e.tensor_copy(out=MP[1][:, 128:256], in_=mLOi[:]), R=[cst], W=[cst])
    K.op(DVE, lambda e: e.tensor_copy(out=ML[0][:], in_=mLOs[:]), R=[cst], W=[cst])
    K.op(DVE, lambda e: e.tensor_copy(out=ML[1][:], in_=mUPs[:]), R=[cst], W=[cst])
    K.barrier()
    K.stacks.pop().close()
    K.op(POOL, lambda e: e.iota(iotac[:], pattern=[[1, 256]], base=0, channel_multiplier=0,
                                allow_small_or_imprecise_dtypes=True), W=[cst])
    K.op(POOL, lambda e: e.iota(iotap[:], pattern=[[128, 2]], base=0, channel_multiplier=1,
                                allow_small_or_imprecise_dtypes=True), W=[cst])
    K.op(DVE, lambda e: e.memset(hmask[:], 0.0), W=[cst])
    K.op(DVE, lambda e: e.memset(hmask[0:64, 0:1], 1.0), W=[cst])
    K.op(DVE, lambda e: e.memset(hmask[64:128, 1:2], 1.0), W=[cst])
    K.op(DVE, lambda e: e.memset(hmask[0:64, 2:3], -1.0), W=[cst])
    K.op(DVE, lambda e: e.memset(hmask[64:128, 3:4], -1.0), W=[cst])

    ckpt('consts')
    def ldsmall(dram, shape, name, dt=F32, eng=None):
        t = K.sb(shape, dt, name)
        c = K.dmac(name)
        tk = Tok()
        K.dma(SP if dt == F32 else POOL, c, t[:], dram, W=[tk])
        return t, tk

    cT, cT_k = ldsmall(cT_d, [128, KD, NSEQ], "cT")
    bada, bada_k = ldsmall(bada_d, [128, 48], "bada")
    gmix, gmix_k = ldsmall(gmix_d, [128, KD], "gmix")
    gffn, gffn_k = ldsmall(gffn_d, [128, KD], "gffn")
    mu, mu_k = ldsmall(mu_d, [128, 14], "mu")
    w0, w0_k = ldsmall(w0_d, [128, 2, 4], "w0")
    a0, a0_k = ldsmall(a0_d, [128, 2, 4], "a0")
    kkp, kkp_k = ldsmall(kk_d, [128, 4], "kkp")
    kap, kap_k = ldsmall(ka_d, [128, 4], "kap")
    rkp, rkp_k = ldsmall(rk_d, [128, 4], "rkp")
    lnw, lnw_k = ldsmall(lnw_d, [128, 4], "lnw")
    lnb, lnb_k = ldsmall(lnb_d, [128, 4], "lnb")
    cosT, cos_k = ldsmall(cos_d, [128, NT, 32], "cos")
    sinT, sin_k = ldsmall(sin_d, [128, NT, 32], "sin")
    gain = K.sb([128, 10, HD], F32, "gain")
    gain_k = Tok()
    gc_ = K.dmac("gain")
    K.dma(SP, gc_, gain[:, 0, :], qn_d.partition_broadcast(128), W=[gain_k])
    K.dma(SP, gc_, gain[:, 8, :], kn_d.partition_broadcast(128), W=[gain_k])
    for h in range(1, 8):
        K.op(DVE, lambda e, h=h: e.tensor_copy(out=gain[:, h, :], in_=gain[:, 0, :]), R=[gain_k], W=[gain_k])
    K.op(DVE, lambda e: e.tensor_copy(out=gain[:, 9, :], in_=gain[:, 8, :]), R=[gain_k], W=[gain_k])
    K.op(DVE, lambda e: e.tensor_scalar(out=gain[:, 0:8, :], in0=gain[:, 0:8, :], scalar1=HD ** -0.5, scalar2=None, op0=ALU.mult),
         R=[gain_k], W=[gain_k])
    hmu = K.sb([128, 14], F32, "hmu")
    omm = K.sb([128, 14], F32, "omm")
    omka = K.sb([128, 4], F32, "omka")
    K.op(DVE, lambda e: e.tensor_scalar(out=hmu[:], in0=mu[:], scalar1=0.5, scalar2=None, op0=ALU.mult), R=[mu_k], W=[mu_k])
    K.op(DVE, lambda e: e.tensor_scalar(out=omm[:], in0=mu[:], scalar1=-1.0, scalar2=1.0, op0=ALU.mult, op1=ALU.add), R=[mu_k], W=[mu_k])
    K.op(DVE, lambda e: e.tensor_scalar(out=omka[:], in0=kap[:], scalar1=-1.0, scalar2=1.0, op0=ALU.mult, op1=ALU.add), R=[kap_k], W=[kap_k])
    wup = K.sb([128, 2, 512], BF16, "wup")
    aup = K.sb([128, 2, 512], BF16, "aup")
    gup = K.sb([128, 512], BF16, "gup")
    wrt = K.sb([128, KD, NE], BF16, "wrt")
    wsm_k = Tok()
    wsc = K.dmac("wsm")
    K.dma(POOL, wsc, wup[0:64, :, :], wup_d.rearrange("d k n -> k d n"), W=[wsm_k])
    K.dma(POOL, wsc, aup[64:128, :, :], aup_d.rearrange("d k n -> k d n"), W=[wsm_k])
    K.dma(POOL, wsc, gup[:], gup_d, W=[wsm_k])
    K.dma(POOL, wsc, wrt[:], wr_d.rearrange("(j p) n -> p j n", p=128), W=[wsm_k])

    ckpt('small')
    modT = K.sb([128, 48, NSEQ], F32, "modT")
    mod_k = Tok()
    S1 = K.sb([128, KD, NSEQ], F32, "S1")
    S2 = K.sb([128, KD, NSEQ], F32, "S2")
    with K.scope():
        cond = K.sb([128, KD, NSEQ], F32, "cond")
        cond_k = Tok()
        K.op(ACT, lambda e: e.activation(out=cond[:], in_=cT[:], func=AF.Silu), R=[cT_k], W=[cond_k])
        wa = [K.sb([128, KD, 512], F32, "wa") for _ in range(2)]
        wa_k = [Tok(), Tok()]
        wac = [K.dmac("wa0"), K.dmac("wa1")]
        pm = [K.ps([128, 4, NSEQ], F32, "pm") for _ in range(2)]
        pm_k = [Tok(), Tok()]
        for g in range(12):
            b = g % 2
            K.dma(SP, wac[b], wa[b][:], wada_d[:, g * 512:(g + 1) * 512].rearrange("(j p) n -> p j n", p=128), W=[wa_k[b]])
            for mm_ in range(4):
                K.mm(pm[b][:, mm_, :], [(wa[b][:, j, mm_ * 128:(mm_ + 1) * 128], cond[:, j, :]) for j in range(KD)],
                     R=[wa_k[b], cond_k], W=[pm_k[b]])
            K.op(DVE, lambda e, b=b, g=g: e.tensor_tensor(out=modT[:, g * 4:(g + 1) * 4, :], in0=pm[b][:],
                                                          in1=bc(bada[:, g * 4:(g + 1) * 4].unsqueeze(2), [128, 4, NSEQ]), op=ALU.add),
                 R=[pm_k[b], bada_k], W=[mod_k])
        for (Sx, gv, gk, off) in ((S1, gmix, gmix_k, 8), (S2, gffn, gffn_k, 32)):
            K.op(DVE, lambda e, Sx=Sx, off=off: e.tensor_scalar(out=Sx[:], in0=modT[:, off:off + 8, :], scalar1=1.0, scalar2=None, op0=ALU.add),
                 R=[mod_k], W=[mod_k])
            K.op(DVE, lambda e, Sx=Sx, gv=gv: e.tensor_tensor(out=Sx[:], in0=Sx[:], in1=bc(gv[:].unsqueeze(2), [128, KD, NSEQ]), op=ALU.mult),
                 R=[mod_k, gk], W=[mod_k])
    dump("modT", modT[:].rearrange("p m b -> p (m b)"), [128, 48 * NSEQ], R=[mod_k])

    ckpt('phaseA')
    catT = K.sb([128, KD, S], BF16, "catT")
    cat_k = [Tok() for _ in range(KD)]
    def bcast_rows(b, bct, bct_k, srcs):
        with K.scope():
            dg = [K.sb([128, 128], F32, "dg") for _ in range(2)]
            dg_k = [Tok(), Tok()]
            pb_ = [K.ps([128, 512], F32, "pb") for _ in range(2)]
            pb_k = [Tok(), Tok()]
            i = 0
            for r, (src, off) in enumerate(srcs):
                for half in range(2):
                    pi = (r * 2 + half) % 2
                    for jj in range(4):
                        j = half * 4 + jj
                        di = i % 2
                        i += 1
                        K.op(DVE, lambda e, di=di, src=src, off=off, j=j: e.tensor_scalar(
                            out=dg[di][:], in0=identf[:], scalar1=src[:, off + j, b:b + 1], scalar2=None, op0=ALU.mult),
                            R=[cst, mod_k], W=[dg_k[di]])
                        K.mm(pb_[pi][:, jj * 128:(jj + 1) * 128], [(onesf[:], dg[di][:])], R=[dg_k[di], cst], W=[pb_k[pi]])
                    K.op(ACT, lambda e, pi=pi, r=r, half=half: e.activation(out=bct[:, r, half * 512:(half + 1) * 512], in_=pb_[pi][:], func=AF.Copy),
                         R=[pb_k[pi]], W=[bct_k])

    for b in range(NSEQ):
        tok0 = b * S
        with K.scope():
            hT = K.sb([128, KD, S], BF16, "hT")
            hT_k = [Tok() for _ in range(NT)]
            with K.scope():
                xt = [K.sb([128, D], F32, "xt") for _ in range(2)]
                xt_k = [Tok(), Tok()]
                xc = [K.dmac("x0"), K.dmac("x1")]
                xn = [K.sb([128, D], BF16, "xn") for _ in range(2)]
                xn_k = [Tok(), Tok()]
                junk = K.sb([128, D], BF16, "junk")
                junk_k = Tok()
                st_ = [K.sb([128, 2], F32, "st") for _ in range(2)]
                st_k = [Tok(), Tok()]
                pt = [K.ps([128, KD, 128], BF16, "pt") for _ in range(2)]
                pt_k = [Tok(), Tok()]
                for i in range(NT):
                    if i % 4 == 0 and i > 0:
                        K.barrier()
                    u = i % 2
                    K.dma(SP, xc[u], xt[u][:], x_d[tok0 + i * 128: tok0 + (i + 1) * 128, :], W=[xt_k[u]])
                    K.op(ACT, lambda e, u=u: e.activation(out=junk[:], in_=xt[u][:], func=AF.Square, accum_out=st_[u][:, 0:1]),
                         R=[xt_k[u]], W=[junk_k, st_k[u]])
                    K.op(ACT, lambda e, u=u: e.activation(out=st_[u][:, 1:2], in_=st_[u][:, 0:1], func=AF.Sqrt, scale=1.0 / D, bias=NORM_EPS),
                         R=[st_k[u]], W=[st_k[u]])
                    K.op(DVE, lambda e, u=u: e.reciprocal(out=st_[u][:, 1:2], in_=st_[u][:, 1:2]), R=[st_k[u]], W=[st_k[u]])
                    K.op(DVE, lambda e, u=u: e.tensor_scalar(out=xn[u][:], in0=xt[u][:], scalar1=st_[u][:, 1:2], scalar2=None, op0=ALU.mult),
                         R=[xt_k[u], st_k[u]], W=[xn_k[u]])
                    for j in range(KD):
                        K.tr(pt[u][:, j, :], xn[u][:, j * 128:(j + 1) * 128], identb[:], R=[xn_k[u], cst], W=[pt_k[u]], inc=(j == KD - 1))
                    for j in range(KD):
                        eng = ACT if j % 2 == 0 else DVE
                        if eng is ACT:
                            K.op(ACT, lambda e, j=j, u=u, i=i: e.activation(out=hT[:, j, i * 128:(i + 1) * 128], in_=pt[u][:, j, :], func=AF.Identity,
                                                                             scale=S1[:, j, b:b + 1], bias=modT[:, j, b:b + 1]),
                                 R=[pt_k[u], mod_k], W=[hT_k[i]])
                        else:
                            K.op(DVE, lambda e, j=j, u=u, i=i: e.tensor_scalar(out=hT[:, j, i * 128:(i + 1) * 128], in0=pt[u][:, j, :],
                                                                                scalar1=S1[:, j, b:b + 1], scalar2=modT[:, j, b:b + 1], op0=ALU.mult, op1=ALU.add),
                                 R=[pt_k[u], mod_k], W=[hT_k[i]])
            if b == 0:
                dump("hT", hT[:, 0, :], [128, S], R=hT_k)

            ckpt('B1')
            with K.scope():
                watt = K.sb([128, KD, 768], BF16, "watt")
                watt_k = Tok()
                K.dma(POOL, K.dmac("watt"), watt[:], win_d[:, 0:768].rearrange("(j p) n -> p j n", p=128), W=[watt_k])
                qT = K.sb([128, 4, S], BF16, "qT")
                qT_k = [Tok() for _ in range(NT)]
                kT2 = K.sb([128, 2, S], BF16, "kT2")
                kT_k = [Tok() for _ in range(NT)]
                vaug = K.sb([128, NT, 2, HD + 1], BF16, "vaug")
                v_k = [Tok() for _ in range(NT)]
                K.op(DVE, lambda e: e.memset(vaug[:], 1.0), W=v_k)
                with K.scope():
                    pq = K.ps([128, 512], F32, "pq")
                    pq_k = Tok()
                    pkv = K.ps([128, 256], F32, "pkv")
                    pkv_k = Tok()
                    ptq = K.ps([128, 4, 128], BF16, "ptq")
                    ptq_k = Tok()
                    ptk = K.ps([128, 2, 128], BF16, "ptk")
                    ptk_k = Tok()
                    qk = K.sb([128, 10, HD], F32, "qk")
                    qk_k = Tok()
                    sq = K.sb([128, 10, HD], F32, "sq")
                    sq_k = Tok()
                    ss = K.sb([128, 10], F32, "ss")
                    ss_k = Tok()
                    t1 = K.sb([128, 10, 32], F32, "t1")
                    t2 = K.sb([128, 10, 32], F32, "t2")
                    t3 = K.sb([128, 10, 32], F32, "t3")
                    t4 = K.sb([128, 10, 32], F32, "t4")
                    t_k = [Tok() for _ in range(4)]
                    qr = K.sb([128, 8, HD], BF16, "qr")
                    qr_k = Tok()
                    kr = K.sb([128, 2, 2, HD], BF16, "kr")
                    kr_k = Tok()
                    for i in range(NT):
                        if i % 4 == 0 and i > 0:
                            K.barrier()
                        sl = slice(i * 128, (i + 1) * 128)
                        K.mm(pq[:], [(hT[:, j, sl], watt[:, j, 0:512]) for j in range(KD)], R=[hT_k[i], watt_k], W=[pq_k])
                        K.mm(pkv[:], [(hT[:, j, sl], watt[:, j, 512:768]) for j in range(KD)], R=[hT_k[i], watt_k], W=[pkv_k])
                        K.op(ACT, lambda e: e.activation(out=qk[:, 0:8, :].rearrange("p h d -> p (h d)"), in_=pq[:], func=AF.Copy), R=[pq_k], W=[qk_k])
                        K.op(ACT, lambda e: e.activation(out=qk[:, 8:10, :].rearrange("p h d -> p (h d)"), in_=pkv[:, 0:128], func=AF.Copy), R=[pkv_k], W=[qk_k])
                        K.op(ACT, lambda e, i=i: e.activation(out=vaug[:, i, :, 0:HD], in_=pkv[:, 128:256].rearrange("p (g d) -> p g d", g=2), func=AF.Copy),
                             R=[pkv_k], W=[v_k[i]])
                        K.op(DVE, lambda e: e.tensor_tensor(out=sq[:], in0=qk[:], in1=qk[:], op=ALU.mult), R=[qk_k], W=[sq_k])
                        K.op(DVE, lambda e: e.tensor_reduce(out=ss[:], in_=sq[:], axis=AX.X, op=ALU.add), R=[sq_k], W=[ss_k])
                        K.op(ACT, lambda e: e.activation(out=ss[:], in_=ss[:], func=AF.Sqrt, scale=1.0 / HD, bias=NORM_EPS), R=[ss_k], W=[ss_k])
                        K.op(DVE, lambda e: e.reciprocal(out=ss[:], in_=ss[:]), R=[ss_k], W=[ss_k])
                        K.op(DVE, lambda e: e.tensor_tensor(out=qk[:], in0=qk[:], in1=bc(ss[:].unsqueeze(2), [128, 10, HD]), op=ALU.mult),
                             R=[qk_k, ss_k], W=[qk_k])
                        K.op(DVE, lambda e: e.tensor_tensor(out=qk[:], in0=qk[:], in1=gain[:], op=ALU.mult), R=[qk_k, gain_k], W=[qk_k])
                        qv = qk[:].rearrange("p h (k two) -> p h k two", two=2)
                        x0, x1 = qv[:, :, :, 0], qv[:, :, :, 1]
                        cb = bc(cosT[:, i, :].unsqueeze(1), [128, 10, 32])
                        sb_ = bc(sinT[:, i, :].unsqueeze(1), [128, 10, 32])
                        K.op(DVE, lambda e: e.tensor_tensor(out=t1[:], in0=x0, in1=cb, op=ALU.mult), R=[qk_k, cos_k], W=[t_k[0]])
                        K.op(PL, lambda e: e.tensor_tensor(out=t2[:], in0=x1, in1=sb_, op=ALU.mult), R=[qk_k, sin_k], W=[t_k[1]])
                        K.op(DVE, lambda e: e.tensor_tensor(out=t3[:], in0=x0, in1=sb_, op=ALU.mult), R=[qk_k, sin_k], W=[t_k[2]])
                        K.op(PL, lambda e: e.tensor_tensor(out=t4[:], in0=x1, in1=cb, op=ALU.mult), R=[qk_k, cos_k], W=[t_k[3]])
                        qrv = qr[:].rearrange("p h (k two) -> p h k two", two=2)
                        krv = kr[:].rearrange("p g u (k two) -> p g u k two", two=2)
                        K.op(DVE, lambda e: e.tensor_tensor(out=qrv[:, :, :, 0], in0=t1[:, 0:8, :], in1=t2[:, 0:8, :], op=ALU.subtract),
                             R=[t_k[0], t_k[1]], W=[qr_k])
                        K.op(DVE, lambda e: e.tensor_tensor(out=qrv[:, :, :, 1], in0=t3[:, 0:8, :], in1=t4[:, 0:8, :], op=ALU.add),
                             R=[t_k[2], t_k[3]], W=[qr_k])
                        for u_ in range(2):
                            K.op(PL, lambda e, u_=u_: e.tensor_tensor(out=krv[:, :, u_, :, 0], in0=t1[:, 8:10, :], in1=t2[:, 8:10, :], op=ALU.subtract),
                                 R=[t_k[0], t_k[1]], W=[kr_k])
                            K.op(PL, lambda e, u_=u_: e.tensor_tensor(out=krv[:, :, u_, :, 1], in0=t3[:, 8:10, :], in1=t4[:, 8:10, :], op=ALU.add),
                                 R=[t_k[2], t_k[3]], W=[kr_k])
                        for pr in range(4):
                            K.tr(ptq[:, pr, :], qr[:, 2 * pr:2 * pr + 2, :].rearrange("p h d -> p (h d)"), identb[:], R=[qr_k, cst], W=[ptq_k], inc=(pr == 3))
                        K.op(ACT, lambda e, sl=sl: e.activation(out=qT[:, :, sl], in_=ptq[:], func=AF.Copy), R=[ptq_k], W=[qT_k[i]])
                        for g in range(2):
                            K.tr(ptk[:, g, :], kr[:, g, :, :].rearrange("p u d -> p (u d)"), identb[:], R=[kr_k, cst], W=[ptk_k], inc=(g == 1))
                        K.op(DVE, lambda e, sl=sl: e.tensor_copy(out=kT2[:, :, sl], in_=ptk[:]), R=[ptk_k], W=[kT_k[i]])
                if b == 0:
                    dump("qT", qT[:, 0, :], [128, S], R=qT_k)
                    dump("kT", kT2[:, 0, :], [128, S], R=kT_k)
                ckpt('attproj')
                with K.scope():
                    NPS = 2
                    psc = [K.ps([128, QB], F32, "psc") for _ in range(NPS)]
                    psc_k = [Tok() for _ in range(NPS)]
                    pTa = [K.sb([128, NT, QB], BF16, "pTa") for _ in range(2)]
                    pTa_k = [Tok(), Tok()]
                    oacc = [K.ps([128, QT, 128], F32, "oacc") for _ in range(2)]
                    oacc_k = [Tok(), Tok()]
                    rs = K.sb([128, QT], F32, "rs")
                    rs_k = Tok()
                    otm = K.sb([128, QT, 512], BF16, "otm")
                    otm_k = Tok()
                    pto = K.ps([128, 4, 128], BF16, "pto")
                    pto_k = Tok()
                    it = 0
                    for qb in range(NQB):
                        qsl = slice(qb * QB, (qb + 1) * QB)
                        qtoks = qT_k[qb * QT:(qb + 1) * QT]
                        for hq in range(8):
                            g = hq // 4
                            pb0 = 64 * (hq % 2)
                            pr = hq // 2
                            oa = oacc[hq % 2]
                            oa_k = oacc_k[hq % 2]
                            pa = pTa[hq % 2]
                            pa_k = pTa_k[hq % 2]
                            for kt in range(NT):
                                u = it % NPS
                                it += 1
                                K.mm(psc[u][:], [(kT2[pb0:pb0 + 64, g, kt * 128:(kt + 1) * 128], qT[pb0:pb0 + 64, pr, qsl])],
                                     R=[kT_k[kt]] + qtoks, W=[psc_k[u]])
                                K.op(ACT, lambda e, u=u, pa=pa, kt=kt: e.activation(out=pa[:, kt, :], in_=psc[u][:], func=AF.Exp), R=[psc_k[u]], W=[pa_k])
                            for qt in range(QT):
                                K.mm(oa[:, qt, 0:HD + 1], [(pa[:, kt, qt * 128:(qt + 1) * 128], vaug[:, kt, g, :]) for kt in range(NT)],
                                     R=[pa_k] + v_k, W=[oa_k], inc=(qt == QT - 1))
                            K.op(DVE, lambda e, oa=oa: e.reciprocal(out=rs[:], in_=oa[:, :, HD]), R=[oa_k], W=[rs_k])
                            K.op(DVE, lambda e, oa=oa, hq=hq: e.tensor_tensor(out=otm[:, :, hq * HD:(hq + 1) * HD], in0=oa[:, :, 0:HD],
                                                                               in1=bc(rs[:].unsqueeze(2), [128, QT, HD]), op=ALU.mult),
                                 R=[oa_k, rs_k], W=[otm_k])
                        for qt in range(QT):
                            ti = qb * QT + qt
                            for c in range(4):
                                K.tr(pto[:, c, :], otm[:, qt, c * 128:(c + 1) * 128], identb[:], R=[otm_k, cst], W=[pto_k], inc=(c == 3))
                            K.op(ACT, lambda e, ti=ti: e.activation(out=catT[:, 0:4, ti * 128:(ti + 1) * 128], in_=pto[:], func=AF.Copy),
                                 R=[pto_k], W=cat_k[0:4])
            if b == 0:
                dump("oatt", catT[:, 0, :], [128, S], R=cat_k[0:4])

            ckpt('attcore')
            with K.scope():
                NC = NT
                c_ = DECAY
                wrw = [K.sb([128, KD, 128], BF16, "wrw") for _ in range(1)]
                wrw_k = [Tok() for _ in range(1)]
                wrc = [K.dmac("wrw") for _ in range(1)]
                wri = [0]
                T1 = K.sb([128, S + 2], F32, "T1")
                T2 = K.sb([128, S], F32, "T2")
                T3 = K.sb([128, S], F32, "T3")
                T4 = K.sb([128, S], F32, "T4")
                T_k = [Tok() for _ in range(4)]
                r32 = K.sb([128, S], BF16, "r32")
                k32 = K.sb([128, S], F32, "k32")
                kk32 = K.sb([128, S], F32, "kk32")
                yacc = K.sb([128, S], F32, "yacc")
                bacc = K.sb([128, S], BF16, "bacc")
                r_k_, k_k_, kk_k_, ya_k, ba_k = Tok(), Tok(), Tok(), Tok(), Tok()
                twda = K.sb([128, S], BF16, "twda")
                sg = K.sb([128, S], BF16, "sg")
                vb = K.sb([128, S], BF16, "vb")
                gTb = K.sb([128, S], BF16, "gTb")
                sqb = K.sb([128, S], BF16, "sqb")
                twda_k, sg_k, vb_k, gT_k, sqb_k = Tok(), Tok(), Tok(), Tok(), Tok()
                ART = K.sb([128, 2, NC, 2, 128], BF16, "ART")
                BT = K.sb([128, S], BF16, "BT")
                KT = K.sb([128, S], BF16, "KT")
                ART_k, BT_k, KT_k = Tok(), Tok(), Tok()
                gC = K.sb([128, NC], F32, "gC")
                gC_k = Tok()
                ppj = [K.ps([128, 512], F32, "ppj") for _ in range(2)]
                ppj_k = [Tok(), Tok()]
                pji = [0]
                K.op(DVE, lambda e: e.memset(T1[:, 0:1], 0.0), W=[T_k[0]])
                K.op(DVE, lambda e: e.memset(T1[:, S + 1:S + 2], 0.0), W=[T_k[0]])

                def project_shift(m, dst_fn):
                    wi = 0
                    wri[0] += 1
                    K.dma(POOL, wrc[wi], wrw[wi][:], win_d[:, 768 + m * 128: 768 + (m + 1) * 128].rearrange("(j p) n -> p j n", p=128), W=[wrw_k[wi]])
                    for tb in range(NTB):
                        u = pji[0] % 2
                        pji[0] += 1
                        K.mm(ppj[u][:, 0:TB], [(wrw[wi][:, j, :], hT[:, j, tb * TB:(tb + 1) * TB]) for j in range(KD)],
                             R=[wrw_k[wi]] + hT_k[tb * (TB // 128):(tb + 1) * (TB // 128)], W=[ppj_k[u]])
                        K.op(ACT, lambda e, u=u, tb=tb: e.activation(out=T1[:, 1 + tb * TB:1 + (tb + 1) * TB], in_=ppj[u][:, 0:TB], func=AF.Copy),
                             R=[ppj_k[u]], W=[T_k[0]])
                    K.op(PL, lambda e: e.tensor_tensor(out=T2[:], in0=T1[:, 0:S], in1=T1[:, 2:S + 2], op=ALU.add), R=[T_k[0]], W=[T_k[1]])
                    K.op(DVE, lambda e: e.tensor_scalar(out=T2[:], in0=T2[:], scalar1=hmu[:, m:m + 1], scalar2=None, op0=ALU.mult), R=[T_k[1], mu_k], W=[T_k[1]])
                    K.op(DVE, lambda e: e.scalar_tensor_tensor(out=T3[:], in0=T1[:, 1:S + 1], scalar=omm[:, m:m + 1], in1=T2[:], op0=ALU.mult, op1=ALU.add),
                         R=[T_k[0], T_k[1], mu_k], W=[T_k[2]])
                    if b == 0 and m == 4:
                        dump("T1k", T1[:, 0:S], [128, S], R=[T_k[0]])
                        dump("T2k", T2[:], [128, S], R=[T_k[1]])
                        dump("T3k", T3[:], [128, S], R=[T_k[2]])
                        dump("hmu", hmu[:], [128, 14], R=[mu_k])
                        dump("omm", omm[:], [128, 14], R=[mu_k])
                    dst_fn()

                def d12():
                    K.op(ACT, lambda e: e.activation(out=twda[0:64, :], in_=T3[0:64, :], func=AF.Tanh), R=[T_k[2]], W=[twda_k])
                    K.op(DVE, lambda e: e.tensor_copy(out=twda[64:128, :], in_=T3[64:128, :]), R=[T_k[2]], W=[twda_k])
                project_shift(12, d12)

                def d13():
                    K.op(ACT, lambda e: e.activation(out=sg[:], in_=T3[:], func=AF.Sigmoid), R=[T_k[2]], W=[sg_k])
                project_shift(13, d13)

                ckpt('lora')
                pbd = [K.ps([128, 512], F32, "pbd") for _ in range(2)]
                pbd_k = [Tok(), Tok()]
                bdi = [0]
                nck = [0]
                ptk3 = K.ps([128, 3, 128], BF16, "ptk3")
                ptk3_k = Tok()
                pP = K.ps([128, 2, 256], F32, "pP")
                pP_k = Tok()
                pQ = K.ps([128, 2, 128], F32, "pQ")
                pQ_k = Tok()
                pZY = K.ps([128, 512], F32, "pZY")
                pZ = pZY[:, 0:256].rearrange("p (a v) -> p a v", v=64)
                pZY_k = Tok()
                pZ_k = [pZY_k, pZY_k]
                pY = pZY[:, 256:448]
                pY_k = [pZY_k, pZY_k]
                tok3s = [K.sb([128, 3, 2, 128], BF16, "tok3") for _ in range(2)]
                tok3s_k = [Tok(), Tok()]
                for q_ in range(2):
                    K.op(DVE, lambda e, q_=q_: e.memset(tok3s[q_][:], 0.0), W=[tok3s_k[q_]])
                Ub2 = K.sb([128, 2, 128], BF16, "Ub2")
                Ub2_k = Tok()
                K.op(DVE, lambda e: e.memset(Ub2[:], 0.0), W=[Ub2_k])
                Hbd = K.sb([128, 128], BF16, "Hbd")
                Hbd_k = Tok()
                M1s = [K.sb([128, 2, 256], BF16, "M1") for _ in range(2)]
                M2s = [K.sb([128, 2, 256], BF16, "M2") for _ in range(2)]
                M1s_k, M2s_k = [Tok(), Tok()], [Tok(), Tok()]
                XAs = [[K.sb([128, 2, 2, 128], BF16, "XA") for _ in range(2)] for _ in range(2)]
                XAs_k = [[Tok(), Tok()], [Tok(), Tok()]]
                PAs = [[K.sb([128, 2, 128], BF16, "PA") for _ in range(2)] for _ in range(2)]
                PAs_k = [[Tok(), Tok()], [Tok(), Tok()]]
                Zb = K.sb([128, 2, 64], BF16, "Zb")
                Ub = K.sb([128, 2, 64], BF16, "Ub")
                Zb_k, Ub_k = Tok(), Tok()
                H32 = K.sb([128, 64], F32, "H32")
                Hb = K.sb([128, 64], BF16, "Hb")
                Htmp = K.sb([128, 64], F32, "Htmp")
                H_k, Hb_k, Ht_k = Tok(), Tok(), Tok()
                identb2 = bc(identb[:].unsqueeze(1), [128, 2, 128])
                T4b = T4[:].bitcast(BF16)
                if 2 * S >= 3328:
                    tok3s.append(T4b[:, 0:768].rearrange("p (x h c) -> p x h c", x=3, h=2))
                    M1s.append(T4b[:, 768:1280].rearrange("p (h t) -> p h t", h=2))
                    M2s.append(T4b[:, 1280:1792].rearrange("p (h t) -> p h t", h=2))
                    XAs.append([T4b[:, 1792 + i_ * 512:1792 + (i_ + 1) * 512].rearrange("p (h a t) -> p h a t", h=2, a=2) for i_ in range(2)])
                    PAs.append([T4b[:, 2816 + i_ * 256:2816 + (i_ + 1) * 256].rearrange("p (h t) -> p h t", h=2) for i_ in range(2)])
                    tok3s_k.append(Tok()); M1s_k.append(Tok()); M2s_k.append(Tok())
                    XAs_k.append([Tok(), Tok()]); PAs_k.append([Tok(), Tok()])
                    T3b = T3[:].bitcast(BF16)
                    tok3s.append(T3b[:, 0:768].rearrange("p (x h c) -> p x h c", x=3, h=2))
                    M1s.append(T3b[:, 768:1280].rearrange("p (h t) -> p h t", h=2))
                    M2s.append(T3b[:, 1280:1792].rearrange("p (h t) -> p h t", h=2))
                    XAs.append([T3b[:, 1792 + i_ * 512:1792 + (i_ + 1) * 512].rearrange("p (h a t) -> p h a t", h=2, a=2) for i_ in range(2)])
                    PAs.append([T3b[:, 2816 + i_ * 256:2816 + (i_ + 1) * 256].rearrange("p (h t) -> p h t", h=2) for i_ in range(2)])
                    tok3s_k.append(Tok()); M1s_k.append(Tok()); M2s_k.append(Tok())
                    XAs_k.append([Tok(), Tok()]); PAs_k.append([Tok(), Tok()])
                NSETS = len(tok3s)
                pPs = [pP, ppj[1][:].rearrange("p (a t) -> p a t", a=2), ppj[0][:].rearrange("p (a t) -> p a t", a=2)]
                pP_ks = [pP_k, ppj_k[1], ppj_k[0]]
                pQs = [pQ, pbd[1][:, 0:256].rearrange("p (a t) -> p a t", a=2), pbd[0][:, 0:256].rearrange("p (a t) -> p a t", a=2)]
                pQ_ks = [pQ_k, pbd_k[1], pbd_k[0]]
                NPIPE = 3 if NSETS >= 4 else 2

                def bdsum(src_bf, src_k, consume):
                    for tb in range(NTB):
                        u = bdi[0] % 2
                        bdi[0] += 1
                        K.mm(pbd[u][:, 0:TB], [(bdones[:], src_bf[:, tb * TB:(tb + 1) * TB])], R=[src_k, cst], W=[pbd_k[u]])
                        consume(pbd[u][:, 0:TB], pbd_k[u], tb)

                import os as _os
                for c4 in [int(q) for q in _os.environ.get('C4LIST', '0,1,2,3').split(',')]:
                    K.barrier()
                    csl = slice(c4 * 128, (c4 + 1) * 128)
                    project_shift(c4, lambda: K.op(PL, lambda e: e.tensor_copy(out=r32[:], in_=T3[:]), R=[T_k[2]], W=[r_k_]))
                    project_shift(4 + c4, lambda: K.op(PL, lambda e: e.tensor_copy(out=k32[:], in_=T3[:]), R=[T_k[2]], W=[k_k_]))
                    project_shift(8 + c4, lambda: K.op(ACT, lambda e: e.activation(out=vb[:], in_=T3[:], func=AF.Copy), R=[T_k[2]], W=[vb_k]))
                    ckpt('rw_proj%d' % c4)
                    for tb in range(NTB):
                        u = pji[0] % 2
                        pji[0] += 1
                        K.mm(ppj[u][:, 0:TB], [(gup[:, csl], sg[:, tb * TB:(tb + 1) * TB])], R=[wsm_k, sg_k], W=[ppj_k[u]])
                        K.op(ACT, lambda e, u=u, tb=tb: e.activation(out=gTb[:, tb * TB:(tb + 1) * TB], in_=ppj[u][:, 0:TB], func=AF.Copy), R=[ppj_k[u]], W=[gT_k])
                    K.op(DVE, lambda e: e.tensor_scalar(out=kk32[:], in0=k32[:], scalar1=kkp[:, c4:c4 + 1], scalar2=None, op0=ALU.mult), R=[k_k_, kkp_k], W=[kk_k_])
                    K.op(PL, lambda e: e.tensor_tensor(out=sqb[:], in0=kk32[:], in1=kk32[:], op=ALU.mult), R=[kk_k_], W=[sqb_k])

                    def cons_kk(ps_, pk, tb):
                        tsl = slice(tb * TB, (tb + 1) * TB)
                        K.op(ACT, lambda e: e.activation(out=T4[:, tsl], in_=ps_[:], func=AF.Sqrt, bias=1e-24), R=[pk], W=[T_k[3]])
                        K.op(DVE, lambda e: e.reciprocal(out=T4[:, tsl], in_=T4[:, tsl]), R=[T_k[3]], W=[T_k[3]])
                    bdsum(sqb, sqb_k, cons_kk)
                    K.op(DVE, lambda e: e.tensor_tensor(out=kk32[:], in0=kk32[:], in1=T4[:], op=ALU.mult), R=[kk_k_, T_k[3]], W=[kk_k_])
                    if b == 0 and c4 == 0:
                        dump("kk", kk32[:], [128, S], R=[kk_k_])
                        pass

                    ckpt('rw_kk%d' % c4)
                    for d in range(2):
                        lw = T1[:, 1:S + 1]
                        for tb in range(NTB):
                            tsl = slice(tb * TB, (tb + 1) * TB)
                            u = pji[0] % 2
                            pji[0] += 1
                            K.mm(ppj[u][:, 0:TB], [(wup[0:64, d, csl], twda[0:64, tsl])], R=[wsm_k, twda_k], W=[ppj_k[u]])
                            K.op(ACT, lambda e, u=u, tsl=tsl: e.activation(out=lw[:, tsl], in_=ppj[u][:, 0:TB], func=AF.Sigmoid, bias=w0[:, d, c4:c4 + 1]),
                                 R=[ppj_k[u], w0_k], W=[T_k[0]])
                        for n_ in range(NC):
                            K.op(DVE, lambda e, n_=n_: e.tensor_tensor_scan(out=T2[:, n_ * 128:(n_ + 1) * 128], data0=onesf[:], data1=lw[:, n_ * 128:(n_ + 1) * 128],
                                                                        initial=0.0, op0=ALU.mult, op1=ALU.add),
                                 R=[T_k[0], cst], W=[T_k[1]])
                        cs3 = T2[:].rearrange("p (c t) -> p c t", t=128)
                        K.op(ACT, lambda e: e.activation(out=gC[:], in_=cs3[:, :, 127], func=AF.Exp, scale=-c_), R=[T_k[1]], W=[gC_k])
                        if d == 0:
                            K.op(DVE, lambda e: e.tensor_tensor(out=lw, in0=T2[:], in1=lw, op=ALU.subtract), R=[T_k[0], T_k[1]], W=[T_k[0]])
                            gexc, gexc_k, ginc, ginc_k = lw, T_k[0], T2[:], T_k[1]
                        else:
                            K.op(PL, lambda e: e.tensor_copy(out=T4[:].rearrange("p (c t) -> p c t", t=128), in_=bc(cs3[:, :, 127:128], [128, NC, 128])),
                                 R=[T_k[1]], W=[T_k[3]])
                            K.op(DVE, lambda e: e.tensor_tensor(out=T2[:], in0=T4[:], in1=T2[:], op=ALU.subtract), R=[T_k[1], T_k[3]], W=[T_k[1]])
                            K.op(DVE, lambda e: e.tensor_tensor(out=lw, in0=lw, in1=T2[:], op=ALU.add), R=[T_k[0], T_k[1]], W=[T_k[0]])
                            gexc, gexc_k, ginc, ginc_k = T2[:], T_k[1], lw, T_k[0]
                        A3 = [ART[:, 0, :, 0, :], ART[:, 1, :, 0, :]]
                        R3 = [ART[:, 0, :, 1, :], ART[:, 1, :, 1, :]]
                        v3 = lambda ap: ap.rearrange("p (c t) -> p c t", t=128)
                        K.op(ACT, lambda e: e.activation(out=gexc, in_=gexc, func=AF.Exp, scale=-c_), R=[gexc_k], W=[gexc_k])
                        for hh in range(2):
                            K.op(DVE, lambda e, hh=hh: e.scalar_tensor_tensor(out=A3[hh], in0=v3(kk32[:]), scalar=hmask[:, 2 + hh:3 + hh], in1=v3(gexc), op0=ALU.mult, op1=ALU.mult),
                                 R=[kk_k_, gexc_k, cst], W=[ART_k])
                        K.op(ACT, lambda e: e.activation(out=T3[:], in_=ginc, func=AF.Exp, scale=-c_), R=[ginc_k], W=[T_k[2]])
                        for hh in range(2):
                            K.op(DVE, lambda e, hh=hh: e.scalar_tensor_tensor(out=R3[hh], in0=v3(r32[:]), scalar=hmask[:, hh:hh + 1], in1=v3(T3[:]), op0=ALU.mult, op1=ALU.mult),
                                 R=[r_k_, T_k[2], cst], W=[ART_k])
                        K.op(ACT, lambda e: e.activation(out=ginc, in_=ginc, func=AF.Exp, scale=c_), R=[ginc_k], W=[ginc_k])
                        for tb in range(NTB):
                            tsl = slice(tb * TB, (tb + 1) * TB)
                            u = pji[0] % 2
                            pji[0] += 1
                            K.mm(ppj[u][:, 0:TB], [(aup[64:128, d, csl], twda[64:128, tsl])], R=[wsm_k, twda_k], W=[ppj_k[u]])
                            K.op(ACT, lambda e, u=u, tsl=tsl: e.activation(out=T3[:, tsl], in_=ppj[u][:, 0:TB], func=AF.Sigmoid, bias=a0[:, d, c4:c4 + 1]),
                                 R=[ppj_k[u], a0_k], W=[T_k[2]])
                        Tg = gexc
                        K.op(DVE, lambda e: e.tensor_tensor(out=Tg, in0=kk32[:], in1=T3[:], op=ALU.mult), R=[kk_k_, T_k[2], ART_k], W=[gexc_k])
                        K.op(DVE, lambda e: e.tensor_tensor(out=BT[:], in0=Tg, in1=ginc, op=ALU.mult), R=[gexc_k, ginc_k], W=[BT_k])
                        K.op(DVE, lambda e: e.tensor_scalar(out=Tg, in0=T3[:], scalar1=kap[:, c4:c4 + 1], scalar2=omka[:, c4:c4 + 1], op0=ALU.mult, op1=ALU.add),
                             R=[T_k[2], kap_k, BT_k], W=[gexc_k])
                        K.op(DVE, lambda e: e.tensor_tensor(out=Tg, in0=Tg, in1=k32[:], op=ALU.mult), R=[gexc_k, k_k_], W=[gexc_k])
                        K.op(PL, lambda e: e.tensor_tensor(out=KT[:], in0=Tg, in1=ginc, op=ALU.mult), R=[gexc_k, ginc_k], W=[KT_k])
                        K.op(DVE, lambda e: e.scalar_tensor_tensor(out=sqb[:], in0=r32[:], scalar=rkp[:, c4:c4 + 1], in1=Tg, op0=ALU.mult, op1=ALU.mult),
                             R=[r_k_, gexc_k, rkp_k], W=[sqb_k])

                        def cons_b(ps_, pk, tb, d=d):
                            tsl = slice(tb * TB, (tb + 1) * TB)
                            if d == 0:
                                K.op(DVE, lambda e: e.tensor_tensor(out=bacc[:, tsl], in0=ps_[:], in1=vb[:, tsl], op=ALU.mult), R=[pk, vb_k], W=[ba_k])
                            else:
                                K.op(DVE, lambda e: e.tensor_tensor(out=T3[:, tsl], in0=ps_[:], in1=vb[:, tsl], op=ALU.mult), R=[pk, vb_k], W=[T_k[2]])
                                K.op(PL, lambda e: e.tensor_tensor(out=bacc[:, tsl], in0=bacc[:, tsl], in1=T3[:, tsl], op=ALU.add), R=[T_k[2]], W=[ba_k])
                        bdsum(sqb, sqb_k, cons_b)
                        if b == 0 and c4 == 0:
                            dump(f"AT{d}", ART[:, 0, :, 0, :], [128, NC, 128], R=[ART_k])
                            dump(f"BT{d}", BT[:], [128, S], R=[BT_k])

                        ckpt('rw_prep%d_%d' % (c4, d))
                        K.op(DVE, lambda e: e.memset(H32[:], 0.0), W=[H_k])
                        K.op(DVE, lambda e: e.memset(Hb[:], 0.0), W=[Hb_k])
                        K.op(DVE, lambda e: e.memset(Hbd[:], 0.0), W=[Hbd_k])
                        order = range(NC) if d == 0 else range(NC - 1, -1, -1)
                        hs = [slice(0, 64), slice(64, 128)]

                        def prep(n, q, r, d=d):
                            nsl = slice(n * 128, (n + 1) * 128)
                            tk, tk_k = tok3s[q], tok3s_k[q]
                            m1, m1_k, m2, m2_k = M1s[q], M1s_k[q], M2s[q], M2s_k[q]
                            xa, xa_k, pa, pa_k = XAs[q], XAs_k[q], PAs[q], PAs_k[q]
                            pP, pP_k, pQ, pQ_k = pPs[r], pP_ks[r], pQs[r], pQ_ks[r]
                            K.tr(ptk3[:, 0, :], BT[:, nsl], identb[:], R=[BT_k, cst], W=[ptk3_k], inc=False)
                            K.tr(ptk3[:, 1, :], KT[:, nsl], identb[:], R=[KT_k], W=[ptk3_k], inc=False)
                            K.tr(ptk3[:, 2, :], vb[:, nsl], identb[:], R=[vb_k], W=[ptk3_k])
                            for hh in range(2):
                                K.op(ACT if hh == 0 else DVE, (lambda e, hh=hh: e.activation(out=tk[:, :, hh, hh * 64:(hh + 1) * 64], in_=ptk3[:, :, hh * 64:(hh + 1) * 64], func=AF.Copy)) if hh == 0 else
                                     (lambda e, hh=hh: e.tensor_copy(out=tk[:, :, hh, hh * 64:(hh + 1) * 64], in_=ptk3[:, :, hh * 64:(hh + 1) * 64])), R=[ptk3_k], W=[tk_k])
                            yield
                            for hh in range(2):
                                K.mm(pP[:, hh, :], [(BT[:, nsl], ART[:, hh, n, :, :].rearrange("p a t -> p (a t)"))], R=[BT_k, ART_k], W=[pP_k], inc=(hh == 1))
                            for hh in range(2):
                                K.op(DVE, lambda e, hh=hh: e.tensor_tensor(out=m1[:, hh, :], in0=pP[:, hh, :], in1=MP[d][:], op=ALU.mult), R=[pP_k, cst], W=[m1_k])
                            yield
                            for hh in range(2):
                                K.mm(pP[:, hh, :], [(KT[:, nsl], ART[:, hh, n, :, :].rearrange("p a t -> p (a t)"))], R=[KT_k, ART_k], W=[pP_k], inc=(hh == 1))
                            for hh in range(2):
                                K.op(DVE, lambda e, hh=hh: e.tensor_tensor(out=m2[:, hh, :], in0=pP[:, hh, :], in1=MP[d][:], op=ALU.mult), R=[pP_k, cst], W=[m2_k])
                            yield
                            for hh in range(2):
                                K.mm(pQ[:, hh, :], [(ART[:, hh, n, 0, :], BT[:, nsl])], R=[BT_k, ART_k], W=[pQ_k], inc=(hh == 1))
                            for hh in range(2):
                                K.op(DVE, lambda e, hh=hh: e.tensor_tensor(out=pa[0][:, hh, :], in0=pQ[:, hh, :], in1=ML[d][:], op=ALU.mult), R=[pQ_k, cst], W=[pa_k[0]])
                            yield
                            K.op(PL, lambda e: e.tensor_tensor(out=xa[1][:, :, 1, :], in0=m1[:, :, 0:128], in1=identb2, op=ALU.add), R=[m1_k, cst], W=[xa_k[1]])
                            for hh in range(2):
                                K.mm(pP[:, hh, 0:128], [(pa[0][:, hh, :], m1[:, hh, 0:128])], R=[pa_k[0], m1_k], W=[pP_k], inc=(hh == 1))
                            K.op(ACT, lambda e: e.activation(out=xa[1][:, :, 0, :], in_=pP[:, :, 0:128], func=AF.Copy), R=[pP_k], W=[xa_k[1]])
                            for hh in range(2):
                                K.mm(pQ[:, hh, :], [(m1[:, hh, 0:128], pa[0][:, hh, :])], R=[pa_k[0], m1_k], W=[pQ_k], inc=(hh == 1))
                            K.op(ACT, lambda e: e.activation(out=pa[1][:], in_=pQ[:], func=AF.Copy), R=[pQ_k], W=[pa_k[1]])
                            yield
                            cur = 1
                            for lev in range(1, 7):
                                nx = 1 - cur
                                if lev < 6:
                                    for hh in range(2):
                                        K.mm(pP[:, hh, :], [(pa[cur][:, hh, :], xa[cur][:, hh, :, :].rearrange("p a t -> p (a t)"))],
                                             R=[pa_k[cur], xa_k[cur]], W=[pP_k], inc=(hh == 1))
                                    K.op(ACT, lambda e, nx=nx: e.activation(out=xa[nx][:, :, 0, :], in_=pP[:, :, 0:128], func=AF.Copy), R=[pP_k], W=[xa_k[nx]])
                                    K.op(DVE, lambda e, nx=nx, cur=cur: e.tensor_tensor(out=xa[nx][:, :, 1, :], in0=pP[:, :, 128:256], in1=xa[cur][:, :, 1, :], op=ALU.add),
                                         R=[pP_k, xa_k[cur]], W=[xa_k[nx]])
                                    for hh in range(2):
                                        K.mm(pQ[:, hh, :], [(xa[cur][:, hh, 0, :], pa[cur][:, hh, :])], R=[pa_k[cur], xa_k[cur]], W=[pQ_k], inc=(hh == 1))
                                    K.op(ACT, lambda e, nx=nx: e.activation(out=pa[nx][:], in_=pQ[:], func=AF.Copy), R=[pQ_k], W=[pa_k[nx]])
                                else:
                                    for hh in range(2):
                                        K.mm(pP[:, hh, 128:256], [(pa[cur][:, hh, :], xa[cur][:, hh, 1, :])], R=[pa_k[cur], xa_k[cur]], W=[pP_k], inc=(hh == 1))
                                    K.op(DVE, lambda e, nx=nx, cur=cur: e.tensor_tensor(out=xa[nx][:, :, 1, :], in0=pP[:, :, 128:256], in1=xa[cur][:, :, 1, :], op=ALU.add),
                                         R=[pP_k, xa_k[cur]], W=[xa_k[nx]])
                                cur = nx
                                yield
                            assert cur == 1

                        def chain(n, q, d=d):
                            nsl = slice(n * 128, (n + 1) * 128)
                            tk, tk_k = tok3s[q], tok3s_k[q]
                            m1, m1_k, m2, m2_k = M1s[q], M1s_k[q], M2s[q], M2s_k[q]
                            Wf, Wf_k = XAs[q][1], XAs_k[q][1]
                            for hh in range(2):
                                K.mm(pZ[:, hh, :], [(ART[:, hh, n, 0, :], Hb[:, :]), (m2[:, hh, 0:128], tk[:, 2, hh, hs[hh]])],
                                     R=[ART_k, Hb_k, m2_k, tk_k], W=[pZ_k[0]])
                            K.op(ACT, lambda e: e.activation(out=Zb[:], in_=pZ[:, 0:2, :], func=AF.Copy), R=[pZ_k[0]], W=[Zb_k])
                            yield
                            for hh in range(2):
                                K.mm(pZ[:, 2 + hh, :], [(Wf[:, hh, 1, :], Zb[:, hh, :])], R=[Wf_k, Zb_k], W=[pZ_k[1]])
                            K.op(ACT, lambda e: e.activation(out=Ub[:], in_=pZ[:, 2:4, :], func=AF.Copy), R=[pZ_k[1]], W=[Ub_k])
                            for hh in range(2):
                                K.op(DVE, lambda e, hh=hh: e.tensor_copy(out=Ub2[:, hh, hh * 64:(hh + 1) * 64], in_=Ub[:, hh, :]), R=[Ub_k], W=[Ub2_k])
                            yield
                            K.mm(pY[:, 128:192], [(tk[:, 0, 0, :], Ub[:, 0, :]), (tk[:, 0, 1, :], Ub[:, 1, :]),
                                                  (tk[:, 1, 0, :], tk[:, 2, 0, 0:64]), (tk[:, 1, 1, :], tk[:, 2, 1, 64:128])],
                                 R=[tk_k, Ub_k], W=[pY_k[1]])
                            K.mm(pY[:, 0:128], [(Hbd[:], ART[:, 0, n, 1, :]), (Hbd[:], ART[:, 1, n, 1, :]),
                                                (Ub2[:, 0, :], m1[:, 0, 128:256]), (Ub2[:, 1, :], m1[:, 1, 128:256]),
                                                (tk[:, 2, 0, :], m2[:, 0, 128:256]), (tk[:, 2, 1, :], m2[:, 1, 128:256])],
                                 R=[Hbd_k, ART_k, Ub2_k, m1_k, m2_k, tk_k], W=[pY_k[0]])
                            K.op(DVE, lambda e: e.tensor_tensor(out=Htmp[:], in0=pY[:, 128:192], in1=H32[:], op=ALU.add), R=[pY_k[1], H_k], W=[Ht_k])
                            if d == 0:
                                K.op(DVE, lambda e, nsl=nsl: e.tensor_copy(out=yacc[:, nsl], in_=pY[:, 0:128]), R=[pY_k[0]], W=[ya_k])
                            else:
                                K.op(DVE, lambda e, nsl=nsl: e.tensor_tensor(out=yacc[:, nsl], in0=pY[:, 0:128], in1=yacc[:, nsl], op=ALU.add), R=[pY_k[0], ya_k], W=[ya_k])
                            yield
                            K.op(DVE, lambda e, n=n: e.tensor_scalar(out=H32[:], in0=Htmp[:], scalar1=gC[:, n:n + 1], scalar2=None, op0=ALU.mult), R=[Ht_k, gC_k], W=[H_k])
                            K.op(ACT, lambda e, n=n: e.activation(out=Hb[:], in_=Htmp[:], func=AF.Copy, scale=gC[:, n:n + 1]), R=[Ht_k, gC_k], W=[Hb_k])
                            for hh in range(2):
                                K.op(PL, lambda e, hh=hh: e.tensor_copy(out=Hbd[hs[hh], hh * 64:(hh + 1) * 64], in_=H32[hs[hh], :]), R=[H_k], W=[Hbd_k])
                            yield

                        order = list(order)
                        K.barrier()
                        for q_ in range(2, NSETS):
                            K.op(DVE, lambda e, q_=q_: e.memset(tok3s[q_], 0.0), W=[tok3s_k[q_]])
                        nch = len(order)
                        act_preps, done_prep = [], set()
                        next_prep, chain_k, completed, chain_gen = 0, 0, 0, None
                        while chain_k < nch:
                            while len(act_preps) < NPIPE and next_prep < nch and next_prep <= completed + NSETS - 1:
                                act_preps.append((next_prep, prep(order[next_prep], next_prep % NSETS, next_prep % NPIPE)))
                                next_prep += 1
                            if chain_gen is None and chain_k in done_prep:
                                chain_gen = chain(order[chain_k], chain_k % NSETS)
                            if chain_gen is not None:
                                try:
                                    next(chain_gen)
                                except StopIteration:
                                    chain_gen = None
                                    completed += 1
                                    chain_k += 1
                                    nck[0] += 1
                            for it_ in list(act_preps):
                                try:
                                    next(it_[1])
                                except StopIteration:
                                    act_preps.remove(it_)
                                    done_prep.add(it_[0])
                        K.barrier()
                    if b == 0 and c4 == 0:
                        dump("yacc", yacc[:], [128, S], R=[ya_k])
                        dump("bacc", bacc[:], [128, S], R=[ba_k])
                    ckpt('rw_loops%d' % c4)
                    K.op(ACT, lambda e: e.activation(out=sqb[:], in_=yacc[:], func=AF.Copy), R=[ya_k], W=[sqb_k])

                    def cons_m(ps_, pk, tb):
                        tsl = slice(tb * TB, (tb + 1) * TB)
                        K.op(DVE, lambda e: e.scalar_tensor_tensor(out=yacc[:, tsl], in0=ps_[:], scalar=-1.0 / 64, in1=yacc[:, tsl], op0=ALU.mult, op1=ALU.add),
                             R=[pk, ya_k], W=[ya_k])
                    bdsum(sqb, sqb_k, cons_m)
                    K.op(PL, lambda e: e.tensor_tensor(out=sqb[:], in0=yacc[:], in1=yacc[:], op=ALU.mult), R=[ya_k], W=[sqb_k])

                    def cons_v(ps_, pk, tb):
                        tsl = slice(tb * TB, (tb + 1) * TB)
                        K.op(ACT, lambda e: e.activation(out=T4[:, tsl], in_=ps_[:], func=AF.Sqrt, scale=1.0 / 64, bias=GN_EPS), R=[pk], W=[T_k[3]])
                        K.op(DVE, lambda e: e.reciprocal(out=T4[:, tsl], in_=T4[:, tsl]), R=[T_k[3]], W=[T_k[3]])
                    bdsum(sqb, sqb_k, cons_v)
                    K.op(DVE, lambda e: e.tensor_tensor(out=yacc[:], in0=yacc[:], in1=T4[:], op=ALU.mult), R=[ya_k, T_k[3]], W=[ya_k])
                    K.op(DVE, lambda e: e.tensor_scalar(out=yacc[:], in0=yacc[:], scalar1=lnw[:, c4:c4 + 1], scalar2=lnb[:, c4:c4 + 1], op0=ALU.mult, op1=ALU.add),
                         R=[ya_k, lnw_k, lnb_k], W=[ya_k])
                    K.op(PL, lambda e: e.tensor_tensor(out=yacc[:], in0=yacc[:], in1=bacc[:], op=ALU.add), R=[ya_k, ba_k], W=[ya_k])
                    K.op(DVE, lambda e: e.tensor_tensor(out=catT[:, 4 + c4, :], in0=yacc[:], in1=gTb[:], op=ALU.mult), R=[ya_k, gT_k], W=[cat_k[4 + c4]])
                    ckpt('rw_gn%d' % c4)
            if b == 0:
                dump("orw", catT[:, 4, :], [128, S], R=cat_k)

        ckpt('rwkv')
        with K.scope():
            x1 = K.sb([128, NT, D], F32, "x1")
            x1_k = [Tok() for _ in range(NT)]
            h2t = K.sb([128, NT, D], BF16, "h2t")
            h2_k = [Tok() for _ in range(NT)]
            afft = K.sb([128, NT, NE], F32, "afft")
            aff_k = [Tok() for _ in range(NT)]
            posm = K.sb([16, S], F32, "posm")
            posm_k = Tok()
            post = K.sb([128, NT, NE], F32, "post")
            post_k = Tok()
            gt2b = K.sb([128, 1, D], F32, "gt2b")
            gt2b_k = Tok()
            bcast_rows(b, gt2b, gt2b_k, [(modT, 40)])
            K.stacks.append(ExitStack())
            affT = K.sb([16, S], F32, "affT")
            affT_k = Tok()
            with K.scope():
                bct = K.sb([128, 3, D], F32, "bct")
                bct_k = Tok()
                bcast_rows(b, bct, bct_k, [(modT, 16), (S2, 0), (modT, 24)])
                wo = K.sb([128, KD, D], BF16, "wo")
                wo_k = Tok()
                K.dma(POOL, K.dmac("wo"), wo[:], wout_d.rearrange("(j p) n -> p j n", p=128), W=[wo_k])
                xc = [K.dmac("x0")]
                xt = [K.sb([128, D], F32, "xt")]
                xt_k = [Tok()]
                po = [K.ps([128, 512], F32, "po") for _ in range(2)]
                po_k = [Tok(), Tok()]
                st_ = [K.sb([128, 4], F32, "st") for _ in range(2)]
                st_k = [Tok(), Tok()]
                pt = [K.ps([128, KD, 128], BF16, "pt") for _ in range(2)]
                pt_k = [Tok(), Tok()]
                h2T = [K.sb([128, KD, 128], BF16, "h2T") for _ in range(2)]
                h2T_k = [Tok(), Tok()]
                plg = K.ps([128, NE], F32, "plg")
                plg_k = Tok()
                lg = K.sb([128, NE], F32, "lg")
                lg_k = Tok()
                paT = K.ps([16, 128], F32, "paT")
                paT_k = Tok()
                for i in range(NT):
                    if i % 4 == 0 and i > 0:
                        K.barrier()
                    u = i % 2
                    sl = slice(i * 128, (i + 1) * 128)
                    K.dma(SP, xc[0], xt[0][:], x_d[tok0 + i * 128: tok0 + (i + 1) * 128, :], W=[xt_k[0]])
                    for half in range(2):
                        hsl = slice(half * 512, (half + 1) * 512)
                        K.mm(po[half][:], [(catT[:, j, sl], wo[:, j, hsl]) for j in range(KD)], R=cat_k + [wo_k], W=[po_k[half]])
                        K.op(DVE, lambda e, half=half, hsl=hsl, i=i: e.tensor_tensor(out=x1[:, i, hsl], in0=po[half][:], in1=bct[:, 0, hsl], op=ALU.mult),
                             R=[po_k[half], bct_k], W=[x1_k[i]])
                    K.op(PL, lambda e, i=i: e.tensor_tensor(out=x1[:, i, :], in0=x1[:, i, :], in1=xt[0][:], op=ALU.add), R=[x1_k[i], xt_k[0]], W=[x1_k[i]])
                    K.op(ACT, lambda e, u=u, i=i: e.activation(out=h2T[u][:].rearrange("p j t -> p (j t)"), in_=x1[:, i, :], func=AF.Square, accum_out=st_[u][:, 0:1]),
                         R=[x1_k[i]], W=[h2T_k[u], st_k[u]])
                    K.op(ACT, lambda e, u=u: e.activation(out=st_[u][:, 1:2], in_=st_[u][:, 0:1], func=AF.Sqrt, scale=1.0 / D, bias=NORM_EPS), R=[st_k[u]], W=[st_k[u]])
                    K.op(DVE, lambda e, u=u: e.reciprocal(out=st_[u][:, 1:2], in_=st_[u][:, 1:2]), R=[st_k[u]], W=[st_k[u]])
                    K.op(DVE, lambda e, u=u, i=i: e.scalar_tensor_tensor(out=h2t[:, i, :], in0=x1[:, i, :], scalar=st_[u][:, 1:2], in1=bct[:, 1, :], op0=ALU.mult, op1=ALU.mult),
                         R=[x1_k[i], st_k[u], bct_k], W=[h2_k[i]])
                    K.op(PL, lambda e, i=i: e.tensor_tensor(out=h2t[:, i, :], in0=h2t[:, i, :], in1=bct[:, 2, :], op=ALU.add), R=[h2_k[i], bct_k], W=[h2_k[i]])
                    for j in range(KD):
                        K.tr(pt[u][:, j, :], h2t[:, i, j * 128:(j + 1) * 128], identb[:], R=[h2_k[i], cst], W=[pt_k[u]], inc=(j == KD - 1))
                    K.op(ACT, lambda e, u=u: e.activation(out=h2T[u][:], in_=pt[u][:], func=AF.Copy), R=[pt_k[u]], W=[h2T_k[u]])
                    K.mm(plg[:], [(h2T[u][:, j, :], wrt[:, j, :]) for j in range(KD)], R=[h2T_k[u], wsm_k], W=[plg_k])
                    K.op(DVE, lambda e, u=u: e.tensor_reduce(out=st_[u][:, 2:3], in_=plg[:], axis=AX.X, op=ALU.max), R=[plg_k], W=[st_k[u]])
                    K.op(DVE, lambda e, u=u: e.tensor_scalar(out=st_[u][:, 2:3], in0=st_[u][:, 2:3], scalar1=-1.0, scalar2=None, op0=ALU.mult), R=[st_k[u]], W=[st_k[u]])
                    K.op(ACT, lambda e, u=u: e.activation(out=lg[:], in_=plg[:], func=AF.Exp, bias=st_[u][:, 2:3], accum_out=st_[u][:, 3:4]),
                         R=[plg_k, st_k[u]], W=[lg_k, st_k[u]])
                    K.op(DVE, lambda e, u=u: e.reciprocal(out=st_[u][:, 3:4], in_=st_[u][:, 3:4]), R=[st_k[u]], W=[st_k[u]])
                    K.op(DVE, lambda e, u=u, i=i: e.tensor_scalar(out=afft[:, i, :], in0=lg[:], scalar1=st_[u][:, 3:4], scalar2=None, op0=ALU.mult),
                         R=[lg_k, st_k[u]], W=[aff_k[i]])
                    K.tr(paT[:], afft[:, i, :], identf[:], R=[aff_k[i], cst], W=[paT_k])
                    K.op(DVE, lambda e, sl=sl: e.tensor_copy(out=affT[:, sl], in_=paT[:]), R=[paT_k], W=[affT_k])
            if b == 0:
                dump("x1", x1[:, 0, :], [128, D], R=x1_k)
                dump("affT", affT[:], [16, S], R=[affT_k])

            ckpt('outproj')
            with K.scope():
                wk = [K.sb([16, S], F32, "wk") for _ in range(2)]
                wk_k = [Tok(), Tok()]
                m8 = K.sb([16, 8], F32, "m8")
                m8_k = Tok()
                mk_ = K.sb([16, S], F32, "mk")
                mk_k = Tok()
                ppo = K.ps([128, NE], F32, "ppo")
                ppo_k = Tok()
                K.op(DVE, lambda e: e.tensor_copy(out=wk[0][:], in_=affT[:]), R=[affT_k], W=[wk_k[0]])
                nit = CAP // 8
                cur = 0
                for it in range(nit):
                    K.op(DVE, lambda e, cur=cur: e.max(out=m8[:], in_=wk[cur][:]), R=[wk_k[cur]], W=[m8_k])
                    if it < nit - 1:
                        K.op(DVE, lambda e, cur=cur: e.match_replace(out=wk[1 - cur][:], in_to_replace=m8[:], in_values=wk[cur][:], imm_value=-1.0),
                             R=[wk_k[cur], m8_k], W=[wk_k[1 - cur]])
                        cur = 1 - cur
                K.op(DVE, lambda e: e.tensor_scalar(out=mk_[:], in0=affT[:], scalar1=m8[:, 7:8], scalar2=None, op0=ALU.is_ge), R=[affT_k, m8_k], W=[mk_k])
                K.op(DVE, lambda e: e.tensor_tensor_scan(out=posm[:], data0=onesf[0:16, 0:1].to_broadcast([16, S]), data1=mk_[:], initial=0.0, op0=ALU.mult, op1=ALU.add),
                     R=[mk_k, cst], W=[posm_k])
                K.op(DVE, lambda e: e.tensor_tensor(out=posm[:], in0=posm[:], in1=mk_[:], op=ALU.mult), R=[posm_k, mk_k], W=[posm_k])
                K.op(DVE, lambda e: e.tensor_scalar(out=posm[:], in0=posm[:], scalar1=-1.0, scalar2=None, op0=ALU.add), R=[posm_k], W=[posm_k])
                for i in range(NT):
                    K.tr(ppo[:], posm[:, i * 128:(i + 1) * 128], identf[0:16, 0:16], R=[posm_k, cst], W=[ppo_k])
                    K.op(DVE, lambda e, i=i: e.tensor_copy(out=post[:, i, :], in_=ppo[:]), R=[ppo_k], W=[post_k])
            if b == 0:
                dump("posm", posm[:], [16, S], R=[posm_k])

            K.barrier()
            K.stacks.pop().close()
            ckpt('topk')
            with K.scope():
                NWS = 6
                if S >= 2048:
                    wsl = [catT[:, :, q * 512:(q + 1) * 512] for q in range(4)]
                    wsl += [K.sb([128, KD, 512], BF16, "wsl") for _ in range(NWS - 4)]
                else:
                    wsl = [K.sb([128, KD, 512], BF16, "wsl") for _ in range(NWS)]
                wsl_k = [Tok() for _ in range(NWS)]
                wsc_ = [K.dmac("wsl") for _ in range(NWS)]
                Sel = K.sb([128, NT, CAP], BF16, "Sel")
                Sel_k = Tok()
                SelT = K.sb([128, NCT, S], BF16, "SelT")
                SelT_k = Tok()
                hgT = K.sb([128, KD, CAP], BF16, "hgT")
                hgT_k = Tok()
                hidT = K.sb([128, KD, CAP], BF16, "hidT")
                hid_k = Tok()
                sgt = K.sb([128, CAP], F32, "sgt")
                sgt_k = Tok()
                ysb = K.sb([128, NCT, D], BF16, "ysb")
                ysb_k = Tok()
                ppb = K.ps([128, TB], F32, "ppb")
                ppb_k = Tok()
                pg = K.ps([128, CAP], F32, "pg")
                pg_k = Tok()
                pu = K.ps([128, CAP], F32, "pu")
                pu_k = Tok()
                ph = [K.ps([128, CAP], F32, "ph") for _ in range(2)]
                ph_k = [Tok(), Tok()]
                py = [K.ps([128, 512], F32, "py") for _ in range(2)]
                py_k = [Tok(), Tok()]
                wcount = [0]
                ohe = K.sb([16, 128], F32, "ohe")
                ohe_k = Tok()

                def wload(src_d, e_, half):
                    s = wcount[0] % NWS
                    wcount[0] += 1
                    K.dma(POOL, wsc_[s], wsl[s][:], src_d[e_, :, half * 512:(half + 1) * 512].rearrange("(j p) n -> p j n", p=128), W=[wsl_k[s]])
                    return s

                for e_ in range(NE):
                    sg0 = wload(wg_d, e_, 0)
                    sg1 = wload(wg_d, e_, 1)
                    su0 = wload(wu_d, e_, 0)
                    su1 = wload(wu_d, e_, 1)
                    for i in range(NT):
                        K.op(DVE, lambda e, i=i, e_=e_: e.tensor_scalar(out=Sel[:, i, :], in0=iotac[:, 0:CAP], scalar1=post[:, i, e_:e_ + 1], scalar2=None, op0=ALU.is_equal),
                             R=[post_k, cst], W=[Sel_k])
                    K.op(DVE, lambda e, e_=e_: e.tensor_copy(out=ohe[:], in_=bc(identf[0:16, e_:e_ + 1], [16, 128])), R=[cst], W=[ohe_k])
                    for tb in range(NTB):
                        tsl = slice(tb * TB, (tb + 1) * TB)
                        K.mm(ppb[:], [(ohe[:], posm[:, tsl])], R=[posm_k, ohe_k], W=[ppb_k])
                        for ct in range(NCT):
                            K.op(DVE, lambda e, ct=ct, tsl=tsl: e.tensor_scalar(out=SelT[:, ct, tsl], in0=ppb[:], scalar1=iotap[:, ct:ct + 1], scalar2=None, op0=ALU.is_equal),
                                 R=[ppb_k, cst], W=[SelT_k])
                    for fc in range(KD):
                        u = fc % 2
                        K.mm(ph[u][:], [(h2t[:, i, fc * 128:(fc + 1) * 128], Sel[:, i, :]) for i in range(NT)], R=h2_k + [Sel_k], W=[ph_k[u]])
                        K.op(ACT if u == 0 else DVE, (lambda e, u=u, fc=fc: e.activation(out=hgT[:, fc, :], in_=ph[u][:], func=AF.Copy)) if u == 0 else
                             (lambda e, u=u, fc=fc: e.tensor_copy(out=hgT[:, fc, :], in_=ph[u][:])), R=[ph_k[u]], W=[hgT_k])
                    for fc in range(KD):
                        gs = sg0 if fc < 4 else sg1
                        us = su0 if fc < 4 else su1
                        fo = (fc % 4) * 128
                        K.mm(pg[:], [(wsl[gs][:, j, fo:fo + 128], hgT[:, j, :]) for j in range(KD)], R=[wsl_k[gs], hgT_k], W=[pg_k])
                        K.mm(pu[:], [(wsl[us][:, j, fo:fo + 128], hgT[:, j, :]) for j in range(KD)], R=[wsl_k[us], hgT_k], W=[pu_k])
                        K.op(ACT, lambda e: e.activation(out=sgt[:], in_=pg[:], func=AF.Silu), R=[pg_k], W=[sgt_k])
                        K.op(DVE, lambda e, fc=fc: e.tensor_tensor(out=hidT[:, fc, :], in0=pu[:], in1=sgt[:], op=ALU.mult), R=[pu_k, sgt_k], W=[hid_k])
                    sd0 = wload(wd_d, e_, 0)
                    sd1 = wload(wd_d, e_, 1)
                    for ct in range(NCT):
                        for half in range(2):
                            ds_ = sd0 if half == 0 else sd1
                            hsl = slice(half * 512, (half + 1) * 512)
                            K.mm(py[half][0:CP, :], [(hidT[:, fc, ct * 128:ct * 128 + CP], wsl[ds_][:, fc, :]) for fc in range(KD)], R=[hid_k, wsl_k[ds_]], W=[py_k[half]])
                            K.op(DVE, lambda e, ct=ct, half=half, hsl=hsl: e.tensor_tensor(out=ysb[0:CP, ct, hsl], in0=py[half][0:CP, :], in1=gt2b[0:CP, 0, hsl], op=ALU.mult),
                                 R=[py_k[half], gt2b_k], W=[ysb_k])
                    for i in range(NT):
                        sl = slice(i * 128, (i + 1) * 128)
                        for half in range(2):
                            hsl = slice(half * 512, (half + 1) * 512)
                            K.mm(py[half][:], [(SelT[0:CP, ct, sl], ysb[0:CP, ct, hsl]) for ct in range(NCT)], R=[SelT_k, ysb_k], W=[py_k[half]])
                            K.op(DVE, lambda e, i=i, half=half, hsl=hsl, e_=e_: e.scalar_tensor_tensor(out=x1[:, i, hsl], in0=py[half][:], scalar=afft[:, i, e_:e_ + 1],
                                                                                                    in1=x1[:, i, hsl], op0=ALU.mult, op1=ALU.add),
                                 R=[py_k[half], aff_k[i], x1_k[i]], W=[x1_k[i]])
                for i in range(NT):
                    K.dma(SP, outc, out_d[tok0 + i * 128: tok0 + (i + 1) * 128, :], x1[:, i, :], R=[x1_k[i]])
    K.barrier()
    K.stacks[0].close()
    return nc, dump_d


def rope_tables(S):
    rows = S // 64
    row = np.repeat(np.arange(rows, dtype=np.float32), 64)
    col = np.tile(np.arange(64, dtype=np.float32), rows)
    freqs = (np.float32(10000.0) ** (-np.arange(16, dtype=np.float32) / np.float32(16))).astype(np.float32)
    ang = np.concatenate([row[:, None] * freqs, col[:, None] * freqs], axis=-1).astype(np.float32)
    return np.cos(ang).astype(np.float32), np.sin(ang).astype(np.float32)


def fm(v, n):
    return np.ascontiguousarray(np.asarray(v, np.float32).reshape(n, 128).T)


def make_in_maps(inputs, S, NSEQ, ncores):
    f = lambda a: np.ascontiguousarray(np.asarray(a, np.float32))
    NT = S // 128
    cos, sin = rope_tables(S)
    cosl = np.ascontiguousarray(cos.reshape(NT, 128, 32).transpose(1, 0, 2))
    sinl = np.ascontiguousarray(sin.reshape(NT, 128, 32).transpose(1, 0, 2))
    x = f(inputs["x"])
    c = f(inputs["c"])
    shared = {
        "w_ada": f(inputs["w_ada"][0]),
        "b_ada": fm(inputs["b_ada"][0], 48),
        "g_mix": fm(inputs["g_mix"][0], 8),
        "g_ffn": fm(inputs["g_ffn"][0], 8),
        "w_in": f(inputs["w_in"][0]),
        "q_norm": f(inputs["q_norm"][0]).reshape(1, 64),
        "k_norm": f(inputs["k_norm"][0]).reshape(1, 64),
        "mu": fm(inputs["mu_shift"][0], 14),
        "w0": np.ascontiguousarray(f(inputs["w0"][0]).reshape(2, 4, 128).transpose(2, 0, 1)),
        "a0": np.ascontiguousarray(f(inputs["a0"][0]).reshape(2, 4, 128).transpose(2, 0, 1)),
        "w_up": f(inputs["w_up"][0]),
        "a_up": f(inputs["a_up"][0]),
        "g_up": f(inputs["g_up"][0]),
        "k_k": fm(inputs["k_k"][0], 4),
        "k_a": fm(inputs["k_a"][0], 4),
        "r_k": fm(f(inputs["r_k"][0]).reshape(-1), 4),
        "ln_w": fm(inputs["ln_w"][0], 4),
        "ln_b": fm(inputs["ln_b"][0], 4),
        "w_out": f(inputs["w_out"][0]),
        "w_router": f(inputs["w_router"][0]),
        "w_gate": f(inputs["w_gate"][0]),
        "w_up_e": f(inputs["w_up_e"][0]),
        "w_down": f(inputs["w_down"][0]),
        "cos": cosl,
        "sin": sinl,
    }
    maps = []
    for i in range(ncores):
        m = dict(shared)
        m["x"] = np.ascontiguousarray(x[i * NSEQ:(i + 1) * NSEQ].reshape(NSEQ * S, D))
        cc = c[i * NSEQ:(i + 1) * NSEQ]
        m["cT"] = np.ascontiguousarray(cc.reshape(NSEQ, KD, 128).transpose(2, 1, 0))
        maps.append(m)
    return maps


def kernel(**inputs):
    x = np.asarray(inputs["x"])
    B, S, _ = x.shape
    ncores = 8
    NSEQ = B // ncores
    nc, _ = build(S=S, NSEQ=NSEQ)
    maps = make_in_maps(inputs, S, NSEQ, ncores)
    res = run_bass_kernel_spmd(nc, maps, core_ids=list(range(ncores)))
    outs = [np.asarray(r["out"]).reshape(NSEQ, S, D) for r in res.results]
    return np.concatenate(outs, axis=0).astype(np.float32)
```

```python
import numpy as np
from contextlib import ExitStack, contextmanager
import concourse.bass as bass
import concourse.mybir as mybir
from concourse.bass_utils import run_bass_kernel_spmd

F32 = mybir.dt.float32
BF16 = mybir.dt.bfloat16
AF = mybir.ActivationFunctionType
ALU = mybir.AluOpType
AX = mybir.AxisListType

D = 1024
KD = 8
HD = 64
NE = 16
DECAY = 0.606531
GN_EPS = 64e-5
NORM_EPS = 1e-6
N_IN = 2560
REBASE_T = 3000
BAR_EVERY = 4


class Tok:
    __slots__ = ("w", "r")

    def __init__(self):
        self.w = None
        self.r = {}


class Cnt:
    def __init__(self, sem, incv, eng=None, name=""):
        self.sem = sem
        self.incv = incv
        self.cnt = 0
        self.eng = eng
        self.seen = {}
        self.name = name
        self.gen = 0


class Ctx:
    def __init__(self, nc):
        self.nc = nc
        self.stacks = [ExitStack()]
        self.uid = 0
        self.allc = []
        mk = self._mkc
        self.PE = mk(nc.tensor, 1, "pe")
        self.ACT = mk(nc.scalar, 1, "act")
        self.DVE = mk(nc.vector, 1, "dve")
        self.POOL = mk(nc.gpsimd, 1, "pool")
        self.SP = Cnt(None, 0, nc.sync, "sp")
        self.dma_free = []
        self.outc = []

    def _mkc(self, eng, incv, name):
        sem = self.stacks[0].enter_context(self.nc.semaphore(f"s_{name}_{self.uid}"))
        self.uid += 1
        c = Cnt(sem, incv, eng, name)
        self.allc.append(c)
        return c

    def dmac(self, name="d"):
        return self._mkc(None, 16, name)

    def nm(self, s):
        self.uid += 1
        return f"{s}_{self.uid}"

    def sb(self, shape, dt, name="t"):
        return self.stacks[-1].enter_context(self.nc.sbuf_tensor(self.nm(name), list(shape), dt))

    def ps(self, shape, dt=F32, name="p"):
        return self.stacks[-1].enter_context(self.nc.psum_tensor(self.nm(name), list(shape), dt))

    @contextmanager
    def scope(self):
        self.stacks.append(ExitStack())
        try:
            yield
        finally:
            self.barrier()
            self.stacks.pop().close()

    def barrier(self):
        for e in (self.PE, self.ACT, self.DVE, self.POOL, self.SP):
            for f in self.allc:
                if f.cnt > 0 and e.seen.get(f, 0) < f.cnt:
                    e.eng.wait_ge(f.sem, f.cnt * f.incv)
                    e.seen[f] = f.cnt
        for f in (self.PE, self.ACT, self.DVE, self.POOL):
            if f.cnt > REBASE_T:
                f.sem = self.stacks[0].enter_context(self.nc.semaphore(f"s_{f.name}_rb{self.uid}"))
                self.uid += 1
                f.cnt = 0
                f.gen += 1
                for e in (self.PE, self.ACT, self.DVE, self.POOL, self.SP):
                    e.seen.pop(f, None)

    def op(self, e, fn, R=(), W=(), comp=None, inc=True):
        comp = comp or e
        need = {}
        for t in R:
            if t.w is not None:
                f, c, g = t.w
                if g == f.gen and c > need.get(f, 0):
                    need[f] = c
        for t in W:
            if t.w is not None:
                f, c, g = t.w
                if g == f.gen and c > need.get(f, 0):
                    need[f] = c
            for f, (c, g) in t.r.items():
                if g == f.gen and c > need.get(f, 0):
                    need[f] = c
        for f, c in need.items():
            if f is e and e is self.PE:
                continue
            if e.seen.get(f, 0) < c:
                e.eng.wait_ge(f.sem, c * f.incv)
                e.seen[f] = c
        ins = fn(e.eng)
        if inc:
            comp.cnt += 1
            ins.then_inc(comp.sem, comp.incv)
            cc = comp.cnt
        else:
            cc = comp.cnt + 1
        for t in R:
            pr = t.r.get(comp)
            if pr is None or pr[1] != comp.gen or pr[0] < cc:
                t.r[comp] = (cc, comp.gen)
        for t in W:
            t.w = (comp, cc, comp.gen)
            t.r = {}
        return ins

    def mm(self, out, pairs, R=(), W=(), start=True, stop=True, inc=True):
        n = len(pairs)
        for i, (l, r) in enumerate(pairs):
            last = i == n - 1
            self.op(self.PE,
                    lambda e, l=l, r=r, i=i, last=last: e.matmul(out, lhsT=l, rhs=r, start=(start and i == 0), stop=(stop and last)),
                    R=R if i == 0 else (), W=W, inc=(inc and last))

    def tr(self, out, in_, ident, R=(), W=(), inc=True):
        self.op(self.PE, lambda e: e.transpose(out, in_, ident), R=R, W=W, inc=inc)

    def dma(self, issuer, comp, out, in_, R=(), W=()):
        self.op(issuer, lambda e: e.dma_start(out=out, in_=in_), R=R, W=W, comp=comp)


def bc(ap, shape):
    return ap.to_broadcast(list(shape))


class _Stop(Exception):
    pass


def build(S=2048, NSEQ=4, dumps=(), stop=None):
    try:
        return _build(S, NSEQ, dumps, stop)
    except _Stop as ex:
        ex.args[1].barrier()
        ex.args[1].stacks[0].close()
        return ex.args[0]


def _build(S, NSEQ, dumps, stop):
    NT = S // 128
    QB = min(512, S)
    NQB = S // QB
    QT = QB // 128
    CAP = 2 * S // NE
    NCT = max(1, CAP // 128)
    CP = min(CAP, 128)
    TB = min(512, S)
    NTB = S // TB
    TOKS = NSEQ * S
    nc = bass.Bass("TRN2", target_bir_lowering=False)
    K = Ctx(nc)

    def din(name, shape):
        return nc.dram_tensor(name, list(shape), F32, kind="ExternalInput").ap()

    x_d = din("x", [TOKS, D])
    cT_d = din("cT", [128, KD, NSEQ])
    wada_d = din("w_ada", [D, 6 * D])
    bada_d = din("b_ada", [128, 48])
    gmix_d = din("g_mix", [128, KD])
    gffn_d = din("g_ffn", [128, KD])
    win_d = din("w_in", [D, N_IN])
    qn_d = din("q_norm", [1, HD])
    kn_d = din("k_norm", [1, HD])
    mu_d = din("mu", [128, 14])
    w0_d = din("w0", [128, 2, 4])
    a0_d = din("a0", [128, 2, 4])
    wup_d = din("w_up", [2, 64, 512])
    aup_d = din("a_up", [2, 64, 512])
    gup_d = din("g_up", [128, 512])
    kk_d = din("k_k", [128, 4])
    ka_d = din("k_a", [128, 4])
    rk_d = din("r_k", [128, 4])
    lnw_d = din("ln_w", [128, 4])
    lnb_d = din("ln_b", [128, 4])
    wout_d = din("w_out", [D, D])
    wr_d = din("w_router", [D, NE])
    wg_d = din("w_gate", [NE, D, D])
    wu_d = din("w_up_e", [NE, D, D])
    wd_d = din("w_down", [NE, D, D])
    cos_d = din("cos", [128, NT, 32])
    sin_d = din("sin", [128, NT, 32])
    out_d = nc.dram_tensor("out", [TOKS, D], F32, kind="ExternalOutput").ap()
    dump_d = {}

    PE, ACT, DVE, POOL, SP = K.PE, K.ACT, K.DVE, K.POOL, K.SP
    import os as _os0
    PL = POOL if _os0.environ.get('POOLC', '1') == '1' else DVE
    outc = K.dmac("outc")

    def ckpt(name):
        if stop == name:
            raise _Stop((nc, dump_d), K)

    def dump(name, src_ap, shape, R=()):
        if name not in dumps:
            return
        dd = nc.dram_tensor("dump_" + name, list(shape), F32, kind="ExternalOutput").ap()
        dump_d[name] = dd
        tmp = K.sb(shape, F32, "dmp")
        tk = Tok()
        K.op(DVE, lambda e: e.tensor_copy(out=tmp[:], in_=src_ap), R=R, W=[tk])
        K.dma(SP, outc, dd, tmp[:], R=[tk])

    cst = Tok()
    identf = K.sb([128, 128], F32, "identf")
    identb = K.sb([128, 128], BF16, "identb")
    onesf = K.sb([128, 128], F32, "onesf")
    bdones = K.sb([128, 128], BF16, "bdones")
    MP = [K.sb([128, 256], BF16, "MP0"), K.sb([128, 256], BF16, "MP1")]
    ML = [K.sb([128, 128], BF16, "ML0"), K.sb([128, 128], BF16, "ML1")]
    iotac = K.sb([128, 256], F32, "iotac")
    iotap = K.sb([128, 2], F32, "iotap")
    hmask = K.sb([128, 4], F32, "hmask")

    K.op(POOL, lambda e: e.memset(onesf[:], 1.0), W=[cst])
    K.stacks.append(ExitStack())
    mUPs = K.sb([128, 128], F32, "mUPs")
    mUPi = K.sb([128, 128], F32, "mUPi")
    mLOs = K.sb([128, 128], F32, "mLOs")
    mLOi = K.sb([128, 128], F32, "mLOi")
    def aff(dst, base, cm, step, cmp):
        K.op(POOL, lambda e: e.affine_select(out=dst[:], in_=onesf[:], pattern=[[step, 128]], compare_op=cmp,
                                             fill=0.0, base=base, channel_multiplier=cm), R=[cst], W=[cst])
    aff(identf, 0, 1, -1, ALU.is_equal)
    aff(mUPs, 0, -1, 1, ALU.is_gt)
    aff(mUPi, 0, -1, 1, ALU.is_ge)
    aff(mLOs, 0, 1, -1, ALU.is_gt)
    aff(mLOi, 0, 1, -1, ALU.is_ge)
    K.op(DVE, lambda e: e.tensor_copy(out=identb[:], in_=identf[:]), R=[cst], W=[cst])
    K.op(DVE, lambda e: e.memset(bdones[:], 0.0), W=[cst])
    K.op(DVE, lambda e: e.memset(bdones[0:64, 0:64], 1.0), W=[cst])
    K.op(DVE, lambda e: e.memset(bdones[64:128, 64:128], 1.0), W=[cst])
    K.op(DVE, lambda e: e.tensor_copy(out=MP[0][:, 0:128], in_=mUPs[:]), R=[cst], W=[cst])
    K.op(DVE, lambda e: e.tensor_copy(out=MP[0][:, 128:256], in_=mUPi[:]), R=[cst], W=[cst])
    K.op(DVE, lambda e: e.tensor_copy(out=MP[1][:, 0:128], in_=mLOs[:]), R=[cst], W=[cst])
    K.op(DVE, lambda e: e.tensor_copy(out=MP[1][:, 128:256], in_=mLOi[:]), R=[cst], W=[cst])
    K.op(DVE, lambda e: e.tensor_copy(out=ML[0][:], in_=mLOs[:]), R=[cst], W=[cst])
    K.op(DVE, lambda e: e.tensor_copy(out=ML[1][:], in_=mUPs[:]), R=[cst], W=[cst])
    K.barrier()
    K.stacks.pop().close()
    K.op(POOL, lambda e: e.iota(iotac[:], pattern=[[1, 256]], base=0, channel_multiplier=0,
                                allow_small_or_imprecise_dtypes=True), W=[cst])
    K.op(POOL, lambda e: e.iota(iotap[:], pattern=[[128, 2]], base=0, channel_multiplier=1,
                                allow_small_or_imprecise_dtypes=True), W=[cst])
    K.op(DVE, lambda e: e.memset(hmask[:], 0.0), W=[cst])
    K.op(DVE, lambda e: e.memset(hmask[0:64, 0:1], 1.0), W=[cst])
    K.op(DVE, lambda e: e.memset(hmask[64:128, 1:2], 1.0), W=[cst])
    K.op(DVE, lambda e: e.memset(hmask[0:64, 2:3], -1.0), W=[cst])
    K.op(DVE, lambda e: e.memset(hmask[64:128, 3:4], -1.0), W=[cst])

    ckpt('consts')
    def ldsmall(dram, shape, name, dt=F32, eng=None):
        t = K.sb(shape, dt, name)
        c = K.dmac(name)
        tk = Tok()
        K.dma(SP if dt == F32 else POOL, c, t[:], dram, W=[tk])
        return t, tk

    cT, cT_k = ldsmall(cT_d, [128, KD, NSEQ], "cT")
    bada, bada_k = ldsmall(bada_d, [128, 48], "bada")
    gmix, gmix_k = ldsmall(gmix_d, [128, KD], "gmix")
    gffn, gffn_k = ldsmall(gffn_d, [128, KD], "gffn")
    mu, mu_k = ldsmall(mu_d, [128, 14], "mu")
    w0, w0_k = ldsmall(w0_d, [128, 2, 4], "w0")
    a0, a0_k = ldsmall(a0_d, [128, 2, 4], "a0")
    kkp, kkp_k = ldsmall(kk_d, [128, 4], "kkp")
    kap, kap_k = ldsmall(ka_d, [128, 4], "kap")
    rkp, rkp_k = ldsmall(rk_d, [128, 4], "rkp")
    lnw, lnw_k = ldsmall(lnw_d, [128, 4], "lnw")
    lnb, lnb_k = ldsmall(lnb_d, [128, 4], "lnb")
    cosT, cos_k = ldsmall(cos_d, [128, NT, 32], "cos")
    sinT, sin_k = ldsmall(sin_d, [128, NT, 32], "sin")
    gain = K.sb([128, 10, HD], F32, "gain")
    gain_k = Tok()
    gc_ = K.dmac("gain")
    K.dma(SP, gc_, gain[:, 0, :], qn_d.partition_broadcast(128), W=[gain_k])
    K.dma(SP, gc_, gain[:, 8, :], kn_d.partition_broadcast(128), W=[gain_k])
    for h in range(1, 8):
        K.op(DVE, lambda e, h=h: e.tensor_copy(out=gain[:, h, :], in_=gain[:, 0, :]), R=[gain_k], W=[gain_k])
    K.op(DVE, lambda e: e.tensor_copy(out=gain[:, 9, :], in_=gain[:, 8, :]), R=[gain_k], W=[gain_k])
    K.op(DVE, lambda e: e.tensor_scalar(out=gain[:, 0:8, :], in0=gain[:, 0:8, :], scalar1=HD ** -0.5, scalar2=None, op0=ALU.mult),
         R=[gain_k], W=[gain_k])
    hmu = K.sb([128, 14], F32, "hmu")
    omm = K.sb([128, 14], F32, "omm")
    omka = K.sb([128, 4], F32, "omka")
    K.op(DVE, lambda e: e.tensor_scalar(out=hmu[:], in0=mu[:], scalar1=0.5, scalar2=None, op0=ALU.mult), R=[mu_k], W=[mu_k])
    K.op(DVE, lambda e: e.tensor_scalar(out=omm[:], in0=mu[:], scalar1=-1.0, scalar2=1.0, op0=ALU.mult, op1=ALU.add), R=[mu_k], W=[mu_k])
    K.op(DVE, lambda e: e.tensor_scalar(out=omka[:], in0=kap[:], scalar1=-1.0, scalar2=1.0, op0=ALU.mult, op1=ALU.add), R=[kap_k], W=[kap_k])
    wup = K.sb([128, 2, 512], BF16, "wup")
    aup = K.sb([128, 2, 512], BF16, "aup")
    gup = K.sb([128, 512], BF16, "gup")
    wrt = K.sb([128, KD, NE], BF16, "wrt")
    wsm_k = Tok()
    wsc = K.dmac("wsm")
    K.dma(POOL, wsc, wup[0:64, :, :], wup_d.rearrange("d k n -> k d n"), W=[wsm_k])
    K.dma(POOL, wsc, aup[64:128, :, :], aup_d.rearrange("d k n -> k d n"), W=[wsm_k])
    K.dma(POOL, wsc, gup[:], gup_d, W=[wsm_k])
    K.dma(POOL, wsc, wrt[:], wr_d.rearrange("(j p) n -> p j n", p=128), W=[wsm_k])

    ckpt('small')
    modT = K.sb([128, 48, NSEQ], F32, "modT")
    mod_k = Tok()
    S1 = K.sb([128, KD, NSEQ], F32, "S1")
    S2 = K.sb([128, KD, NSEQ], F32, "S2")
    with K.scope():
        cond = K.sb([128, KD, NSEQ], F32, "cond")
        cond_k = Tok()
        K.op(ACT, lambda e: e.activation(out=cond[:], in_=cT[:], func=AF.Silu), R=[cT_k], W=[cond_k])
        wa = [K.sb([128, KD, 512], F32, "wa") for _ in range(2)]
        wa_k = [Tok(), Tok()]
        wac = [K.dmac("wa0"), K.dmac("wa1")]
        pm = [K.ps([128, 4, NSEQ], F32, "pm") for _ in range(2)]
        pm_k = [Tok(), Tok()]
        for g in range(12):
            b = g % 2
            K.dma(SP, wac[b], wa[b][:], wada_d[:, g * 512:(g + 1) * 512].rearrange("(j p) n -> p j n", p=128), W=[wa_k[b]])
            for mm_ in range(4):
                K.mm(pm[b][:, mm_, :], [(wa[b][:, j, mm_ * 128:(mm_ + 1) * 128], cond[:, j, :]) for j in range(KD)],
                     R=[wa_k[b], cond_k], W=[pm_k[b]])
            K.op(DVE, lambda e, b=b, g=g: e.tensor_tensor(out=modT[:, g * 4:(g + 1) * 4, :], in0=pm[b][:],
                                                          in1=bc(bada[:, g * 4:(g + 1) * 4].unsqueeze(2), [128, 4, NSEQ]), op=ALU.add),
                 R=[pm_k[b], bada_k], W=[mod_k])
        for (Sx, gv, gk, off) in ((S1, gmix, gmix_k, 8), (S2, gffn, gffn_k, 32)):
            K.op(DVE, lambda e, Sx=Sx, off=off: e.tensor_scalar(out=Sx[:], in0=modT[:, off:off + 8, :], scalar1=1.0, scalar2=None, op0=ALU.add),
                 R=[mod_k], W=[mod_k])
            K.op(DVE, lambda e, Sx=Sx, gv=gv: e.tensor_tensor(out=Sx[:], in0=Sx[:], in1=bc(gv[:].unsqueeze(2), [128, KD, NSEQ]), op=ALU.mult),
                 R=[mod_k, gk], W=[mod_k])
    dump("modT", modT[:].rearrange("p m b -> p (m b)"), [128, 48 * NSEQ], R=[mod_k])

    ckpt('phaseA')
    catT = K.sb([128, KD, S], BF16, "catT")
    cat_k = [Tok() for _ in range(KD)]
    def bcast_rows(b, bct, bct_k, srcs):
        with K.scope():
            dg = [K.sb([128, 128], F32, "dg") for _ in range(2)]
            dg_k = [Tok(), Tok()]
            pb_ = [K.ps([128, 512], F32, "pb") for _ in range(2)]
            pb_k = [Tok(), Tok()]
            i = 0
            for r, (src, off) in enumerate(srcs):
                for half in range(2):
                    pi = (r * 2 + half) % 2
                    for jj in range(4):
                        j = half * 4 + jj
                        di = i % 2
                        i += 1
                        K.op(DVE, lambda e, di=di, src=src, off=off, j=j: e.tensor_scalar(
                            out=dg[di][:], in0=identf[:], scalar1=src[:, off + j, b:b + 1], scalar2=None, op0=ALU.mult),
                            R=[cst, mod_k], W=[dg_k[di]])
                        K.mm(pb_[pi][:, jj * 128:(jj + 1) * 128], [(onesf[:], dg[di][:])], R=[dg_k[di], cst], W=[pb_k[pi]])
                    K.op(ACT, lambda e, pi=pi, r=r, half=half: e.activation(out=bct[:, r, half * 512:(half + 1) * 512], in_=pb_[pi][:], func=AF.Copy),
                         R=[pb_k[pi]], W=[bct_k])

    for b in range(NSEQ):
        tok0 = b * S
        with K.scope():
            hT = K.sb([128, KD, S], BF16, "hT")
            hT_k = [Tok() for _ in range(NT)]
            with K.scope():
                xt = [K.sb([128, D], F32, "xt") for _ in range(2)]
                xt_k = [Tok(), Tok()]
                xc = [K.dmac("x0"), K.dmac("x1")]
                xn = [K.sb([128, D], BF16, "xn") for _ in range(2)]
                xn_k = [Tok(), Tok()]
                junk = K.sb([128, D], BF16, "junk")
                junk_k = Tok()
                st_ = [K.sb([128, 2], F32, "st") for _ in range(2)]
                st_k = [Tok(), Tok()]
                pt = [K.ps([128, KD, 128], BF16, "pt") for _ in range(2)]
                pt_k = [Tok(), Tok()]
                for i in range(NT):
                    u = i % 2
                    K.dma(SP, xc[u], xt[u][:], x_d[tok0 + i * 128: tok0 + (i + 1) * 128, :], W=[xt_k[u]])
                    K.op(ACT, lambda e, u=u: e.activation(out=junk[:], in_=xt[u][:], func=AF.Square, accum_out=st_[u][:, 0:1]),
                         R=[xt_k[u]], W=[junk_k, st_k[u]])
                    K.op(ACT, lambda e, u=u: e.activation(out=st_[u][:, 1:2], in_=st_[u][:, 0:1], func=AF.Sqrt, scale=1.0 / D, bias=NORM_EPS),
                         R=[st_k[u]], W=[st_k[u]])
                    K.op(DVE, lambda e, u=u: e.reciprocal(out=st_[u][:, 1:2], in_=st_[u][:, 1:2]), R=[st_k[u]], W=[st_k[u]])
                    K.op(DVE, lambda e, u=u: e.tensor_scalar(out=xn[u][:], in0=xt[u][:], scalar1=st_[u][:, 1:2], scalar2=None, op0=ALU.mult),
                         R=[xt_k[u], st_k[u]], W=[xn_k[u]])
                    for j in range(KD):
                        K.tr(pt[u][:, j, :], xn[u][:, j * 128:(j + 1) * 128], identb[:], R=[xn_k[u], cst], W=[pt_k[u]], inc=(j == KD - 1))
                    for j in range(KD):
                        eng = ACT if j % 2 == 0 else DVE
                        if eng is ACT:
                            K.op(ACT, lambda e, j=j, u=u, i=i: e.activation(out=hT[:, j, i * 128:(i + 1) * 128], in_=pt[u][:, j, :], func=AF.Identity,
                                                                             scale=S1[:, j, b:b + 1], bias=modT[:, j, b:b + 1]),
                                 R=[pt_k[u], mod_k], W=[hT_k[i]])
                        else:
                            K.op(DVE, lambda e, j=j, u=u, i=i: e.tensor_scalar(out=hT[:, j, i * 128:(i + 1) * 128], in0=pt[u][:, j, :],
                                                                                scalar1=S1[:, j, b:b + 1], scalar2=modT[:, j, b:b + 1], op0=ALU.mult, op1=ALU.add),
                                 R=[pt_k[u], mod_k], W=[hT_k[i]])
            if b == 0:
                dump("hT", hT[:, 0, :], [128, S], R=hT_k)

            ckpt('B1')
            with K.scope():
                watt = K.sb([128, KD, 768], BF16, "watt")
                watt_k = Tok()
                K.dma(POOL, K.dmac("watt"), watt[:], win_d[:, 0:768].rearrange("(j p) n -> p j n", p=128), W=[watt_k])
                qT = K.sb([128, 4, S], BF16, "qT")
                qT_k = [Tok() for _ in range(NT)]
                kT2 = K.sb([128, 2, S], BF16, "kT2")
                kT_k = [Tok() for _ in range(NT)]
                vaug = K.sb([128, NT, 2, HD + 1], BF16, "vaug")
                v_k = [Tok() for _ in range(NT)]
                K.op(DVE, lambda e: e.memset(vaug[:], 1.0), W=v_k)
                with K.scope():
                    pq = K.ps([128, 512], F32, "pq")
                    pq_k = Tok()
                    pkv = K.ps([128, 256], F32, "pkv")
                    pkv_k = Tok()
                    ptq = K.ps([128, 4, 128], BF16, "ptq")
                    ptq_k = Tok()
                    ptk = K.ps([128, 2, 128], BF16, "ptk")
                    ptk_k = Tok()
                    qk = K.sb([128, 10, HD], F32, "qk")
                    qk_k = Tok()
                    sq = K.sb([128, 10, HD], F32, "sq")
                    sq_k = Tok()
                    ss = K.sb([128, 10], F32, "ss")
                    ss_k = Tok()
                    t1 = K.sb([128, 10, 32], F32, "t1")
                    t2 = K.sb([128, 10, 32], F32, "t2")
                    t3 = K.sb([128, 10, 32], F32, "t3")
                    t4 = K.sb([128, 10, 32], F32, "t4")
                    t_k = [Tok() for _ in range(4)]
                    qr = K.sb([128, 8, HD], BF16, "qr")
                    qr_k = Tok()
                    kr = K.sb([128, 2, 2, HD], BF16, "kr")
                    kr_k = Tok()
                    for i in range(NT):
                        sl = slice(i * 128, (i + 1) * 128)
                        K.mm(pq[:], [(hT[:, j, sl], watt[:, j, 0:512]) for j in range(KD)], R=[hT_k[i], watt_k], W=[pq_k])
                        K.mm(pkv[:], [(hT[:, j, sl], watt[:, j, 512:768]) for j in range(KD)], R=[hT_k[i], watt_k], W=[pkv_k])
                        K.op(ACT, lambda e: e.activation(out=qk[:, 0:8, :].rearrange("p h d -> p (h d)"), in_=pq[:], func=AF.Copy), R=[pq_k], W=[qk_k])
                        K.op(ACT, lambda e: e.activation(out=qk[:, 8:10, :].rearrange("p h d -> p (h d)"), in_=pkv[:, 0:128], func=AF.Copy), R=[pkv_k], W=[qk_k])
                        K.op(ACT, lambda e, i=i: e.activation(out=vaug[:, i, :, 0:HD], in_=pkv[:, 128:256].rearrange("p (g d) -> p g d", g=2), func=AF.Copy),
                             R=[pkv_k], W=[v_k[i]])
                        K.op(DVE, lambda e: e.tensor_tensor(out=sq[:], in0=qk[:], in1=qk[:], op=ALU.mult), R=[qk_k], W=[sq_k])
                        K.op(DVE, lambda e: e.tensor_reduce(out=ss[:], in_=sq[:], axis=AX.X, op=ALU.add), R=[sq_k], W=[ss_k])
                        K.op(ACT, lambda e: e.activation(out=ss[:], in_=ss[:], func=AF.Sqrt, scale=1.0 / HD, bias=NORM_EPS), R=[ss_k], W=[ss_k])
                        K.op(DVE, lambda e: e.reciprocal(out=ss[:], in_=ss[:]), R=[ss_k], W=[ss_k])
                        K.op(DVE, lambda e: e.tensor_tensor(out=qk[:], in0=qk[:], in1=bc(ss[:].unsqueeze(2), [128, 10, HD]), op=ALU.mult),
                             R=[qk_k, ss_k], W=[qk_k])
                        K.op(DVE, lambda e: e.tensor_tensor(out=qk[:], in0=qk[:], in1=gain[:], op=ALU.mult), R=[qk_k, gain_k], W=[qk_k])
                        qv = qk[:].rearrange("p h (k two) -> p h k two", two=2)
                        x0, x1 = qv[:, :, :, 0], qv[:, :, :, 1]
                        cb = bc(cosT[:, i, :].unsqueeze(1), [128, 10, 32])
                        sb_ = bc(sinT[:, i, :].unsqueeze(1), [128, 10, 32])
                        K.op(DVE, lambda e: e.tensor_tensor(out=t1[:], in0=x0, in1=cb, op=ALU.mult), R=[qk_k, cos_k], W=[t_k[0]])
                        K.op(PL, lambda e: e.tensor_tensor(out=t2[:], in0=x1, in1=sb_, op=ALU.mult), R=[qk_k, sin_k], W=[t_k[1]])
                        K.op(DVE, lambda e: e.tensor_tensor(out=t3[:], in0=x0, in1=sb_, op=ALU.mult), R=[qk_k, sin_k], W=[t_k[2]])
                        K.op(PL, lambda e: e.tensor_tensor(out=t4[:], in0=x1, in1=cb, op=ALU.mult), R=[qk_k, cos_k], W=[t_k[3]])
                        qrv = qr[:].rearrange("p h (k two) -> p h k two", two=2)
                        krv = kr[:].rearrange("p g u (k two) -> p g u k two", two=2)
                        K.op(DVE, lambda e: e.tensor_tensor(out=qrv[:, :, :, 0], in0=t1[:, 0:8, :], in1=t2[:, 0:8, :], op=ALU.subtract),
                             R=[t_k[0], t_k[1]], W=[qr_k])
                        K.op(DVE, lambda e: e.tensor_tensor(out=qrv[:, :, :, 1], in0=t3[:, 0:8, :], in1=t4[:, 0:8, :], op=ALU.add),
                             R=[t_k[2], t_k[3]], W=[qr_k])
                        for u_ in range(2):
                            K.op(PL, lambda e, u_=u_: e.tensor_tensor(out=krv[:, :, u_, :, 0], in0=t1[:, 8:10, :], in1=t2[:, 8:10, :], op=ALU.subtract),
                                 R=[t_k[0], t_k[1]], W=[kr_k])
                            K.op(PL, lambda e, u_=u_: e.tensor_tensor(out=krv[:, :, u_, :, 1], in0=t3[:, 8:10, :], in1=t4[:, 8:10, :], op=ALU.add),
                                 R=[t_k[2], t_k[3]], W=[kr_k])
                        for pr in range(4):
                            K.tr(ptq[:, pr, :], qr[:, 2 * pr:2 * pr + 2, :].rearrange("p h d -> p (h d)"), identb[:], R=[qr_k, cst], W=[ptq_k], inc=(pr == 3))
                        K.op(ACT, lambda e, sl=sl: e.activation(out=qT[:, :, sl], in_=ptq[:], func=AF.Copy), R=[ptq_k], W=[qT_k[i]])
                        for g in range(2):
                            K.tr(ptk[:, g, :], kr[:, g, :, :].rearrange("p u d -> p (u d)"), identb[:], R=[kr_k, cst], W=[ptk_k], inc=(g == 1))
                        K.op(DVE, lambda e, sl=sl: e.tensor_copy(out=kT2[:, :, sl], in_=ptk[:]), R=[ptk_k], W=[kT_k[i]])
                if b == 0:
                    dump("qT", qT[:, 0, :], [128, S], R=qT_k)
                    dump("kT", kT2[:, 0, :], [128, S], R=kT_k)
                ckpt('attproj')
                with K.scope():
                    NPS = 2
                    psc = [K.ps([128, QB], F32, "psc") for _ in range(NPS)]
                    psc_k = [Tok() for _ in range(NPS)]
                    pTa = [K.sb([128, NT, QB], BF16, "pTa") for _ in range(2)]
                    pTa_k = [Tok(), Tok()]
                    oacc = [K.ps([128, QT, 128], F32, "oacc") for _ in range(2)]
                    oacc_k = [Tok(), Tok()]
                    rs = K.sb([128, QT], F32, "rs")
                    rs_k = Tok()
                    otm = K.sb([128, QT, 512], BF16, "otm")
                    otm_k = Tok()
                    pto = K.ps([128, 4, 128], BF16, "pto")
                    pto_k = Tok()
                    it = 0
                    for qb in range(NQB):
                        qsl = slice(qb * QB, (qb + 1) * QB)
                        qtoks = qT_k[qb * QT:(qb + 1) * QT]
                        for hq in range(8):
                            g = hq // 4
                            pb0 = 64 * (hq % 2)
                            pr = hq // 2
                            oa = oacc[hq % 2]
                            oa_k = oacc_k[hq % 2]
                            pa = pTa[hq % 2]
                            pa_k = pTa_k[hq % 2]
                            for kt in range(NT):
                                u = it % NPS
                                it += 1
                                K.mm(psc[u][:], [(kT2[pb0:pb0 + 64, g, kt * 128:(kt + 1) * 128], qT[pb0:pb0 + 64, pr, qsl])],
                                     R=[kT_k[kt]] + qtoks, W=[psc_k[u]])
                                K.op(ACT, lambda e, u=u, pa=pa, kt=kt: e.activation(out=pa[:, kt, :], in_=psc[u][:], func=AF.Exp), R=[psc_k[u]], W=[pa_k])
                            for qt in range(QT):
                                K.mm(oa[:, qt, 0:HD + 1], [(pa[:, kt, qt * 128:(qt + 1) * 128], vaug[:, kt, g, :]) for kt in range(NT)],
                                     R=[pa_k] + v_k, W=[oa_k], inc=(qt == QT - 1))
                            K.op(DVE, lambda e, oa=oa: e.reciprocal(out=rs[:], in_=oa[:, :, HD]), R=[oa_k], W=[rs_k])
                            K.op(DVE, lambda e, oa=oa, hq=hq: e.tensor_tensor(out=otm[:, :, hq * HD:(hq + 1) * HD], in0=oa[:, :, 0:HD],
                                                                               in1=bc(rs[:].unsqueeze(2), [128, QT, HD]), op=ALU.mult),
                                 R=[oa_k, rs_k], W=[otm_k])
                        for qt in range(QT):
                            ti = qb * QT + qt
                            for c in range(4):
                                K.tr(pto[:, c, :], otm[:, qt, c * 128:(c + 1) * 128], identb[:], R=[otm_k, cst], W=[pto_k], inc=(c == 3))
                            K.op(ACT, lambda e, ti=ti: e.activation(out=catT[:, 0:4, ti * 128:(ti + 1) * 128], in_=pto[:], func=AF.Copy),
                                 R=[pto_k], W=cat_k[0:4])
            if b == 0:
                dump("oatt", catT[:, 0, :], [128, S], R=cat_k[0:4])

            ckpt('attcore')
            with K.scope():
                NC = NT
                c_ = DECAY
                wrw = [K.sb([128, KD, 128], BF16, "wrw") for _ in range(1)]
                wrw_k = [Tok() for _ in range(1)]
                wrc = [K.dmac("wrw") for _ in range(1)]
                wri = [0]
                T1 = K.sb([128, S + 2], F32, "T1")
                T2 = K.sb([128, S], F32, "T2")
                T3 = K.sb([128, S], F32, "T3")
                T4 = K.sb([128, S], F32, "T4")
                T_k = [Tok() for _ in range(4)]
                r32 = K.sb([128, S], BF16, "r32")
                k32 = K.sb([128, S], F32, "k32")
                kk32 = K.sb([128, S], F32, "kk32")
                yacc = K.sb([128, S], F32, "yacc")
                bacc = K.sb([128, S], BF16, "bacc")
                r_k_, k_k_, kk_k_, ya_k, ba_k = Tok(), Tok(), Tok(), Tok(), Tok()
                twda = K.sb([128, S], BF16, "twda")
                sg = K.sb([128, S], BF16, "sg")
                vb = K.sb([128, S], BF16, "vb")
                gTb = K.sb([128, S], BF16, "gTb")
                sqb = K.sb([128, S], BF16, "sqb")
                twda_k, sg_k, vb_k, gT_k, sqb_k = Tok(), Tok(), Tok(), Tok(), Tok()
                ART = K.sb([128, 2, NC, 2, 128], BF16, "ART")
                BT = K.sb([128, S], BF16, "BT")
                KT = K.sb([128, S], BF16, "KT")
                ART_k, BT_k, KT_k = Tok(), Tok(), Tok()
                gC = K.sb([128, NC], F32, "gC")
                gC_k = Tok()
                ppj = [K.ps([128, 512], F32, "ppj") for _ in range(2)]
                ppj_k = [Tok(), Tok()]
                pji = [0]
                K.op(DVE, lambda e: e.memset(T1[:, 0:1], 0.0), W=[T_k[0]])
                K.op(DVE, lambda e: e.memset(T1[:, S + 1:S + 2], 0.0), W=[T_k[0]])

                def project_shift(m, dst_fn):
                    wi = 0
                    wri[0] += 1
                    K.dma(POOL, wrc[wi], wrw[wi][:], win_d[:, 768 + m * 128: 768 + (m + 1) * 128].rearrange("(j p) n -> p j n", p=128), W=[wrw_k[wi]])
                    for tb in range(NTB):
                        u = pji[0] % 2
                        pji[0] += 1
                        K.mm(ppj[u][:, 0:TB], [(wrw[wi][:, j, :], hT[:, j, tb * TB:(tb + 1) * TB]) for j in range(KD)],
                             R=[wrw_k[wi]] + hT_k[tb * (TB // 128):(tb + 1) * (TB // 128)], W=[ppj_k[u]])
                        K.op(ACT, lambda e, u=u, tb=tb: e.activation(out=T1[:, 1 + tb * TB:1 + (tb + 1) * TB], in_=ppj[u][:, 0:TB], func=AF.Copy),
                             R=[ppj_k[u]], W=[T_k[0]])
                    K.op(PL, lambda e: e.tensor_tensor(out=T2[:], in0=T1[:, 0:S], in1=T1[:, 2:S + 2], op=ALU.add), R=[T_k[0]], W=[T_k[1]])
                    K.op(DVE, lambda e: e.tensor_scalar(out=T2[:], in0=T2[:], scalar1=hmu[:, m:m + 1], scalar2=None, op0=ALU.mult), R=[T_k[1], mu_k], W=[T_k[1]])
                    K.op(DVE, lambda e: e.scalar_tensor_tensor(out=T3[:], in0=T1[:, 1:S + 1], scalar=omm[:, m:m + 1], in1=T2[:], op0=ALU.mult, op1=ALU.add),
                         R=[T_k[0], T_k[1], mu_k], W=[T_k[2]])
                    if b == 0 and m == 4:
                        dump("T1k", T1[:, 0:S], [128, S], R=[T_k[0]])
                        dump("T2k", T2[:], [128, S], R=[T_k[1]])
                        dump("T3k", T3[:], [128, S], R=[T_k[2]])
                        dump("hmu", hmu[:], [128, 14], R=[mu_k])
                        dump("omm", omm[:], [128, 14], R=[mu_k])
                    dst_fn()

                def d12():
                    K.op(ACT, lambda e: e.activation(out=twda[0:64, :], in_=T3[0:64, :], func=AF.Tanh), R=[T_k[2]], W=[twda_k])
                    K.op(DVE, lambda e: e.tensor_copy(out=twda[64:128, :], in_=T3[64:128, :]), R=[T_k[2]], W=[twda_k])
                project_shift(12, d12)

                def d13():
                    K.op(ACT, lambda e: e.activation(out=sg[:], in_=T3[:], func=AF.Sigmoid), R=[T_k[2]], W=[sg_k])
                project_shift(13, d13)

                ckpt('lora')
                pbd = [K.ps([128, 512], F32, "pbd") for _ in range(2)]
                pbd_k = [Tok(), Tok()]
                bdi = [0]
                nck = [0]
                ptk3 = K.ps([128, 3, 128], BF16, "ptk3")
                ptk3_k = Tok()
                pP = K.ps([128, 2, 256], F32, "pP")
                pP_k = Tok()
                pQ = K.ps([128, 2, 128], F32, "pQ")
                pQ_k = Tok()
                pZY = K.ps([128, 512], F32, "pZY")
                pZ = pZY[:, 0:256].rearrange("p (a v) -> p a v", v=64)
                pZY_k = Tok()
                pZ_k = [pZY_k, pZY_k]
                pY = pZY[:, 256:448]
                pY_k = [pZY_k, pZY_k]
                tok3s = [K.sb([128, 3, 2, 128], BF16, "tok3") for _ in range(2)]
                tok3s_k = [Tok(), Tok()]
                for q_ in range(2):
                    K.op(DVE, lambda e, q_=q_: e.memset(tok3s[q_][:], 0.0), W=[tok3s_k[q_]])
                Ub2 = K.sb([128, 2, 128], BF16, "Ub2")
                Ub2_k = Tok()
                K.op(DVE, lambda e: e.memset(Ub2[:], 0.0), W=[Ub2_k])
                Hbd = K.sb([128, 128], BF16, "Hbd")
                Hbd_k = Tok()
                M1s = [K.sb([128, 2, 256], BF16, "M1") for _ in range(2)]
                M2s = [K.sb([128, 2, 256], BF16, "M2") for _ in range(2)]
                M1s_k, M2s_k = [Tok(), Tok()], [Tok(), Tok()]
                XAs = [[K.sb([128, 2, 2, 128], BF16, "XA") for _ in range(2)] for _ in range(2)]
                XAs_k = [[Tok(), Tok()], [Tok(), Tok()]]
                PAs = [[K.sb([128, 2, 128], BF16, "PA") for _ in range(2)] for _ in range(2)]
                PAs_k = [[Tok(), Tok()], [Tok(), Tok()]]
                Zb = K.sb([128, 2, 64], BF16, "Zb")
                Ub = K.sb([128, 2, 64], BF16, "Ub")
                Zb_k, Ub_k = Tok(), Tok()
                H32 = K.sb([128, 64], F32, "H32")
                Hb = K.sb([128, 64], BF16, "Hb")
                Htmp = K.sb([128, 64], F32, "Htmp")
                H_k, Hb_k, Ht_k = Tok(), Tok(), Tok()
                identb2 = bc(identb[:].unsqueeze(1), [128, 2, 128])
                T4b = T4[:].bitcast(BF16)
                if 2 * S >= 3328:
                    tok3s.append(T4b[:, 0:768].rearrange("p (x h c) -> p x h c", x=3, h=2))
                    M1s.append(T4b[:, 768:1280].rearrange("p (h t) -> p h t", h=2))
                    M2s.append(T4b[:, 1280:1792].rearrange("p (h t) -> p h t", h=2))
                    XAs.append([T4b[:, 1792 + i_ * 512:1792 + (i_ + 1) * 512].rearrange("p (h a t) -> p h a t", h=2, a=2) for i_ in range(2)])
                    PAs.append([T4b[:, 2816 + i_ * 256:2816 + (i_ + 1) * 256].rearrange("p (h t) -> p h t", h=2) for i_ in range(2)])
                    tok3s_k.append(Tok()); M1s_k.append(Tok()); M2s_k.append(Tok())
                    XAs_k.append([Tok(), Tok()]); PAs_k.append([Tok(), Tok()])
                    T3b = T3[:].bitcast(BF16)
                    tok3s.append(T3b[:, 0:768].rearrange("p (x h c) -> p x h c", x=3, h=2))
                    M1s.append(T3b[:, 768:1280].rearrange("p (h t) -> p h t", h=2))
                    M2s.append(T3b[:, 1280:1792].rearrange("p (h t) -> p h t", h=2))
                    XAs.append([T3b[:, 1792 + i_ * 512:1792 + (i_ + 1) * 512].rearrange("p (h a t) -> p h a t", h=2, a=2) for i_ in range(2)])
                    PAs.append([T3b[:, 2816 + i_ * 256:2816 + (i_ + 1) * 256].rearrange("p (h t) -> p h t", h=2) for i_ in range(2)])
                    tok3s_k.append(Tok()); M1s_k.append(Tok()); M2s_k.append(Tok())
                    XAs_k.append([Tok(), Tok()]); PAs_k.append([Tok(), Tok()])
                NSETS = len(tok3s)
                pPs = [pP, ppj[1][:].rearrange("p (a t) -> p a t", a=2), ppj[0][:].rearrange("p (a t) -> p a t", a=2)]
                pP_ks = [pP_k, ppj_k[1], ppj_k[0]]
                pQs = [pQ, pbd[1][:, 0:256].rearrange("p (a t) -> p a t", a=2), pbd[0][:, 0:256].rearrange("p (a t) -> p a t", a=2)]
                pQ_ks = [pQ_k, pbd_k[1], pbd_k[0]]
                NPIPE = 3 if NSETS >= 4 else 2

                def bdsum(src_bf, src_k, consume):
                    for tb in range(NTB):
                        u = bdi[0] % 2
                        bdi[0] += 1
                        K.mm(pbd[u][:, 0:TB], [(bdones[:], src_bf[:, tb * TB:(tb + 1) * TB])], R=[src_k, cst], W=[pbd_k[u]])
                        consume(pbd[u][:, 0:TB], pbd_k[u], tb)

                import os as _os
                for c4 in [int(q) for q in _os.environ.get('C4LIST', '0,1,2,3').split(',')]:
                    csl = slice(c4 * 128, (c4 + 1) * 128)
                    project_shift(c4, lambda: K.op(PL, lambda e: e.tensor_copy(out=r32[:], in_=T3[:]), R=[T_k[2]], W=[r_k_]))
                    project_shift(4 + c4, lambda: K.op(PL, lambda e: e.tensor_copy(out=k32[:], in_=T3[:]), R=[T_k[2]], W=[k_k_]))
                    project_shift(8 + c4, lambda: K.op(ACT, lambda e: e.activation(out=vb[:], in_=T3[:], func=AF.Copy), R=[T_k[2]], W=[vb_k]))
                    ckpt('rw_proj%d' % c4)
                    for tb in range(NTB):
                        u = pji[0] % 2
                        pji[0] += 1
                        K.mm(ppj[u][:, 0:TB], [(gup[:, csl], sg[:, tb * TB:(tb + 1) * TB])], R=[wsm_k, sg_k], W=[ppj_k[u]])
                        K.op(ACT, lambda e, u=u, tb=tb: e.activation(out=gTb[:, tb * TB:(tb + 1) * TB], in_=ppj[u][:, 0:TB], func=AF.Copy), R=[ppj_k[u]], W=[gT_k])
                    K.op(DVE, lambda e: e.tensor_scalar(out=kk32[:], in0=k32[:], scalar1=kkp[:, c4:c4 + 1], scalar2=None, op0=ALU.mult), R=[k_k_, kkp_k], W=[kk_k_])
                    K.op(PL, lambda e: e.tensor_tensor(out=sqb[:], in0=kk32[:], in1=kk32[:], op=ALU.mult), R=[kk_k_], W=[sqb_k])

                    def cons_kk(ps_, pk, tb):
                        tsl = slice(tb * TB, (tb + 1) * TB)
                        K.op(ACT, lambda e: e.activation(out=T4[:, tsl], in_=ps_[:], func=AF.Sqrt, bias=1e-24), R=[pk], W=[T_k[3]])
                        K.op(DVE, lambda e: e.reciprocal(out=T4[:, tsl], in_=T4[:, tsl]), R=[T_k[3]], W=[T_k[3]])
                    bdsum(sqb, sqb_k, cons_kk)
                    K.op(DVE, lambda e: e.tensor_tensor(out=kk32[:], in0=kk32[:], in1=T4[:], op=ALU.mult), R=[kk_k_, T_k[3]], W=[kk_k_])
                    if b == 0 and c4 == 0:
                        dump("kk", kk32[:], [128, S], R=[kk_k_])
                        pass

                    ckpt('rw_kk%d' % c4)
                    for d in range(2):
                        lw = T1[:, 1:S + 1]
                        for tb in range(NTB):
                            tsl = slice(tb * TB, (tb + 1) * TB)
                            u = pji[0] % 2
                            pji[0] += 1
                            K.mm(ppj[u][:, 0:TB], [(wup[0:64, d, csl], twda[0:64, tsl])], R=[wsm_k, twda_k], W=[ppj_k[u]])
                            K.op(ACT, lambda e, u=u, tsl=tsl: e.activation(out=lw[:, tsl], in_=ppj[u][:, 0:TB], func=AF.Sigmoid, bias=w0[:, d, c4:c4 + 1]),
                                 R=[ppj_k[u], w0_k], W=[T_k[0]])
                        for n_ in range(NC):
                            K.op(DVE, lambda e, n_=n_: e.tensor_tensor_scan(out=T2[:, n_ * 128:(n_ + 1) * 128], data0=onesf[:], data1=lw[:, n_ * 128:(n_ + 1) * 128],
                                                                        initial=0.0, op0=ALU.mult, op1=ALU.add),
                                 R=[T_k[0], cst], W=[T_k[1]])
                        cs3 = T2[:].rearrange("p (c t) -> p c t", t=128)
                        K.op(ACT, lambda e: e.activation(out=gC[:], in_=cs3[:, :, 127], func=AF.Exp, scale=-c_), R=[T_k[1]], W=[gC_k])
                        if d == 0:
                            K.op(DVE, lambda e: e.tensor_tensor(out=lw, in0=T2[:], in1=lw, op=ALU.subtract), R=[T_k[0], T_k[1]], W=[T_k[0]])
                            gexc, gexc_k, ginc, ginc_k = lw, T_k[0], T2[:], T_k[1]
                        else:
                            K.op(PL, lambda e: e.tensor_copy(out=T4[:].rearrange("p (c t) -> p c t", t=128), in_=bc(cs3[:, :, 127:128], [128, NC, 128])),
                                 R=[T_k[1]], W=[T_k[3]])
                            K.op(DVE, lambda e: e.tensor_tensor(out=T2[:], in0=T4[:], in1=T2[:], op=ALU.subtract), R=[T_k[1], T_k[3]], W=[T_k[1]])
                            K.op(DVE, lambda e: e.tensor_tensor(out=lw, in0=lw, in1=T2[:], op=ALU.add), R=[T_k[0], T_k[1]], W=[T_k[0]])
                            gexc, gexc_k, ginc, ginc_k = T2[:], T_k[1], lw, T_k[0]
                        A3 = [ART[:, 0, :, 0, :], ART[:, 1, :, 0, :]]
                        R3 = [ART[:, 0, :, 1, :], ART[:, 1, :, 1, :]]
                        v3 = lambda ap: ap.rearrange("p (c t) -> p c t", t=128)
                        K.op(ACT, lambda e: e.activation(out=gexc, in_=gexc, func=AF.Exp, scale=-c_), R=[gexc_k], W=[gexc_k])
                        for hh in range(2):
                            K.op(DVE, lambda e, hh=hh: e.scalar_tensor_tensor(out=A3[hh], in0=v3(kk32[:]), scalar=hmask[:, 2 + hh:3 + hh], in1=v3(gexc), op0=ALU.mult, op1=ALU.mult),
                                 R=[kk_k_, gexc_k, cst], W=[ART_k])
                        K.op(ACT, lambda e: e.activation(out=T3[:], in_=ginc, func=AF.Exp, scale=-c_), R=[ginc_k], W=[T_k[2]])
                        for hh in range(2):
                            K.op(DVE, lambda e, hh=hh: e.scalar_tensor_tensor(out=R3[hh], in0=v3(r32[:]), scalar=hmask[:, hh:hh + 1], in1=v3(T3[:]), op0=ALU.mult, op1=ALU.mult),
                                 R=[r_k_, T_k[2], cst], W=[ART_k])
                        K.op(ACT, lambda e: e.activation(out=ginc, in_=ginc, func=AF.Exp, scale=c_), R=[ginc_k], W=[ginc_k])
                        for tb in range(NTB):
                            tsl = slice(tb * TB, (tb + 1) * TB)
                            u = pji[0] % 2
                            pji[0] += 1
                            K.mm(ppj[u][:, 0:TB], [(aup[64:128, d, csl], twda[64:128, tsl])], R=[wsm_k, twda_k], W=[ppj_k[u]])
                            K.op(ACT, lambda e, u=u, tsl=tsl: e.activation(out=T3[:, tsl], in_=ppj[u][:, 0:TB], func=AF.Sigmoid, bias=a0[:, d, c4:c4 + 1]),
                                 R=[ppj_k[u], a0_k], W=[T_k[2]])
                        Tg = gexc
                        K.op(DVE, lambda e: e.tensor_tensor(out=Tg, in0=kk32[:], in1=T3[:], op=ALU.mult), R=[kk_k_, T_k[2], ART_k], W=[gexc_k])
                        K.op(DVE, lambda e: e.tensor_tensor(out=BT[:], in0=Tg, in1=ginc, op=ALU.mult), R=[gexc_k, ginc_k], W=[BT_k])
                        K.op(DVE, lambda e: e.tensor_scalar(out=Tg, in0=T3[:], scalar1=kap[:, c4:c4 + 1], scalar2=omka[:, c4:c4 + 1], op0=ALU.mult, op1=ALU.add),
                             R=[T_k[2], kap_k, BT_k], W=[gexc_k])
                        K.op(DVE, lambda e: e.tensor_tensor(out=Tg, in0=Tg, in1=k32[:], op=ALU.mult), R=[gexc_k, k_k_], W=[gexc_k])
                        K.op(PL, lambda e: e.tensor_tensor(out=KT[:], in0=Tg, in1=ginc, op=ALU.mult), R=[gexc_k, ginc_k], W=[KT_k])
                        K.op(DVE, lambda e: e.scalar_tensor_tensor(out=sqb[:], in0=r32[:], scalar=rkp[:, c4:c4 + 1], in1=Tg, op0=ALU.mult, op1=ALU.mult),
                             R=[r_k_, gexc_k, rkp_k], W=[sqb_k])

                        def cons_b(ps_, pk, tb, d=d):
                            tsl = slice(tb * TB, (tb + 1) * TB)
                            if d == 0:
                                K.op(DVE, lambda e: e.tensor_tensor(out=bacc[:, tsl], in0=ps_[:], in1=vb[:, tsl], op=ALU.mult), R=[pk, vb_k], W=[ba_k])
                            else:
                                K.op(DVE, lambda e: e.tensor_tensor(out=T3[:, tsl], in0=ps_[:], in1=vb[:, tsl], op=ALU.mult), R=[pk, vb_k], W=[T_k[2]])
                                K.op(PL, lambda e: e.tensor_tensor(out=bacc[:, tsl], in0=bacc[:, tsl], in1=T3[:, tsl], op=ALU.add), R=[T_k[2]], W=[ba_k])
                        bdsum(sqb, sqb_k, cons_b)
                        if b == 0 and c4 == 0:
                            dump(f"AT{d}", ART[:, 0, :, 0, :], [128, NC, 128], R=[ART_k])
                            dump(f"BT{d}", BT[:], [128, S], R=[BT_k])

                        ckpt('rw_prep%d_%d' % (c4, d))
                        K.op(DVE, lambda e: e.memset(H32[:], 0.0), W=[H_k])
                        K.op(DVE, lambda e: e.memset(Hb[:], 0.0), W=[Hb_k])
                        K.op(DVE, lambda e: e.memset(Hbd[:], 0.0), W=[Hbd_k])
                        order = range(NC) if d == 0 else range(NC - 1, -1, -1)
                        hs = [slice(0, 64), slice(64, 128)]

                        def prep(n, q, r, d=d):
                            nsl = slice(n * 128, (n + 1) * 128)
                            tk, tk_k = tok3s[q], tok3s_k[q]
                            m1, m1_k, m2, m2_k = M1s[q], M1s_k[q], M2s[q], M2s_k[q]
                            xa, xa_k, pa, pa_k = XAs[q], XAs_k[q], PAs[q], PAs_k[q]
                            pP, pP_k, pQ, pQ_k = pPs[r], pP_ks[r], pQs[r], pQ_ks[r]
                            K.tr(ptk3[:, 0, :], BT[:, nsl], identb[:], R=[BT_k, cst], W=[ptk3_k], inc=False)
                            K.tr(ptk3[:, 1, :], KT[:, nsl], identb[:], R=[KT_k], W=[ptk3_k], inc=False)
                            K.tr(ptk3[:, 2, :], vb[:, nsl], identb[:], R=[vb_k], W=[ptk3_k])
                            for hh in range(2):
                                K.op(ACT if hh == 0 else DVE, (lambda e, hh=hh: e.activation(out=tk[:, :, hh, hh * 64:(hh + 1) * 64], in_=ptk3[:, :, hh * 64:(hh + 1) * 64], func=AF.Copy)) if hh == 0 else
                                     (lambda e, hh=hh: e.tensor_copy(out=tk[:, :, hh, hh * 64:(hh + 1) * 64], in_=ptk3[:, :, hh * 64:(hh + 1) * 64])), R=[ptk3_k], W=[tk_k])
                            yield
                            for hh in range(2):
                                K.mm(pP[:, hh, :], [(BT[:, nsl], ART[:, hh, n, :, :].rearrange("p a t -> p (a t)"))], R=[BT_k, ART_k], W=[pP_k], inc=(hh == 1))
                            for hh in range(2):
                                K.op(DVE, lambda e, hh=hh: e.tensor_tensor(out=m1[:, hh, :], in0=pP[:, hh, :], in1=MP[d][:], op=ALU.mult), R=[pP_k, cst], W=[m1_k])
                            yield
                            for hh in range(2):
                                K.mm(pP[:, hh, :], [(KT[:, nsl], ART[:, hh, n, :, :].rearrange("p a t -> p (a t)"))], R=[KT_k, ART_k], W=[pP_k], inc=(hh == 1))
                            for hh in range(2):
                                K.op(DVE, lambda e, hh=hh: e.tensor_tensor(out=m2[:, hh, :], in0=pP[:, hh, :], in1=MP[d][:], op=ALU.mult), R=[pP_k, cst], W=[m2_k])
                            yield
                            for hh in range(2):
                                K.mm(pQ[:, hh, :], [(ART[:, hh, n, 0, :], BT[:, nsl])], R=[BT_k, ART_k], W=[pQ_k], inc=(hh == 1))
                            for hh in range(2):
                                K.op(DVE, lambda e, hh=hh: e.tensor_tensor(out=pa[0][:, hh, :], in0=pQ[:, hh, :], in1=ML[d][:], op=ALU.mult), R=[pQ_k, cst], W=[pa_k[0]])
                            yield
                            K.op(PL, lambda e: e.tensor_tensor(out=xa[1][:, :, 1, :], in0=m1[:, :, 0:128], in1=identb2, op=ALU.add), R=[m1_k, cst], W=[xa_k[1]])
                            for hh in range(2):
                                K.mm(pP[:, hh, 0:128], [(pa[0][:, hh, :], m1[:, hh, 0:128])], R=[pa_k[0], m1_k], W=[pP_k], inc=(hh == 1))
                            K.op(ACT, lambda e: e.activation(out=xa[1][:, :, 0, :], in_=pP[:, :, 0:128], func=AF.Copy), R=[pP_k], W=[xa_k[1]])
                            for hh in range(2):
                                K.mm(pQ[:, hh, :], [(m1[:, hh, 0:128], pa[0][:, hh, :])], R=[pa_k[0], m1_k], W=[pQ_k], inc=(hh == 1))
                            K.op(ACT, lambda e: e.activation(out=pa[1][:], in_=pQ[:], func=AF.Copy), R=[pQ_k], W=[pa_k[1]])
                            yield
                            cur = 1
                            for lev in range(1, 7):
                                nx = 1 - cur
                                if lev < 6:
                                    for hh in range(2):
                                        K.mm(pP[:, hh, :], [(pa[cur][:, hh, :], xa[cur][:, hh, :, :].rearrange("p a t -> p (a t)"))],
                                             R=[pa_k[cur], xa_k[cur]], W=[pP_k], inc=(hh == 1))
                                    K.op(ACT, lambda e, nx=nx: e.activation(out=xa[nx][:, :, 0, :], in_=pP[:, :, 0:128], func=AF.Copy), R=[pP_k], W=[xa_k[nx]])
                                    K.op(DVE, lambda e, nx=nx, cur=cur: e.tensor_tensor(out=xa[nx][:, :, 1, :], in0=pP[:, :, 128:256], in1=xa[cur][:, :, 1, :], op=ALU.add),
                                         R=[pP_k, xa_k[cur]], W=[xa_k[nx]])
                                    for hh in range(2):
                                        K.mm(pQ[:, hh, :], [(xa[cur][:, hh, 0, :], pa[cur][:, hh, :])], R=[pa_k[cur], xa_k[cur]], W=[pQ_k], inc=(hh == 1))
                                    K.op(ACT, lambda e, nx=nx: e.activation(out=pa[nx][:], in_=pQ[:], func=AF.Copy), R=[pQ_k], W=[pa_k[nx]])
                                else:
                                    for hh in range(2):
                                        K.mm(pP[:, hh, 128:256], [(pa[cur][:, hh, :], xa[cur][:, hh, 1, :])], R=[pa_k[cur], xa_k[cur]], W=[pP_k], inc=(hh == 1))
                                    K.op(DVE, lambda e, nx=nx, cur=cur: e.tensor_tensor(out=xa[nx][:, :, 1, :], in0=pP[:, :, 128:256], in1=xa[cur][:, :, 1, :], op=ALU.add),
                                         R=[pP_k, xa_k[cur]], W=[xa_k[nx]])
                                cur = nx
                                yield
                            assert cur == 1

                        def chain(n, q, d=d):
                            nsl = slice(n * 128, (n + 1) * 128)
                            tk, tk_k = tok3s[q], tok3s_k[q]
                            m1, m1_k, m2, m2_k = M1s[q], M1s_k[q], M2s[q], M2s_k[q]
                            Wf, Wf_k = XAs[q][1], XAs_k[q][1]
                            for hh in range(2):
                                K.mm(pZ[:, hh, :], [(ART[:, hh, n, 0, :], Hb[:, :]), (m2[:, hh, 0:128], tk[:, 2, hh, hs[hh]])],
                                     R=[ART_k, Hb_k, m2_k, tk_k], W=[pZ_k[0]])
                            K.op(ACT, lambda e: e.activation(out=Zb[:], in_=pZ[:, 0:2, :], func=AF.Copy), R=[pZ_k[0]], W=[Zb_k])
                            yield
                            for hh in range(2):
                                K.mm(pZ[:, 2 + hh, :], [(Wf[:, hh, 1, :], Zb[:, hh, :])], R=[Wf_k, Zb_k], W=[pZ_k[1]])
                            K.op(ACT, lambda e: e.activation(out=Ub[:], in_=pZ[:, 2:4, :], func=AF.Copy), R=[pZ_k[1]], W=[Ub_k])
                            for hh in range(2):
                                K.op(DVE, lambda e, hh=hh: e.tensor_copy(out=Ub2[:, hh, hh * 64:(hh + 1) * 64], in_=Ub[:, hh, :]), R=[Ub_k], W=[Ub2_k])
                            yield
                            K.mm(pY[:, 128:192], [(tk[:, 0, 0, :], Ub[:, 0, :]), (tk[:, 0, 1, :], Ub[:, 1, :]),
                                                  (tk[:, 1, 0, :], tk[:, 2, 0, 0:64]), (tk[:, 1, 1, :], tk[:, 2, 1, 64:128])],
                                 R=[tk_k, Ub_k], W=[pY_k[1]])
                            K.mm(pY[:, 0:128], [(Hbd[:], ART[:, 0, n, 1, :]), (Hbd[:], ART[:, 1, n, 1, :]),
                                                (Ub2[:, 0, :], m1[:, 0, 128:256]), (Ub2[:, 1, :], m1[:, 1, 128:256]),
                                                (tk[:, 2, 0, :], m2[:, 0, 128:256]), (tk[:, 2, 1, :], m2[:, 1, 128:256])],
                                 R=[Hbd_k, ART_k, Ub2_k, m1_k, m2_k, tk_k], W=[pY_k[0]])
                            K.op(DVE, lambda e: e.tensor_tensor(out=Htmp[:], in0=pY[:, 128:192], in1=H32[:], op=ALU.add), R=[pY_k[1], H_k], W=[Ht_k])
                            if d == 0:
                                K.op(DVE, lambda e, nsl=nsl: e.tensor_copy(out=yacc[:, nsl], in_=pY[:, 0:128]), R=[pY_k[0]], W=[ya_k])
                            else:
                                K.op(DVE, lambda e, nsl=nsl: e.tensor_tensor(out=yacc[:, nsl], in0=pY[:, 0:128], in1=yacc[:, nsl], op=ALU.add), R=[pY_k[0], ya_k], W=[ya_k])
                            yield
                            K.op(DVE, lambda e, n=n: e.tensor_scalar(out=H32[:], in0=Htmp[:], scalar1=gC[:, n:n + 1], scalar2=None, op0=ALU.mult), R=[Ht_k, gC_k], W=[H_k])
                            K.op(ACT, lambda e, n=n: e.activation(out=Hb[:], in_=Htmp[:], func=AF.Copy, scale=gC[:, n:n + 1]), R=[Ht_k, gC_k], W=[Hb_k])
                            for hh in range(2):
                                K.op(PL, lambda e, hh=hh: e.tensor_copy(out=Hbd[hs[hh], hh * 64:(hh + 1) * 64], in_=H32[hs[hh], :]), R=[H_k], W=[Hbd_k])
                            yield

                        order = list(order)
                        K.barrier()
                        for q_ in range(2, NSETS):
                            K.op(DVE, lambda e, q_=q_: e.memset(tok3s[q_], 0.0), W=[tok3s_k[q_]])
                        nch = len(order)
                        act_preps, done_prep = [], set()
                        next_prep, chain_k, completed, chain_gen = 0, 0, 0, None
                        while chain_k < nch:
                            while len(act_preps) < NPIPE and next_prep < nch and next_prep <= completed + NSETS - 1:
                                act_preps.append((next_prep, prep(order[next_prep], next_prep % NSETS, next_prep % NPIPE)))
                                next_prep += 1
                            if chain_gen is None and chain_k in done_prep:
                                chain_gen = chain(order[chain_k], chain_k % NSETS)
                            if chain_gen is not None:
                                try:
                                    next(chain_gen)
                                except StopIteration:
                                    chain_gen = None
                                    completed += 1
                                    chain_k += 1
                                    nck[0] += 1
                            for it_ in list(act_preps):
                                try:
                                    next(it_[1])
                                except StopIteration:
                                    act_preps.remove(it_)
                                    done_prep.add(it_[0])
                        K.barrier()
                    if b == 0 and c4 == 0:
                        dump("yacc", yacc[:], [128, S], R=[ya_k])
                        dump("bacc", bacc[:], [128, S], R=[ba_k])
                    ckpt('rw_loops%d' % c4)
                    K.op(ACT, lambda e: e.activation(out=sqb[:], in_=yacc[:], func=AF.Copy), R=[ya_k], W=[sqb_k])

                    def cons_m(ps_, pk, tb):
                        tsl = slice(tb * TB, (tb + 1) * TB)
                        K.op(DVE, lambda e: e.scalar_tensor_tensor(out=yacc[:, tsl], in0=ps_[:], scalar=-1.0 / 64, in1=yacc[:, tsl], op0=ALU.mult, op1=ALU.add),
                             R=[pk, ya_k], W=[ya_k])
                    bdsum(sqb, sqb_k, cons_m)
                    K.op(PL, lambda e: e.tensor_tensor(out=sqb[:], in0=yacc[:], in1=yacc[:], op=ALU.mult), R=[ya_k], W=[sqb_k])

                    def cons_v(ps_, pk, tb):
                        tsl = slice(tb * TB, (tb + 1) * TB)
                        K.op(ACT, lambda e: e.activation(out=T4[:, tsl], in_=ps_[:], func=AF.Sqrt, scale=1.0 / 64, bias=GN_EPS), R=[pk], W=[T_k[3]])
                        K.op(DVE, lambda e: e.reciprocal(out=T4[:, tsl], in_=T4[:, tsl]), R=[T_k[3]], W=[T_k[3]])
                    bdsum(sqb, sqb_k, cons_v)
                    K.op(DVE, lambda e: e.tensor_tensor(out=yacc[:], in0=yacc[:], in1=T4[:], op=ALU.mult), R=[ya_k, T_k[3]], W=[ya_k])
                    K.op(DVE, lambda e: e.tensor_scalar(out=yacc[:], in0=yacc[:], scalar1=lnw[:, c4:c4 + 1], scalar2=lnb[:, c4:c4 + 1], op0=ALU.mult, op1=ALU.add),
                         R=[ya_k, lnw_k, lnb_k], W=[ya_k])
                    K.op(PL, lambda e: e.tensor_tensor(out=yacc[:], in0=yacc[:], in1=bacc[:], op=ALU.add), R=[ya_k, ba_k], W=[ya_k])
                    K.op(DVE, lambda e: e.tensor_tensor(out=catT[:, 4 + c4, :], in0=yacc[:], in1=gTb[:], op=ALU.mult), R=[ya_k, gT_k], W=[cat_k[4 + c4]])
                    ckpt('rw_gn%d' % c4)
            if b == 0:
                dump("orw", catT[:, 4, :], [128, S], R=cat_k)

        ckpt('rwkv')
        with K.scope():
            x1 = K.sb([128, NT, D], F32, "x1")
            x1_k = [Tok() for _ in range(NT)]
            h2t = K.sb([128, NT, D], BF16, "h2t")
            h2_k = [Tok() for _ in range(NT)]
            afft = K.sb([128, NT, NE], F32, "afft")
            aff_k = [Tok() for _ in range(NT)]
            posm = K.sb([16, S], F32, "posm")
            posm_k = Tok()
            post = K.sb([128, NT, NE], F32, "post")
            post_k = Tok()
            gt2b = K.sb([128, 1, D], F32, "gt2b")
            gt2b_k = Tok()
            bcast_rows(b, gt2b, gt2b_k, [(modT, 40)])
            K.stacks.append(ExitStack())
            affT = K.sb([16, S], F32, "affT")
            affT_k = Tok()
            with K.scope():
                bct = K.sb([128, 3, D], F32, "bct")
                bct_k = Tok()
                bcast_rows(b, bct, bct_k, [(modT, 16), (S2, 0), (modT, 24)])
                wo = K.sb([128, KD, D], BF16, "wo")
                wo_k = Tok()
                K.dma(POOL, K.dmac("wo"), wo[:], wout_d.rearrange("(j p) n -> p j n", p=128), W=[wo_k])
                xc = [K.dmac("x0")]
                xt = [K.sb([128, D], F32, "xt")]
                xt_k = [Tok()]
                po = [K.ps([128, 512], F32, "po") for _ in range(2)]
                po_k = [Tok(), Tok()]
                st_ = [K.sb([128, 4], F32, "st") for _ in range(2)]
                st_k = [Tok(), Tok()]
                pt = [K.ps([128, KD, 128], BF16, "pt") for _ in range(2)]
                pt_k = [Tok(), Tok()]
                h2T = [K.sb([128, KD, 128], BF16, "h2T") for _ in range(2)]
                h2T_k = [Tok(), Tok()]
                plg = K.ps([128, NE], F32, "plg")
                plg_k = Tok()
                lg = K.sb([128, NE], F32, "lg")
                lg_k = Tok()
                paT = K.ps([16, 128], F32, "paT")
                paT_k = Tok()
                for i in range(NT):
                    u = i % 2
                    sl = slice(i * 128, (i + 1) * 128)
                    K.dma(SP, xc[0], xt[0][:], x_d[tok0 + i * 128: tok0 + (i + 1) * 128, :], W=[xt_k[0]])
                    for half in range(2):
                        hsl = slice(half * 512, (half + 1) * 512)
                        K.mm(po[half][:], [(catT[:, j, sl], wo[:, j, hsl]) for j in range(KD)], R=cat_k + [wo_k], W=[po_k[half]])
                        K.op(DVE, lambda e, half=half, hsl=hsl, i=i: e.tensor_tensor(out=x1[:, i, hsl], in0=po[half][:], in1=bct[:, 0, hsl], op=ALU.mult),
                             R=[po_k[half], bct_k], W=[x1_k[i]])
                    K.op(PL, lambda e, i=i: e.tensor_tensor(out=x1[:, i, :], in0=x1[:, i, :], in1=xt[0][:], op=ALU.add), R=[x1_k[i], xt_k[0]], W=[x1_k[i]])
                    K.op(ACT, lambda e, u=u, i=i: e.activation(out=h2T[u][:].rearrange("p j t -> p (j t)"), in_=x1[:, i, :], func=AF.Square, accum_out=st_[u][:, 0:1]),
                         R=[x1_k[i]], W=[h2T_k[u], st_k[u]])
                    K.op(ACT, lambda e, u=u: e.activation(out=st_[u][:, 1:2], in_=st_[u][:, 0:1], func=AF.Sqrt, scale=1.0 / D, bias=NORM_EPS), R=[st_k[u]], W=[st_k[u]])
                    K.op(DVE, lambda e, u=u: e.reciprocal(out=st_[u][:, 1:2], in_=st_[u][:, 1:2]), R=[st_k[u]], W=[st_k[u]])
                    K.op(DVE, lambda e, u=u, i=i: e.scalar_tensor_tensor(out=h2t[:, i, :], in0=x1[:, i, :], scalar=st_[u][:, 1:2], in1=bct[:, 1, :], op0=ALU.mult, op1=ALU.mult),
                         R=[x1_k[i], st_k[u], bct_k], W=[h2_k[i]])
                    K.op(PL, lambda e, i=i: e.tensor_tensor(out=h2t[:, i, :], in0=h2t[:, i, :], in1=bct[:, 2, :], op=ALU.add), R=[h2_k[i], bct_k], W=[h2_k[i]])
                    for j in range(KD):
                        K.tr(pt[u][:, j, :], h2t[:, i, j * 128:(j + 1) * 128], identb[:], R=[h2_k[i], cst], W=[pt_k[u]], inc=(j == KD - 1))
                    K.op(ACT, lambda e, u=u: e.activation(out=h2T[u][:], in_=pt[u][:], func=AF.Copy), R=[pt_k[u]], W=[h2T_k[u]])
                    K.mm(plg[:], [(h2T[u][:, j, :], wrt[:, j, :]) for j in range(KD)], R=[h2T_k[u], wsm_k], W=[plg_k])
                    K.op(DVE, lambda e, u=u: e.tensor_reduce(out=st_[u][:, 2:3], in_=plg[:], axis=AX.X, op=ALU.max), R=[plg_k], W=[st_k[u]])
                    K.op(DVE, lambda e, u=u: e.tensor_scalar(out=st_[u][:, 2:3], in0=st_[u][:, 2:3], scalar1=-1.0, scalar2=None, op0=ALU.mult), R=[st_k[u]], W=[st_k[u]])
                    K.op(ACT, lambda e, u=u: e.activation(out=lg[:], in_=plg[:], func=AF.Exp, bias=st_[u][:, 2:3], accum_out=st_[u][:, 3:4]),
                         R=[plg_k, st_k[u]], W=[lg_k, st_k[u]])
                    K.op(DVE, lambda e, u=u: e.reciprocal(out=st_[u][:, 3:4], in_=st_[u][:, 3:4]), R=[st_k[u]], W=[st_k[u]])
                    K.op(DVE, lambda e, u=u, i=i: e.tensor_scalar(out=afft[:, i, :], in0=lg[:], scalar1=st_[u][:, 3:4], scalar2=None, op0=ALU.mult),
                         R=[lg_k, st_k[u]], W=[aff_k[i]])
                    K.tr(paT[:], afft[:, i, :], identf[:], R=[aff_k[i], cst], W=[paT_k])
                    K.op(DVE, lambda e, sl=sl: e.tensor_copy(out=affT[:, sl], in_=paT[:]), R=[paT_k], W=[affT_k])
            if b == 0:
                dump("x1", x1[:, 0, :], [128, D], R=x1_k)
                dump("affT", affT[:], [16, S], R=[affT_k])

            ckpt('outproj')
            with K.scope():
                wk = [K.sb([16, S], F32, "wk") for _ in range(2)]
                wk_k = [Tok(), Tok()]
                m8 = K.sb([16, 8], F32, "m8")
                m8_k = Tok()
                mk_ = K.sb([16, S], F32, "mk")
                mk_k = Tok()
                ppo = K.ps([128, NE], F32, "ppo")
                ppo_k = Tok()
                K.op(DVE, lambda e: e.tensor_copy(out=wk[0][:], in_=affT[:]), R=[affT_k], W=[wk_k[0]])
                nit = CAP // 8
                cur = 0
                for it in range(nit):
                    K.op(DVE, lambda e, cur=cur: e.max(out=m8[:], in_=wk[cur][:]), R=[wk_k[cur]], W=[m8_k])
                    if it < nit - 1:
                        K.op(DVE, lambda e, cur=cur: e.match_replace(out=wk[1 - cur][:], in_to_replace=m8[:], in_values=wk[cur][:], imm_value=-1.0),
                             R=[wk_k[cur], m8_k], W=[wk_k[1 - cur]])
                        cur = 1 - cur
                K.op(DVE, lambda e: e.tensor_scalar(out=mk_[:], in0=affT[:], scalar1=m8[:, 7:8], scalar2=None, op0=ALU.is_ge), R=[affT_k, m8_k], W=[mk_k])
                K.op(DVE, lambda e: e.tensor_tensor_scan(out=posm[:], data0=onesf[0:16, 0:1].to_broadcast([16, S]), data1=mk_[:], initial=0.0, op0=ALU.mult, op1=ALU.add),
                     R=[mk_k, cst], W=[posm_k])
                K.op(DVE, lambda e: e.tensor_tensor(out=posm[:], in0=posm[:], in1=mk_[:], op=ALU.mult), R=[posm_k, mk_k], W=[posm_k])
                K.op(DVE, lambda e: e.tensor_scalar(out=posm[:], in0=posm[:], scalar1=-1.0, scalar2=None, op0=ALU.add), R=[posm_k], W=[posm_k])
                for i in range(NT):
                    K.tr(ppo[:], posm[:, i * 128:(i + 1) * 128], identf[0:16, 0:16], R=[posm_k, cst], W=[ppo_k])
                    K.op(DVE, lambda e, i=i: e.tensor_copy(out=post[:, i, :], in_=ppo[:]), R=[ppo_k], W=[post_k])
            if b == 0:
                dump("posm", posm[:], [16, S], R=[posm_k])

            K.barrier()
            K.stacks.pop().close()
            ckpt('topk')
            with K.scope():
                NWS = 6
                if S >= 2048:
                    wsl = [catT[:, :, q * 512:(q + 1) * 512] for q in range(4)]
                    wsl += [K.sb([128, KD, 512], BF16, "wsl") for _ in range(NWS - 4)]
                else:
                    wsl = [K.sb([128, KD, 512], BF16, "wsl") for _ in range(NWS)]
                wsl_k = [Tok() for _ in range(NWS)]
                wsc_ = [K.dmac("wsl") for _ in range(NWS)]
                Sel = K.sb([128, NT, CAP], BF16, "Sel")
                Sel_k = Tok()
                SelT = K.sb([128, NCT, S], BF16, "SelT")
                SelT_k = Tok()
                hgT = K.sb([128, KD, CAP], BF16, "hgT")
                hgT_k = Tok()
                hidT = K.sb([128, KD, CAP], BF16, "hidT")
                hid_k = Tok()
                sgt = K.sb([128, CAP], F32, "sgt")
                sgt_k = Tok()
                ysb = K.sb([128, NCT, D], BF16, "ysb")
                ysb_k = Tok()
                ppb = K.ps([128, TB], F32, "ppb")
                ppb_k = Tok()
                pg = K.ps([128, CAP], F32, "pg")
                pg_k = Tok()
                pu = K.ps([128, CAP], F32, "pu")
                pu_k = Tok()
                ph = [K.ps([128, CAP], F32, "ph") for _ in range(2)]
                ph_k = [Tok(), Tok()]
                py = [K.ps([128, 512], F32, "py") for _ in range(2)]
                py_k = [Tok(), Tok()]
                wcount = [0]
                ohe = K.sb([16, 128], F32, "ohe")
                ohe_k = Tok()

                def wload(src_d, e_, half):
                    s = wcount[0] % NWS
                    wcount[0] += 1
                    K.dma(POOL, wsc_[s], wsl[s][:], src_d[e_, :, half * 512:(half + 1) * 512].rearrange("(j p) n -> p j n", p=128), W=[wsl_k[s]])
                    return s

                for e_ in range(NE):
                    sg0 = wload(wg_d, e_, 0)
                    sg1 = wload(wg_d, e_, 1)
                    su0 = wload(wu_d, e_, 0)
                    su1 = wload(wu_d, e_, 1)
                    for i in range(NT):
                        K.op(DVE, lambda e, i=i, e_=e_: e.tensor_scalar(out=Sel[:, i, :], in0=iotac[:, 0:CAP], scalar1=post[:, i, e_:e_ + 1], scalar2=None, op0=ALU.is_equal),
                             R=[post_k, cst], W=[Sel_k])
                    K.op(DVE, lambda e, e_=e_: e.tensor_copy(out=ohe[:], in_=bc(identf[0:16, e_:e_ + 1], [16, 128])), R=[cst], W=[ohe_k])
                    for tb in range(NTB):
                        tsl = slice(tb * TB, (tb + 1) * TB)
                        K.mm(ppb[:], [(ohe[:], posm[:, tsl])], R=[posm_k, ohe_k], W=[ppb_k])
                        for ct in range(NCT):
                            K.op(DVE, lambda e, ct=ct, tsl=tsl: e.tensor_scalar(out=SelT[:, ct, tsl], in0=ppb[:], scalar1=iotap[:, ct:ct + 1], scalar2=None, op0=ALU.is_equal),
                                 R=[ppb_k, cst], W=[SelT_k])
                    for fc in range(KD):
                        u = fc % 2
                        K.mm(ph[u][:], [(h2t[:, i, fc * 128:(fc + 1) * 128], Sel[:, i, :]) for i in range(NT)], R=h2_k + [Sel_k], W=[ph_k[u]])
                        K.op(ACT if u == 0 else DVE, (lambda e, u=u, fc=fc: e.activation(out=hgT[:, fc, :], in_=ph[u][:], func=AF.Copy)) if u == 0 else
                             (lambda e, u=u, fc=fc: e.tensor_copy(out=hgT[:, fc, :], in_=ph[u][:])), R=[ph_k[u]], W=[hgT_k])
                    for fc in range(KD):
                        gs = sg0 if fc < 4 else sg1
                        us = su0 if fc < 4 else su1
                        fo = (fc % 4) * 128
                        K.mm(pg[:], [(wsl[gs][:, j, fo:fo + 128], hgT[:, j, :]) for j in range(KD)], R=[wsl_k[gs], hgT_k], W=[pg_k])
                        K.mm(pu[:], [(wsl[us][:, j, fo:fo + 128], hgT[:, j, :]) for j in range(KD)], R=[wsl_k[us], hgT_k], W=[pu_k])
                        K.op(ACT, lambda e: e.activation(out=sgt[:], in_=pg[:], func=AF.Silu), R=[pg_k], W=[sgt_k])
                        K.op(DVE, lambda e, fc=fc: e.tensor_tensor(out=hidT[:, fc, :], in0=pu[:], in1=sgt[:], op=ALU.mult), R=[pu_k, sgt_k], W=[hid_k])
                    sd0 = wload(wd_d, e_, 0)
                    sd1 = wload(wd_d, e_, 1)
                    for ct in range(NCT):
                        for half in range(2):
                            ds_ = sd0 if half == 0 else sd1
                            hsl = slice(half * 512, (half + 1) * 512)
                            K.mm(py[half][0:CP, :], [(hidT[:, fc, ct * 128:ct * 128 + CP], wsl[ds_][:, fc, :]) for fc in range(KD)], R=[hid_k, wsl_k[ds_]], W=[py_k[half]])
                            K.op(DVE, lambda e, ct=ct, half=half, hsl=hsl: e.tensor_tensor(out=ysb[0:CP, ct, hsl], in0=py[half][0:CP, :], in1=gt2b[0:CP, 0, hsl], op=ALU.mult),
                                 R=[py_k[half], gt2b_k], W=[ysb_k])
                    for i in range(NT):
                        sl = slice(i * 128, (i + 1) * 128)
                        for half in range(2):
                            hsl = slice(half * 512, (half + 1) * 512)
                            K.mm(py[half][:], [(SelT[0:CP, ct, sl], ysb[0:CP, ct, hsl]) for ct in range(NCT)], R=[SelT_k, ysb_k], W=[py_k[half]])
                            K.op(DVE, lambda e, i=i, half=half, hsl=hsl, e_=e_: e.scalar_tensor_tensor(out=x1[:, i, hsl], in0=py[half][:], scalar=afft[:, i, e_:e_ + 1],
                                                                                                    in1=x1[:, i, hsl], op0=ALU.mult, op1=ALU.add),
                                 R=[py_k[half], aff_k[i], x1_k[i]], W=[x1_k[i]])
                for i in range(NT):
                    K.dma(SP, outc, out_d[tok0 + i * 128: tok0 + (i + 1) * 128, :], x1[:, i, :], R=[x1_k[i]])
    K.barrier()
    K.stacks[0].close()
    return nc, dump_d


def rope_tables(S):
    rows = S // 64
    row = np.repeat(np.arange(rows, dtype=np.float32), 64)
    col = np.tile(np.arange(64, dtype=np.float32), rows)
    freqs = (np.float32(10000.0) ** (-np.arange(16, dtype=np.float32) / np.float32(16))).astype(np.float32)
    ang = np.concatenate([row[:, None] * freqs, col[:, None] * freqs], axis=-1).astype(np.float32)
    return np.cos(ang).astype(np.float32), np.sin(ang).astype(np.float32)


def fm(v, n):
    return np.ascontiguousarray(np.asarray(v, np.float32).reshape(n, 128).T)


def make_in_maps(inputs, S, NSEQ, ncores):
    f = lambda a: np.ascontiguousarray(np.asarray(a, np.float32))
    NT = S // 128
    cos, sin = rope_tables(S)
    cosl = np.ascontiguousarray(cos.reshape(NT, 128, 32).transpose(1, 0, 2))
    sinl = np.ascontiguousarray(sin.reshape(NT, 128, 32).transpose(1, 0, 2))
    x = f(inputs["x"])
    c = f(inputs["c"])
    shared = {
        "w_ada": f(inputs["w_ada"][0]),
        "b_ada": fm(inputs["b_ada"][0], 48),
        "g_mix": fm(inputs["g_mix"][0], 8),
        "g_ffn": fm(inputs["g_ffn"][0], 8),
        "w_in": f(inputs["w_in"][0]),
        "q_norm": f(inputs["q_norm"][0]).reshape(1, 64),
        "k_norm": f(inputs["k_norm"][0]).reshape(1, 64),
        "mu": fm(inputs["mu_shift"][0], 14),
        "w0": np.ascontiguousarray(f(inputs["w0"][0]).reshape(2, 4, 128).transpose(2, 0, 1)),
        "a0": np.ascontiguousarray(f(inputs["a0"][0]).reshape(2, 4, 128).transpose(2, 0, 1)),
        "w_up": f(inputs["w_up"][0]),
        "a_up": f(inputs["a_up"][0]),
        "g_up": f(inputs["g_up"][0]),
        "k_k": fm(inputs["k_k"][0], 4),
        "k_a": fm(inputs["k_a"][0], 4),
        "r_k": fm(f(inputs["r_k"][0]).reshape(-1), 4),
        "ln_w": fm(inputs["ln_w"][0], 4),
        "ln_b": fm(inputs["ln_b"][0], 4),
        "w_out": f(inputs["w_out"][0]),
        "w_router": f(inputs["w_router"][0]),
        "w_gate": f(inputs["w_gate"][0]),
        "w_up_e": f(inputs["w_up_e"][0]),
        "w_down": f(inputs["w_down"][0]),
        "cos": cosl,
        "sin": sinl,
    }
    maps = []
    for i in range(ncores):
        m = dict(shared)
        m["x"] = np.ascontiguousarray(x[i * NSEQ:(i + 1) * NSEQ].reshape(NSEQ * S, D))
        cc = c[i * NSEQ:(i + 1) * NSEQ]
        m["cT"] = np.ascontiguousarray(cc.reshape(NSEQ, KD, 128).transpose(2, 1, 0))
        maps.append(m)
    return maps


def kernel(**inputs):
    x = np.asarray(inputs["x"])
    B, S, _ = x.shape
    ncores = 8
    NSEQ = B // ncores
    nc, _ = build(S=S, NSEQ=NSEQ)
    maps = make_in_maps(inputs, S, NSEQ, ncores)
    res = run_bass_kernel_spmd(nc, maps, core_ids=list(range(ncores)))
    outs = [np.asarray(r["out"]).reshape(NSEQ, S, D) for r in res.results]
    return np.concatenate(outs, axis=0).astype(np.float32)
```

```python
import numpy as np
from contextlib import ExitStack, contextmanager
import concourse.bass as bass
import concourse.mybir as mybir
from concourse.bass_utils import run_bass_kernel_spmd

F32 = mybir.dt.float32
BF16 = mybir.dt.bfloat16
AF = mybir.ActivationFunctionType
ALU = mybir.AluOpType
AX = mybir.AxisListType

D = 1024
KD = 8
HD = 64
NE = 16
DECAY = 0.606531
GN_EPS = 64e-5
NORM_EPS = 1e-6
N_IN = 2560
REBASE_T = 3000
BAR_EVERY = 4


class Tok:
    __slots__ = ("w", "r")

    def __init__(self):
        self.w = None
        self.r = {}


class Cnt:
    def __init__(self, sem, incv, eng=None, name=""):
        self.sem = sem
        self.incv = incv
        self.cnt = 0
        self.eng = eng
        self.seen = {}
        self.name = name
        self.gen = 0


class Ctx:
    def __init__(self, nc):
        self.nc = nc
        self.stacks = [ExitStack()]
        self.uid = 0
        self.allc = []
        mk = self._mkc
        self.PE = mk(nc.tensor, 1, "pe")
        self.ACT = mk(nc.scalar, 1, "act")
        self.DVE = mk(nc.vector, 1, "dve")
        self.POOL = mk(nc.gpsimd, 1, "pool")
        self.SP = Cnt(None, 0, nc.sync, "sp")
        self.dma_free = []
        self.outc = []

    def _mkc(self, eng, incv, name):
        sem = self.stacks[0].enter_context(self.nc.semaphore(f"s_{name}_{self.uid}"))
        self.uid += 1
        c = Cnt(sem, incv, eng, name)
        self.allc.append(c)
        return c

    def dmac(self, name="d"):
        return self._mkc(None, 16, name)

    def nm(self, s):
        self.uid += 1
        return f"{s}_{self.uid}"

    def sb(self, shape, dt, name="t"):
        return self.stacks[-1].enter_context(self.nc.sbuf_tensor(self.nm(name), list(shape), dt))

    def ps(self, shape, dt=F32, name="p"):
        return self.stacks[-1].enter_context(self.nc.psum_tensor(self.nm(name), list(shape), dt))

    @contextmanager
    def scope(self):
        self.stacks.append(ExitStack())
        try:
            yield
        finally:
            self.barrier()
            self.stacks.pop().close()

    def barrier(self):
        for e in (self.PE, self.ACT, self.DVE, self.POOL, self.SP):
            for f in self.allc:
                if f.cnt > 0 and e.seen.get(f, 0) < f.cnt:
                    e.eng.wait_ge(f.sem, f.cnt * f.incv)
                    e.seen[f] = f.cnt
        for f in (self.PE, self.ACT, self.DVE, self.POOL):
            if f.cnt > REBASE_T:
                f.sem = self.stacks[0].enter_context(self.nc.semaphore(f"s_{f.name}_rb{self.uid}"))
                self.uid += 1
                f.cnt = 0
                f.gen += 1
                for e in (self.PE, self.ACT, self.DVE, self.POOL, self.SP):
                    e.seen.pop(f, None)

    def op(self, e, fn, R=(), W=(), comp=None, inc=True):
        comp = comp or e
        need = {}
        for t in R:
            if t.w is not None:
                f, c, g = t.w
                if g == f.gen and c > need.get(f, 0):
                    need[f] = c
        for t in W:
            if t.w is not None:
                f, c, g = t.w
                if g == f.gen and c > need.get(f, 0):
                    need[f] = c
            for f, (c, g) in t.r.items():
                if g == f.gen and c > need.get(f, 0):
                    need[f] = c
        for f, c in need.items():
            if f is e and e is self.PE:
                continue
            if e.seen.get(f, 0) < c:
                e.eng.wait_ge(f.sem, c * f.incv)
                e.seen[f] = c
        ins = fn(e.eng)
        if inc:
            comp.cnt += 1
            ins.then_inc(comp.sem, comp.incv)
            cc = comp.cnt
        else:
            cc = comp.cnt + 1
        for t in R:
            pr = t.r.get(comp)
            if pr is None or pr[1] != comp.gen or pr[0] < cc:
                t.r[comp] = (cc, comp.gen)
        for t in W:
            t.w = (comp, cc, comp.gen)
            t.r = {}
        return ins

    def mm(self, out, pairs, R=(), W=(), start=True, stop=True, inc=True):
        n = len(pairs)
        for i, (l, r) in enumerate(pairs):
            last = i == n - 1
            self.op(self.PE,
                    lambda e, l=l, r=r, i=i, last=last: e.matmul(out, lhsT=l, rhs=r, start=(start and i == 0), stop=(stop and last)),
                    R=R if i == 0 else (), W=W, inc=(inc and last))

    def tr(self, out, in_, ident, R=(), W=(), inc=True):
        self.op(self.PE, lambda e: e.transpose(out, in_, ident), R=R, W=W, inc=inc)

    def dma(self, issuer, comp, out, in_, R=(), W=()):
        self.op(issuer, lambda e: e.dma_start(out=out, in_=in_), R=R, W=W, comp=comp)


def bc(ap, shape):
    return ap.to_broadcast(list(shape))


class _Stop(Exception):
    pass


def build(S=2048, NSEQ=4, dumps=(), stop=None):
    try:
        return _build(S, NSEQ, dumps, stop)
    except _Stop as ex:
        ex.args[1].barrier()
        ex.args[1].stacks[0].close()
        return ex.args[0]


def _build(S, NSEQ, dumps, stop):
    NT = S // 128
    QB = min(512, S)
    NQB = S // QB
    QT = QB // 128
    CAP = 2 * S // NE
    NCT = max(1, CAP // 128)
    CP = min(CAP, 128)
    TB = min(512, S)
    NTB = S // TB
    TOKS = NSEQ * S
    nc = bass.Bass("TRN2", target_bir_lowering=False)
    K = Ctx(nc)

    def din(name, shape):
        return nc.dram_tensor(name, list(shape), F32, kind="ExternalInput").ap()

    x_d = din("x", [TOKS, D])
    cT_d = din("cT", [128, KD, NSEQ])
    wada_d = din("w_ada", [D, 6 * D])
    bada_d = din("b_ada", [128, 48])
    gmix_d = din("g_mix", [128, KD])
    gffn_d = din("g_ffn", [128, KD])
    win_d = din("w_in", [D, N_IN])
    qn_d = din("q_norm", [1, HD])
    kn_d = din("k_norm", [1, HD])
    mu_d = din("mu", [128, 14])
    w0_d = din("w0", [128, 2, 4])
    a0_d = din("a0", [128, 2, 4])
    wup_d = din("w_up", [2, 64, 512])
    aup_d = din("a_up", [2, 64, 512])
    gup_d = din("g_up", [128, 512])
    kk_d = din("k_k", [128, 4])
    ka_d = din("k_a", [128, 4])
    rk_d = din("r_k", [128, 4])
    lnw_d = din("ln_w", [128, 4])
    lnb_d = din("ln_b", [128, 4])
    wout_d = din("w_out", [D, D])
    wr_d = din("w_router", [D, NE])
    wg_d = din("w_gate", [NE, D, D])
    wu_d = din("w_up_e", [NE, D, D])
    wd_d = din("w_down", [NE, D, D])
    cos_d = din("cos", [128, NT, 32])
    sin_d = din("sin", [128, NT, 32])
    out_d = nc.dram_tensor("out", [TOKS, D], F32, kind="ExternalOutput").ap()
    dump_d = {}

    PE, ACT, DVE, POOL, SP = K.PE, K.ACT, K.DVE, K.POOL, K.SP
    import os as _os0
    PL = POOL if _os0.environ.get('POOLC', '1') == '1' else DVE
    outc = K.dmac("outc")

    def ckpt(name):
        if stop == name:
            raise _Stop((nc, dump_d), K)

    def dump(name, src_ap, shape, R=()):
        if name not in dumps:
            return
        dd = nc.dram_tensor("dump_" + name, list(shape), F32, kind="ExternalOutput").ap()
        dump_d[name] = dd
        tmp = K.sb(shape, F32, "dmp")
        tk = Tok()
        K.op(DVE, lambda e: e.tensor_copy(out=tmp[:], in_=src_ap), R=R, W=[tk])
        K.dma(SP, outc, dd, tmp[:], R=[tk])

    cst = Tok()
    identf = K.sb([128, 128], F32, "identf")
    identb = K.sb([128, 128], BF16, "identb")
    onesf = K.sb([128, 128], F32, "onesf")
    bdones = K.sb([128, 128], BF16, "bdones")
    MP = [K.sb([128, 256], BF16, "MP0"), K.sb([128, 256], BF16, "MP1")]
    ML = [K.sb([128, 128], BF16, "ML0"), K.sb([128, 128], BF16, "ML1")]
    iotac = K.sb([128, 256], F32, "iotac")
    iotap = K.sb([128, 2], F32, "iotap")
    hmask = K.sb([128, 4], F32, "hmask")

    K.op(POOL, lambda e: e.memset(onesf[:], 1.0), W=[cst])
    K.stacks.append(ExitStack())
    mUPs = K.sb([128, 128], F32, "mUPs")
    mUPi = K.sb([128, 128], F32, "mUPi")
    mLOs = K.sb([128, 128], F32, "mLOs")
    mLOi = K.sb([128, 128], F32, "mLOi")
    def aff(dst, base, cm, step, cmp):
        K.op(POOL, lambda e: e.affine_select(out=dst[:], in_=onesf[:], pattern=[[step, 128]], compare_op=cmp,
                                             fill=0.0, base=base, channel_multiplier=cm), R=[cst], W=[cst])
    aff(identf, 0, 1, -1, ALU.is_equal)
    aff(mUPs, 0, -1, 1, ALU.is_gt)
    aff(mUPi, 0, -1, 1, ALU.is_ge)
    aff(mLOs, 0, 1, -1, ALU.is_gt)
    aff(mLOi, 0, 1, -1, ALU.is_ge)
    K.op(DVE, lambda e: e.tensor_copy(out=identb[:], in_=identf[:]), R=[cst], W=[cst])
    K.op(DVE, lambda e: e.memset(bdones[:], 0.0), W=[cst])
    K.op(DVE, lambda e: e.memset(bdones[0:64, 0:64], 1.0), W=[cst])
    K.op(DVE, lambda e: e.memset(bdones[64:128, 64:128], 1.0), W=[cst])
    K.op(DVE, lambda e: e.tensor_copy(out=MP[0][:, 0:128], in_=mUPs[:]), R=[cst], W=[cst])
    K.op(DVE, lambda e: e.tensor_copy(out=MP[0][:, 128:256], in_=mUPi[:]), R=[cst], W=[cst])
    K.op(DVE, lambda e: e.tensor_copy(out=MP[1][:, 0:128], in_=mLOs[:]), R=[cst], W=[cst])
    K.op(DVE, lambda e: e.tensor_copy(out=MP[1][:, 128:256], in_=mLOi[:]), R=[cst], W=[cst])
    K.op(DVE, lambda e: e.tensor_copy(out=ML[0][:], in_=mLOs[:]), R=[cst], W=[cst])
    K.op(DVE, lambda e: e.tensor_copy(out=ML[1][:], in_=mUPs[:]), R=[cst], W=[cst])
    K.barrier()
    K.stacks.pop().close()
    K.op(POOL, lambda e: e.iota(iotac[:], pattern=[[1, 256]], base=0, channel_multiplier=0,
                                allow_small_or_imprecise_dtypes=True), W=[cst])
    K.op(POOL, lambda e: e.iota(iotap[:], pattern=[[128, 2]], base=0, channel_multiplier=1,
                                allow_small_or_imprecise_dtypes=True), W=[cst])
    K.op(DVE, lambda e: e.memset(hmask[:], 0.0), W=[cst])
    K.op(DVE, lambda e: e.memset(hmask[0:64, 0:1], 1.0), W=[cst])
    K.op(DVE, lambda e: e.memset(hmask[64:128, 1:2], 1.0), W=[cst])
    K.op(DVE, lambda e: e.memset(hmask[0:64, 2:3], -1.0), W=[cst])
    K.op(DVE, lambda e: e.memset(hmask[64:128, 3:4], -1.0), W=[cst])

    ckpt('consts')
    def ldsmall(dram, shape, name, dt=F32, eng=None):
        t = K.sb(shape, dt, name)
        c = K.dmac(name)
        tk = Tok()
        K.dma(SP if dt == F32 else POOL, c, t[:], dram, W=[tk])
        return t, tk

    cT, cT_k = ldsmall(cT_d, [128, KD, NSEQ], "cT")
    bada, bada_k = ldsmall(bada_d, [128, 48], "bada")
    gmix, gmix_k = ldsmall(gmix_d, [128, KD], "gmix")
    gffn, gffn_k = ldsmall(gffn_d, [128, KD], "gffn")
    mu, mu_k = ldsmall(mu_d, [128, 14], "mu")
    w0, w0_k = ldsmall(w0_d, [128, 2, 4], "w0")
    a0, a0_k = ldsmall(a0_d, [128, 2, 4], "a0")
    kkp, kkp_k = ldsmall(kk_d, [128, 4], "kkp")
    kap, kap_k = ldsmall(ka_d, [128, 4], "kap")
    rkp, rkp_k = ldsmall(rk_d, [128, 4], "rkp")
    lnw, lnw_k = ldsmall(lnw_d, [128, 4], "lnw")
    lnb, lnb_k = ldsmall(lnb_d, [128, 4], "lnb")
    cosT, cos_k = ldsmall(cos_d, [128, NT, 32], "cos")
    sinT, sin_k = ldsmall(sin_d, [128, NT, 32], "sin")
    gain = K.sb([128, 10, HD], F32, "gain")
    gain_k = Tok()
    gc_ = K.dmac("gain")
    K.dma(SP, gc_, gain[:, 0, :], qn_d.partition_broadcast(128), W=[gain_k])
    K.dma(SP, gc_, gain[:, 8, :], kn_d.partition_broadcast(128), W=[gain_k])
    for h in range(1, 8):
        K.op(DVE, lambda e, h=h: e.tensor_copy(out=gain[:, h, :], in_=gain[:, 0, :]), R=[gain_k], W=[gain_k])
    K.op(DVE, lambda e: e.tensor_copy(out=gain[:, 9, :], in_=gain[:, 8, :]), R=[gain_k], W=[gain_k])
    K.op(DVE, lambda e: e.tensor_scalar(out=gain[:, 0:8, :], in0=gain[:, 0:8, :], scalar1=HD ** -0.5, scalar2=None, op0=ALU.mult),
         R=[gain_k], W=[gain_k])
    hmu = K.sb([128, 14], F32, "hmu")
    omm = K.sb([128, 14], F32, "omm")
    omka = K.sb([128, 4], F32, "omka")
    K.op(DVE, lambda e: e.tensor_scalar(out=hmu[:], in0=mu[:], scalar1=0.5, scalar2=None, op0=ALU.mult), R=[mu_k], W=[mu_k])
    K.op(DVE, lambda e: e.tensor_scalar(out=omm[:], in0=mu[:], scalar1=-1.0, scalar2=1.0, op0=ALU.mult, op1=ALU.add), R=[mu_k], W=[mu_k])
    K.op(DVE, lambda e: e.tensor_scalar(out=omka[:], in0=kap[:], scalar1=-1.0, scalar2=1.0, op0=ALU.mult, op1=ALU.add), R=[kap_k], W=[kap_k])
    wup = K.sb([128, 2, 512], BF16, "wup")
    aup = K.sb([128, 2, 512], BF16, "aup")
    gup = K.sb([128, 512], BF16, "gup")
    wrt = K.sb([128, KD, NE], BF16, "wrt")
    wsm_k = Tok()
    wsc = K.dmac("wsm")
    K.dma(POOL, wsc, wup[0:64, :, :], wup_d.rearrange("d k n -> k d n"), W=[wsm_k])
    K.dma(POOL, wsc, aup[64:128, :, :], aup_d.rearrange("d k n -> k d n"), W=[wsm_k])
    K.dma(POOL, wsc, gup[:], gup_d, W=[wsm_k])
    K.dma(POOL, wsc, wrt[:], wr_d.rearrange("(j p) n -> p j n", p=128), W=[wsm_k])

    ckpt('small')
    modT = K.sb([128, 48, NSEQ], F32, "modT")
    mod_k = Tok()
    S1 = K.sb([128, KD, NSEQ], F32, "S1")
    S2 = K.sb([128, KD, NSEQ], F32, "S2")
    with K.scope():
        cond = K.sb([128, KD, NSEQ], F32, "cond")
        cond_k = Tok()
        K.op(ACT, lambda e: e.activation(out=cond[:], in_=cT[:], func=AF.Silu), R=[cT_k], W=[cond_k])
        wa = [K.sb([128, KD, 512], F32, "wa") for _ in range(2)]
        wa_k = [Tok(), Tok()]
        wac = [K.dmac("wa0"), K.dmac("wa1")]
        pm = [K.ps([128, 4, NSEQ], F32, "pm") for _ in range(2)]
        pm_k = [Tok(), Tok()]
        for g in range(12):
            b = g % 2
            K.dma(SP, wac[b], wa[b][:], wada_d[:, g * 512:(g + 1) * 512].rearrange("(j p) n -> p j n", p=128), W=[wa_k[b]])
            for mm_ in range(4):
                K.mm(pm[b][:, mm_, :], [(wa[b][:, j, mm_ * 128:(mm_ + 1) * 128], cond[:, j, :]) for j in range(KD)],
                     R=[wa_k[b], cond_k], W=[pm_k[b]])
            K.op(DVE, lambda e, b=b, g=g: e.tensor_tensor(out=modT[:, g * 4:(g + 1) * 4, :], in0=pm[b][:],
                                                          in1=bc(bada[:, g * 4:(g + 1) * 4].unsqueeze(2), [128, 4, NSEQ]), op=ALU.add),
                 R=[pm_k[b], bada_k], W=[mod_k])
        for (Sx, gv, gk, off) in ((S1, gmix, gmix_k, 8), (S2, gffn, gffn_k, 32)):
            K.op(DVE, lambda e, Sx=Sx, off=off: e.tensor_scalar(out=Sx[:], in0=modT[:, off:off + 8, :], scalar1=1.0, scalar2=None, op0=ALU.add),
                 R=[mod_k], W=[mod_k])
            K.op(DVE, lambda e, Sx=Sx, gv=gv: e.tensor_tensor(out=Sx[:], in0=Sx[:], in1=bc(gv[:].unsqueeze(2), [128, KD, NSEQ]), op=ALU.mult),
                 R=[mod_k, gk], W=[mod_k])
    dump("modT", modT[:].rearrange("p m b -> p (m b)"), [128, 48 * NSEQ], R=[mod_k])

    ckpt('phaseA')
    catT = K.sb([128, KD, S], BF16, "catT")
    cat_k = [Tok() for _ in range(KD)]
    def bcast_rows(b, bct, bct_k, srcs):
        with K.scope():
            dg = [K.sb([128, 128], F32, "dg") for _ in range(2)]
            dg_k = [Tok(), Tok()]
            pb_ = [K.ps([128, 512], F32, "pb") for _ in range(2)]
            pb_k = [Tok(), Tok()]
            i = 0
            for r, (src, off) in enumerate(srcs):
                for half in range(2):
                    pi = (r * 2 + half) % 2
                    for jj in range(4):
                        j = half * 4 + jj
                        di = i % 2
                        i += 1
                        K.op(DVE, lambda e, di=di, src=src, off=off, j=j: e.tensor_scalar(
                            out=dg[di][:], in0=identf[:], scalar1=src[:, off + j, b:b + 1], scalar2=None, op0=ALU.mult),
                            R=[cst, mod_k], W=[dg_k[di]])
                        K.mm(pb_[pi][:, jj * 128:(jj + 1) * 128], [(onesf[:], dg[di][:])], R=[dg_k[di], cst], W=[pb_k[pi]])
                    K.op(ACT, lambda e, pi=pi, r=r, half=half: e.activation(out=bct[:, r, half * 512:(half + 1) * 512], in_=pb_[pi][:], func=AF.Copy),
                         R=[pb_k[pi]], W=[bct_k])

    for b in range(NSEQ):
        tok0 = b * S
        with K.scope():
            hT = K.sb([128, KD, S], BF16, "hT")
            hT_k = [Tok() for _ in range(NT)]
            with K.scope():
                xt = [K.sb([128, D], F32, "xt") for _ in range(2)]
                xt_k = [Tok(), Tok()]
                xc = [K.dmac("x0"), K.dmac("x1")]
                xn = [K.sb([128, D], BF16, "xn") for _ in range(2)]
                xn_k = [Tok(), Tok()]
                junk = K.sb([128, D], BF16, "junk")
                junk_k = Tok()
                st_ = [K.sb([128, 2], F32, "st") for _ in range(2)]
                st_k = [Tok(), Tok()]
                pt = [K.ps([128, KD, 128], BF16, "pt") for _ in range(2)]
                pt_k = [Tok(), Tok()]
                for i in range(NT):
                    u = i % 2
                    K.dma(SP, xc[u], xt[u][:], x_d[tok0 + i * 128: tok0 + (i + 1) * 128, :], W=[xt_k[u]])
                    K.op(ACT, lambda e, u=u: e.activation(out=junk[:], in_=xt[u][:], func=AF.Square, accum_out=st_[u][:, 0:1]),
                         R=[xt_k[u]], W=[junk_k, st_k[u]])
                    K.op(ACT, lambda e, u=u: e.activation(out=st_[u][:, 1:2], in_=st_[u][:, 0:1], func=AF.Sqrt, scale=1.0 / D, bias=NORM_EPS),
                         R=[st_k[u]], W=[st_k[u]])
                    K.op(DVE, lambda e, u=u: e.reciprocal(out=st_[u][:, 1:2], in_=st_[u][:, 1:2]), R=[st_k[u]], W=[st_k[u]])
                    K.op(DVE, lambda e, u=u: e.tensor_scalar(out=xn[u][:], in0=xt[u][:], scalar1=st_[u][:, 1:2], scalar2=None, op0=ALU.mult),
                         R=[xt_k[u], st_k[u]], W=[xn_k[u]])
                    for j in range(KD):
                        K.tr(pt[u][:, j, :], xn[u][:, j * 128:(j + 1) * 128], identb[:], R=[xn_k[u], cst], W=[pt_k[u]], inc=(j == KD - 1))
                    for j in range(KD):
                        eng = ACT if j % 2 == 0 else DVE
                        if eng is ACT:
                            K.op(ACT, lambda e, j=j, u=u, i=i: e.activation(out=hT[:, j, i * 128:(i + 1) * 128], in_=pt[u][:, j, :], func=AF.Identity,
                                                                             scale=S1[:, j, b:b + 1], bias=modT[:, j, b:b + 1]),
                                 R=[pt_k[u], mod_k], W=[hT_k[i]])
                        else:
                            K.op(DVE, lambda e, j=j, u=u, i=i: e.tensor_scalar(out=hT[:, j, i * 128:(i + 1) * 128], in0=pt[u][:, j, :],
                                                                                scalar1=S1[:, j, b:b + 1], scalar2=modT[:, j, b:b + 1], op0=ALU.mult, op1=ALU.add),
                                 R=[pt_k[u], mod_k], W=[hT_k[i]])
            if b == 0:
                dump("hT", hT[:, 0, :], [128, S], R=hT_k)

            ckpt('B1')
            with K.scope():
                watt = K.sb([128, KD, 768], BF16, "watt")
                watt_k = Tok()
                K.dma(POOL, K.dmac("watt"), watt[:], win_d[:, 0:768].rearrange("(j p) n -> p j n", p=128), W=[watt_k])
                qT = K.sb([128, 4, S], BF16, "qT")
                qT_k = [Tok() for _ in range(NT)]
                kT2 = K.sb([128, 2, S], BF16, "kT2")
                kT_k = [Tok() for _ in range(NT)]
                vaug = K.sb([128, NT, 2, HD + 1], BF16, "vaug")
                v_k = [Tok() for _ in range(NT)]
                K.op(DVE, lambda e: e.memset(vaug[:], 1.0), W=v_k)
                with K.scope():
                    pq = K.ps([128, 512], F32, "pq")
                    pq_k = Tok()
                    pkv = K.ps([128, 256], F32, "pkv")
                    pkv_k = Tok()
                    ptq = K.ps([128, 4, 128], BF16, "ptq")
                    ptq_k = Tok()
                    ptk = K.ps([128, 2, 128], BF16, "ptk")
                    ptk_k = Tok()
                    qk = K.sb([128, 10, HD], F32, "qk")
                    qk_k = Tok()
                    sq = K.sb([128, 10, HD], F32, "sq")
                    sq_k = Tok()
                    ss = K.sb([128, 10], F32, "ss")
                    ss_k = Tok()
                    t1 = K.sb([128, 10, 32], F32, "t1")
                    t2 = K.sb([128, 10, 32], F32, "t2")
                    t3 = K.sb([128, 10, 32], F32, "t3")
                    t4 = K.sb([128, 10, 32], F32, "t4")
                    t_k = [Tok() for _ in range(4)]
                    qr = K.sb([128, 8, HD], BF16, "qr")
                    qr_k = Tok()
                    kr = K.sb([128, 2, 2, HD], BF16, "kr")
                    kr_k = Tok()
                    for i in range(NT):
                        sl = slice(i * 128, (i + 1) * 128)
                        K.mm(pq[:], [(hT[:, j, sl], watt[:, j, 0:512]) for j in range(KD)], R=[hT_k[i], watt_k], W=[pq_k])
                        K.mm(pkv[:], [(hT[:, j, sl], watt[:, j, 512:768]) for j in range(KD)], R=[hT_k[i], watt_k], W=[pkv_k])
                        K.op(ACT, lambda e: e.activation(out=qk[:, 0:8, :].rearrange("p h d -> p (h d)"), in_=pq[:], func=AF.Copy), R=[pq_k], W=[qk_k])
                        K.op(ACT, lambda e: e.activation(out=qk[:, 8:10, :].rearrange("p h d -> p (h d)"), in_=pkv[:, 0:128], func=AF.Copy), R=[pkv_k], W=[qk_k])
                        K.op(ACT, lambda e, i=i: e.activation(out=vaug[:, i, :, 0:HD], in_=pkv[:, 128:256].rearrange("p (g d) -> p g d", g=2), func=AF.Copy),
                             R=[pkv_k], W=[v_k[i]])
                        K.op(DVE, lambda e: e.tensor_tensor(out=sq[:], in0=qk[:], in1=qk[:], op=ALU.mult), R=[qk_k], W=[sq_k])
                        K.op(DVE, lambda e: e.tensor_reduce(out=ss[:], in_=sq[:], axis=AX.X, op=ALU.add), R=[sq_k], W=[ss_k])
                        K.op(ACT, lambda e: e.activation(out=ss[:], in_=ss[:], func=AF.Sqrt, scale=1.0 / HD, bias=NORM_EPS), R=[ss_k], W=[ss_k])
                        K.op(DVE, lambda e: e.reciprocal(out=ss[:], in_=ss[:]), R=[ss_k], W=[ss_k])
                        K.op(DVE, lambda e: e.tensor_tensor(out=qk[:], in0=qk[:], in1=bc(ss[:].unsqueeze(2), [128, 10, HD]), op=ALU.mult),
                             R=[qk_k, ss_k], W=[qk_k])
                        K.op(DVE, lambda e: e.tensor_tensor(out=qk[:], in0=qk[:], in1=gain[:], op=ALU.mult), R=[qk_k, gain_k], W=[qk_k])
                        qv = qk[:].rearrange("p h (k two) -> p h k two", two=2)
                        x0, x1 = qv[:, :, :, 0], qv[:, :, :, 1]
                        cb = bc(cosT[:, i, :].unsqueeze(1), [128, 10, 32])
                        sb_ = bc(sinT[:, i, :].unsqueeze(1), [128, 10, 32])
                        K.op(DVE, lambda e: e.tensor_tensor(out=t1[:], in0=x0, in1=cb, op=ALU.mult), R=[qk_k, cos_k], W=[t_k[0]])
                        K.op(PL, lambda e: e.tensor_tensor(out=t2[:], in0=x1, in1=sb_, op=ALU.mult), R=[qk_k, sin_k], W=[t_k[1]])
                        K.op(DVE, lambda e: e.tensor_tensor(out=t3[:], in0=x0, in1=sb_, op=ALU.mult), R=[qk_k, sin_k], W=[t_k[2]])
                        K.op(PL, lambda e: e.tensor_tensor(out=t4[:], in0=x1, in1=cb, op=ALU.mult), R=[qk_k, cos_k], W=[t_k[3]])
                        qrv = qr[:].rearrange("p h (k two) -> p h k two", two=2)
                        krv = kr[:].rearrange("p g u (k two) -> p g u k two", two=2)
                        K.op(DVE, lambda e: e.tensor_tensor(out=qrv[:, :, :, 0], in0=t1[:, 0:8, :], in1=t2[:, 0:8, :], op=ALU.subtract),
                             R=[t_k[0], t_k[1]], W=[qr_k])
                        K.op(DVE, lambda e: e.tensor_tensor(out=qrv[:, :, :, 1], in0=t3[:, 0:8, :], in1=t4[:, 0:8, :], op=ALU.add),
                             R=[t_k[2], t_k[3]], W=[qr_k])
                        for u_ in range(2):
                            K.op(PL, lambda e, u_=u_: e.tensor_tensor(out=krv[:, :, u_, :, 0], in0=t1[:, 8:10, :], in1=t2[:, 8:10, :], op=ALU.subtract),
                                 R=[t_k[0], t_k[1]], W=[kr_k])
                            K.op(PL, lambda e, u_=u_: e.tensor_tensor(out=krv[:, :, u_, :, 1], in0=t3[:, 8:10, :], in1=t4[:, 8:10, :], op=ALU.add),
                                 R=[t_k[2], t_k[3]], W=[kr_k])
                        for pr in range(4):
                            K.tr(ptq[:, pr, :], qr[:, 2 * pr:2 * pr + 2, :].rearrange("p h d -> p (h d)"), identb[:], R=[qr_k, cst], W=[ptq_k], inc=(pr == 3))
                        K.op(ACT, lambda e, sl=sl: e.activation(out=qT[:, :, sl], in_=ptq[:], func=AF.Copy), R=[ptq_k], W=[qT_k[i]])
                        for g in range(2):
                            K.tr(ptk[:, g, :], kr[:, g, :, :].rearrange("p u d -> p (u d)"), identb[:], R=[kr_k, cst], W=[ptk_k], inc=(g == 1))
                        K.op(DVE, lambda e, sl=sl: e.tensor_copy(out=kT2[:, :, sl], in_=ptk[:]), R=[ptk_k], W=[kT_k[i]])
                if b == 0:
                    dump("qT", qT[:, 0, :], [128, S], R=qT_k)
                    dump("kT", kT2[:, 0, :], [128, S], R=kT_k)
                ckpt('attproj')
                with K.scope():
                    NPS = 4
                    psc = [K.ps([128, QB], F32, "psc") for _ in range(NPS)]
                    psc_k = [Tok() for _ in range(NPS)]
                    pTa = [K.sb([128, NT, QB], BF16, "pTa") for _ in range(2)]
                    pTa_k = [Tok(), Tok()]
                    oacc = [K.ps([128, QT, 128], F32, "oacc") for _ in range(2)]
                    oacc_k = [Tok(), Tok()]
                    rs = K.sb([128, QT], F32, "rs")
                    rs_k = Tok()
                    otm = K.sb([128, QT, 512], BF16, "otm")
                    otm_k = Tok()
                    pto = K.ps([128, 4, 128], BF16, "pto")
                    pto_k = Tok()
                    it = 0
                    for qb in range(NQB):
                        qsl = slice(qb * QB, (qb + 1) * QB)
                        qtoks = qT_k[qb * QT:(qb + 1) * QT]
                        for hq in range(8):
                            g = hq // 4
                            pb0 = 64 * (hq % 2)
                            pr = hq // 2
                            oa = oacc[hq % 2]
                            oa_k = oacc_k[hq % 2]
                            pa = pTa[hq % 2]
                            pa_k = pTa_k[hq % 2]
                            for kt in range(NT):
                                u = it % NPS
                                it += 1
                                K.mm(psc[u][:], [(kT2[pb0:pb0 + 64, g, kt * 128:(kt + 1) * 128], qT[pb0:pb0 + 64, pr, qsl])],
                                     R=[kT_k[kt]] + qtoks, W=[psc_k[u]])
                                K.op(ACT, lambda e, u=u, pa=pa, kt=kt: e.activation(out=pa[:, kt, :], in_=psc[u][:], func=AF.Exp), R=[psc_k[u]], W=[pa_k])
                            for qt in range(QT):
                                K.mm(oa[:, qt, 0:HD + 1], [(pa[:, kt, qt * 128:(qt + 1) * 128], vaug[:, kt, g, :]) for kt in range(NT)],
                                     R=[pa_k] + v_k, W=[oa_k], inc=(qt == QT - 1))
                            K.op(DVE, lambda e, oa=oa: e.reciprocal(out=rs[:], in_=oa[:, :, HD]), R=[oa_k], W=[rs_k])
                            K.op(DVE, lambda e, oa=oa, hq=hq: e.tensor_tensor(out=otm[:, :, hq * HD:(hq + 1) * HD], in0=oa[:, :, 0:HD],
                                                                               in1=bc(rs[:].unsqueeze(2), [128, QT, HD]), op=ALU.mult),
                                 R=[oa_k, rs_k], W=[otm_k])
                        for qt in range(QT):
                            ti = qb * QT + qt
                            for c in range(4):
                                K.tr(pto[:, c, :], otm[:, qt, c * 128:(c + 1) * 128], identb[:], R=[otm_k, cst], W=[pto_k], inc=(c == 3))
                            K.op(ACT, lambda e, ti=ti: e.activation(out=catT[:, 0:4, ti * 128:(ti + 1) * 128], in_=pto[:], func=AF.Copy),
                                 R=[pto_k], W=cat_k[0:4])
            if b == 0:
                dump("oatt", catT[:, 0, :], [128, S], R=cat_k[0:4])

            ckpt('attcore')
            with K.scope():
                NC = NT
                c_ = DECAY
                wrw = [K.sb([128, KD, 128], BF16, "wrw") for _ in range(1)]
                wrw_k = [Tok() for _ in range(1)]
                wrc = [K.dmac("wrw") for _ in range(1)]
                wri = [0]
                T1 = K.sb([128, S + 2], F32, "T1")
                T2 = K.sb([128, S], F32, "T2")
                T3 = K.sb([128, S], F32, "T3")
                T4 = K.sb([128, S], F32, "T4")
                T_k = [Tok() for _ in range(4)]
                r32 = K.sb([128, S], BF16, "r32")
                k32 = K.sb([128, S], F32, "k32")
                kk32 = K.sb([128, S], F32, "kk32")
                yacc = K.sb([128, S], F32, "yacc")
                bacc = K.sb([128, S], BF16, "bacc")
                r_k_, k_k_, kk_k_, ya_k, ba_k = Tok(), Tok(), Tok(), Tok(), Tok()
                twda = K.sb([128, S], BF16, "twda")
                sg = K.sb([128, S], BF16, "sg")
                vb = K.sb([128, S], BF16, "vb")
                gTb = K.sb([128, S], BF16, "gTb")
                sqb = K.sb([128, S], BF16, "sqb")
                twda_k, sg_k, vb_k, gT_k, sqb_k = Tok(), Tok(), Tok(), Tok(), Tok()
                ART = K.sb([128, 2, NC, 2, 128], BF16, "ART")
                BT = K.sb([128, S], BF16, "BT")
                KT = K.sb([128, S], BF16, "KT")
                ART_k, BT_k, KT_k = Tok(), Tok(), Tok()
                gC = K.sb([128, NC], F32, "gC")
                gC_k = Tok()
                ppj = [K.ps([128, 512], F32, "ppj") for _ in range(2)]
                ppj_k = [Tok(), Tok()]
                pji = [0]
                K.op(DVE, lambda e: e.memset(T1[:, 0:1], 0.0), W=[T_k[0]])
                K.op(DVE, lambda e: e.memset(T1[:, S + 1:S + 2], 0.0), W=[T_k[0]])

                def project_shift(m, dst_fn):
                    wi = 0
                    wri[0] += 1
                    K.dma(POOL, wrc[wi], wrw[wi][:], win_d[:, 768 + m * 128: 768 + (m + 1) * 128].rearrange("(j p) n -> p j n", p=128), W=[wrw_k[wi]])
                    for tb in range(NTB):
                        u = pji[0] % 2
                        pji[0] += 1
                        K.mm(ppj[u][:, 0:TB], [(wrw[wi][:, j, :], hT[:, j, tb * TB:(tb + 1) * TB]) for j in range(KD)],
                             R=[wrw_k[wi]] + hT_k[tb * (TB // 128):(tb + 1) * (TB // 128)], W=[ppj_k[u]])
                        K.op(ACT, lambda e, u=u, tb=tb: e.activation(out=T1[:, 1 + tb * TB:1 + (tb + 1) * TB], in_=ppj[u][:, 0:TB], func=AF.Copy),
                             R=[ppj_k[u]], W=[T_k[0]])
                    K.op(PL, lambda e: e.tensor_tensor(out=T2[:], in0=T1[:, 0:S], in1=T1[:, 2:S + 2], op=ALU.add), R=[T_k[0]], W=[T_k[1]])
                    K.op(DVE, lambda e: e.tensor_scalar(out=T2[:], in0=T2[:], scalar1=hmu[:, m:m + 1], scalar2=None, op0=ALU.mult), R=[T_k[1], mu_k], W=[T_k[1]])
                    K.op(DVE, lambda e: e.scalar_tensor_tensor(out=T3[:], in0=T1[:, 1:S + 1], scalar=omm[:, m:m + 1], in1=T2[:], op0=ALU.mult, op1=ALU.add),
                         R=[T_k[0], T_k[1], mu_k], W=[T_k[2]])
                    if b == 0 and m == 4:
                        dump("T1k", T1[:, 0:S], [128, S], R=[T_k[0]])
                        dump("T2k", T2[:], [128, S], R=[T_k[1]])
                        dump("T3k", T3[:], [128, S], R=[T_k[2]])
                        dump("hmu", hmu[:], [128, 14], R=[mu_k])
                        dump("omm", omm[:], [128, 14], R=[mu_k])
                    dst_fn()

                def d12():
                    K.op(ACT, lambda e: e.activation(out=twda[0:64, :], in_=T3[0:64, :], func=AF.Tanh), R=[T_k[2]], W=[twda_k])
                    K.op(DVE, lambda e: e.tensor_copy(out=twda[64:128, :], in_=T3[64:128, :]), R=[T_k[2]], W=[twda_k])
                project_shift(12, d12)

                def d13():
                    K.op(ACT, lambda e: e.activation(out=sg[:], in_=T3[:], func=AF.Sigmoid), R=[T_k[2]], W=[sg_k])
                project_shift(13, d13)

                ckpt('lora')
                pbd = [K.ps([128, 512], F32, "pbd") for _ in range(2)]
                pbd_k = [Tok(), Tok()]
                bdi = [0]
                nck = [0]
                ptk3 = K.ps([128, 3, 128], BF16, "ptk3")
                ptk3_k = Tok()
                pP = K.ps([128, 2, 256], F32, "pP")
                pP_k = Tok()
                pQ = K.ps([128, 2, 128], F32, "pQ")
                pQ_k = Tok()
                pZY = K.ps([128, 512], F32, "pZY")
                pZ = pZY[:, 0:256].rearrange("p (a v) -> p a v", v=64)
                pZY_k = Tok()
                pZ_k = [pZY_k, pZY_k]
                pY = pZY[:, 256:448]
                pY_k = [pZY_k, pZY_k]
                tok3s = [K.sb([128, 3, 2, 128], BF16, "tok3") for _ in range(2)]
                tok3s_k = [Tok(), Tok()]
                for q_ in range(2):
                    K.op(DVE, lambda e, q_=q_: e.memset(tok3s[q_][:], 0.0), W=[tok3s_k[q_]])
                Ub2 = K.sb([128, 2, 128], BF16, "Ub2")
                Ub2_k = Tok()
                K.op(DVE, lambda e: e.memset(Ub2[:], 0.0), W=[Ub2_k])
                Hbd = K.sb([128, 128], BF16, "Hbd")
                Hbd_k = Tok()
                M1s = [K.sb([128, 2, 256], BF16, "M1") for _ in range(2)]
                M2s = [K.sb([128, 2, 256], BF16, "M2") for _ in range(2)]
                M1s_k, M2s_k = [Tok(), Tok()], [Tok(), Tok()]
                XAs = [[K.sb([128, 2, 2, 128], BF16, "XA") for _ in range(2)] for _ in range(2)]
                XAs_k = [[Tok(), Tok()], [Tok(), Tok()]]
                PAs = [[K.sb([128, 2, 128], BF16, "PA") for _ in range(2)] for _ in range(2)]
                PAs_k = [[Tok(), Tok()], [Tok(), Tok()]]
                Zb = K.sb([128, 2, 64], BF16, "Zb")
                Ub = K.sb([128, 2, 64], BF16, "Ub")
                Zb_k, Ub_k = Tok(), Tok()
                H32 = K.sb([128, 64], F32, "H32")
                Hb = K.sb([128, 64], BF16, "Hb")
                Htmp = K.sb([128, 64], F32, "Htmp")
                H_k, Hb_k, Ht_k = Tok(), Tok(), Tok()
                identb2 = bc(identb[:].unsqueeze(1), [128, 2, 128])
                T4b = T4[:].bitcast(BF16)
                if 2 * S >= 3328:
                    tok3s.append(T4b[:, 0:768].rearrange("p (x h c) -> p x h c", x=3, h=2))
                    M1s.append(T4b[:, 768:1280].rearrange("p (h t) -> p h t", h=2))
                    M2s.append(T4b[:, 1280:1792].rearrange("p (h t) -> p h t", h=2))
                    XAs.append([T4b[:, 1792 + i_ * 512:1792 + (i_ + 1) * 512].rearrange("p (h a t) -> p h a t", h=2, a=2) for i_ in range(2)])
                    PAs.append([T4b[:, 2816 + i_ * 256:2816 + (i_ + 1) * 256].rearrange("p (h t) -> p h t", h=2) for i_ in range(2)])
                    tok3s_k.append(Tok()); M1s_k.append(Tok()); M2s_k.append(Tok())
                    XAs_k.append([Tok(), Tok()]); PAs_k.append([Tok(), Tok()])
                    T3b = T3[:].bitcast(BF16)
                    tok3s.append(T3b[:, 0:768].rearrange("p (x h c) -> p x h c", x=3, h=2))
                    M1s.append(T3b[:, 768:1280].rearrange("p (h t) -> p h t", h=2))
                    M2s.append(T3b[:, 1280:1792].rearrange("p (h t) -> p h t", h=2))
                    XAs.append([T3b[:, 1792 + i_ * 512:1792 + (i_ + 1) * 512].rearrange("p (h a t) -> p h a t", h=2, a=2) for i_ in range(2)])
                    PAs.append([T3b[:, 2816 + i_ * 256:2816 + (i_ + 1) * 256].rearrange("p (h t) -> p h t", h=2) for i_ in range(2)])
                    tok3s_k.append(Tok()); M1s_k.append(Tok()); M2s_k.append(Tok())
                    XAs_k.append([Tok(), Tok()]); PAs_k.append([Tok(), Tok()])
                NSETS = len(tok3s)
                pPs = [pP, ppj[1][:].rearrange("p (a t) -> p a t", a=2), ppj[0][:].rearrange("p (a t) -> p a t", a=2)]
                pP_ks = [pP_k, ppj_k[1], ppj_k[0]]
                pQs = [pQ, pbd[1][:, 0:256].rearrange("p (a t) -> p a t", a=2), pbd[0][:, 0:256].rearrange("p (a t) -> p a t", a=2)]
                pQ_ks = [pQ_k, pbd_k[1], pbd_k[0]]
                NPIPE = 3 if NSETS >= 4 else 2

                def bdsum(src_bf, src_k, consume):
                    for tb in range(NTB):
                        u = bdi[0] % 2
                        bdi[0] += 1
                        K.mm(pbd[u][:, 0:TB], [(bdones[:], src_bf[:, tb * TB:(tb + 1) * TB])], R=[src_k, cst], W=[pbd_k[u]])
                        consume(pbd[u][:, 0:TB], pbd_k[u], tb)

                import os as _os
                for c4 in [int(q) for q in _os.environ.get('C4LIST', '0,1,2,3').split(',')]:
                    csl = slice(c4 * 128, (c4 + 1) * 128)
                    project_shift(c4, lambda: K.op(PL, lambda e: e.tensor_copy(out=r32[:], in_=T3[:]), R=[T_k[2]], W=[r_k_]))
                    project_shift(4 + c4, lambda: K.op(PL, lambda e: e.tensor_copy(out=k32[:], in_=T3[:]), R=[T_k[2]], W=[k_k_]))
                    project_shift(8 + c4, lambda: K.op(ACT, lambda e: e.activation(out=vb[:], in_=T3[:], func=AF.Copy), R=[T_k[2]], W=[vb_k]))
                    ckpt('rw_proj%d' % c4)
                    for tb in range(NTB):
                        u = pji[0] % 2
                        pji[0] += 1
                        K.mm(ppj[u][:, 0:TB], [(gup[:, csl], sg[:, tb * TB:(tb + 1) * TB])], R=[wsm_k, sg_k], W=[ppj_k[u]])
                        K.op(ACT, lambda e, u=u, tb=tb: e.activation(out=gTb[:, tb * TB:(tb + 1) * TB], in_=ppj[u][:, 0:TB], func=AF.Copy), R=[ppj_k[u]], W=[gT_k])
                    K.op(DVE, lambda e: e.tensor_scalar(out=kk32[:], in0=k32[:], scalar1=kkp[:, c4:c4 + 1], scalar2=None, op0=ALU.mult), R=[k_k_, kkp_k], W=[kk_k_])
                    K.op(PL, lambda e: e.tensor_tensor(out=sqb[:], in0=kk32[:], in1=kk32[:], op=ALU.mult), R=[kk_k_], W=[sqb_k])

                    def cons_kk(ps_, pk, tb):
                        tsl = slice(tb * TB, (tb + 1) * TB)
                        K.op(ACT, lambda e: e.activation(out=T4[:, tsl], in_=ps_[:], func=AF.Sqrt, bias=1e-24), R=[pk], W=[T_k[3]])
                        K.op(DVE, lambda e: e.reciprocal(out=T4[:, tsl], in_=T4[:, tsl]), R=[T_k[3]], W=[T_k[3]])
                    bdsum(sqb, sqb_k, cons_kk)
                    K.op(DVE, lambda e: e.tensor_tensor(out=kk32[:], in0=kk32[:], in1=T4[:], op=ALU.mult), R=[kk_k_, T_k[3]], W=[kk_k_])
                    if b == 0 and c4 == 0:
                        dump("kk", kk32[:], [128, S], R=[kk_k_])
                        pass

                    ckpt('rw_kk%d' % c4)
                    for d in range(2):
                        lw = T1[:, 1:S + 1]
                        for tb in range(NTB):
                            tsl = slice(tb * TB, (tb + 1) * TB)
                            u = pji[0] % 2
                            pji[0] += 1
                            K.mm(ppj[u][:, 0:TB], [(wup[0:64, d, csl], twda[0:64, tsl])], R=[wsm_k, twda_k], W=[ppj_k[u]])
                            K.op(ACT, lambda e, u=u, tsl=tsl: e.activation(out=lw[:, tsl], in_=ppj[u][:, 0:TB], func=AF.Sigmoid, bias=w0[:, d, c4:c4 + 1]),
                                 R=[ppj_k[u], w0_k], W=[T_k[0]])
                        for n_ in range(NC):
                            K.op(DVE, lambda e, n_=n_: e.tensor_tensor_scan(out=T2[:, n_ * 128:(n_ + 1) * 128], data0=onesf[:], data1=lw[:, n_ * 128:(n_ + 1) * 128],
                                                                        initial=0.0, op0=ALU.mult, op1=ALU.add),
                                 R=[T_k[0], cst], W=[T_k[1]])
                        cs3 = T2[:].rearrange("p (c t) -> p c t", t=128)
                        K.op(ACT, lambda e: e.activation(out=gC[:], in_=cs3[:, :, 127], func=AF.Exp, scale=-c_), R=[T_k[1]], W=[gC_k])
                        if d == 0:
                            K.op(DVE, lambda e: e.tensor_tensor(out=lw, in0=T2[:], in1=lw, op=ALU.subtract), R=[T_k[0], T_k[1]], W=[T_k[0]])
                            gexc, gexc_k, ginc, ginc_k = lw, T_k[0], T2[:], T_k[1]
                        else:
                            K.op(PL, lambda e: e.tensor_copy(out=T4[:].rearrange("p (c t) -> p c t", t=128), in_=bc(cs3[:, :, 127:128], [128, NC, 128])),
                                 R=[T_k[1]], W=[T_k[3]])
                            K.op(DVE, lambda e: e.tensor_tensor(out=T2[:], in0=T4[:], in1=T2[:], op=ALU.subtract), R=[T_k[1], T_k[3]], W=[T_k[1]])
                            K.op(DVE, lambda e: e.tensor_tensor(out=lw, in0=lw, in1=T2[:], op=ALU.add), R=[T_k[0], T_k[1]], W=[T_k[0]])
                            gexc, gexc_k, ginc, ginc_k = T2[:], T_k[1], lw, T_k[0]
                        A3 = [ART[:, 0, :, 0, :], ART[:, 1, :, 0, :]]
                        R3 = [ART[:, 0, :, 1, :], ART[:, 1, :, 1, :]]
                        v3 = lambda ap: ap.rearrange("p (c t) -> p c t", t=128)
                        K.op(ACT, lambda e: e.activation(out=gexc, in_=gexc, func=AF.Exp, scale=-c_), R=[gexc_k], W=[gexc_k])
                        for hh in range(2):
                            K.op(DVE, lambda e, hh=hh: e.scalar_tensor_tensor(out=A3[hh], in0=v3(kk32[:]), scalar=hmask[:, 2 + hh:3 + hh], in1=v3(gexc), op0=ALU.mult, op1=ALU.mult),
                                 R=[kk_k_, gexc_k, cst], W=[ART_k])
                        K.op(ACT, lambda e: e.activation(out=T3[:], in_=ginc, func=AF.Exp, scale=-c_), R=[ginc_k], W=[T_k[2]])
                        for hh in range(2):
                            K.op(DVE, lambda e, hh=hh: e.scalar_tensor_tensor(out=R3[hh], in0=v3(r32[:]), scalar=hmask[:, hh:hh + 1], in1=v3(T3[:]), op0=ALU.mult, op1=ALU.mult),
                                 R=[r_k_, T_k[2], cst], W=[ART_k])
                        K.op(ACT, lambda e: e.activation(out=ginc, in_=ginc, func=AF.Exp, scale=c_), R=[ginc_k], W=[ginc_k])
                        for tb in range(NTB):
                            tsl = slice(tb * TB, (tb + 1) * TB)
                            u = pji[0] % 2
                            pji[0] += 1
                            K.mm(ppj[u][:, 0:TB], [(aup[64:128, d, csl], twda[64:128, tsl])], R=[wsm_k, twda_k], W=[ppj_k[u]])
                            K.op(ACT, lambda e, u=u, tsl=tsl: e.activation(out=T3[:, tsl], in_=ppj[u][:, 0:TB], func=AF.Sigmoid, bias=a0[:, d, c4:c4 + 1]),
                                 R=[ppj_k[u], a0_k], W=[T_k[2]])
                        Tg = gexc
                        K.op(DVE, lambda e: e.tensor_tensor(out=Tg, in0=kk32[:], in1=T3[:], op=ALU.mult), R=[kk_k_, T_k[2], ART_k], W=[gexc_k])
                        K.op(DVE, lambda e: e.tensor_tensor(out=BT[:], in0=Tg, in1=ginc, op=ALU.mult), R=[gexc_k, ginc_k], W=[BT_k])
                        K.op(DVE, lambda e: e.tensor_scalar(out=Tg, in0=T3[:], scalar1=kap[:, c4:c4 + 1], scalar2=omka[:, c4:c4 + 1], op0=ALU.mult, op1=ALU.add),
                             R=[T_k[2], kap_k, BT_k], W=[gexc_k])
                        K.op(DVE, lambda e: e.tensor_tensor(out=Tg, in0=Tg, in1=k32[:], op=ALU.mult), R=[gexc_k, k_k_], W=[gexc_k])
                        K.op(PL, lambda e: e.tensor_tensor(out=KT[:], in0=Tg, in1=ginc, op=ALU.mult), R=[gexc_k, ginc_k], W=[KT_k])
                        K.op(DVE, lambda e: e.scalar_tensor_tensor(out=sqb[:], in0=r32[:], scalar=rkp[:, c4:c4 + 1], in1=Tg, op0=ALU.mult, op1=ALU.mult),
                             R=[r_k_, gexc_k, rkp_k], W=[sqb_k])

                        def cons_b(ps_, pk, tb, d=d):
                            tsl = slice(tb * TB, (tb + 1) * TB)
                            if d == 0:
                                K.op(DVE, lambda e: e.tensor_tensor(out=bacc[:, tsl], in0=ps_[:], in1=vb[:, tsl], op=ALU.mult), R=[pk, vb_k], W=[ba_k])
                            else:
                                K.op(DVE, lambda e: e.tensor_tensor(out=T3[:, tsl], in0=ps_[:], in1=vb[:, tsl], op=ALU.mult), R=[pk, vb_k], W=[T_k[2]])
                                K.op(PL, lambda e: e.tensor_tensor(out=bacc[:, tsl], in0=bacc[:, tsl], in1=T3[:, tsl], op=ALU.add), R=[T_k[2]], W=[ba_k])
                        bdsum(sqb, sqb_k, cons_b)
                        if b == 0 and c4 == 0:
                            dump(f"AT{d}", ART[:, 0, :, 0, :], [128, NC, 128], R=[ART_k])
                            dump(f"BT{d}", BT[:], [128, S], R=[BT_k])

                        ckpt('rw_prep%d_%d' % (c4, d))
                        K.op(DVE, lambda e: e.memset(H32[:], 0.0), W=[H_k])
                        K.op(DVE, lambda e: e.memset(Hb[:], 0.0), W=[Hb_k])
                        K.op(DVE, lambda e: e.memset(Hbd[:], 0.0), W=[Hbd_k])
                        order = range(NC) if d == 0 else range(NC - 1, -1, -1)
                        hs = [slice(0, 64), slice(64, 128)]

                        def prep(n, q, r, d=d):
                            nsl = slice(n * 128, (n + 1) * 128)
                            tk, tk_k = tok3s[q], tok3s_k[q]
                            m1, m1_k, m2, m2_k = M1s[q], M1s_k[q], M2s[q], M2s_k[q]
                            xa, xa_k, pa, pa_k = XAs[q], XAs_k[q], PAs[q], PAs_k[q]
                            pP, pP_k, pQ, pQ_k = pPs[r], pP_ks[r], pQs[r], pQ_ks[r]
                            K.tr(ptk3[:, 0, :], BT[:, nsl], identb[:], R=[BT_k, cst], W=[ptk3_k], inc=False)
                            K.tr(ptk3[:, 1, :], KT[:, nsl], identb[:], R=[KT_k], W=[ptk3_k], inc=False)
                            K.tr(ptk3[:, 2, :], vb[:, nsl], identb[:], R=[vb_k], W=[ptk3_k])
                            for hh in range(2):
                                K.op(ACT if hh == 0 else DVE, (lambda e, hh=hh: e.activation(out=tk[:, :, hh, hh * 64:(hh + 1) * 64], in_=ptk3[:, :, hh * 64:(hh + 1) * 64], func=AF.Copy)) if hh == 0 else
                                     (lambda e, hh=hh: e.tensor_copy(out=tk[:, :, hh, hh * 64:(hh + 1) * 64], in_=ptk3[:, :, hh * 64:(hh + 1) * 64])), R=[ptk3_k], W=[tk_k])
                            yield
                            for hh in range(2):
                                K.mm(pP[:, hh, :], [(BT[:, nsl], ART[:, hh, n, :, :].rearrange("p a t -> p (a t)"))], R=[BT_k, ART_k], W=[pP_k], inc=(hh == 1))
                            for hh in range(2):
                                K.op(DVE, lambda e, hh=hh: e.tensor_tensor(out=m1[:, hh, :], in0=pP[:, hh, :], in1=MP[d][:], op=ALU.mult), R=[pP_k, cst], W=[m1_k])
                            yield
                            for hh in range(2):
                                K.mm(pP[:, hh, :], [(KT[:, nsl], ART[:, hh, n, :, :].rearrange("p a t -> p (a t)"))], R=[KT_k, ART_k], W=[pP_k], inc=(hh == 1))
                            for hh in range(2):
                                K.op(DVE, lambda e, hh=hh: e.tensor_tensor(out=m2[:, hh, :], in0=pP[:, hh, :], in1=MP[d][:], op=ALU.mult), R=[pP_k, cst], W=[m2_k])
                            yield
                            for hh in range(2):
                                K.mm(pQ[:, hh, :], [(ART[:, hh, n, 0, :], BT[:, nsl])], R=[BT_k, ART_k], W=[pQ_k], inc=(hh == 1))
                            for hh in range(2):
                                K.op(DVE, lambda e, hh=hh: e.tensor_tensor(out=pa[0][:, hh, :], in0=pQ[:, hh, :], in1=ML[d][:], op=ALU.mult), R=[pQ_k, cst], W=[pa_k[0]])
                            yield
                            K.op(PL, lambda e: e.tensor_tensor(out=xa[1][:, :, 1, :], in0=m1[:, :, 0:128], in1=identb2, op=ALU.add), R=[m1_k, cst], W=[xa_k[1]])
                            for hh in range(2):
                                K.mm(pP[:, hh, 0:128], [(pa[0][:, hh, :], m1[:, hh, 0:128])], R=[pa_k[0], m1_k], W=[pP_k], inc=(hh == 1))
                            K.op(ACT, lambda e: e.activation(out=xa[1][:, :, 0, :], in_=pP[:, :, 0:128], func=AF.Copy), R=[pP_k], W=[xa_k[1]])
                            for hh in range(2):
                                K.mm(pQ[:, hh, :], [(m1[:, hh, 0:128], pa[0][:, hh, :])], R=[pa_k[0], m1_k], W=[pQ_k], inc=(hh == 1))
                            K.op(ACT, lambda e: e.activation(out=pa[1][:], in_=pQ[:], func=AF.Copy), R=[pQ_k], W=[pa_k[1]])
                            yield
                            cur = 1
                            for lev in range(1, 7):
                                nx = 1 - cur
                                if lev < 6:
                                    for hh in range(2):
                                        K.mm(pP[:, hh, :], [(pa[cur][:, hh, :], xa[cur][:, hh, :, :].rearrange("p a t -> p (a t)"))],
                                             R=[pa_k[cur], xa_k[cur]], W=[pP_k], inc=(hh == 1))
                                    K.op(ACT, lambda e, nx=nx: e.activation(out=xa[nx][:, :, 0, :], in_=pP[:, :, 0:128], func=AF.Copy), R=[pP_k], W=[xa_k[nx]])
                                    K.op(DVE, lambda e, nx=nx, cur=cur: e.tensor_tensor(out=xa[nx][:, :, 1, :], in0=pP[:, :, 128:256], in1=xa[cur][:, :, 1, :], op=ALU.add),
                                         R=[pP_k, xa_k[cur]], W=[xa_k[nx]])
                                    for hh in range(2):
                                        K.mm(pQ[:, hh, :], [(xa[cur][:, hh, 0, :], pa[cur][:, hh, :])], R=[pa_k[cur], xa_k[cur]], W=[pQ_k], inc=(hh == 1))
                                    K.op(ACT, lambda e, nx=nx: e.activation(out=pa[nx][:], in_=pQ[:], func=AF.Copy), R=[pQ_k], W=[pa_k[nx]])
                                else:
                                    for hh in range(2):
                                        K.mm(pP[:, hh, 128:256], [(pa[cur][:, hh, :], xa[cur][:, hh, 1, :])], R=[pa_k[cur], xa_k[cur]], W=[pP_k], inc=(hh == 1))
                                    K.op(DVE, lambda e, nx=nx, cur=cur: e.tensor_tensor(out=xa[nx][:, :, 1, :], in0=pP[:, :, 128:256], in1=xa[cur][:, :, 1, :], op=ALU.add),
                                         R=[pP_k, xa_k[cur]], W=[xa_k[nx]])
                                cur = nx
                                yield
                            assert cur == 1

                        def chain(n, q, d=d):
                            nsl = slice(n * 128, (n + 1) * 128)
                            tk, tk_k = tok3s[q], tok3s_k[q]
                            m1, m1_k, m2, m2_k = M1s[q], M1s_k[q], M2s[q], M2s_k[q]
                            Wf, Wf_k = XAs[q][1], XAs_k[q][1]
                            for hh in range(2):
                                K.mm(pZ[:, hh, :], [(ART[:, hh, n, 0, :], Hb[:, :]), (m2[:, hh, 0:128], tk[:, 2, hh, hs[hh]])],
                                     R=[ART_k, Hb_k, m2_k, tk_k], W=[pZ_k[0]])
                            K.op(ACT, lambda e: e.activation(out=Zb[:], in_=pZ[:, 0:2, :], func=AF.Copy), R=[pZ_k[0]], W=[Zb_k])
                            for hh in range(2):
                                K.mm(pZ[:, 2 + hh, :], [(Wf[:, hh, 1, :], Zb[:, hh, :])], R=[Wf_k, Zb_k], W=[pZ_k[1]])
                            K.op(ACT, lambda e: e.activation(out=Ub[:], in_=pZ[:, 2:4, :], func=AF.Copy), R=[pZ_k[1]], W=[Ub_k])
                            for hh in range(2):
                                K.op(DVE, lambda e, hh=hh: e.tensor_copy(out=Ub2[:, hh, hh * 64:(hh + 1) * 64], in_=Ub[:, hh, :]), R=[Ub_k], W=[Ub2_k])
                            yield
                            K.mm(pY[:, 128:192], [(tk[:, 0, 0, :], Ub[:, 0, :]), (tk[:, 0, 1, :], Ub[:, 1, :]),
                                                  (tk[:, 1, 0, :], tk[:, 2, 0, 0:64]), (tk[:, 1, 1, :], tk[:, 2, 1, 64:128])],
                                 R=[tk_k, Ub_k], W=[pY_k[1]])
                            K.mm(pY[:, 0:128], [(Hbd[:], ART[:, 0, n, 1, :]), (Hbd[:], ART[:, 1, n, 1, :]),
                                                (Ub2[:, 0, :], m1[:, 0, 128:256]), (Ub2[:, 1, :], m1[:, 1, 128:256]),
                                                (tk[:, 2, 0, :], m2[:, 0, 128:256]), (tk[:, 2, 1, :], m2[:, 1, 128:256])],
                                 R=[Hbd_k, ART_k, Ub2_k, m1_k, m2_k, tk_k], W=[pY_k[0]])
                            K.op(DVE, lambda e: e.tensor_tensor(out=Htmp[:], in0=pY[:, 128:192], in1=H32[:], op=ALU.add), R=[pY_k[1], H_k], W=[Ht_k])
                            if d == 0:
                                K.op(DVE, lambda e, nsl=nsl: e.tensor_copy(out=yacc[:, nsl], in_=pY[:, 0:128]), R=[pY_k[0]], W=[ya_k])
                            else:
                                K.op(DVE, lambda e, nsl=nsl: e.tensor_tensor(out=yacc[:, nsl], in0=pY[:, 0:128], in1=yacc[:, nsl], op=ALU.add), R=[pY_k[0], ya_k], W=[ya_k])
                            K.op(DVE, lambda e, n=n: e.tensor_scalar(out=H32[:], in0=Htmp[:], scalar1=gC[:, n:n + 1], scalar2=None, op0=ALU.mult), R=[Ht_k, gC_k], W=[H_k])
                            K.op(ACT, lambda e, n=n: e.activation(out=Hb[:], in_=Htmp[:], func=AF.Copy, scale=gC[:, n:n + 1]), R=[Ht_k, gC_k], W=[Hb_k])
                            for hh in range(2):
                                K.op(PL, lambda e, hh=hh: e.tensor_copy(out=Hbd[hs[hh], hh * 64:(hh + 1) * 64], in_=H32[hs[hh], :]), R=[H_k], W=[Hbd_k])
                            yield

                        order = list(order)
                        K.barrier()
                        for q_ in range(2, NSETS):
                            K.op(DVE, lambda e, q_=q_: e.memset(tok3s[q_], 0.0), W=[tok3s_k[q_]])
                        nch = len(order)
                        act_preps, done_prep = [], set()
                        next_prep, chain_k, completed, chain_gen = 0, 0, 0, None
                        while chain_k < nch:
                            while len(act_preps) < NPIPE and next_prep < nch and next_prep <= completed + NSETS - 1:
                                act_preps.append((next_prep, prep(order[next_prep], next_prep % NSETS, next_prep % NPIPE)))
                                next_prep += 1
                            if chain_gen is None and chain_k in done_prep:
                                chain_gen = chain(order[chain_k], chain_k % NSETS)
                            if chain_gen is not None:
                                try:
                                    next(chain_gen)
                                except StopIteration:
                                    chain_gen = None
                                    completed += 1
                                    chain_k += 1
                                    nck[0] += 1
                            for it_ in list(act_preps):
                                try:
                                    next(it_[1])
                                except StopIteration:
                                    act_preps.remove(it_)
                                    done_prep.add(it_[0])
                        K.barrier()
                    if b == 0 and c4 == 0:
                        dump("yacc", yacc[:], [128, S], R=[ya_k])
                        dump("bacc", bacc[:], [128, S], R=[ba_k])
                    ckpt('rw_loops%d' % c4)
                    K.op(ACT, lambda e: e.activation(out=sqb[:], in_=yacc[:], func=AF.Copy), R=[ya_k], W=[sqb_k])

                    def cons_m(ps_, pk, tb):
                        tsl = slice(tb * TB, (tb + 1) * TB)
                        K.op(DVE, lambda e: e.scalar_tensor_tensor(out=yacc[:, tsl], in0=ps_[:], scalar=-1.0 / 64, in1=yacc[:, tsl], op0=ALU.mult, op1=ALU.add),
                             R=[pk, ya_k], W=[ya_k])
                    bdsum(sqb, sqb_k, cons_m)
                    K.op(PL, lambda e: e.tensor_tensor(out=sqb[:], in0=yacc[:], in1=yacc[:], op=ALU.mult), R=[ya_k], W=[sqb_k])

                    def cons_v(ps_, pk, tb):
                        tsl = slice(tb * TB, (tb + 1) * TB)
                        K.op(ACT, lambda e: e.activation(out=T4[:, tsl], in_=ps_[:], func=AF.Sqrt, scale=1.0 / 64, bias=GN_EPS), R=[pk], W=[T_k[3]])
                        K.op(DVE, lambda e: e.reciprocal(out=T4[:, tsl], in_=T4[:, tsl]), R=[T_k[3]], W=[T_k[3]])
                    bdsum(sqb, sqb_k, cons_v)
                    K.op(DVE, lambda e: e.tensor_tensor(out=yacc[:], in0=yacc[:], in1=T4[:], op=ALU.mult), R=[ya_k, T_k[3]], W=[ya_k])
                    K.op(DVE, lambda e: e.tensor_scalar(out=yacc[:], in0=yacc[:], scalar1=lnw[:, c4:c4 + 1], scalar2=lnb[:, c4:c4 + 1], op0=ALU.mult, op1=ALU.add),
                         R=[ya_k, lnw_k, lnb_k], W=[ya_k])
                    K.op(PL, lambda e: e.tensor_tensor(out=yacc[:], in0=yacc[:], in1=bacc[:], op=ALU.add), R=[ya_k, ba_k], W=[ya_k])
                    K.op(DVE, lambda e: e.tensor_tensor(out=catT[:, 4 + c4, :], in0=yacc[:], in1=gTb[:], op=ALU.mult), R=[ya_k, gT_k], W=[cat_k[4 + c4]])
                    ckpt('rw_gn%d' % c4)
            if b == 0:
                dump("orw", catT[:, 4, :], [128, S], R=cat_k)

        ckpt('rwkv')
        with K.scope():
            x1 = K.sb([128, NT, D], F32, "x1")
            x1_k = [Tok() for _ in range(NT)]
            h2t = K.sb([128, NT, D], BF16, "h2t")
            h2_k = [Tok() for _ in range(NT)]
            afft = K.sb([128, NT, NE], F32, "afft")
            aff_k = [Tok() for _ in range(NT)]
            posm = K.sb([16, S], F32, "posm")
            posm_k = Tok()
            post = K.sb([128, NT, NE], F32, "post")
            post_k = Tok()
            gt2b = K.sb([128, 1, D], F32, "gt2b")
            gt2b_k = Tok()
            bcast_rows(b, gt2b, gt2b_k, [(modT, 40)])
            K.stacks.append(ExitStack())
            affT = K.sb([16, S], F32, "affT")
            affT_k = Tok()
            with K.scope():
                bct = K.sb([128, 3, D], F32, "bct")
                bct_k = Tok()
                bcast_rows(b, bct, bct_k, [(modT, 16), (S2, 0), (modT, 24)])
                wo = K.sb([128, KD, D], BF16, "wo")
                wo_k = Tok()
                K.dma(POOL, K.dmac("wo"), wo[:], wout_d.rearrange("(j p) n -> p j n", p=128), W=[wo_k])
                xc = [K.dmac("x0")]
                xt = [K.sb([128, D], F32, "xt")]
                xt_k = [Tok()]
                po = [K.ps([128, 512], F32, "po") for _ in range(2)]
                po_k = [Tok(), Tok()]
                st_ = [K.sb([128, 4], F32, "st") for _ in range(2)]
                st_k = [Tok(), Tok()]
                pt = [K.ps([128, KD, 128], BF16, "pt") for _ in range(2)]
                pt_k = [Tok(), Tok()]
                h2T = [K.sb([128, KD, 128], BF16, "h2T") for _ in range(2)]
                h2T_k = [Tok(), Tok()]
                plg = K.ps([128, NE], F32, "plg")
                plg_k = Tok()
                lg = K.sb([128, NE], F32, "lg")
                lg_k = Tok()
                paT = K.ps([16, 128], F32, "paT")
                paT_k = Tok()
                for i in range(NT):
                    u = i % 2
                    sl = slice(i * 128, (i + 1) * 128)
                    K.dma(SP, xc[0], xt[0][:], x_d[tok0 + i * 128: tok0 + (i + 1) * 128, :], W=[xt_k[0]])
                    for half in range(2):
                        hsl = slice(half * 512, (half + 1) * 512)
                        K.mm(po[half][:], [(catT[:, j, sl], wo[:, j, hsl]) for j in range(KD)], R=cat_k + [wo_k], W=[po_k[half]])
                        K.op(DVE, lambda e, half=half, hsl=hsl, i=i: e.tensor_tensor(out=x1[:, i, hsl], in0=po[half][:], in1=bct[:, 0, hsl], op=ALU.mult),
                             R=[po_k[half], bct_k], W=[x1_k[i]])
                    K.op(PL, lambda e, i=i: e.tensor_tensor(out=x1[:, i, :], in0=x1[:, i, :], in1=xt[0][:], op=ALU.add), R=[x1_k[i], xt_k[0]], W=[x1_k[i]])
                    K.op(ACT, lambda e, u=u, i=i: e.activation(out=h2T[u][:].rearrange("p j t -> p (j t)"), in_=x1[:, i, :], func=AF.Square, accum_out=st_[u][:, 0:1]),
                         R=[x1_k[i]], W=[h2T_k[u], st_k[u]])
                    K.op(ACT, lambda e, u=u: e.activation(out=st_[u][:, 1:2], in_=st_[u][:, 0:1], func=AF.Sqrt, scale=1.0 / D, bias=NORM_EPS), R=[st_k[u]], W=[st_k[u]])
                    K.op(DVE, lambda e, u=u: e.reciprocal(out=st_[u][:, 1:2], in_=st_[u][:, 1:2]), R=[st_k[u]], W=[st_k[u]])
                    K.op(DVE, lambda e, u=u, i=i: e.scalar_tensor_tensor(out=h2t[:, i, :], in0=x1[:, i, :], scalar=st_[u][:, 1:2], in1=bct[:, 1, :], op0=ALU.mult, op1=ALU.mult),
                         R=[x1_k[i], st_k[u], bct_k], W=[h2_k[i]])
                    K.op(PL, lambda e, i=i: e.tensor_tensor(out=h2t[:, i, :], in0=h2t[:, i, :], in1=bct[:, 2, :], op=ALU.add), R=[h2_k[i], bct_k], W=[h2_k[i]])
                    for j in range(KD):
                        K.tr(pt[u][:, j, :], h2t[:, i, j * 128:(j + 1) * 128], identb[:], R=[h2_k[i], cst], W=[pt_k[u]], inc=(j == KD - 1))
                    K.op(ACT, lambda e, u=u: e.activation(out=h2T[u][:], in_=pt[u][:], func=AF.Copy), R=[pt_k[u]], W=[h2T_k[u]])
                    K.mm(plg[:], [(h2T[u][:, j, :], wrt[:, j, :]) for j in range(KD)], R=[h2T_k[u], wsm_k], W=[plg_k])
                    K.op(DVE, lambda e, u=u: e.tensor_reduce(out=st_[u][:, 2:3], in_=plg[:], axis=AX.X, op=ALU.max), R=[plg_k], W=[st_k[u]])
                    K.op(DVE, lambda e, u=u: e.tensor_scalar(out=st_[u][:, 2:3], in0=st_[u][:, 2:3], scalar1=-1.0, scalar2=None, op0=ALU.mult), R=[st_k[u]], W=[st_k[u]])
                    K.op(ACT, lambda e, u=u: e.activation(out=lg[:], in_=plg[:], func=AF.Exp, bias=st_[u][:, 2:3], accum_out=st_[u][:, 3:4]),
                         R=[plg_k, st_k[u]], W=[lg_k, st_k[u]])
                    K.op(DVE, lambda e, u=u: e.reciprocal(out=st_[u][:, 3:4], in_=st_[u][:, 3:4]), R=[st_k[u]], W=[st_k[u]])
                    K.op(DVE, lambda e, u=u, i=i: e.tensor_scalar(out=afft[:, i, :], in0=lg[:], scalar1=st_[u][:, 3:4], scalar2=None, op0=ALU.mult),
                         R=[lg_k, st_k[u]], W=[aff_k[i]])
                    K.tr(paT[:], afft[:, i, :], identf[:], R=[aff_k[i], cst], W=[paT_k])
                    K.op(DVE, lambda e, sl=sl: e.tensor_copy(out=affT[:, sl], in_=paT[:]), R=[paT_k], W=[affT_k])
            if b == 0:
                dump("x1", x1[:, 0, :], [128, D], R=x1_k)
                dump("affT", affT[:], [16, S], R=[affT_k])

            ckpt('outproj')
            with K.scope():
                wk = [K.sb([16, S], F32, "wk") for _ in range(2)]
                wk_k = [Tok(), Tok()]
                m8 = K.sb([16, 8], F32, "m8")
                m8_k = Tok()
                mk_ = K.sb([16, S], F32, "mk")
                mk_k = Tok()
                ppo = K.ps([128, NE], F32, "ppo")
                ppo_k = Tok()
                K.op(DVE, lambda e: e.tensor_copy(out=wk[0][:], in_=affT[:]), R=[affT_k], W=[wk_k[0]])
                nit = CAP // 8
                cur = 0
                for it in range(nit):
                    K.op(DVE, lambda e, cur=cur: e.max(out=m8[:], in_=wk[cur][:]), R=[wk_k[cur]], W=[m8_k])
                    if it < nit - 1:
                        K.op(DVE, lambda e, cur=cur: e.match_replace(out=wk[1 - cur][:], in_to_replace=m8[:], in_values=wk[cur][:], imm_value=-1.0),
                             R=[wk_k[cur], m8_k], W=[wk_k[1 - cur]])
                        cur = 1 - cur
                K.op(DVE, lambda e: e.tensor_scalar(out=mk_[:], in0=affT[:], scalar1=m8[:, 7:8], scalar2=None, op0=ALU.is_ge), R=[affT_k, m8_k], W=[mk_k])
                K.op(DVE, lambda e: e.tensor_tensor_scan(out=posm[:], data0=onesf[0:16, 0:1].to_broadcast([16, S]), data1=mk_[:], initial=0.0, op0=ALU.mult, op1=ALU.add),
                     R=[mk_k, cst], W=[posm_k])
                K.op(DVE, lambda e: e.tensor_tensor(out=posm[:], in0=posm[:], in1=mk_[:], op=ALU.mult), R=[posm_k, mk_k], W=[posm_k])
                K.op(DVE, lambda e: e.tensor_scalar(out=posm[:], in0=posm[:], scalar1=-1.0, scalar2=None, op0=ALU.add), R=[posm_k], W=[posm_k])
                for i in range(NT):
                    K.tr(ppo[:], posm[:, i * 128:(i + 1) * 128], identf[0:16, 0:16], R=[posm_k, cst], W=[ppo_k])
                    K.op(DVE, lambda e, i=i: e.tensor_copy(out=post[:, i, :], in_=ppo[:]), R=[ppo_k], W=[post_k])
            if b == 0:
                dump("posm", posm[:], [16, S], R=[posm_k])

            K.barrier()
            K.stacks.pop().close()
            ckpt('topk')
            with K.scope():
                NWS = 6
                if S >= 2048:
                    wsl = [catT[:, :, q * 512:(q + 1) * 512] for q in range(4)]
                    wsl += [K.sb([128, KD, 512], BF16, "wsl") for _ in range(NWS - 4)]
                else:
                    wsl = [K.sb([128, KD, 512], BF16, "wsl") for _ in range(NWS)]
                wsl_k = [Tok() for _ in range(NWS)]
                wsc_ = [K.dmac("wsl") for _ in range(NWS)]
                Sel = K.sb([128, NT, CAP], BF16, "Sel")
                Sel_k = Tok()
                SelT = K.sb([128, NCT, S], BF16, "SelT")
                SelT_k = Tok()
                hgT = K.sb([128, KD, CAP], BF16, "hgT")
                hgT_k = Tok()
                hidT = K.sb([128, KD, CAP], BF16, "hidT")
                hid_k = Tok()
                sgt = K.sb([128, CAP], F32, "sgt")
                sgt_k = Tok()
                ysb = K.sb([128, NCT, D], BF16, "ysb")
                ysb_k = Tok()
                ppb = K.ps([128, TB], F32, "ppb")
                ppb_k = Tok()
                pg = K.ps([128, CAP], F32, "pg")
                pg_k = Tok()
                pu = K.ps([128, CAP], F32, "pu")
                pu_k = Tok()
                ph = [K.ps([128, CAP], F32, "ph") for _ in range(2)]
                ph_k = [Tok(), Tok()]
                py = [K.ps([128, 512], F32, "py") for _ in range(2)]
                py_k = [Tok(), Tok()]
                wcount = [0]
                ohe = K.sb([16, 128], F32, "ohe")
                ohe_k = Tok()

                def wload(src_d, e_, half):
                    s = wcount[0] % NWS
                    wcount[0] += 1
                    K.dma(POOL, wsc_[s], wsl[s][:], src_d[e_, :, half * 512:(half + 1) * 512].rearrange("(j p) n -> p j n", p=128), W=[wsl_k[s]])
                    return s

                for e_ in range(NE):
                    sg0 = wload(wg_d, e_, 0)
                    sg1 = wload(wg_d, e_, 1)
                    su0 = wload(wu_d, e_, 0)
                    su1 = wload(wu_d, e_, 1)
                    for i in range(NT):
                        K.op(DVE, lambda e, i=i, e_=e_: e.tensor_scalar(out=Sel[:, i, :], in0=iotac[:, 0:CAP], scalar1=post[:, i, e_:e_ + 1], scalar2=None, op0=ALU.is_equal),
                             R=[post_k, cst], W=[Sel_k])
                    K.op(DVE, lambda e, e_=e_: e.tensor_copy(out=ohe[:], in_=bc(identf[0:16, e_:e_ + 1], [16, 128])), R=[cst], W=[ohe_k])
                    for tb in range(NTB):
                        tsl = slice(tb * TB, (tb + 1) * TB)
                        K.mm(ppb[:], [(ohe[:], posm[:, tsl])], R=[posm_k, ohe_k], W=[ppb_k])
                        for ct in range(NCT):
                            K.op(DVE, lambda e, ct=ct, tsl=tsl: e.tensor_scalar(out=SelT[:, ct, tsl], in0=ppb[:], scalar1=iotap[:, ct:ct + 1], scalar2=None, op0=ALU.is_equal),
                                 R=[ppb_k, cst], W=[SelT_k])
                    for fc in range(KD):
                        u = fc % 2
                        K.mm(ph[u][:], [(h2t[:, i, fc * 128:(fc + 1) * 128], Sel[:, i, :]) for i in range(NT)], R=h2_k + [Sel_k], W=[ph_k[u]])
                        K.op(ACT if u == 0 else DVE, (lambda e, u=u, fc=fc: e.activation(out=hgT[:, fc, :], in_=ph[u][:], func=AF.Copy)) if u == 0 else
                             (lambda e, u=u, fc=fc: e.tensor_copy(out=hgT[:, fc, :], in_=ph[u][:])), R=[ph_k[u]], W=[hgT_k])
                    for fc in range(KD):
                        gs = sg0 if fc < 4 else sg1
                        us = su0 if fc < 4 else su1
                        fo = (fc % 4) * 128
                        K.mm(pg[:], [(wsl[gs][:, j, fo:fo + 128], hgT[:, j, :]) for j in range(KD)], R=[wsl_k[gs], hgT_k], W=[pg_k])
                        K.mm(pu[:], [(wsl[us][:, j, fo:fo + 128], hgT[:, j, :]) for j in range(KD)], R=[wsl_k[us], hgT_k], W=[pu_k])
                        K.op(ACT, lambda e: e.activation(out=sgt[:], in_=pg[:], func=AF.Silu), R=[pg_k], W=[sgt_k])
                        K.op(DVE, lambda e, fc=fc: e.tensor_tensor(out=hidT[:, fc, :], in0=pu[:], in1=sgt[:], op=ALU.mult), R=[pu_k, sgt_k], W=[hid_k])
                    sd0 = wload(wd_d, e_, 0)
                    sd1 = wload(wd_d, e_, 1)
                    for ct in range(NCT):
                        for half in range(2):
                            ds_ = sd0 if half == 0 else sd1
                            hsl = slice(half * 512, (half + 1) * 512)
                            K.mm(py[half][0:CP, :], [(hidT[:, fc, ct * 128:ct * 128 + CP], wsl[ds_][:, fc, :]) for fc in range(KD)], R=[hid_k, wsl_k[ds_]], W=[py_k[half]])
                            K.op(DVE, lambda e, ct=ct, half=half, hsl=hsl: e.tensor_tensor(out=ysb[0:CP, ct, hsl], in0=py[half][0:CP, :], in1=gt2b[0:CP, 0, hsl], op=ALU.mult),
                                 R=[py_k[half], gt2b_k], W=[ysb_k])
                    for i in range(NT):
                        sl = slice(i * 128, (i + 1) * 128)
                        for half in range(2):
                            hsl = slice(half * 512, (half + 1) * 512)
                            K.mm(py[half][:], [(SelT[0:CP, ct, sl], ysb[0:CP, ct, hsl]) for ct in range(NCT)], R=[SelT_k, ysb_k], W=[py_k[half]])
                            K.op(DVE, lambda e, i=i, half=half, hsl=hsl, e_=e_: e.scalar_tensor_tensor(out=x1[:, i, hsl], in0=py[half][:], scalar=afft[:, i, e_:e_ + 1],
                                                                                                    in1=x1[:, i, hsl], op0=ALU.mult, op1=ALU.add),
                                 R=[py_k[half], aff_k[i], x1_k[i]], W=[x1_k[i]])
                for i in range(NT):
                    K.dma(SP, outc, out_d[tok0 + i * 128: tok0 + (i + 1) * 128, :], x1[:, i, :], R=[x1_k[i]])
    K.barrier()
    K.stacks[0].close()
    return nc, dump_d


def rope_tables(S):
    rows = S // 64
    row = np.repeat(np.arange(rows, dtype=np.float32), 64)
    col = np.tile(np.arange(64, dtype=np.float32), rows)
    freqs = (np.float32(10000.0) ** (-np.arange(16, dtype=np.float32) / np.float32(16))).astype(np.float32)
    ang = np.concatenate([row[:, None] * freqs, col[:, None] * freqs], axis=-1).astype(np.float32)
    return np.cos(ang).astype(np.float32), np.sin(ang).astype(np.float32)


def fm(v, n):
    return np.ascontiguousarray(np.asarray(v, np.float32).reshape(n, 128).T)


def make_in_maps(inputs, S, NSEQ, ncores):
    f = lambda a: np.ascontiguousarray(np.asarray(a, np.float32))
    NT = S // 128
    cos, sin = rope_tables(S)
    cosl = np.ascontiguousarray(cos.reshape(NT, 128, 32).transpose(1, 0, 2))
    sinl = np.ascontiguousarray(sin.reshape(NT, 128, 32).transpose(1, 0, 2))
    x = f(inputs["x"])
    c = f(inputs["c"])
    shared = {
        "w_ada": f(inputs["w_ada"][0]),
        "b_ada": fm(inputs["b_ada"][0], 48),
        "g_mix": fm(inputs["g_mix"][0], 8),
        "g_ffn": fm(inputs["g_ffn"][0], 8),
        "w_in": f(inputs["w_in"][0]),
        "q_norm": f(inputs["q_norm"][0]).reshape(1, 64),
        "k_norm": f(inputs["k_norm"][0]).reshape(1, 64),
        "mu": fm(inputs["mu_shift"][0], 14),
        "w0": np.ascontiguousarray(f(inputs["w0"][0]).reshape(2, 4, 128).transpose(2, 0, 1)),
        "a0": np.ascontiguousarray(f(inputs["a0"][0]).reshape(2, 4, 128).transpose(2, 0, 1)),
        "w_up": f(inputs["w_up"][0]),
        "a_up": f(inputs["a_up"][0]),
        "g_up": f(inputs["g_up"][0]),
        "k_k": fm(inputs["k_k"][0], 4),
        "k_a": fm(inputs["k_a"][0], 4),
        "r_k": fm(f(inputs["r_k"][0]).reshape(-1), 4),
        "ln_w": fm(inputs["ln_w"][0], 4),
        "ln_b": fm(inputs["ln_b"][0], 4),
        "w_out": f(inputs["w_out"][0]),
        "w_router": f(inputs["w_router"][0]),
        "w_gate": f(inputs["w_gate"][0]),
        "w_up_e": f(inputs["w_up_e"][0]),
        "w_down": f(inputs["w_down"][0]),
        "cos": cosl,
        "sin": sinl,
    }
    maps = []
    for i in range(ncores):
        m = dict(shared)
        m["x"] = np.ascontiguousarray(x[i * NSEQ:(i + 1) * NSEQ].reshape(NSEQ * S, D))
        cc = c[i * NSEQ:(i + 1) * NSEQ]
        m["cT"] = np.ascontiguousarray(cc.reshape(NSEQ, KD, 128).transpose(2, 1, 0))
        maps.append(m)
    return maps


def kernel(**inputs):
    x = np.asarray(inputs["x"])
    B, S, _ = x.shape
    ncores = 8
    NSEQ = B // ncores
    nc, _ = build(S=S, NSEQ=NSEQ)
    maps = make_in_maps(inputs, S, NSEQ, ncores)
    res = run_bass_kernel_spmd(nc, maps, core_ids=list(range(ncores)))
    outs = [np.asarray(r["out"]).reshape(NSEQ, S, D) for r in res.results]
    return np.concatenate(outs, axis=0).astype(np.float32)
```
